# Optimizing a Trainium2 kernel written in Bass

```python
import jax, jax.numpy as jnp
from jax import lax
import numpy as np

D_MODEL = 1024
BATCH = 4
SEQ = 8192
DEPTH = 2

BRANCH_WIDTH = D_MODEL // 2
RWKV_HEAD_DIM = 64
RWKV_HEADS = BRANCH_WIDTH // RWKV_HEAD_DIM
RWKV_WIDTH = RWKV_HEADS * RWKV_HEAD_DIM
DECAY_LORA = 64
ICLR_LORA = 64
GATE_LORA = 128
RWKV_LN_EPS = 64e-5
CONV_CH = BRANCH_WIDTH
CONV_TAPS = 3
FOX_HEAD_DIM = 64
FOX_HEADS = BRANCH_WIDTH // FOX_HEAD_DIM
FOX_WIDTH = FOX_HEADS * FOX_HEAD_DIM
Q_BLOCK = 128
N_BRANCHES = 3
RWKV_COLS = 3 * RWKV_WIDTH + DECAY_LORA + ICLR_LORA + GATE_LORA
CONV_COLS = 3 * CONV_CH
FOX_COLS = 3 * FOX_WIDTH + FOX_HEADS
GATE_COLS = N_BRANCHES * D_MODEL
IN_COLS = RWKV_COLS + CONV_COLS + FOX_COLS + GATE_COLS
D_FF = 2816
N_EXPERTS = 8
TOP_K = 2
D_FF_EXPERT = 3584
N_DENSE = (DEPTH + 1) // 2
N_MOE = DEPTH // 2
DEEPNORM_ALPHA = (2 * DEPTH) ** 0.25
DEEPNORM_BETA = (8 * DEPTH) ** -0.25
LN_EPS = 1e-5

kernel_name = 'hybrid_rwkv7_conv_fox_moe_deepnorm'


def _split_cols(t, sizes):
    return jnp.split(t, np.cumsum(sizes)[:-1].tolist(), axis=-1)


def _heads(t, n_heads):
    return t.reshape(t.shape[:-1] + (n_heads, t.shape[-1] // n_heads))


def layer_norm(x, g, b):
    xf = x.astype(jnp.float32)
    mu = jnp.mean(xf, axis=-1, keepdims=True)
    var = jnp.mean(jnp.square(xf - mu), axis=-1, keepdims=True)
    return ((xf - mu) * lax.rsqrt(var + LN_EPS) * g + b).astype(x.dtype)


def token_shift(t):
    return jnp.pad(t, ((0, 0), (1, 0), (0, 0)))[:, :-1]


def rwkv7_scan(r, decay, k, v, a_vec, b_vec):
    bsz, _, h, n = r.shape

    def step(state, inp):
        r_t, d_t, k_t, v_t, a_t, b_t = inp
        sa = jnp.einsum('bhvk,bhk->bhv', state, a_t)
        state = (state * d_t[:, :, None, :] + sa[..., None] * b_t[:, :, None, :]
                 + v_t[..., None] * k_t[:, :, None, :])
        return state, jnp.einsum('bhvk,bhk->bhv', state, r_t)

    xs = tuple(jnp.moveaxis(t, 1, 0) for t in (r, decay, k, v, a_vec, b_vec))
    s0 = jnp.zeros((bsz, h, n, n), jnp.float32)
    _, ys = lax.scan(step, s0, xs)
    return jnp.moveaxis(ys, 0, 1)


def rwkv7_time_mix(p, mu_shift, w0_decay, w2_decay, a0, w2_iclr, w2_gate, k_k, k_a, r_k, lnx_g, lnx_b):
    f32 = jnp.float32
    bsz, t_len, _ = p.shape
    p = p + mu_shift * (token_shift(p) - p)
    r, k, v, w_lo, a_lo, g_lo = _split_cols(p, [RWKV_WIDTH] * 3 + [DECAY_LORA, ICLR_LORA, GATE_LORA])
    w_raw = (w0_decay + jnp.tanh(w_lo) @ w2_decay).astype(f32)
    decay = jnp.exp(-jnp.exp(-jax.nn.softplus(-w_raw) - 0.5))
    a = jax.nn.sigmoid(a0 + a_lo @ w2_iclr).astype(f32)
    g = jax.nn.sigmoid(g_lo) @ w2_gate
    r, k, v = r.astype(f32), k.astype(f32), v.astype(f32)
    kk = _heads(k * k_k, RWKV_HEADS)
    kk = kk / jnp.maximum(jnp.linalg.norm(kk, axis=-1, keepdims=True), 1e-12)
    k = k * (1.0 + (a - 1.0) * k_a)
    rh, kh, vh, ah = (_heads(t, RWKV_HEADS) for t in (r, k, v, a))
    y = rwkv7_scan(rh, _heads(decay, RWKV_HEADS), kh, vh, -kk, kk * ah)
    mu = jnp.mean(y, axis=-1, keepdims=True)
    var = jnp.mean(jnp.square(y - mu), axis=-1, keepdims=True)
    y = ((y - mu) * lax.rsqrt(var + RWKV_LN_EPS)).reshape(bsz, t_len, RWKV_WIDTH) * lnx_g + lnx_b
    bonus = jnp.sum(rh * kh * r_k, axis=-1, keepdims=True) * vh
    y = y + bonus.reshape(bsz, t_len, RWKV_WIDTH)
    return y.astype(p.dtype) * g


def short_conv_mix(p, conv_w):
    t_len = p.shape[1]
    b_gate, c_gate, h = _split_cols(p, [CONV_CH] * 3)
    u = c_gate * h
    u_pad = jnp.pad(u, ((0, 0), (CONV_TAPS - 1, 0), (0, 0)))
    y = conv_w[0] * u_pad[:, 0:t_len]
    for i in range(1, CONV_TAPS):
        y = y + conv_w[i] * u_pad[:, i:i + t_len]
    return b_gate * y


def forgetting_attention(p, b_forget):
    f32 = jnp.float32
    bsz, t_len, _ = p.shape
    q, k, v, f = _split_cols(p, [FOX_WIDTH] * 3 + [FOX_HEADS])
    q, k, v = (_heads(t, FOX_HEADS) for t in (q, k, v))
    log_f = jax.nn.log_sigmoid((f + b_forget).astype(f32))
    cum = jnp.transpose(jnp.cumsum(log_f, axis=1), (0, 2, 1))
    scale = FOX_HEAD_DIM ** -0.5
    q_pos = jnp.arange(Q_BLOCK)
    outs = []
    for blk in range(t_len // Q_BLOCK):
        lo = blk * Q_BLOCK
        hi = lo + Q_BLOCK
        s = jnp.einsum('bqhd,bkhd->bhqk', q[:, lo:hi], k[:, :hi]).astype(f32) * scale
        s = s + cum[:, :, lo:hi, None] - cum[:, :, None, :hi]
        causal = jnp.arange(hi)[None, :] <= (lo + q_pos)[:, None]
        s = jnp.where(causal, s, -jnp.inf)
        prob = jax.nn.softmax(s, axis=-1).astype(v.dtype)
        outs.append(jnp.einsum('bhqk,bkhd->bqhd', prob, v[:, :hi]))
    return jnp.concatenate(outs, axis=1).reshape(bsz, t_len, FOX_WIDTH)


def hybrid_token_mixer(x, w_in, mu_shift, w0_decay, w2_decay, a0, w2_iclr, w2_gate, k_k, k_a, r_k,
                       lnx_g, lnx_b, conv_w, b_forget, b_gate, w_up_rwkv, w_up_conv, w_up_attn, w_out):
    bsz, t_len, _ = x.shape
    proj = x @ w_in
    p_rwkv, p_conv, p_fox, p_gate = _split_cols(proj, [RWKV_COLS, CONV_COLS, FOX_COLS, GATE_COLS])
    o_a = rwkv7_time_mix(p_rwkv, mu_shift, w0_decay, w2_decay, a0, w2_iclr, w2_gate, k_k, k_a, r_k, lnx_g, lnx_b)
    o_b = short_conv_mix(p_conv, conv_w)
    o_c = forgetting_attention(p_fox, b_forget)
    gates = jax.nn.sigmoid(p_gate.reshape(bsz, t_len, N_BRANCHES, D_MODEL) + b_gate)
    merged = (gates[:, :, 0] * (o_a @ w_up_rwkv) + gates[:, :, 1] * (o_b @ w_up_conv)
              + gates[:, :, 2] * (o_c @ w_up_attn))
    return merged @ w_out


def swiglu(x, w1, w3, w2):
    return (jax.nn.silu(x @ w1) * (x @ w3)) @ w2


def moe_swiglu(x, router_w, router_b, w1, w3, w2):
    f32 = jnp.float32
    logits = (x @ router_w).astype(f32) + router_b
    top_v, top_i = lax.top_k(logits, TOP_K)
    top_g = jax.nn.softmax(top_v, axis=-1)
    combine = jnp.sum(jax.nn.one_hot(top_i, N_EXPERTS, dtype=f32) * top_g[..., None], axis=-2)
    y = jnp.zeros_like(x)
    for e in range(N_EXPERTS):
        y = y + combine[..., e:e + 1].astype(x.dtype) * swiglu(x, w1[e], w3[e], w2[e])
    return y


def setup_inputs(seed: int = 0) -> dict:
    key = jax.random.key(seed)
    ks = iter(jax.random.split(key, 48))
    f32 = jnp.float32
    L = DEPTH
    D = D_MODEL

    def nrm(shape, scale):
        return jax.random.normal(next(ks), shape, f32) * scale

    return {
        'x': nrm((BATCH, SEQ, D), 1.0),
        'ln0_g': 1.0 + nrm((D,), 0.02),
        'ln0_b': nrm((D,), 0.02),
        'w_in': nrm((L, D, IN_COLS), D ** -0.5),
        'mu_shift': jax.random.uniform(next(ks), (L, RWKV_COLS), f32),
        'w0_decay': -1.5 + nrm((L, RWKV_WIDTH), 0.5),
        'w2_decay': nrm((L, DECAY_LORA, RWKV_WIDTH), 0.1),
        'a0': nrm((L, RWKV_WIDTH), 0.5),
        'w2_iclr': nrm((L, ICLR_LORA, RWKV_WIDTH), ICLR_LORA ** -0.5),
        'w2_gate': nrm((L, GATE_LORA, RWKV_WIDTH), GATE_LORA ** -0.5),
        'k_k': 0.85 + nrm((L, RWKV_WIDTH), 0.05),
        'k_a': 1.0 + nrm((L, RWKV_WIDTH), 0.05),
        'r_k': nrm((L, RWKV_HEADS, RWKV_HEAD_DIM), 0.1),
        'lnx_g': 1.0 + nrm((L, RWKV_WIDTH), 0.02),
        'lnx_b': nrm((L, RWKV_WIDTH), 0.02),
        'conv_w': nrm((L, CONV_TAPS, CONV_CH), CONV_TAPS ** -0.5),
        'b_forget': 3.0 + nrm((L, FOX_HEADS), 1.0),
        'b_gate': nrm((L, N_BRANCHES, D), 0.1),
        'w_up_rwkv': nrm((L, RWKV_WIDTH, D), RWKV_WIDTH ** -0.5),
        'w_up_conv': nrm((L, CONV_CH, D), CONV_CH ** -0.5),
        'w_up_attn': nrm((L, FOX_WIDTH, D), FOX_WIDTH ** -0.5),
        'w_out': nrm((L, D, D), D ** -0.5 * DEEPNORM_BETA),
        'ln1_g': 1.0 + nrm((L, D), 0.02),
        'ln1_b': nrm((L, D), 0.02),
        'ln2_g': 1.0 + nrm((L, D), 0.02),
        'ln2_b': nrm((L, D), 0.02),
        'ffn_w1': nrm((N_DENSE, D, D_FF), D ** -0.5),
        'ffn_w3': nrm((N_DENSE, D, D_FF), D ** -0.5),
        'ffn_w2': nrm((N_DENSE, D_FF, D), D_FF ** -0.5 * DEEPNORM_BETA),
        'router_w': nrm((N_MOE, D, N_EXPERTS), D ** -0.5),
        'router_b': nrm((N_MOE, N_EXPERTS), 0.01),
        'moe_w1': nrm((N_MOE, N_EXPERTS, D, D_FF_EXPERT), D ** -0.5),
        'moe_w3': nrm((N_MOE, N_EXPERTS, D, D_FF_EXPERT), D ** -0.5),
        'moe_w2': nrm((N_MOE, N_EXPERTS, D_FF_EXPERT, D), D_FF_EXPERT ** -0.5 * DEEPNORM_BETA),
    }


def reference(x, ln0_g, ln0_b, w_in, mu_shift, w0_decay, w2_decay, a0, w2_iclr, w2_gate, k_k, k_a, r_k,
              lnx_g, lnx_b, conv_w, b_forget, b_gate, w_up_rwkv, w_up_conv, w_up_attn, w_out,
              ln1_g, ln1_b, ln2_g, ln2_b, ffn_w1, ffn_w3, ffn_w2, router_w, router_b,
              moe_w1, moe_w3, moe_w2):
    x = layer_norm(x, ln0_g, ln0_b)
    for l in range(DEPTH):
        mix = hybrid_token_mixer(x, w_in[l], mu_shift[l], w0_decay[l], w2_decay[l], a0[l], w2_iclr[l],
                                 w2_gate[l], k_k[l], k_a[l], r_k[l], lnx_g[l], lnx_b[l], conv_w[l],
                                 b_forget[l], b_gate[l], w_up_rwkv[l], w_up_conv[l], w_up_attn[l], w_out[l])
        x = layer_norm(DEEPNORM_ALPHA * x + mix, ln1_g[l], ln1_b[l])
        i = l // 2
        if l % 2 == 0:
            ffn = swiglu(x, ffn_w1[i], ffn_w3[i], ffn_w2[i])
        else:
            ffn = moe_swiglu(x, router_w[i], router_b[i], moe_w1[i], moe_w3[i], moe_w2[i])
        x = layer_norm(DEEPNORM_ALPHA * x + ffn, ln2_g[l], ln2_b[l])
    return x
```

```python
import contextlib
import numpy as np
import ml_dtypes
import concourse.bass as bass
import concourse.mybir as mybir
from concourse.bass_utils import run_bass_kernel_spmd

F32 = mybir.dt.float32
BF16 = mybir.dt.bfloat16
AF = mybir.ActivationFunctionType
ALU = mybir.AluOpType
AX = mybir.AxisListType

D = 1024
DEPTH = 2
ALPHA = (2 * DEPTH) ** 0.25
LN_EPS = 1e-5
D_FF = 2816
N_EXP = 8
D_FF_E = 3584
NBR = 3
BW = 512


class Res:
    __slots__ = ("name", "w", "r", "dsem", "dval", "excl")

    def __init__(self, name, excl=False):
        self.name = name
        self.excl = excl
        self.w = None
        self.r = {}
        self.dsem = None
        self.dval = 0

    def absorb(self, others):
        for o in others:
            if o.w is not None:
                self.r[("w", id(o))] = o.w
            for k, t in o.r.items():
                self.r[(k, id(o))] = t


class Prog:
    ROLL = 20000

    NPOOL = 20

    def __init__(self, nc, es):
        self.nc = nc
        self.es = es
        self.dpool = {q: dict(sems=[], vals=[], idx=0) for q in ("sp", "act", "pool")}
        self.cc_tags = []
        self.E = {}
        for name, obj in (("pe", nc.tensor), ("dve", nc.vector), ("act", nc.scalar),
                          ("pool", nc.gpsimd), ("sp", nc.sync)):
            self.E[name] = dict(obj=obj, sem=None, val=0, seen={}, nsem=0, pending=False)
        self.nsems = 0
        self.n_inst = 0

    def new_sem(self, name):
        self.nsems += 1
        return self.es.enter_context(self.nc.semaphore(f"{name}_{self.nsems}"))

    def _eng_sem(self, en):
        E = self.E[en]
        if E["sem"] is None or E["val"] >= self.ROLL:
            assert not E["pending"]
            E["sem"] = self.new_sem("e" + en)
            E["val"] = 0
        return E

    def _wait(self, en, tag, same_ok=True):
        E = self.E[en]
        ten, sem, val = tag
        if ten == en and sem is E["sem"]:
            if en == "pe":
                return
            if val < E["val"] - 1:
                return
        elif ten == en:
            return
        key = id(sem)
        if E["seen"].get(key, 0) >= val:
            return
        E["obj"].wait_ge(sem, val)
        E["seen"][key] = val
        self.n_inst += 1

    def _deps(self, en, reads, writes):
        for r in reads:
            if r.w is not None:
                self._wait(en, r.w)
            if r.excl:
                for t in r.r.values():
                    if t[0] != en:
                        self._wait(en, t)
        for w in writes:
            if w.w is not None:
                self._wait(en, w.w)
            for t in w.r.values():
                if t[0] == en and t[1] is self.E[en]["sem"] and en != "pe":
                    continue
                self._wait(en, t)

    def op(self, en, fn, reads=(), writes=(), inc=True):
        E = self._eng_sem(en)
        self._deps(en, reads, writes)
        ins = fn(E["obj"])
        self.n_inst += 1
        if inc:
            E["val"] += 1
            ins.then_inc(E["sem"], 1)
            tag = (en, E["sem"], E["val"])
            E["pending"] = False
        else:
            tag = (en, E["sem"], E["val"] + 1)
            E["pending"] = True
        for r in reads:
            r.r[en] = tag
        for w in writes:
            w.w = tag
            w.r = {}
        return ins

    def dma(self, q, out, in_, reads=(), writes=(), sem_res=None):
        E = self.E[q]
        self._deps(q, reads, writes)
        pool = self.dpool[q]
        i = pool["idx"] % self.NPOOL
        pool["idx"] += 1
        if len(pool["sems"]) <= i:
            pool["sems"].append(self.new_sem("d" + q))
            pool["vals"].append(0)
        sem, prev = pool["sems"][i], pool["vals"][i]
        if prev > 0:
            self._wait(q, ("dma", sem, prev))
        ins = E["obj"].dma_start(out=out, in_=in_)
        ins.then_inc(sem, 16)
        pool["vals"][i] = prev + 16
        self.n_inst += 1
        tag = ("dma", sem, prev + 16)
        for r in reads:
            r.r[("dma", id(sem))] = tag
        for w in writes:
            w.w = tag
            w.r = {}
        return tag

    def collective_allgather(self, in_ap, out_ap, groups, dep_tags):
        for t in dep_tags:
            self._wait("pool", t)
        sem = self.new_sem("cc")
        ins = self.nc.gpsimd.collective_compute("AllGather", ALU.bypass, replica_groups=groups,
                                                ins=[in_ap], outs=[out_ap])
        ins.then_inc(sem)
        self.n_inst += 1
        tag = ("cc", sem, 1)
        self.cc_tags.append(tag)
        return tag

    def barrier(self, extra_tags=()):
        tags = [(en, E["sem"], E["val"]) for en, E in self.E.items() if E["sem"] is not None and E["val"] > 0]
        for pool in self.dpool.values():
            for sem, v in zip(pool["sems"], pool["vals"]):
                if v > 0:
                    tags.append(("dma", sem, v))
        tags += list(self.cc_tags) + list(extra_tags)
        for en, E in self.E.items():
            assert not E["pending"], en
            for t in tags:
                if t[0] == en:
                    continue
                self._wait(en, t)

    def finish(self, out_tags):
        for t in out_tags:
            self._wait("sp", t)
        for en, E in self.E.items():
            assert not E["pending"], en


class Ctx:
    _uid = [0]

    def __init__(self, nc, P=None):
        self.nc = nc
        self.es = contextlib.ExitStack()
        self.P = P if P is not None else Prog(nc, self.es)
        Ctx._uid[0] += 1
        self.n = Ctx._uid[0] * 1000

    def sb(self, shape, dt, name=None):
        self.n += 1
        return self.es.enter_context(self.nc.sbuf_tensor(f"{name or 't'}{self.n}", list(shape), dt))

    def ps(self, shape, dt=F32, name=None):
        self.n += 1
        return self.es.enter_context(self.nc.psum_tensor(f"{name or 'p'}{self.n}", list(shape), dt))

    def close(self):
        self.es.close()


class WStream:
    def __init__(self, cx, nbuf, nbytes, name, lookahead=None, q="pool"):
        self.cx = cx
        self.P = cx.P
        self.bufs = [cx.sb([128, nbytes // 2], BF16, name) for _ in range(nbuf)]
        self.res = [Res(f"{name}{i}") for i in range(nbuf)]
        self.reqs = []
        self.issued = 0
        self.taken = 0
        self.la = lookahead if lookahead is not None else nbuf - 1
        self.q = q

    def plan(self, dram_ap, shape):
        self.reqs.append((dram_ap, tuple(shape)))

    def _view(self, i, shape):
        n = int(np.prod(shape))
        b = self.bufs[i % len(self.bufs)]
        v = b[:, 0:n]
        if len(shape) == 2:
            v = v.rearrange("p (a b) -> p a b", a=shape[0])
        return v

    def _issue(self, i):
        ap, shape = self.reqs[i]
        r = self.res[i % len(self.bufs)]
        self.P.dma(self.q, self._view(i, shape), ap, writes=[r])

    def next(self):
        i = self.taken
        while self.issued < min(len(self.reqs), i + 1 + self.la):
            self._issue(self.issued)
            self.issued += 1
        self.taken += 1
        return self.res[i % len(self.bufs)], self._view(i, self.reqs[i][1])


def layer_norm_rows(P, nc, x_ap, xres, gB, bB, stats, mv, rstd, sres, cres, eps=LN_EPS):
    P.op("dve", lambda e: e.bn_stats(out=stats[:, 0, :], in_=x_ap[:, 0:512]), reads=[xres], writes=[sres])
    P.op("dve", lambda e: e.bn_stats(out=stats[:, 1, :], in_=x_ap[:, 512:1024]), reads=[xres], writes=[sres])
    P.op("dve", lambda e: e.bn_aggr(out=mv[:, :], in_=stats[:, :, :].rearrange("p a b -> p (a b)")),
         reads=[sres], writes=[sres])
    P.op("act", lambda e: e.activation(out=rstd[:, :], in_=mv[:, 1:2], func=AF.Sqrt, bias=float(eps), scale=1.0),
         reads=[sres], writes=[sres])
    P.op("dve", lambda e: e.reciprocal(out=rstd[:, :], in_=rstd[:, :]), reads=[sres], writes=[sres])
    P.op("dve", lambda e: e.tensor_scalar(out=x_ap, in0=x_ap, scalar1=mv[:, 0:1], scalar2=rstd[:, 0:1],
                                          op0=ALU.subtract, op1=ALU.mult), reads=[sres, xres], writes=[xres])
    P.op("dve", lambda e: e.tensor_tensor(out=x_ap, in0=x_ap, in1=gB, op=ALU.mult),
         reads=[xres, cres], writes=[xres])
    P.op("dve", lambda e: e.tensor_tensor(out=x_ap, in0=x_ap, in1=bB, op=ALU.add),
         reads=[xres, cres], writes=[xres])


def decl_merge(nc, pre, moe, ne_override=None, ff_override=None):
    dram = lambda n, s_, dt=F32: nc.dram_tensor(pre + n, list(s_), dt, kind="ExternalInput").ap()
    A = dict(
        w_gate=dram("w_gate", [D, NBR * D]), b_gate=dram("b_gate", [128, NBR * 8]), w_up=dram("w_up", [NBR, BW, D]),
        w_out=dram("w_out", [D, D]), lnB=dram("lnB", [6, 128, D]), ident=dram("ident", [128, 128]))
    if moe:
        FF = ff_override or D_FF_E
        NE = ne_override or N_EXP
        A.update(w1=dram("w1", [NE, D, FF]), w3=dram("w3", [NE, D, FF]), w2=dram("w2", [NE, FF, D]),
                 router_w=dram("router_w", [D, N_EXP]), router_b=dram("router_b", [128, N_EXP]))
    else:
        FF, NE = D_FF, 1
        A.update(w1=dram("w1", [1, D, FF]), w3=dram("w3", [1, D, FF]), w2=dram("w2", [1, FF, D]))
    A["FF"], A["NE"] = FF, NE
    return A


def build_merge(NT, do_ln0, moe, ne_override=None, ff_override=None):
    nc = bass.Bass("TRN2", target_bir_lowering=False)
    A = decl_merge(nc, "", moe, ne_override, ff_override)
    x_d = nc.dram_tensor("x", [NT, D], F32, kind="ExternalInput").ap()
    oT_d = nc.dram_tensor("oT", [NBR * BW, NT], BF16, kind="ExternalInput").ap()
    y_d = nc.dram_tensor("y", [NT, D], F32, kind="ExternalOutput").ap()
    cx0 = Ctx(nc)
    x_rows = lambda t0, n: x_d[t0:t0 + n, :]
    o_rows = lambda br, half, t0, n: oT_d[br * BW + half * 256: br * BW + (half + 1) * 256, t0:t0 + n]
    tags, cx = emit_merge(nc, cx0.P, A, NT, do_ln0, moe, x_rows, o_rows, y_d)
    cx0.P.finish(tags)
    cx.close()
    cx0.close()
    return nc


def emit_merge(nc, P, A, NT, do_ln0, moe, x_rows, o_rows, y_d, after_store=None):
    cx = Ctx(nc, P)
    wg_d, bg_d, wup_d, wout_d, lnB_d, ident_d = (A[k] for k in ("w_gate", "b_gate", "w_up", "w_out", "lnB", "ident"))
    FF, NE = A["FF"], A["NE"]
    w1_d, w3_d, w2_d = A["w1"], A["w3"], A["w2"]
    if moe:
        rw_d, rb_d = A["router_w"], A["router_b"]
    NFS = FF // 128

    TT2 = min(NT, 2048 if moe else 1024)
    NHF = 2 if (moe and NFS % 2 == 0) else 1
    NFH = NFS // NHF
    TT1 = min(NT, 512)
    NS2 = TT2 // 128
    NS1 = TT1 // 128
    NH2 = TT2 // TT1
    n_super = NT // TT2

    ident = cx.sb([128, 128], F32, "ident")
    LN0 = 0 if do_ln0 else 2
    lnB = cx.sb([128, 6 - LN0, D], F32, "lnB")
    bg = cx.sb([128, NBR * 8], F32, "bg")
    xs = cx.sb([128, NS2, D], F32, "xs")
    xT = cx.sb([128, 8, TT2], BF16, "xT")
    xTf = cx.sb([128, 8, 128], F32, "xTf") if moe else None
    U = cx.sb([128, max(20 * TT1, NFH * TT2)], BF16, "U")
    gate_sb = [cx.sb([128, TT1], BF16 if moe else F32, "gate") for _ in range(3)]
    prod_sb = [cx.sb([128, TT1], BF16 if moe else F32, "prod") for _ in range(3)]
    silu_sb = [cx.sb([128, 512], BF16, "silu") for _ in range(2)]
    stats = cx.sb([128, 2, 6], F32, "stats")
    mv = cx.sb([128, 2], F32, "mv")
    rstd = cx.sb([128, 1], F32, "rstd")
    if moe:
        rw = cx.sb([128, 8, N_EXP], F32, "rw")
        rb = cx.sb([128, N_EXP], F32, "rb")
        comb = cx.sb([128, NS2, N_EXP], F32, "comb")
        rt = [cx.sb([128, N_EXP], F32, "rt") for _ in range(6)]
        rs = [cx.sb([128, 1], F32, "rs") for _ in range(4)]
    TR = cx.ps([128, 1024], F32, "TR")
    TM = cx.ps([128, 1024], F32, "TM")
    FM = [cx.ps([128, 512], F32, "FM") for _ in range(4)]
    rTR, rTM = Res("TR", True), Res("TM", True)
    rFM = [Res(f"FM{i}", True) for i in range(4)]

    r_ident, r_lnB, r_bg = Res("ident"), Res("lnB"), Res("bg")
    r_xs = [Res(f"xs{i}") for i in range(NS2)]
    r_xT = [Res(f"xT{i}") for i in range(NS2)]
    r_xTf = Res("xTf")
    r_stat = Res("stat")
    r_gate = [Res(f"gate{i}") for i in range(3)]
    r_prod = [Res(f"prod{i}") for i in range(3)]
    r_silu = [Res(f"silu{i}") for i in range(2)]
    r_rt = Res("rt")
    r_comb = [Res(f"comb{i}") for i in range(NS2)]
    r_const = Res("const")

    oT_v = U[:, 0:12 * TT1].rearrange("p (a t) -> p a t", a=12)
    mT_v = U[:, 12 * TT1:20 * TT1].rearrange("p (a t) -> p a t", a=8)
    hT_v = U[:, 0:NFH * TT2].rearrange("p (a t) -> p a t", a=NFH)
    r_oT = [Res(f"oT{i}") for i in range(3)]
    r_mT = [Res(f"mT{i}") for i in range(8)]
    r_hT = [Res(f"hT{i}") for i in range(NFH)]

    P.dma("sp", ident[:, :], ident_d[:, :], writes=[r_ident])
    P.dma("sp", lnB[:, :, :], lnB_d.rearrange("a p d -> p a d")[:, LN0:6, :], writes=[r_lnB])
    P.dma("sp", bg[:, :], bg_d[:, :], writes=[r_bg])
    if moe:
        P.dma("sp", rw[:, :, :], rw_d.rearrange("(kc p) e -> p kc e", p=128), writes=[r_const])
        r_rb = Res("rb")
        P.dma("sp", rb[:, :], rb_d[:, :], writes=[r_rb])

    wsA = WStream(cx, 10 if not moe else 4, 2048, "wA", lookahead=6 if not moe else 1)
    wsB = WStream(cx, 12 if not moe else 8, 2048, "wB", lookahead=4 if not moe else 2)
    wg_v = wg_d.rearrange("(kc p) c -> p kc c", p=128)
    wup_v = wup_d.rearrange("b (kc p) c -> b p kc c", p=128)
    w1_v = w1_d.rearrange("e (kc p) c -> e p kc c", p=128)
    w3_v = w3_d.rearrange("e (kc p) c -> e p kc c", p=128)
    for st in range(n_super):
        for h in range(NH2):
            for fs in range(8):
                for br in range(NBR):
                    wsA.plan(wg_v[:, :, br * D + fs * 128: br * D + (fs + 1) * 128], (8, 128))
                    wsA.plan(wup_v[br][:, :, fs * 128:(fs + 1) * 128], (4, 128))
            for kc in range(8):
                wsB.plan(wout_d[kc * 128:(kc + 1) * 128, :], (1024,))
        for e in range(NE):
            for hfF in range(NHF):
                for fs in range(hfF * NFH, (hfF + 1) * NFH):
                    wsA.plan(w1_v[e][:, :, fs * 128:(fs + 1) * 128], (8, 128))
                    wsA.plan(w3_v[e][:, :, fs * 128:(fs + 1) * 128], (8, 128))
                for kc in range(hfF * NFH, (hfF + 1) * NFH):
                    wsB.plan(w2_d[e, kc * 128:(kc + 1) * 128, :], (1024,))

    out_tags = []

    def transpose_sub(s, want_f32):
        for kc in range(8):
            P.op("pe", lambda e, kc=kc: e.transpose(out=TR[:, kc * 128:(kc + 1) * 128],
                                                    in_=xs[:, s, kc * 128:(kc + 1) * 128], identity=ident[:, :]),
                 reads=[r_xs[s], r_ident], writes=[rTR], inc=(kc == 7))
        if want_f32:
            P.op("dve", lambda e: e.tensor_copy(out=xTf[:, :, :], in_=TR[:, :].rearrange("p (a t) -> p a t", a=8)),
                 reads=[rTR], writes=[r_xTf])
            P.op("act", lambda e: e.copy(out=xT[:, :, s * 128:(s + 1) * 128], in_=xTf[:, :, :]),
                 reads=[r_xTf], writes=[r_xT[s]])
        else:
            P.op("act", lambda e: e.copy(out=xT[:, :, s * 128:(s + 1) * 128],
                                         in_=TR[:, :].rearrange("p (a t) -> p a t", a=8)),
                 reads=[rTR], writes=[r_xT[s]])

    fm_i = [0]

    def next_fm():
        i = fm_i[0] % 4
        fm_i[0] += 1
        return FM[i], rFM[i]

    def ln_sub(s, which):
        layer_norm_rows(P, nc, xs[:, s, :], r_xs[s], lnB[:, 2 * which - LN0, :], lnB[:, 2 * which + 1 - LN0, :],
                        stats, mv, rstd, r_stat, r_lnB)

    for st in range(n_super):
        t0 = st * TT2
        for s in range(NS2):
            P.dma("sp", xs[:, s, :], x_rows(t0 + s * 128, 128), writes=[r_xs[s]])
        for s in range(NS2):
            if do_ln0:
                ln_sub(s, 0)
            transpose_sub(s, False)
        for r in r_oT + r_mT:
            r.absorb(r_hT)
        for h in range(NH2):
            c0 = h * TT1
            for br in range(NBR):
                for half in range(2):
                    P.dma("sp", oT_v[:, br * 4 + half * 2:br * 4 + half * 2 + 2, :],
                          o_rows(br, half, t0 + c0, TT1).rearrange("(kc p) t -> p kc t", p=128),
                          writes=[r_oT[br]])
            for fs in range(8):
                for br in range(NBR):
                    gw_r, gw = wsA.next()
                    uw_r, uw = wsA.next()
                    gp, gp_r = next_fm()
                    for kc in range(8):
                        P.op("pe", lambda e, kc=kc: e.matmul(gp[:, 0:TT1], lhsT=gw[:, kc, :],
                                                             rhs=xT[:, kc, c0:c0 + TT1],
                                                             start=(kc == 0), stop=(kc == 7)),
                             reads=[gw_r] + r_xT[h * NS1:(h + 1) * NS1], writes=[gp_r], inc=(kc == 7))
                    up, up_r = next_fm()
                    for kc in range(4):
                        P.op("pe", lambda e, kc=kc: e.matmul(up[:, 0:TT1], lhsT=uw[:, kc, :],
                                                             rhs=oT_v[:, br * 4 + kc, :],
                                                             start=(kc == 0), stop=(kc == 3)),
                             reads=[uw_r, r_oT[br]], writes=[up_r], inc=(kc == 3))
                    P.op("act", lambda e: e.activation(out=gate_sb[br][:, :], in_=gp[:, 0:TT1], func=AF.Sigmoid,
                                                       bias=bg[:, br * 8 + fs: br * 8 + fs + 1], scale=1.0),
                         reads=[gp_r, r_bg], writes=[r_gate[br]])
                    P.op("dve", lambda e: e.tensor_tensor(out=prod_sb[br][:, :], in0=up[:, 0:TT1],
                                                          in1=gate_sb[br][:, :], op=ALU.mult),
                         reads=[up_r, r_gate[br]], writes=[r_prod[br]])
                P.op("dve", lambda e: e.tensor_tensor(out=prod_sb[0][:, :], in0=prod_sb[0][:, :],
                                                       in1=prod_sb[1][:, :], op=ALU.add),
                     reads=[r_prod[0], r_prod[1]], writes=[r_prod[0]])
                P.op("dve", lambda e: e.tensor_tensor(out=mT_v[:, fs, :], in0=prod_sb[0][:, :],
                                                       in1=prod_sb[2][:, :], op=ALU.add),
                     reads=[r_prod[0], r_prod[2]], writes=[r_mT[fs]])
            for kg in range(2):
                wo = [wsB.next() for _ in range(4)]
                for sl in range(NS1):
                    s = h * NS1 + sl
                    for half in range(2):
                        for k4 in range(4):
                            kc = kg * 4 + k4
                            P.op("pe", lambda e, kc=kc, k4=k4, half=half: e.matmul(
                                TM[:, half * 512:(half + 1) * 512], lhsT=mT_v[:, kc, sl * 128:(sl + 1) * 128],
                                rhs=wo[k4][1][:, half * 512:(half + 1) * 512], start=(k4 == 0), stop=(k4 == 3)),
                                 reads=[r_mT[kc], wo[k4][0]], writes=[rTM], inc=(k4 == 3 and half == 1))
                    if kg == 0:
                        P.op("dve", lambda e: e.scalar_tensor_tensor(out=xs[:, s, :], in0=xs[:, s, :], scalar=float(ALPHA),
                                                                     in1=TM[:, :], op0=ALU.mult, op1=ALU.add),
                             reads=[rTM, r_xs[s]], writes=[r_xs[s]])
                    else:
                        P.op("dve", lambda e: e.tensor_tensor(out=xs[:, s, :], in0=TM[:, :], in1=xs[:, s, :], op=ALU.add),
                             reads=[rTM, r_xs[s]], writes=[r_xs[s]])
                        ln_sub(s, 1)
        for s in range(NS2):
            transpose_sub(s, moe)
            if moe:
                lg, lg_r = next_fm()
                for kc in range(8):
                    P.op("pe", lambda e, kc=kc: e.matmul(lg[:, 0:N_EXP], lhsT=xTf[:, kc, :], rhs=rw[:, kc, :],
                                                         start=(kc == 0), stop=(kc == 7)),
                         reads=[r_xTf, r_const], writes=[lg_r], inc=(kc == 7))
                L, M1, K1, L2, K2, T6 = rt
                m1, m2, g1, g2 = rs
                ops = [
                    lambda e: e.tensor_tensor(out=L[:, :], in0=lg[:, 0:N_EXP], in1=rb[:, :], op=ALU.add),
                    lambda e: e.tensor_reduce(out=m1[:, :], in_=L[:, :], axis=AX.X, op=ALU.max),
                    lambda e: e.tensor_scalar(out=K1[:, :], in0=L[:, :], scalar1=m1[:, 0:1], scalar2=None,
                                              op0=ALU.is_ge),
                    lambda e: e.scalar_tensor_tensor(out=L2[:, :], in0=K1[:, :], scalar=-1e30, in1=L[:, :],
                                                     op0=ALU.mult, op1=ALU.add),
                    lambda e: e.tensor_reduce(out=m2[:, :], in_=L2[:, :], axis=AX.X, op=ALU.max),
                    lambda e: e.tensor_scalar(out=K2[:, :], in0=L2[:, :], scalar1=m2[:, 0:1], scalar2=None,
                                              op0=ALU.is_ge),
                    lambda e: e.tensor_tensor(out=g2[:, :], in0=m2[:, :], in1=m1[:, :], op=ALU.subtract),
                ]
                for i, f in enumerate(ops):
                    P.op("dve", f, reads=[lg_r, r_rt, r_rb] if i == 0 else [r_rt], writes=[r_rt])
                P.op("act", lambda e: e.activation(out=g2[:, :], in_=g2[:, :], func=AF.Sigmoid),
                     reads=[r_rt], writes=[r_rt])
                ops2 = [
                    lambda e: e.tensor_scalar(out=g1[:, :], in0=g2[:, :], scalar1=-1.0, scalar2=1.0,
                                              op0=ALU.mult, op1=ALU.add),
                    lambda e: e.tensor_scalar(out=K2[:, :], in0=K2[:, :], scalar1=g2[:, 0:1], scalar2=None,
                                              op0=ALU.mult),
                ]
                for f in ops2:
                    P.op("dve", f, reads=[r_rt], writes=[r_rt])
                P.op("dve", lambda e: e.scalar_tensor_tensor(out=comb[:, s, :], in0=K1[:, :], scalar=g1[:, 0:1],
                                                             in1=K2[:, :], op0=ALU.mult, op1=ALU.add),
                     reads=[r_rt], writes=[r_comb[s]])
        for s in range(NS2):
            P.op("act", lambda e: e.mul(out=xs[:, s, :], in_=xs[:, s, :], mul=float(ALPHA)),
                 reads=[r_xs[s]], writes=[r_xs[s]])
        for r in r_hT:
            r.absorb(r_oT + r_mT)
        for e_i, hfF in [(a_, b_) for a_ in range(NE) for b_ in range(NHF)]:
            for fs in range(NFH):
                w1r, w1t = wsA.next()
                w3r, w3t = wsA.next()
                for hh in range(TT2 // 512 if TT2 >= 512 else 1):
                    n = min(512, TT2)
                    c0 = hh * n
                    subs = r_xT[c0 // 128:(c0 + n) // 128]
                    a, a_r = next_fm()
                    for kc in range(8):
                        P.op("pe", lambda e, kc=kc: e.matmul(a[:, 0:n], lhsT=w1t[:, kc, :], rhs=xT[:, kc, c0:c0 + n],
                                                             start=(kc == 0), stop=(kc == 7)),
                             reads=[w1r] + subs, writes=[a_r], inc=(kc == 7))
                    b, b_r = next_fm()
                    for kc in range(8):
                        P.op("pe", lambda e, kc=kc: e.matmul(b[:, 0:n], lhsT=w3t[:, kc, :], rhs=xT[:, kc, c0:c0 + n],
                                                             start=(kc == 0), stop=(kc == 7)),
                             reads=[w3r] + subs, writes=[b_r], inc=(kc == 7))
                    si = (fs * 2 + hh) % 2
                    P.op("act", lambda e: e.activation(out=silu_sb[si][:, 0:n], in_=a[:, 0:n], func=AF.Silu),
                         reads=[a_r], writes=[r_silu[si]])
                    P.op("dve", lambda e: e.tensor_tensor(out=hT_v[:, fs, c0:c0 + n], in0=b[:, 0:n],
                                                          in1=silu_sb[si][:, 0:n], op=ALU.mult),
                         reads=[b_r, r_silu[si]], writes=[r_hT[fs]])
            GK = 6 if moe else 8
            for g0 in range(0, NFH, GK):
                gks = list(range(g0, min(NFH, g0 + GK)))
                wts = [wsB.next() for _ in gks]
                for s in range(NS2):
                    for half in range(2):
                        for j, kc in enumerate(gks):
                            P.op("pe", lambda e, j=j, kc=kc, half=half: e.matmul(
                                TM[:, half * 512:(half + 1) * 512], lhsT=hT_v[:, kc, s * 128:(s + 1) * 128],
                                rhs=wts[j][1][:, half * 512:(half + 1) * 512],
                                start=(j == 0), stop=(j == len(gks) - 1)),
                                 reads=[r_hT[kc], wts[j][0]], writes=[rTM],
                                 inc=(j == len(gks) - 1 and half == 1))
                    if moe:
                        P.op("dve", lambda e: e.scalar_tensor_tensor(
                            out=xs[:, s, :], in0=TM[:, :], scalar=comb[:, s, e_i:e_i + 1], in1=xs[:, s, :],
                            op0=ALU.mult, op1=ALU.add), reads=[rTM, r_xs[s], r_comb[s]], writes=[r_xs[s]])
                    else:
                        P.op("dve", lambda e: e.tensor_tensor(out=xs[:, s, :], in0=TM[:, :], in1=xs[:, s, :],
                                                              op=ALU.add),
                             reads=[rTM, r_xs[s]], writes=[r_xs[s]])
        for s in range(NS2):
            ln_sub(s, 2)
            out_tags.append(P.dma("sp", y_d[t0 + s * 128: t0 + (s + 1) * 128, :], xs[:, s, :], reads=[r_xs[s]]))
            if after_store is not None:
                after_store(t0 + s * 128, out_tags[-1])
    return out_tags, cx


RWKV_COLS = 3 * 512 + 64 + 64 + 128
CONV_COLS = 3 * 512
FOX_COLS = 3 * 512 + 8
GATE_OFF = RWKV_COLS + CONV_COLS + FOX_COLS
IDENT = np.eye(128, dtype=np.float32)


def _bc(v):
    return np.ascontiguousarray(np.broadcast_to(np.asarray(v, np.float32)[None, :], (128, v.shape[0])))


def merge_inputs(p, l, moe):
    w_in = p["w_in"][l]
    common = {
        "ident": IDENT,
        "w_gate": np.ascontiguousarray(w_in[:, GATE_OFF:GATE_OFF + NBR * D]),
        "b_gate": np.ascontiguousarray(p["b_gate"][l].reshape(NBR, 8, 128).transpose(2, 0, 1).reshape(128, NBR * 8)),
        "w_up": np.ascontiguousarray(np.stack([p["w_up_rwkv"][l], p["w_up_conv"][l], p["w_up_attn"][l]], 0)),
        "w_out": np.ascontiguousarray(p["w_out"][l]),
        "lnB": np.ascontiguousarray(np.stack([_bc(p["ln0_g"]), _bc(p["ln0_b"]), _bc(p["ln1_g"][l]), _bc(p["ln1_b"][l]),
                                              _bc(p["ln2_g"][l]), _bc(p["ln2_b"][l])], 0)),
    }
    i = l // 2
    if moe:
        common.update({
            "w1": p["moe_w1"][i], "w3": p["moe_w3"][i], "w2": p["moe_w2"][i],
            "router_w": np.ascontiguousarray(p["router_w"][i]), "router_b": _bc(p["router_b"][i]),
        })
    else:
        common.update({"w1": p["ffn_w1"][i][None], "w3": p["ffn_w3"][i][None], "w2": p["ffn_w2"][i][None]})
    return common


def run_merge(x_flat, oT, p, l, do_ln0, moe, n_cores=8):
    ntok = x_flat.shape[0]
    NT = ntok // n_cores
    nc = build_merge(NT, do_ln0, moe)
    common = merge_inputs(p, l, moe)
    in_maps = []
    for c in range(n_cores):
        m = dict(common)
        m["x"] = np.ascontiguousarray(x_flat[c * NT:(c + 1) * NT])
        m["oT"] = np.ascontiguousarray(oT[:, c * NT:(c + 1) * NT])
        in_maps.append(m)
    res = run_bass_kernel_spmd(nc, in_maps, core_ids=list(range(n_cores)))
    return np.concatenate([r["y"] for r in res.results], 0)


NEG = -30000.0


def decl_fox(nc, pre):
    dram = lambda n, s_, dt=F32: nc.dram_tensor(pre + n, list(s_), dt, kind="ExternalInput").ap()
    return dict(wc=dram("wc", [D, 768]), wq=dram("wq", [D, 256]), wk=dram("wk", [D, 256]), wv=dram("wv", [D, 256]),
                wf=dram("wf", [D, 4]), bf=dram("bf", [4, 1]), cw=dram("cw", [128, 6]), lnB=dram("lnB", [2, 128, D]),
                ident=dram("ident", [128, 128]), mask=dram("mask", [128, 128]))


def build_fox(T, do_ln0):
    nc = bass.Bass("TRN2", target_bir_lowering=False)
    A = decl_fox(nc, "")
    x_d = nc.dram_tensor("x", [T, D], F32, kind="ExternalInput").ap()
    obT_d = nc.dram_tensor("obT", [256, T], BF16, kind="ExternalOutput").ap()
    ocT_d = nc.dram_tensor("ocT", [256, T], BF16, kind="ExternalOutput").ap()
    cx0 = Ctx(nc)
    TTf = min(T, 512)
    tags, cx = emit_fox(nc, cx0.P, A, T, do_ln0, lambda t0, n: x_d[t0:t0 + n, :],
                        lambda ti: obT_d[:, ti * TTf:(ti + 1) * TTf], lambda ti: ocT_d[:, ti * TTf:(ti + 1) * TTf])
    cx0.P.finish(tags)
    cx.close()
    cx0.close()
    return nc


def emit_fox(nc, P, A, T, do_ln0, xsrc, ob_dst, oc_dst, after_store=None):
    cx = Ctx(nc, P)
    wc_d, wq_d, wk_d, wv_d, wf_d, bf_d, cw_d, lnB_d, ident_d, mask_d = (
        A[k] for k in ("wc", "wq", "wk", "wv", "wf", "bf", "cw", "lnB", "ident", "mask"))

    TT = min(T, 512)
    NS = TT // 128
    NSB = T // TT
    NB = T // 128
    H = 4

    ident = cx.sb([128, 128], F32, "ident")
    identb = cx.sb([128, 128], BF16, "identb")
    maskf = cx.sb([128, 128], F32, "maskf")
    maskb = cx.sb([128, 128], BF16, "maskb")
    lnB = cx.sb([128, 2, D], F32, "lnB")
    cw = cx.sb([128, 6], F32, "cw")
    bf = cx.sb([4, 1], F32, "bf")
    nbf = cx.sb([4, 1], F32, "nbf")
    wc = cx.sb([128, 8, 768], BF16, "wc")
    wq = cx.sb([128, 8, 256], BF16, "wq")
    wk = cx.sb([128, 8, 256], BF16, "wk")
    wv = cx.sb([128, 8, 256], BF16, "wv")
    wf = cx.sb([128, 8, 4], BF16, "wf")
    xs = cx.sb([128, 2, D], F32, "xs")
    xT = cx.sb([128, 8, TT], BF16, "xT")
    Kaug = [cx.sb([70, T], BF16, "Kaug") for _ in range(H)]
    Vaug = cx.sb([128, NB, H, 65], BF16, "Vaug")
    Qaug = [[cx.sb([70, TT], BF16, "Qaug") for _ in range(H)] for _ in range(2)]
    PT = [cx.sb([128, TT], BF16, "PT") for _ in range(3)]
    cvT = [cx.sb([128, 2, TT], F32, "cvT") for _ in range(2)]
    uT = cx.sb([128, 2, 2 + TT], F32, "uT")
    yT = cx.sb([128, 2, TT], F32, "yT")
    y2T = cx.sb([128, 2, TT], F32, "y2T")
    obT = cx.sb([128, 2, TT], BF16, "obT")
    ones4 = cx.sb([4, TT], F32, "ones4")
    lf = cx.sb([4, TT], F32, "lf")
    cc = cx.sb([4, TT], F32, "cc")
    cprev = cx.sb([4, 1], F32, "cprev")
    csp = cx.sb([4, 3, TT], BF16, "csp")
    csn = cx.sb([4, 3, TT], BF16, "csn")
    ctmp = cx.sb([4, TT], F32, "ctmp")
    ctmp2 = lf
    oc = [cx.sb([128, NS, 256], F32, "oc") for _ in range(2)]
    ocT = [cx.sb([128, 2, TT], BF16, "ocT") for _ in range(2)]
    r_ocT = [Res("ocT0"), Res("ocT1")]
    rinv = cx.sb([128, 4], F32, "rinv")
    stats = cx.sb([128, 2, 6], F32, "stats")
    mv = cx.sb([128, 2], F32, "mv")
    rstd = cx.sb([128, 1], F32, "rstd")

    TR = cx.ps([128, 1024], F32, "TR")
    FM = [cx.ps([128, 512], F32, "FM") for _ in range(2)]
    ST = [cx.ps([128, 512], F32, "ST") for _ in range(2)]
    OA = [cx.ps([128, 512], F32, "OA") for _ in range(2)]
    rTR = Res("TR", True)
    rFM = [Res("FM0", True), Res("FM1", True)]
    rST = [Res("ST0", True), Res("ST1", True)]
    rOA = [Res("OA0", True), Res("OA1", True)]

    r_c = Res("consts")
    r_w = Res("weights")
    r_xs = [Res(f"xs{i}") for i in range(2)]
    r_xT = Res("xT")
    r_K = [[Res(f"K{h}_{i}") for i in range(NSB)] for h in range(H)]
    r_V = [Res(f"V{i}") for i in range(NSB)]
    r_Q = [[Res(f"Q{b}{h}") for h in range(H)] for b in range(2)]
    r_PT = [Res(f"PT{i}") for i in range(3)]
    r_cv = [Res("cvb"), Res("cvc")]
    r_u, r_y, r_ob, r_y2 = Res("u"), Res("y"), Res("ob"), Res("y2")
    r_f = Res("f")
    r_cs = Res("cs")
    r_oc = [Res("oc0"), Res("oc1")]
    r_rinv = Res("rinv")
    r_stat = Res("stat")

    P.dma("sp", ident[:, :], ident_d[:, :], writes=[r_c])
    P.dma("sp", maskf[:, :], mask_d[:, :], writes=[r_c])
    P.dma("sp", lnB[:, :, :], lnB_d.rearrange("a p d -> p a d"), writes=[r_c])
    P.dma("sp", cw[:, :], cw_d[:, :], writes=[r_c])
    P.dma("sp", bf[:, :], bf_d[:, :], writes=[r_c])
    for wt, wd_, n in ((wc, wc_d, 768), (wq, wq_d, 256), (wk, wk_d, 256), (wv, wv_d, 256), (wf, wf_d, 4)):
        P.dma("pool", wt[:, :, :], wd_.rearrange("(kc p) c -> p kc c", p=128), writes=[r_w])
    r_c2 = Res("consts2")
    P.op("dve", lambda e: e.tensor_copy(out=identb[:, :], in_=ident[:, :]), reads=[r_c], writes=[r_c2])
    P.op("dve", lambda e: e.tensor_copy(out=maskb[:, :], in_=maskf[:, :]), reads=[r_c], writes=[r_c2])
    P.op("dve", lambda e: e.tensor_scalar(out=nbf[:, :], in0=bf[:, :], scalar1=-1.0, scalar2=None, op0=ALU.mult),
         reads=[r_c], writes=[r_c2])
    P.op("dve", lambda e: e.memset(ones4[:, :], 1.0), writes=[r_c2])
    P.op("dve", lambda e: e.memset(cprev[:, :], 0.0), writes=[r_cs])
    P.op("dve", lambda e: e.memset(uT[:, :, :], 0.0), writes=[r_u])
    P.op("pool", lambda e: e.memset(Vaug[:, :, :, :], 1.0), writes=r_V)
    for h in range(H):
        P.op("pool", lambda e, h=h: e.memset(Kaug[h][64:70, :], 1.0), writes=r_K[h])
        for b in range(2):
            P.op("pool", lambda e, h=h, b=b: e.memset(Qaug[b][h][64:70, :], 1.0), writes=[r_Q[b][h]])

    fm_i = [0]

    def next_fm():
        i = fm_i[0] % 2
        fm_i[0] += 1
        return FM[i], rFM[i]

    out_tags = []

    def proj(sb_i):
        t0 = sb_i * TT
        qb = sb_i % 2
        for s in range(NS):
            yield
            xb_ = s % 2
            P.dma("sp", xs[:, xb_, :], xsrc(t0 + s * 128, 128), writes=[r_xs[xb_]])
            if do_ln0:
                layer_norm_rows(P, nc, xs[:, xb_, :], r_xs[xb_], lnB[:, 0, :], lnB[:, 1, :], stats, mv, rstd, r_stat, r_c)
            for kc in range(8):
                P.op("pe", lambda e, kc=kc: e.transpose(out=TR[:, kc * 128:(kc + 1) * 128],
                                                        in_=xs[:, xb_, kc * 128:(kc + 1) * 128], identity=ident[:, :]),
                     reads=[r_xs[xb_], r_c], writes=[rTR], inc=(kc == 7))
            P.op("dve", lambda e: e.tensor_copy(out=xT[:, :, s * 128:(s + 1) * 128],
                                                in_=TR[:, :].rearrange("p (a t) -> p a t", a=8)),
                 reads=[rTR], writes=[r_xT])
        yield
        for grp in range(3):
            for hf in range(2):
                yield
                ps, ps_r = next_fm()
                c0 = grp * 256 + hf * 128
                for kc in range(8):
                    P.op("pe", lambda e, kc=kc: e.matmul(ps[:, 0:TT], lhsT=wc[:, kc, c0:c0 + 128], rhs=xT[:, kc, :],
                                                         start=(kc == 0), stop=(kc == 7)),
                         reads=[r_w, r_xT], writes=[ps_r], inc=(kc == 7))
                if grp < 2:
                    P.op("dve", lambda e: e.tensor_copy(out=cvT[grp][:, hf, :], in_=ps[:, 0:TT]),
                         reads=[ps_r], writes=[r_cv[grp]])
                else:
                    P.op("dve", lambda e: e.tensor_tensor(out=uT[:, hf, 2:2 + TT], in0=ps[:, 0:TT],
                                                          in1=cvT[1][:, hf, :], op=ALU.mult),
                         reads=[ps_r, r_cv[1]], writes=[r_u])
        yield
        for hf in range(2):
            P.op("pool", lambda e: e.tensor_scalar(out=yT[:, hf, :], in0=uT[:, hf, 0:TT],
                                                   scalar1=cw[:, hf * 3:hf * 3 + 1], scalar2=None, op0=ALU.mult),
                 reads=[r_u, r_c], writes=[r_y])
            for tap in (1, 2):
                P.op("pool", lambda e, tap=tap: e.tensor_scalar(out=y2T[:, hf, :], in0=uT[:, hf, tap:tap + TT],
                                                                scalar1=cw[:, hf * 3 + tap:hf * 3 + tap + 1],
                                                                scalar2=None, op0=ALU.mult),
                     reads=[r_u, r_c], writes=[r_y2])
                P.op("pool", lambda e: e.tensor_tensor(out=yT[:, hf, :], in0=yT[:, hf, :], in1=y2T[:, hf, :],
                                                       op=ALU.add),
                     reads=[r_y, r_y2], writes=[r_y])
            P.op("pool", lambda e: e.tensor_tensor(out=obT[:, hf, :], in0=yT[:, hf, :], in1=cvT[0][:, hf, :],
                                                   op=ALU.mult),
                 reads=[r_y, r_cv[0]], writes=[r_ob])
        P.op("pool", lambda e: e.tensor_copy(out=uT[:, :, 0:2], in_=uT[:, :, TT:TT + 2]), reads=[r_u], writes=[r_u])
        out_tags.append(P.dma("sp", ob_dst(sb_i).rearrange("(hf p) t -> p hf t", p=128), obT[:, :, :],
                              reads=[r_ob]))
        if after_store is not None:
            after_store(sb_i, out_tags[-1])
        yield
        ps, ps_r = next_fm()
        for kc in range(8):
            P.op("pe", lambda e, kc=kc: e.matmul(ps[0:4, 0:TT], lhsT=wf[:, kc, :], rhs=xT[:, kc, :],
                                                 start=(kc == 0), stop=(kc == 7)),
                 reads=[r_w, r_xT], writes=[ps_r], inc=(kc == 7))
        P.op("act", lambda e: e.activation(out=lf[:, :], in_=ps[0:4, 0:TT], func=AF.Exp, bias=nbf[:, 0:1], scale=-1.0),
             reads=[ps_r, r_c2], writes=[r_f])
        P.op("act", lambda e: e.activation(out=lf[:, :], in_=lf[:, :], func=AF.Ln, bias=1.0, scale=1.0),
             reads=[r_f], writes=[r_f])
        P.op("dve", lambda e: e.tensor_scalar(out=lf[:, :], in0=lf[:, :], scalar1=-1.0, scalar2=None, op0=ALU.mult),
             reads=[r_f], writes=[r_f])
        P.op("dve", lambda e: e.tensor_tensor_scan(out=cc[:, :], data0=ones4[:, :], data1=lf[:, :],
                                                   initial=cprev[:, 0:1], op0=ALU.mult, op1=ALU.add),
             reads=[r_f, r_cs, r_c2], writes=[r_cs])
        P.op("dve", lambda e: e.tensor_copy(out=cprev[:, :], in_=cc[:, TT - 1:TT]), reads=[r_cs], writes=[r_cs])
        P.op("dve", lambda e: e.tensor_copy(out=csp[:, 0, :], in_=cc[:, :]), reads=[r_cs], writes=[r_cs])
        P.op("dve", lambda e: e.tensor_tensor(out=ctmp[:, :], in0=cc[:, :], in1=csp[:, 0, :], op=ALU.subtract),
             reads=[r_cs], writes=[r_cs])
        P.op("dve", lambda e: e.tensor_copy(out=csp[:, 1, :], in_=ctmp[:, :]), reads=[r_cs], writes=[r_cs])
        P.op("dve", lambda e: e.tensor_tensor(out=ctmp2[:, :], in0=ctmp[:, :], in1=csp[:, 1, :], op=ALU.subtract),
             reads=[r_cs], writes=[r_cs])
        P.op("dve", lambda e: e.tensor_copy(out=csp[:, 2, :], in_=ctmp2[:, :]), reads=[r_cs], writes=[r_cs])
        P.op("dve", lambda e: e.tensor_scalar(out=csn[:, :, :], in0=csp[:, :, :], scalar1=-1.0, scalar2=None,
                                              op0=ALU.mult), reads=[r_cs], writes=[r_cs])
        yield
        for h in range(H):
            yield
            ps, ps_r = next_fm()
            for kc in range(8):
                P.op("pe", lambda e, kc=kc: e.matmul(ps[0:64, 0:TT], lhsT=wq[:, kc, h * 64:(h + 1) * 64], rhs=xT[:, kc, :],
                                                     start=(kc == 0), stop=(kc == 7)),
                     reads=[r_w, r_xT], writes=[ps_r], inc=(kc == 7))
            P.op("act", lambda e: e.mul(out=Qaug[qb][h][0:64, :], in_=ps[0:64, 0:TT], mul=0.125),
                 reads=[ps_r], writes=[r_Q[qb][h]])
            for jj in range(3):
                P.dma("sp", Qaug[qb][h][64 + jj:65 + jj, :], csp[h:h + 1, jj, :], reads=[r_cs], writes=[r_Q[qb][h]],
                      sem_res=r_Q[qb][h])
            ps, ps_r = next_fm()
            for kc in range(8):
                P.op("pe", lambda e, kc=kc: e.matmul(ps[0:64, 0:TT], lhsT=wk[:, kc, h * 64:(h + 1) * 64], rhs=xT[:, kc, :],
                                                     start=(kc == 0), stop=(kc == 7)),
                     reads=[r_w, r_xT], writes=[ps_r], inc=(kc == 7))
            P.op("dve", lambda e: e.tensor_copy(out=Kaug[h][0:64, t0:t0 + TT], in_=ps[0:64, 0:TT]),
                 reads=[ps_r], writes=[r_K[h][sb_i]])
            for jj in range(3):
                P.dma("sp", Kaug[h][67 + jj:68 + jj, t0:t0 + TT], csn[h:h + 1, jj, :], reads=[r_cs],
                      writes=[r_K[h][sb_i]], sem_res=r_K[h][sb_i])
        for s in range(NS):
            yield
            ps, ps_r = next_fm()
            for kc in range(8):
                P.op("pe", lambda e, kc=kc: e.matmul(ps[:, 0:256], lhsT=xT[:, kc, s * 128:(s + 1) * 128], rhs=wv[:, kc, :],
                                                     start=(kc == 0), stop=(kc == 7)),
                     reads=[r_w, r_xT], writes=[ps_r], inc=(kc == 7))
            P.op("dve", lambda e: e.tensor_copy(out=Vaug[:, sb_i * NS + s, :, 0:64],
                                                in_=ps[:, 0:256].rearrange("p (h d) -> p h d", h=H)),
                 reads=[ps_r], writes=[r_V[sb_i]])
        yield
    gen = proj(0)
    for _ in gen:
        pass
    for sb_i in range(NSB):
        t0 = sb_i * TT
        qb = sb_i % 2
        gen = proj(sb_i + 1) if sb_i + 1 < NSB else iter(())
        items = []
        for h in range(H):
            nkb = sb_i * NS + NS
            for j in range(nkb):
                items.append((h, j))
        ob_i = sb_i % 2

        def emit_S(idx):
            h, j = items[idx]
            st, st_r = ST[idx % 2], rST[idx % 2]
            dj = j - sb_i * NS
            qlo = max(0, dj) * 128
            N = TT - qlo
            if dj >= 0:
                P.op("pe", lambda e: e.matmul(st[:, 0:128], lhsT=Kaug[h][0:70, j * 128:(j + 1) * 128],
                                              rhs=Qaug[qb][h][0:70, qlo:qlo + 128], start=True, stop=False),
                     reads=[r_K[h][j // NS], r_Q[qb][h]], writes=[st_r], inc=False)
                P.op("pe", lambda e: e.matmul(st[:, 0:128], lhsT=identb[:, :], rhs=maskb[:, :], start=False, stop=True),
                     reads=[r_c2], writes=[st_r], inc=(N == 128))
                if N > 128:
                    P.op("pe", lambda e: e.matmul(st[:, 128:N], lhsT=Kaug[h][0:70, j * 128:(j + 1) * 128],
                                                  rhs=Qaug[qb][h][0:70, qlo + 128:TT], start=True, stop=True),
                         reads=[r_K[h][j // NS], r_Q[qb][h]], writes=[st_r])
            else:
                P.op("pe", lambda e: e.matmul(st[:, 0:N], lhsT=Kaug[h][0:70, j * 128:(j + 1) * 128],
                                              rhs=Qaug[qb][h][0:70, qlo:TT], start=True, stop=True),
                     reads=[r_K[h][j // NS], r_Q[qb][h]], writes=[st_r])

        def emit_rest(idx):
            h, j = items[idx]
            st, st_r = ST[idx % 2], rST[idx % 2]
            pt, pt_r = PT[idx % 3], r_PT[idx % 3]
            dj = j - sb_i * NS
            qlo = max(0, dj) * 128
            N = TT - qlo
            nkb = sb_i * NS + NS
            oa, oa_r = OA[h % 2], rOA[h % 2]
            P.op("act", lambda e: e.activation(out=pt[:, 0:N], in_=st[:, 0:N], func=AF.Exp), reads=[st_r], writes=[pt_r])
            for qq in range(qlo // 128, NS):
                last_j = sb_i * NS + qq
                P.op("pe", lambda e, qq=qq: e.matmul(oa[:, qq * 128:qq * 128 + 65],
                                                     lhsT=pt[:, qq * 128 - qlo:qq * 128 - qlo + 128],
                                                     rhs=Vaug[:, j, h, :], start=(j == 0 and qq == 0),
                                                     stop=(j == last_j), skip_group_check=True),
                     reads=[pt_r, r_V[j // NS]], writes=[oa_r], inc=(qq == NS - 1))
            if j == nkb - 1:
                oav = oa[:, :].rearrange("p (q c) -> p q c", q=4)
                P.op("dve", lambda e: e.reciprocal(out=rinv[:, 0:NS], in_=oav[:, 0:NS, 64]), reads=[oa_r], writes=[r_rinv])
                for qq in range(NS):
                    P.op("dve", lambda e, qq=qq: e.tensor_scalar(out=oc[ob_i][:, qq, h * 64:(h + 1) * 64],
                                                                 in0=oa[:, qq * 128:qq * 128 + 64],
                                                                 scalar1=rinv[:, qq:qq + 1], scalar2=None, op0=ALU.mult),
                         reads=[oa_r, r_rinv], writes=[r_oc[ob_i]])

        emit_S(0)
        nsteps = max(1, -(-48 // len(items)))
        for idx in range(len(items)):
            if idx + 1 < len(items):
                emit_S(idx + 1)
            emit_rest(idx)
            for _ in range(nsteps):
                next(gen, None)
        for _ in gen:
            pass
        for hf in range(2):
            for qq in range(NS):
                P.op("pe", lambda e, hf=hf, qq=qq: e.transpose(out=TR[:, hf * 512 + qq * 128: hf * 512 + (qq + 1) * 128],
                                                               in_=oc[ob_i][:, qq, hf * 128:(hf + 1) * 128],
                                                               identity=ident[:, :]),
                     reads=[r_oc[ob_i], r_c], writes=[rTR], inc=(hf == 1 and qq == NS - 1))
        P.op("act", lambda e: e.copy(out=ocT[ob_i][:, :, :],
                                     in_=TR[:, :].rearrange("p (a t) -> p a t", a=2)[:, :, 0:TT]),
             reads=[rTR], writes=[r_ocT[ob_i]])
        out_tags.append(P.dma("sp", oc_dst(sb_i).rearrange("(hf p) t -> p hf t", p=128), ocT[ob_i][:, :, :],
                              reads=[r_ocT[ob_i]]))
        if after_store is not None:
            after_store(sb_i, out_tags[-1])
    return out_tags, cx


MASK = np.where(np.arange(128)[None, :] >= np.arange(128)[:, None], 0.0, NEG).astype(np.float32)


def fox_inputs(p, l, g):
    w_in = p["w_in"][l]
    c_off = RWKV_COLS
    f_off = RWKV_COLS + CONV_COLS
    if True:
        cs = slice(g * 256, (g + 1) * 256)
        wcv = np.concatenate([w_in[:, c_off + k * 512 + g * 256: c_off + k * 512 + (g + 1) * 256] for k in range(3)], 1)
        m = {
            "wc": np.ascontiguousarray(wcv),
            "wq": np.ascontiguousarray(w_in[:, f_off + g * 256: f_off + (g + 1) * 256]),
            "wk": np.ascontiguousarray(w_in[:, f_off + 512 + g * 256: f_off + 512 + (g + 1) * 256]),
            "wv": np.ascontiguousarray(w_in[:, f_off + 1024 + g * 256: f_off + 1024 + (g + 1) * 256]),
            "wf": np.ascontiguousarray(w_in[:, f_off + 1536 + g * 4: f_off + 1536 + (g + 1) * 4]),
            "bf": np.ascontiguousarray(p["b_forget"][l][g * 4:(g + 1) * 4].reshape(4, 1)),
            "cw": np.ascontiguousarray(p["conv_w"][l][:, cs].reshape(3, 2, 128).transpose(2, 1, 0).reshape(128, 6)),
            "lnB": np.ascontiguousarray(np.stack([_bc(p["ln0_g"]), _bc(p["ln0_b"])], 0)),
            "ident": IDENT, "mask": MASK,
        }
    return m


def run_fox(xb, p, l, do_ln0, n_cores=8):
    B, T, _ = xb.shape
    nc = build_fox(T, do_ln0)
    in_maps = []
    for c in range(n_cores):
        b, g = c // 2, c % 2
        m = fox_inputs(p, l, g)
        m["x"] = np.ascontiguousarray(xb[b])
        in_maps.append(m)
    res = run_bass_kernel_spmd(nc, in_maps, core_ids=list(range(n_cores)))
    obT = np.stack([np.concatenate([res.results[2 * b + g]["obT"] for g in range(2)], 0) for b in range(B)], 0)
    ocT = np.stack([np.concatenate([res.results[2 * b + g]["ocT"] for g in range(2)], 0) for b in range(B)], 0)
    return obT, ocT


RWKV_LN_EPS = 64e-5
RWKV_WINDOW = 16
NPJ = 2
DECAY_SCALE = -0.6065306597126334


class Sched:
    def __init__(self):
        self.tasks = []
        self.done = set()

    def add(self, name, gen, deps=()):
        self.tasks.append([name, gen, set(deps)])

    def run(self, window=10):
        active = []
        pending = list(self.tasks)
        while pending or active:
            i = 0
            while i < len(pending) and len(active) < window:
                t = pending[i]
                if t[2] <= self.done:
                    active.append(t)
                    pending.pop(i)
                else:
                    i += 1
            assert active, ("deadlock", [t[0] for t in pending[:5]])
            for t in list(active):
                try:
                    next(t[1])
                except StopIteration:
                    self.done.add(t[0])
                    active.remove(t)


def decl_rwkv(nc, pre):
    dram = lambda n, s_, dt=F32: nc.dram_tensor(pre + n, list(s_), dt, kind="ExternalInput").ap()
    return dict(wall=dram("wall", [D, 1024]), muB=dram("muB", [128, 1024]), chv=dram("chv", [128, 2, 8]),
                w2ia=dram("w2ia", [128, 256]), w2g=dram("w2g", [128, 256]), lnxB=dram("lnxB", [2, 128, 256]),
                lnB=dram("lnB", [2, 128, D]), ident=dram("ident", [128, 128]), mask4=dram("mask4", [128, 512]),
                maskL=dram("maskL", [128, 128]), bones=dram("bones", [128, 128]), sel=dram("sel", [128, 2]),
                smask=dram("smask", [128, 512]))


def build_rwkv(T, do_ln0):
    nc = bass.Bass("TRN2", target_bir_lowering=False)
    A = decl_rwkv(nc, "")
    x_d = nc.dram_tensor("x", [T, D], F32, kind="ExternalInput").ap()
    oaT_d = nc.dram_tensor("oaT", [256, T], BF16, kind="ExternalOutput").ap()
    cx0 = Ctx(nc)
    TTr = min(T, 512)
    tags, cx = emit_rwkv(nc, cx0.P, A, T, do_ln0, lambda t0, n: x_d[t0:t0 + n, :],
                         lambda ti: oaT_d[:, ti * TTr:(ti + 1) * TTr])
    cx0.P.finish(tags)
    cx.close()
    cx0.close()
    return nc


def emit_rwkv(nc, P, A, T, do_ln0, xsrc, oa_dst, after_store=None):
    cx = Ctx(nc, P)
    (wall_d, muB_d, chv_d, w2ia_d, w2g_d, lnxB_d, lnB_d, ident_d, mask4_d, maskL_d, bones_d, sel_d, smask_d) = (
        A[k] for k in ("wall", "muB", "chv", "w2ia", "w2g", "lnxB", "lnB", "ident", "mask4", "maskL", "bones", "sel",
                       "smask"))

    TT = min(T, 512)
    NCH = TT // 128
    NTI = T // TT

    ident = cx.sb([128, 128], F32, "ident")
    identb = cx.sb([128, 128], BF16, "identb")
    mask4 = cx.sb([128, 512], F32, "mask4")
    maskL = cx.sb([128, 128], F32, "maskL")
    bones = cx.sb([128, 128], F32, "bones")
    self_ = cx.sb([128, 2], F32, "self")
    selb = cx.sb([128, 2], BF16, "selb")
    smask = cx.sb([128, 512], F32, "smask")
    lnB = cx.sb([128, 2, D], F32, "lnB") if do_ln0 else None
    lnxB = cx.sb([128, 2, 256], F32, "lnxB")
    chv = cx.sb([128, 2, 8], F32, "chv")
    omka = cx.sb([128, 2], F32, "omka")
    muB = cx.sb([128, 1024], F32, "muB")
    wtmp = cx.sb([128, 512], F32, "wtmp")
    wtmp2 = cx.sb([128, 512], F32, "wtmp2")
    W0 = cx.sb([128, 8, 1024], BF16, "W0")
    W1 = cx.sb([128, 8, 1024], BF16, "W1")
    w2iaf = cx.sb([128, 256], F32, "w2iaf")
    w2ia = cx.sb([128, 256], BF16, "w2ia")
    w2gf = cx.sb([128, 256], F32, "w2gf")
    w2g = cx.sb([128, 256], BF16, "w2g")
    r_c = Res("consts")
    r_c2 = Res("consts2")
    r_wt = Res("wtmp")
    r_W = Res("W")

    for t_, d_ in ((ident, ident_d), (mask4, mask4_d), (maskL, maskL_d), (bones, bones_d), (self_, sel_d),
                   (smask, smask_d), (muB, muB_d), (w2iaf, w2ia_d), (w2gf, w2g_d)):
        P.dma("sp", t_[:, :], d_[:, :], writes=[r_c])
    if do_ln0:
        P.dma("sp", lnB[:, :, :], lnB_d.rearrange("a p d -> p a d"), writes=[r_c])
    P.dma("sp", lnxB[:, :, :], lnxB_d.rearrange("a p d -> p a d"), writes=[r_c])
    P.dma("sp", chv[:, :, :], chv_d[:, :, :], writes=[r_c])
    P.op("dve", lambda e: e.tensor_copy(out=identb[:, :], in_=ident[:, :]), reads=[r_c], writes=[r_c2])
    P.op("dve", lambda e: e.tensor_copy(out=selb[:, :], in_=self_[:, :]), reads=[r_c], writes=[r_c2])
    P.op("dve", lambda e: e.tensor_copy(out=w2ia[:, :], in_=w2iaf[:, :]), reads=[r_c], writes=[r_c2])
    P.op("dve", lambda e: e.tensor_copy(out=w2g[:, :], in_=w2gf[:, :]), reads=[r_c], writes=[r_c2])
    P.op("dve", lambda e: e.tensor_scalar(out=omka[:, :], in0=chv[:, :, 3], scalar1=-1.0, scalar2=1.0,
                                          op0=ALU.mult, op1=ALU.add), reads=[r_c], writes=[r_c2])
    for kc in range(8):
        for hf in range(2):
            fs_ = slice(hf * 512, (hf + 1) * 512)
            P.dma("sp", wtmp[:, :], wall_d[kc * 128:(kc + 1) * 128, fs_], writes=[r_wt])
            P.op("dve", lambda e: e.tensor_tensor(out=wtmp2[:, :], in0=wtmp[:, :], in1=muB[:, fs_], op=ALU.mult),
                 reads=[r_wt, r_c], writes=[r_W])
            P.op("dve", lambda e, kc=kc: e.tensor_copy(out=W1[:, kc, fs_], in_=wtmp2[:, :]), reads=[r_W], writes=[r_W])
            P.op("dve", lambda e, kc=kc: e.tensor_tensor(out=W0[:, kc, fs_], in0=wtmp[:, :], in1=wtmp2[:, :],
                                                         op=ALU.subtract),
                 reads=[r_wt, r_W], writes=[r_W])

    xs = cx.sb([128, 2, D], F32, "xs")
    xT = cx.sb([128, 8, 1 + TT], BF16, "xT")
    stats = cx.sb([128, 2, 6], F32, "stats")
    mv = cx.sb([128, 2], F32, "mv")
    rstd = cx.sb([128, 1], F32, "rstd")
    r_xs = [Res(f"xs{i}") for i in range(2)]
    r_xT = Res("xT")
    r_stat = Res("stat")
    def pt(name, dt=F32, share=True):
        t_ = cx.sb([128, TT], dt, name)
        return [t_, t_] if share else [t_, cx.sb([128, TT], dt, name)]
    rT, kT, aT, lw, cumI, cumE, Dexcl, invD, E2, kkr, sq, rn, kk, tmpa, kp, bT = (
        pt(n) for n in ("rT", "kT", "aT", "lw", "cumI", "cumE", "Dexcl", "invD", "E2", "kkr", "sq", "rn", "kk",
                        "tmpa", "kp", "bT"))
    BhT, KhT = pt("BhT", share=False), pt("KhT", share=False)
    Dincl1 = cx.sb([128, TT], F32, "Dincl1")
    r_bk = [Res("bhkh0"), Res("bhkh1")]
    lo0T = cx.sb([128, TT], BF16, "lo0T")
    sgT = cx.sb([128, TT], BF16, "sgT")
    _tres = {}

    def qr(t_):
        return _tres.setdefault(id(t_), Res("tmp"))
    r_lo = Res("lo")
    ARt = [[cx.sb([128, NCH, 256], BF16, "ARt") for _ in range(2)] for _ in range(2)]
    BtT = [[cx.sb([128, TT], BF16, "BtT") for _ in range(2)] for _ in range(2)]
    KtT = [[cx.sb([128, TT], BF16, "KtT") for _ in range(2)] for _ in range(2)]
    rkT = [[cx.sb([128, TT], BF16, "rkT") for _ in range(2)] for _ in range(2)]
    DC = [[cx.sb([128, NCH], F32, "DC") for _ in range(2)] for _ in range(2)]
    BKh = [[cx.sb([128, 512], BF16, "BKh") for _ in range(NCH)] for _ in range(2)]
    Vb = [[cx.sb([128, 256], BF16, "Vb") for _ in range(NCH)] for _ in range(2)]
    Gt = [[cx.sb([128, 256], BF16, "Gt") for _ in range(NCH)] for _ in range(2)]
    r_AR = [[Res(f"AR{a}{b}") for b in range(2)] for a in range(2)]
    r_Bt = [[Res(f"Bt{a}{b}") for b in range(2)] for a in range(2)]
    r_Kt = [[Res(f"Kt{a}{b}") for b in range(2)] for a in range(2)]
    r_rk = [[Res(f"rk{a}{b}") for b in range(2)] for a in range(2)]
    r_Di = [[Res(f"Di{a}{b}") for b in range(2)] for a in range(2)]
    r_BKh = [[Res(f"BKh{a}{c}") for c in range(NCH)] for a in range(2)]
    r_Vb = [[Res(f"Vb{a}{c}") for c in range(NCH)] for a in range(2)]
    r_Gt = [[Res(f"Gt{a}{c}") for c in range(NCH)] for a in range(2)]
    NSLOT = NCH
    NM = [[cx.sb([128, 512], BF16, "NM") for _ in range(4)] for _ in range(NSLOT)]
    r_NM = [[Res(f"NM{a}{h}") for h in range(4)] for a in range(NSLOT)]
    Xb = [[[cx.sb([128, 128], BF16, "X") for _ in range(2)] for _ in range(4)] for _ in range(NSLOT)]
    r_X = [[[Res(f"X{a}{h}{k}") for k in range(2)] for h in range(4)] for a in range(NSLOT)]
    NL = [[[cx.sb([128, 256], BF16, "NL") for _ in range(2)] for _ in range(4)] for _ in range(NSLOT)]
    r_NL = [[[Res(f"NL{a}{h}{k}") for k in range(2)] for h in range(4)] for a in range(NSLOT)]
    Sf = [cx.sb([128, 128], F32, "Sf") for _ in range(2)]
    Sb = [cx.sb([128, 128], BF16, "Sb") for _ in range(2)]
    r_S = [Res("S0"), Res("S1")]
    brp = [cx.sb([128, 128], BF16, "brp") for _ in range(2)]
    Wbp = [cx.sb([128, 128], BF16, "Wbp") for _ in range(2)]
    r_br = [Res("br0"), Res("br1")]
    r_Wb = [Res("Wb0"), Res("Wb1")]
    Ysb = [cx.sb([128, 256], F32, "Ysb") for _ in range(2)]
    r_Y = [[Res(f"Y{a}{p}") for p in range(2)] for a in range(2)]
    yn = cx.sb([128, 256], F32, "yn")
    ost = cx.sb([128, 4, 6], F32, "ost")
    omv = cx.sb([128, 4, 2], F32, "omv")
    orstd = cx.sb([128, 4], F32, "orstd")
    bsb = cx.sb([128, 4], F32, "bsb")
    ob = [cx.sb([128, 256], F32, "ob") for _ in range(2)]
    oaT = [cx.sb([128, 2, TT], BF16, "oaT") for _ in range(2)]
    r_o = Res("ostuff")
    r_ob = [Res("ob0"), Res("ob1")]
    r_oaT = [Res("oaT0"), Res("oaT1")]

    PJ = [cx.ps([128, 512], F32, "PJ") for _ in range(NPJ)]
    INV = [cx.ps([128, 512], F32, "INV") for _ in range(6 - NPJ)]
    SEQ = [cx.ps([128, 512], F32, "SEQ") for _ in range(2)]
    rPJ = [Res(f"PJ{i}", True) for i in range(NPJ)]
    rINV = [Res(f"INV{i}", True) for i in range(6 - NPJ)]
    rSEQ = [Res("SEQ0", True), Res("SEQ1", True)]
    pj_i = [0]
    inv_i = [0]

    def next_pj():
        i = pj_i[0] % NPJ
        pj_i[0] += 1
        return PJ[i], rPJ[i]

    def next_inv():
        i = inv_i[0] % (6 - NPJ)
        inv_i[0] += 1
        return INV[i], rINV[i]

    P.op("dve", lambda e: e.memset(xT[:, :, :], 0.0), writes=[r_xT])
    for p in range(2):
        P.op("dve", lambda e, p=p: e.memset(Sf[p][:, :], 0.0), writes=[r_S[p]])
        P.op("dve", lambda e, p=p: e.memset(Sb[p][:, :], 0.0), writes=[r_S[p]])

    out_tags = []
    chs = lambda p, i: chv[:, p, i:i + 1]

    def c3(ap):
        return ap.rearrange("p (c t) -> p c t", c=NCH)

    def prep(ti):
        par = ti % 2
        t0 = ti * TT
        if ti > 0:
            P.op("pool", lambda e: e.tensor_copy(out=xT[:, :, 0:1], in_=xT[:, :, TT:TT + 1]), reads=[r_xT], writes=[r_xT])
        for s in range(NCH):
            xb_ = s % 2
            P.dma("sp", xs[:, xb_, :], xsrc(t0 + s * 128, 128), writes=[r_xs[xb_]])
            if do_ln0:
                layer_norm_rows(P, nc, xs[:, xb_, :], r_xs[xb_], lnB[:, 0, :], lnB[:, 1, :], stats, mv, rstd, r_stat, r_c)
            for half in range(2):
                TR, rTR = next_pj()
                for k4 in range(4):
                    kc = half * 4 + k4
                    P.op("pe", lambda e, kc=kc, k4=k4: e.transpose(out=TR[:, k4 * 128:(k4 + 1) * 128],
                                                                   in_=xs[:, xb_, kc * 128:(kc + 1) * 128],
                                                                   identity=ident[:, :]),
                         reads=[r_xs[xb_], r_c], writes=[rTR], inc=(k4 == 3))
                P.op("act", lambda e, half=half: e.copy(out=xT[:, half * 4:(half + 1) * 4, 1 + s * 128:1 + (s + 1) * 128],
                                                        in_=TR[:, :].rearrange("p (a t) -> p a t", a=4)),
                     reads=[rTR], writes=[r_xT])
            yield

        def proj_cm(c0, ncols=128):
            ps, ps_r = next_pj()
            for kc in range(8):
                P.op("pe", lambda e, kc=kc: e.matmul(ps[0:ncols, 0:TT], lhsT=W0[:, kc, c0:c0 + ncols],
                                                     rhs=xT[:, kc, 1:1 + TT], start=(kc == 0), stop=False),
                     reads=[r_W, r_xT], writes=[ps_r], inc=False)
            for kc in range(8):
                P.op("pe", lambda e, kc=kc: e.matmul(ps[0:ncols, 0:TT], lhsT=W1[:, kc, c0:c0 + ncols],
                                                     rhs=xT[:, kc, 0:TT], start=False, stop=(kc == 7)),
                     reads=[r_W, r_xT], writes=[ps_r], inc=(kc == 7))
            return ps, ps_r

        ps, ps_r = proj_cm(768)
        P.op("act", lambda e: e.activation(out=lo0T[0:64, :], in_=ps[0:64, 0:TT], func=AF.Tanh), reads=[ps_r], writes=[r_lo])
        P.op("act", lambda e: e.copy(out=lo0T[64:128, :], in_=ps[64:128, 0:TT]), reads=[ps_r], writes=[r_lo])
        ps, ps_r = proj_cm(896)
        P.op("act", lambda e: e.activation(out=sgT[:, :], in_=ps[:, 0:TT], func=AF.Sigmoid), reads=[ps_r], writes=[r_lo])
        yield
        for p in range(2):
            ps, ps_r = proj_cm(p * 128)
            P.op("act", lambda e: e.copy(out=rT[p][:, :], in_=ps[:, 0:TT]), reads=[ps_r], writes=[qr(rT[p])])
            ps, ps_r = proj_cm(256 + p * 128)
            P.op("act", lambda e: e.copy(out=kT[p][:, :], in_=ps[:, 0:TT]), reads=[ps_r], writes=[qr(kT[p])])
            ps, ps_r = next_pj()
            P.op("pe", lambda e: e.matmul(ps[:, 0:TT], lhsT=w2ia[0:64, p * 128:(p + 1) * 128], rhs=lo0T[0:64, :],
                                          start=True, stop=True), reads=[r_c2, r_lo], writes=[ps_r])
            P.op("act", lambda e: e.activation(out=lw[p][:, :], in_=ps[:, 0:TT], func=AF.Sigmoid, bias=chs(p, 0), scale=1.0),
                 reads=[ps_r, r_c], writes=[qr(lw[p])])
            ps, ps_r = next_pj()
            P.op("pe", lambda e: e.matmul(ps[:, 0:TT], lhsT=w2ia[64:128, p * 128:(p + 1) * 128], rhs=lo0T[64:128, :],
                                          start=True, stop=True), reads=[r_c2, r_lo], writes=[ps_r])
            P.op("act", lambda e: e.activation(out=aT[p][:, :], in_=ps[:, 0:TT], func=AF.Sigmoid, bias=chs(p, 1), scale=1.0),
                 reads=[ps_r, r_c], writes=[qr(aT[p])])
            yield
            P.op("dve", lambda e: e.tensor_scalar(out=lw[p][:, :], in0=lw[p][:, :], scalar1=DECAY_SCALE, scalar2=None,
                                                  op0=ALU.mult), reads=[qr(lw[p])], writes=[qr(lw[p])])
            P.op("dve", lambda e: e.tensor_tensor_scan(out=cumI[p][:, :], data0=smask[:, 0:TT], data1=lw[p][:, :],
                                                       initial=0.0, op0=ALU.mult, op1=ALU.add),
                 reads=[qr(lw[p]), r_c], writes=[qr(cumI[p])])
            P.op("pool", lambda e: e.tensor_tensor(out=cumE[p][:, :], in0=cumI[p][:, :], in1=lw[p][:, :], op=ALU.subtract),
                 reads=[qr(cumI[p]), qr(lw[p])], writes=[qr(cumE[p])])
            P.op("act", lambda e: e.activation(out=Dincl1[:, :], in_=cumI[p][:, :], func=AF.Exp),
                 reads=[qr(cumI[p])], writes=[qr(Dincl1)])
            P.op("dve", lambda e: e.tensor_copy(out=DC[par][p][:, :], in_=c3(Dincl1[:, :])[:, :, 127]),
                 reads=[qr(Dincl1)], writes=[r_Di[par][p]])
            P.op("act", lambda e: e.activation(out=invD[p][:, :], in_=cumI[p][:, :], func=AF.Exp, scale=-1.0),
                 reads=[qr(cumI[p])], writes=[qr(invD[p])])
            P.op("act", lambda e: e.activation(out=Dexcl[p][:, :], in_=cumE[p][:, :], func=AF.Exp),
                 reads=[qr(cumE[p])], writes=[qr(Dexcl[p])])
            for c in range(NCH):
                P.op("act", lambda e, c=c: e.activation(out=E2[p][:, c * 128:(c + 1) * 128],
                                                        in_=cumI[p][:, c * 128:(c + 1) * 128], func=AF.Exp,
                                                        bias=cumI[p][:, c * 128 + 127:c * 128 + 128], scale=-1.0),
                     reads=[qr(cumI[p])], writes=[qr(E2[p])])
            yield
            P.op("pool", lambda e: e.tensor_scalar(out=kkr[p][:, :], in0=kT[p][:, :], scalar1=chs(p, 2), scalar2=None,
                                                   op0=ALU.mult), reads=[qr(kT[p]), r_c], writes=[qr(kkr[p])])
            P.op("pool", lambda e: e.tensor_tensor(out=sq[p][:, :], in0=kkr[p][:, :], in1=kkr[p][:, :], op=ALU.mult),
                 reads=[qr(kkr[p])], writes=[qr(sq[p])])
            ps, ps_r = next_pj()
            P.op("pe", lambda e: e.matmul(ps[:, 0:TT], lhsT=bones[:, :], rhs=sq[p][:, :], start=True, stop=True),
                 reads=[r_c, qr(sq[p])], writes=[ps_r])
            P.op("act", lambda e: e.activation(out=rn[p][:, :], in_=ps[:, 0:TT], func=AF.Sqrt), reads=[ps_r],
                 writes=[qr(rn[p])])
            P.op("dve", lambda e: e.tensor_scalar(out=rn[p][:, :], in0=rn[p][:, :], scalar1=1e-12, scalar2=None,
                                                  op0=ALU.max), reads=[qr(rn[p])], writes=[qr(rn[p])])
            P.op("dve", lambda e: e.reciprocal(out=rn[p][:, :], in_=rn[p][:, :]), reads=[qr(rn[p])], writes=[qr(rn[p])])
            P.op("dve", lambda e: e.tensor_tensor(out=kk[p][:, :], in0=kkr[p][:, :], in1=rn[p][:, :], op=ALU.mult),
                 reads=[qr(kkr[p]), qr(rn[p])], writes=[qr(kk[p])])
            P.op("pool", lambda e: e.tensor_scalar(out=tmpa[p][:, :], in0=aT[p][:, :], scalar1=chs(p, 3),
                                                   scalar2=omka[:, p:p + 1], op0=ALU.mult, op1=ALU.add),
                 reads=[qr(aT[p]), r_c, r_c2], writes=[qr(tmpa[p])])
            P.op("pool", lambda e: e.tensor_tensor(out=kp[p][:, :], in0=kT[p][:, :], in1=tmpa[p][:, :], op=ALU.mult),
                 reads=[qr(kT[p]), qr(tmpa[p])], writes=[qr(kp[p])])
            yield
            P.op("dve", lambda e: e.scalar_tensor_tensor(out=ARt[par][p][:, :, 0:128], in0=c3(kk[p][:, :]), scalar=-1.0,
                                                         in1=c3(Dexcl[p][:, :]), op0=ALU.mult, op1=ALU.mult),
                 reads=[qr(kk[p]), qr(Dexcl[p])], writes=[r_AR[par][p]])
            P.op("pool", lambda e: e.tensor_tensor(out=ARt[par][p][:, :, 128:256], in0=c3(rT[p][:, :]),
                                                   in1=c3(Dincl1[:, :]), op=ALU.mult),
                 reads=[qr(rT[p]), qr(Dincl1)], writes=[r_AR[par][p]])
            P.op("pool", lambda e: e.tensor_tensor(out=bT[p][:, :], in0=kk[p][:, :], in1=aT[p][:, :], op=ALU.mult),
                 reads=[qr(kk[p]), qr(aT[p])], writes=[qr(bT[p])])
            P.op("dve", lambda e: e.tensor_tensor(out=BtT[par][p][:, :], in0=bT[p][:, :], in1=invD[p][:, :], op=ALU.mult),
                 reads=[qr(bT[p]), qr(invD[p])], writes=[r_Bt[par][p]])
            P.op("pool", lambda e: e.tensor_tensor(out=KtT[par][p][:, :], in0=kp[p][:, :], in1=invD[p][:, :], op=ALU.mult),
                 reads=[qr(kp[p]), qr(invD[p])], writes=[r_Kt[par][p]])
            P.op("dve", lambda e: e.tensor_tensor(out=BhT[p][:, :], in0=bT[p][:, :], in1=E2[p][:, :], op=ALU.mult),
                 reads=[qr(bT[p]), qr(E2[p])], writes=[r_bk[p]])
            P.op("pool", lambda e: e.tensor_tensor(out=KhT[p][:, :], in0=kp[p][:, :], in1=E2[p][:, :], op=ALU.mult),
                 reads=[qr(kp[p]), qr(E2[p]), r_bk[p]], writes=[r_bk[p]])
            P.op("dve", lambda e: e.scalar_tensor_tensor(out=rkT[par][p][:, :], in0=rT[p][:, :], scalar=chs(p, 4),
                                                         in1=kp[p][:, :], op0=ALU.mult, op1=ALU.mult),
                 reads=[qr(rT[p]), qr(kp[p]), r_c], writes=[r_rk[par][p]])
            yield
        for c in range(NCH):
            ps, ps_r = next_pj()
            for kc in range(8):
                P.op("pe", lambda e, kc=kc: e.matmul(ps[:, 0:256], lhsT=xT[:, kc, 1 + c * 128:1 + (c + 1) * 128],
                                                     rhs=W0[:, kc, 512:768], start=(kc == 0), stop=False),
                     reads=[r_W, r_xT], writes=[ps_r], inc=False)
            for kc in range(8):
                P.op("pe", lambda e, kc=kc: e.matmul(ps[:, 0:256], lhsT=xT[:, kc, c * 128:(c + 1) * 128],
                                                     rhs=W1[:, kc, 512:768], start=False, stop=(kc == 7)),
                     reads=[r_W, r_xT], writes=[ps_r], inc=(kc == 7))
            P.op("act", lambda e: e.copy(out=Vb[par][c][:, :], in_=ps[:, 0:256]), reads=[ps_r], writes=[r_Vb[par][c]])
            ps, ps_r = next_pj()
            P.op("pe", lambda e: e.matmul(ps[:, 0:256], lhsT=sgT[:, c * 128:(c + 1) * 128], rhs=w2g[:, :], start=True, stop=True),
                 reads=[r_lo, r_c2], writes=[ps_r])
            P.op("act", lambda e: e.copy(out=Gt[par][c][:, :], in_=ps[:, 0:256]), reads=[ps_r], writes=[r_Gt[par][c]])
            TR, rTR = next_pj()
            for q in range(4):
                src = (BhT, KhT)[q // 2][q % 2]
                P.op("pe", lambda e, q=q, src=src: e.transpose(out=TR[:, q * 128:(q + 1) * 128],
                                                               in_=src[:, c * 128:(c + 1) * 128], identity=ident[:, :]),
                     reads=[r_bk[q % 2], r_c], writes=[rTR], inc=(q == 3))
            P.op("dve", lambda e: e.tensor_copy(out=BKh[par][c][:, :], in_=TR[:, :]), reads=[rTR], writes=[r_BKh[par][c]])
            yield

    def inv(ti, c, h):
        par = ti % 2
        slot = c
        sp = slot
        p, hb = h // 2, (h % 2) * 64
        cs = slice(c * 128, (c + 1) * 128)
        a12, a12_r = next_inv()
        P.op("pe", lambda e: e.matmul(a12[:, 0:256], lhsT=BtT[par][p][hb:hb + 64, cs], rhs=ARt[par][p][hb:hb + 64, c, :],
                                      start=True, stop=True),
             reads=[r_Bt[par][p], r_AR[par][p]], writes=[a12_r], inc=False)
        P.op("pe", lambda e: e.matmul(a12[:, 256:512], lhsT=KtT[par][p][hb:hb + 64, cs], rhs=ARt[par][p][hb:hb + 64, c, :],
                                      start=False, stop=True, skip_group_check=True),
             reads=[r_Kt[par][p], r_AR[par][p]], writes=[a12_r])
        a3, a3_r = next_inv()
        P.op("pe", lambda e: e.matmul(a3[:, 0:128], lhsT=ARt[par][p][hb:hb + 64, c, 0:128], rhs=BtT[par][p][hb:hb + 64, cs],
                                      start=True, stop=True),
             reads=[r_Bt[par][p], r_AR[par][p]], writes=[a3_r])
        nm, nm_r = NM[slot][h], r_NM[slot][h]
        P.op("dve", lambda e: e.tensor_tensor(out=nm[:, :], in0=a12[:, :], in1=mask4[:, :], op=ALU.mult),
             reads=[a12_r, r_c], writes=[nm_r])
        nl = NL[sp][h]
        nl_r = r_NL[sp][h]
        P.op("dve", lambda e: e.tensor_tensor(out=nl[0][:, 128:256], in0=a3[:, 0:128], in1=maskL[:, :], op=ALU.mult),
             reads=[a3_r, r_c], writes=[nl_r[0]])
        X, X_r = Xb[slot][h], r_X[slot][h]
        P.op("pool", lambda e: e.tensor_tensor(out=X[0][:, :], in0=nm[:, 0:128], in1=identb[:, :], op=ALU.add),
             reads=[nm_r, r_c2], writes=[X_r[0]])
        yield
        Np, Np_r = nm[:, 0:128], nm_r
        Lp, Lp_r = nl[0][:, 128:256], nl_r[0]
        xi = 0
        for rnd in range(6):
            last = (rnd == 5)
            pp = (rnd + 1) % 2
            bk, bk_r = next_inv()
            if not last:
                P.op("pe", lambda e: e.matmul(bk[:, 0:128], lhsT=Lp, rhs=Np, start=True, stop=True),
                     reads=[Lp_r, Np_r], writes=[bk_r], inc=False)
            P.op("pe", lambda e: e.matmul(bk[:, 128:256], lhsT=Np, rhs=Lp, start=last, stop=True,
                                          skip_group_check=True),
                 reads=[Lp_r, Np_r], writes=[bk_r])
            if not last:
                P.op("act", lambda e: e.copy(out=nl[pp][:, :], in_=bk[:, 0:256]), reads=[bk_r], writes=[nl_r[pp]])
            else:
                P.op("act", lambda e: e.copy(out=nl[pp][:, 128:256], in_=bk[:, 128:256]), reads=[bk_r], writes=[nl_r[pp]])
            Np, Np_r = nl[pp][:, 0:128], nl_r[pp]
            Lp, Lp_r = nl[pp][:, 128:256], nl_r[pp]
            yield
            px, px_r = next_inv()
            P.op("pe", lambda e: e.matmul(px[:, 0:128], lhsT=Lp, rhs=X[xi][:, :], start=True, stop=True),
                 reads=[Lp_r, X_r[xi]], writes=[px_r])
            P.op("dve", lambda e: e.tensor_tensor(out=X[1 - xi][:, :], in0=px[:, 0:128], in1=X[xi][:, :], op=ALU.add),
                 reads=[px_r, X_r[xi]], writes=[X_r[1 - xi]])
            xi = 1 - xi
            yield
        assert xi == 0

    def seq(ti, c, p):
        par = ti % 2
        slot = c
        sq_, sq_r = SEQ[p], rSEQ[p]
        cs = slice(c * 128, (c + 1) * 128)
        for hh in range(2):
            h = 2 * p + hh
            hb = hh * 64
            P.op("pe", lambda e: e.matmul(sq_[:, hh * 64:(hh + 1) * 64], lhsT=ARt[par][p][hb:hb + 64, c, 0:128],
                                          rhs=Sb[p][hb:hb + 64, hh * 64:(hh + 1) * 64], start=(hh == 0), stop=False,
                                          skip_group_check=True),
                 reads=[r_AR[par][p], r_S[p]], writes=[sq_r], inc=False)
            P.op("pe", lambda e: e.matmul(sq_[:, hh * 64:(hh + 1) * 64], lhsT=NM[slot][h][:, 256:384],
                                          rhs=Vb[par][c][:, h * 64:(h + 1) * 64], start=False, stop=True,
                                          skip_group_check=True),
                 reads=[r_NM[slot][h], r_Vb[par][c]], writes=[sq_r], inc=(hh == 1))
        P.op("act", lambda e: e.copy(out=brp[p][:, :], in_=sq_[:, 0:128]), reads=[sq_r], writes=[r_br[p]])
        yield
        for hh in range(2):
            h = 2 * p + hh
            P.op("pe", lambda e: e.matmul(sq_[:, 128 + hh * 64:128 + (hh + 1) * 64], lhsT=Xb[slot][h][0][:, :],
                                          rhs=brp[p][:, hh * 64:(hh + 1) * 64], start=False, stop=True,
                                          skip_group_check=True),
                 reads=[r_X[slot][h][0], r_br[p]], writes=[sq_r], inc=(hh == 1))
        P.op("dve", lambda e: e.tensor_copy(out=Wbp[p][:, :], in_=sq_[:, 128:256]), reads=[sq_r], writes=[r_Wb[p]])
        yield
        P.op("pe", lambda e: e.matmul(sq_[:, 256:384], lhsT=BKh[par][c][:, p * 128:(p + 1) * 128], rhs=Wbp[p][:, :],
                                      start=False, stop=False, skip_group_check=True),
             reads=[r_BKh[par][c], r_Wb[p]], writes=[sq_r], inc=False)
        P.op("pe", lambda e: e.matmul(sq_[:, 256:384], lhsT=BKh[par][c][:, 256 + p * 128:256 + (p + 1) * 128],
                                      rhs=Vb[par][c][:, p * 128:(p + 1) * 128], start=False, stop=True,
                                      skip_group_check=True),
             reads=[r_BKh[par][c], r_Vb[par][c]], writes=[sq_r], inc=False)
        for hh in range(2):
            h = 2 * p + hh
            hb = hh * 64
            yc = slice(384 + hh * 64, 384 + (hh + 1) * 64)
            P.op("pe", lambda e: e.matmul(sq_[:, yc], lhsT=ARt[par][p][hb:hb + 64, c, 128:256],
                                          rhs=Sb[p][hb:hb + 64, hh * 64:(hh + 1) * 64], start=False, stop=False,
                                          skip_group_check=True),
                 reads=[r_AR[par][p], r_S[p]], writes=[sq_r], inc=False)
            P.op("pe", lambda e: e.matmul(sq_[:, yc], lhsT=NM[slot][h][:, 128:256], rhs=Wbp[p][:, hh * 64:(hh + 1) * 64],
                                          start=False, stop=False, skip_group_check=True),
                 reads=[r_NM[slot][h], r_Wb[p]], writes=[sq_r], inc=False)
            P.op("pe", lambda e: e.matmul(sq_[:, yc], lhsT=NM[slot][h][:, 384:512], rhs=Vb[par][c][:, h * 64:(h + 1) * 64],
                                          start=False, stop=True, skip_group_check=True),
                 reads=[r_NM[slot][h], r_Vb[par][c]], writes=[sq_r], inc=(hh == 1))
        P.op("dve", lambda e: e.scalar_tensor_tensor(out=Sf[p][:, :], in0=Sf[p][:, :],
                                                     scalar=DC[par][p][:, c:c + 1],
                                                     in1=sq_[:, 256:384], op0=ALU.mult, op1=ALU.add),
             reads=[sq_r, r_S[p], r_Di[par][p]], writes=[r_S[p]])
        P.op("act", lambda e: e.copy(out=Sb[p][:, :], in_=Sf[p][:, :]), reads=[r_S[p]], writes=[r_S[p]])
        P.op("dve", lambda e: e.tensor_copy(out=Ysb[c % 2][:, p * 128:(p + 1) * 128], in_=sq_[:, 384:512]),
             reads=[sq_r], writes=[r_Y[c % 2][p]])
        yield

    def outp(ti, c):
        par = ti % 2
        t0 = ti * TT + c * 128
        Y = Ysb[c % 2]
        ry = r_Y[c % 2]
        cs = slice(c * 128, (c + 1) * 128)
        for h in range(4):
            P.op("dve", lambda e, h=h: e.bn_stats(out=ost[:, h, :], in_=Y[:, h * 64:(h + 1) * 64]), reads=ry, writes=[r_o])
        for h in range(4):
            P.op("dve", lambda e, h=h: e.bn_aggr(out=omv[:, h, :], in_=ost[:, h, :]), reads=[r_o], writes=[r_o])
        P.op("act", lambda e: e.activation(out=orstd[:, :], in_=omv[:, :, 1], func=AF.Sqrt, bias=float(RWKV_LN_EPS), scale=1.0),
             reads=[r_o], writes=[r_o])
        P.op("dve", lambda e: e.reciprocal(out=orstd[:, :], in_=orstd[:, :]), reads=[r_o], writes=[r_o])
        yield
        for h in range(4):
            P.op("dve", lambda e, h=h: e.tensor_scalar(out=yn[:, h * 64:(h + 1) * 64], in0=Y[:, h * 64:(h + 1) * 64],
                                                       scalar1=omv[:, h, 0:1], scalar2=orstd[:, h:h + 1],
                                                       op0=ALU.subtract, op1=ALU.mult),
                 reads=ry + [r_o], writes=[r_o])
        P.op("pool", lambda e: e.tensor_tensor(out=yn[:, :], in0=yn[:, :], in1=lnxB[:, 0, :], op=ALU.mult),
             reads=[r_o, r_c], writes=[r_o])
        P.op("pool", lambda e: e.tensor_tensor(out=yn[:, :], in0=yn[:, :], in1=lnxB[:, 1, :], op=ALU.add),
             reads=[r_o, r_c], writes=[r_o])
        ps, ps_r = next_pj()
        for p in range(2):
            P.op("pe", lambda e, p=p: e.matmul(ps[:, p * 2:(p + 1) * 2], lhsT=rkT[par][p][:, cs], rhs=selb[:, :],
                                               start=(p == 0), stop=True, skip_group_check=True),
                 reads=[r_rk[par][p], r_c2], writes=[ps_r], inc=(p == 1))
        P.op("dve", lambda e: e.tensor_copy(out=bsb[:, :], in_=ps[:, 0:4]), reads=[ps_r], writes=[r_o])
        yield
        for h in range(4):
            P.op("dve", lambda e, h=h: e.scalar_tensor_tensor(out=yn[:, h * 64:(h + 1) * 64],
                                                              in0=Vb[par][c][:, h * 64:(h + 1) * 64],
                                                              scalar=bsb[:, h:h + 1], in1=yn[:, h * 64:(h + 1) * 64],
                                                              op0=ALU.mult, op1=ALU.add),
                 reads=[r_o, r_Vb[par][c]], writes=[r_o])
        oi = c % 2
        P.op("pool", lambda e: e.tensor_tensor(out=ob[oi][:, :], in0=yn[:, :], in1=Gt[par][c][:, :], op=ALU.mult),
             reads=[r_o, r_Gt[par][c]], writes=[r_ob[oi]])
        TR, rTR = next_pj()
        for pp in range(2):
            P.op("pe", lambda e, pp=pp: e.transpose(out=TR[:, pp * 128:(pp + 1) * 128], in_=ob[oi][:, pp * 128:(pp + 1) * 128],
                                                    identity=ident[:, :]),
                 reads=[r_ob[oi], r_c], writes=[rTR], inc=(pp == 1))
        P.op("act", lambda e: e.copy(out=oaT[par][:, :, c * 128:(c + 1) * 128],
                                     in_=TR[:, 0:256].rearrange("p (a t) -> p a t", a=2)),
             reads=[rTR], writes=[r_oaT[par]])
        if c == NCH - 1:
            out_tags.append(P.dma("sp", oa_dst(ti).rearrange("(pp p) t -> p pp t", p=128),
                                  oaT[par][:, :, :], reads=[r_oaT[par]]))
            if after_store is not None:
                after_store(ti, out_tags[-1])
        yield

    S = Sched()

    def add_prep(ti):
        deps = [f"prep{ti - 1}"] if ti > 0 else []
        if ti > 1:
            deps += [f"out{ti - 2}_{NCH - 1}"]
        S.add(f"prep{ti}", prep(ti), deps)

    add_prep(0)
    for ti in range(NTI):
        if ti + 1 < NTI:
            add_prep(ti + 1)
        for c in range(NCH):
            for h in range(4):
                d = [f"prep{ti}"]
                if ti >= 1:
                    d.append(f"seq{ti - 1}_{c}_{h // 2}")
                S.add(f"inv{ti}_{c}_{h}", inv(ti, c, h), d)
        for c in range(NCH):
            for p in range(2):
                d = [f"inv{ti}_{c}_{2 * p}", f"inv{ti}_{c}_{2 * p + 1}"]
                if c > 0:
                    d.append(f"seq{ti}_{c - 1}_{p}")
                elif ti > 0:
                    d.append(f"seq{ti - 1}_{NCH - 1}_{p}")
                S.add(f"seq{ti}_{c}_{p}", seq(ti, c, p), d)
            d = [f"seq{ti}_{c}_0", f"seq{ti}_{c}_1"]
            if c > 0:
                d.append(f"out{ti}_{c - 1}")
            elif ti > 0:
                d.append(f"out{ti - 1}_{NCH - 1}")
            S.add(f"out{ti}_{c}", outp(ti, c), d)
    S.run(window=RWKV_WINDOW)
    return out_tags, cx


_ar = np.arange(128)
MASK4 = np.concatenate([(_ar[:, None] < _ar[None, :]), (_ar[:, None] <= _ar[None, :])] * 2, 1).astype(np.float32)
MASKL = (_ar[None, :] < _ar[:, None]).astype(np.float32)
BONES = (_ar[:, None] // 64 == _ar[None, :] // 64).astype(np.float32)
SEL = (_ar[:, None] // 64 == np.arange(2)[None, :]).astype(np.float32)
SMASK = np.ascontiguousarray(np.broadcast_to((np.arange(512) % 128 != 0).astype(np.float32)[None, :], (128, 512)))


def rwkv_inputs(p, l, g):
    w_in = p["w_in"][l]
    mu = p["mu_shift"][l]
    if True:
        gc = slice(g * 256, (g + 1) * 256)
        cols = np.concatenate([np.arange(g * 256, (g + 1) * 256), 512 + np.arange(g * 256, (g + 1) * 256),
                               1024 + np.arange(g * 256, (g + 1) * 256), np.arange(1536, 1792)])
        vecs = [p["w0_decay"][l][gc], p["a0"][l][gc], p["k_k"][l][gc], p["k_a"][l][gc], p["r_k"][l].reshape(-1)[gc]]
        chv = np.zeros((128, 2, 8), np.float32)
        for i, v in enumerate(vecs):
            chv[:, :, i] = v.reshape(2, 128).T
        m = {
            "wall": np.ascontiguousarray(w_in[:, cols]),
            "muB": _bc(mu[cols]),
            "chv": chv,
            "w2ia": np.ascontiguousarray(np.concatenate([p["w2_decay"][l][:, gc], p["w2_iclr"][l][:, gc]], 0)),
            "w2g": np.ascontiguousarray(p["w2_gate"][l][:, gc]),
            "lnxB": np.ascontiguousarray(np.stack([_bc(p["lnx_g"][l][gc]), _bc(p["lnx_b"][l][gc])], 0)),
            "lnB": np.ascontiguousarray(np.stack([_bc(p["ln0_g"]), _bc(p["ln0_b"])], 0)),
            "ident": IDENT, "mask4": MASK4, "maskL": MASKL, "bones": BONES, "sel": SEL, "smask": SMASK,
        }
    return m


def run_rwkv(xb, p, l, do_ln0, n_cores=8):
    B, T, _ = xb.shape
    nc = build_rwkv(T, do_ln0)
    in_maps = []
    for c in range(n_cores):
        b, g = c // 2, c % 2
        m = rwkv_inputs(p, l, g)
        m["x"] = np.ascontiguousarray(xb[b])
        in_maps.append(m)
    res = run_bass_kernel_spmd(nc, in_maps, core_ids=list(range(n_cores)))
    oaT = np.stack([np.concatenate([res.results[2 * b + g]["oaT"] for g in range(2)], 0) for b in range(B)], 0)
    return oaT


PAIRS = [[0, 1], [2, 3], [4, 5], [6, 7]]


def build_fused(T, ff_override=None):
    nc = bass.Bass("TRN2", target_bir_lowering=False)
    NT = T // 2
    TT = min(T, 512)
    CW = min(1024, NT)
    NCK = T // CW
    HCK = NCK // 2
    RY = min(512, NT)
    NYC = NT // RY
    x_d = nc.dram_tensor("x", [T, D], F32, kind="ExternalInput").ap()
    y_d = nc.dram_tensor("y", [NT, D], F32, kind="ExternalOutput").ap()
    A_r = [decl_rwkv(nc, f"r{l}_") for l in range(DEPTH)]
    A_f = [decl_fox(nc, f"f{l}_") for l in range(DEPTH)]
    A_m = [decl_merge(nc, f"m{l}_", moe=(l % 2 == 1), ff_override=ff_override) for l in range(DEPTH)]
    ola = [nc.dram_tensor(f"ola{l}", [NCK, 256, CW], BF16).ap() for l in range(DEPTH)]
    olf = [nc.dram_tensor(f"olf{l}", [NCK, 512, CW], BF16).ap() for l in range(DEPTH)]
    oalla = [nc.dram_tensor(f"oalla{l}", [NCK, 512, CW], BF16).ap() for l in range(DEPTH)]
    oallf = [nc.dram_tensor(f"oallf{l}", [NCK, 1024, CW], BF16).ap() for l in range(DEPTH)]
    omia = [nc.dram_tensor(f"omia{l}", [HCK, 512, CW], BF16).ap() for l in range(DEPTH)]
    omif = [nc.dram_tensor(f"omif{l}", [HCK, 1024, CW], BF16).ap() for l in range(DEPTH)]
    yloc = nc.dram_tensor("yloc", [NT, D], F32).ap()
    yall = nc.dram_tensor("yall", [NYC, 2 * RY, D], F32).ap()
    xh = nc.dram_tensor("xh", [NT, D], F32).ap()
    cx0 = Ctx(nc)
    P = cx0.P
    pid = nc.sync.partition_id()
    g = pid % 2
    for i in range(2):
        P.dma("sp", xh[i * (NT // 2):(i + 1) * (NT // 2), :], x_d[bass.ds(g * NT + i * (NT // 2), NT // 2), :])

    def odst(buf, r0):
        def f(ti):
            j, c = (ti * TT) // CW, (ti * TT) % CW
            return buf[j, r0:r0 + 256, c:c + TT]
        return f

    def chunk_gather(src, dst, per_chunk):
        acc = {}

        def cb(ti, tag):
            j = (ti * TT) // CW
            acc.setdefault(j, []).append(tag)
            if len(acc[j]) == per_chunk:
                P.collective_allgather(src[j].opt(), dst[j].opt(), PAIRS, acc[j])
        return cb

    def xsrc1(t0, n):
        rank, j, r = t0 // NT, (t0 % NT) // RY, t0 % RY
        return yall[j, rank * RY + r: rank * RY + r + n, :]

    xsrc = lambda t0, n: x_d[t0:t0 + n, :]
    tags_m = []
    for l in range(DEPTH):
        do_ln0 = (l == 0)
        tags_r, cx = emit_rwkv(nc, P, A_r[l], T, do_ln0, xsrc, odst(ola[l], 0),
                               after_store=chunk_gather(ola[l], oalla[l], CW // TT))
        P.barrier()
        cx.close()
        tags_f, cx = emit_fox(nc, P, A_f[l], T, do_ln0, xsrc, odst(olf[l], 0), odst(olf[l], 256),
                              after_store=chunk_gather(olf[l], oallf[l], 2 * (CW // TT)))
        P.barrier()
        cx.close()
        P.dma("sp", omia[l].rearrange("j r c -> (j r) c"),
              oalla[l].rearrange("j r c -> (j r) c")[bass.ds(g * (HCK * 512), HCK * 512), :])
        P.dma("sp", omif[l].rearrange("j r c -> (j r) c"),
              oallf[l].rearrange("j r c -> (j r) c")[bass.ds(g * (HCK * 1024), HCK * 1024), :])
        P.barrier()
        if l == 0:
            x_rows = lambda t0, n: xh[t0:t0 + n, :]
        else:
            x_rows = lambda t0, n: yloc[t0:t0 + n, :]

        def o_rows(br, half, t0, n, l=l):
            jj, c = t0 // CW, t0 % CW
            if br == 0:
                return omia[l][jj, half * 256:(half + 1) * 256, c:c + n]
            return omif[l][jj, half * 512 + (br - 1) * 256: half * 512 + br * 256, c:c + n]

        last = (l == DEPTH - 1)
        dst = y_d if last else yloc
        cb = None
        if not last:
            accy = {}

            def cb(t0, tag):
                j = t0 // RY
                accy.setdefault(j, []).append(tag)
                if len(accy[j]) == RY // 128:
                    P.collective_allgather(yloc[j * RY:(j + 1) * RY, :].opt(), yall[j].opt(), PAIRS, accy[j])
        tags_m, cx = emit_merge(nc, P, A_m[l], NT, do_ln0, l % 2 == 1, x_rows, o_rows, dst, after_store=cb)
        P.barrier()
        cx.close()
        if not last:
            xsrc = xsrc1
    P.finish(tags_m)
    cx0.close()
    return nc


def fused_in_maps(x, p, n_cores=8):
    in_maps = []
    mcommon = [merge_inputs(p, l, l % 2 == 1) for l in range(DEPTH)]
    for c in range(n_cores):
        b, g = c // 2, c % 2
        m = {"x": np.ascontiguousarray(x[b])}
        for l in range(DEPTH):
            for k, v in rwkv_inputs(p, l, g).items():
                m[f"r{l}_{k}"] = v
            for k, v in fox_inputs(p, l, g).items():
                m[f"f{l}_{k}"] = v
            for k, v in mcommon[l].items():
                m[f"m{l}_{k}"] = v
        in_maps.append(m)
    return in_maps


def kernel(**inputs):
    p = {k: np.asarray(v) for k, v in inputs.items()}
    x = np.ascontiguousarray(p["x"], dtype=np.float32)
    B, T, _ = x.shape
    nc = build_fused(T)
    in_maps = fused_in_maps(x, p)
    res = run_bass_kernel_spmd(nc, in_maps, core_ids=list(range(8)))
    NT = T // 2
    out = np.empty((B, T, D), np.float32)
    for c in range(8):
        b, g = c // 2, c % 2
        out[b, g * NT:(g + 1) * NT] = res.results[c]["y"]
    return out
```

```python
import contextlib
import numpy as np
import ml_dtypes
import concourse.bass as bass
import concourse.mybir as mybir
from concourse.bass_utils import run_bass_kernel_spmd

F32 = mybir.dt.float32
BF16 = mybir.dt.bfloat16
AF = mybir.ActivationFunctionType
ALU = mybir.AluOpType
AX = mybir.AxisListType

D = 1024
DEPTH = 2
ALPHA = (2 * DEPTH) ** 0.25
LN_EPS = 1e-5
D_FF = 2816
N_EXP = 8
D_FF_E = 3584
NBR = 3
BW = 512


class Res:
    __slots__ = ("name", "w", "r", "dsem", "dval", "excl")

    def __init__(self, name, excl=False):
        self.name = name
        self.excl = excl
        self.w = None
        self.r = {}
        self.dsem = None
        self.dval = 0

    def absorb(self, others):
        for o in others:
            if o.w is not None:
                self.r[("w", id(o))] = o.w
            for k, t in o.r.items():
                self.r[(k, id(o))] = t


class Prog:
    ROLL = 20000

    NPOOL = 20

    def __init__(self, nc, es):
        self.nc = nc
        self.es = es
        self.dpool = {q: dict(sems=[], vals=[], idx=0) for q in ("sp", "act", "pool")}
        self.cc_tags = []
        self.E = {}
        for name, obj in (("pe", nc.tensor), ("dve", nc.vector), ("act", nc.scalar),
                          ("pool", nc.gpsimd), ("sp", nc.sync)):
            self.E[name] = dict(obj=obj, sem=None, val=0, seen={}, nsem=0, pending=False)
        self.nsems = 0
        self.n_inst = 0

    def new_sem(self, name):
        self.nsems += 1
        return self.es.enter_context(self.nc.semaphore(f"{name}_{self.nsems}"))

    def _eng_sem(self, en):
        E = self.E[en]
        if E["sem"] is None or E["val"] >= self.ROLL:
            assert not E["pending"]
            E["sem"] = self.new_sem("e" + en)
            E["val"] = 0
        return E

    def _wait(self, en, tag, same_ok=True):
        E = self.E[en]
        ten, sem, val = tag
        if ten == en and sem is E["sem"]:
            if en == "pe":
                return
            if val < E["val"] - 1:
                return
        elif ten == en:
            return
        key = id(sem)
        if E["seen"].get(key, 0) >= val:
            return
        E["obj"].wait_ge(sem, val)
        E["seen"][key] = val
        self.n_inst += 1

    def _deps(self, en, reads, writes):
        for r in reads:
            if r.w is not None:
                self._wait(en, r.w)
            if r.excl:
                for t in r.r.values():
                    if t[0] != en:
                        self._wait(en, t)
        for w in writes:
            if w.w is not None:
                self._wait(en, w.w)
            for t in w.r.values():
                if t[0] == en and t[1] is self.E[en]["sem"] and en != "pe":
                    continue
                self._wait(en, t)

    def op(self, en, fn, reads=(), writes=(), inc=True):
        E = self._eng_sem(en)
        self._deps(en, reads, writes)
        ins = fn(E["obj"])
        self.n_inst += 1
        if inc:
            E["val"] += 1
            ins.then_inc(E["sem"], 1)
            tag = (en, E["sem"], E["val"])
            E["pending"] = False
        else:
            tag = (en, E["sem"], E["val"] + 1)
            E["pending"] = True
        for r in reads:
            r.r[en] = tag
        for w in writes:
            w.w = tag
            w.r = {}
        return ins

    def dma(self, q, out, in_, reads=(), writes=(), sem_res=None):
        E = self.E[q]
        self._deps(q, reads, writes)
        pool = self.dpool[q]
        i = pool["idx"] % self.NPOOL
        pool["idx"] += 1
        if len(pool["sems"]) <= i:
            pool["sems"].append(self.new_sem("d" + q))
            pool["vals"].append(0)
        sem, prev = pool["sems"][i], pool["vals"][i]
        if prev > 0:
            self._wait(q, ("dma", sem, prev))
        ins = E["obj"].dma_start(out=out, in_=in_)
        ins.then_inc(sem, 16)
        pool["vals"][i] = prev + 16
        self.n_inst += 1
        tag = ("dma", sem, prev + 16)
        for r in reads:
            r.r[("dma", id(sem))] = tag
        for w in writes:
            w.w = tag
            w.r = {}
        return tag

    def collective_allgather(self, in_ap, out_ap, groups, dep_tags):
        for t in dep_tags:
            self._wait("pool", t)
        sem = self.new_sem("cc")
        ins = self.nc.gpsimd.collective_compute("AllGather", ALU.bypass, replica_groups=groups,
                                                ins=[in_ap], outs=[out_ap])
        ins.then_inc(sem)
        self.n_inst += 1
        tag = ("cc", sem, 1)
        self.cc_tags.append(tag)
        return tag

    def barrier(self, extra_tags=()):
        tags = [(en, E["sem"], E["val"]) for en, E in self.E.items() if E["sem"] is not None and E["val"] > 0]
        for pool in self.dpool.values():
            for sem, v in zip(pool["sems"], pool["vals"]):
                if v > 0:
                    tags.append(("dma", sem, v))
        tags += list(self.cc_tags) + list(extra_tags)
        for en, E in self.E.items():
            assert not E["pending"], en
            for t in tags:
                if t[0] == en:
                    continue
                self._wait(en, t)

    def finish(self, out_tags):
        for t in out_tags:
            self._wait("sp", t)
        for en, E in self.E.items():
            assert not E["pending"], en


class Ctx:
    _uid = [0]

    def __init__(self, nc, P=None):
        self.nc = nc
        self.es = contextlib.ExitStack()
        self.P = P if P is not None else Prog(nc, self.es)
        Ctx._uid[0] += 1
        self.n = Ctx._uid[0] * 1000

    def sb(self, shape, dt, name=None):
        self.n += 1
        return self.es.enter_context(self.nc.sbuf_tensor(f"{name or 't'}{self.n}", list(shape), dt))

    def ps(self, shape, dt=F32, name=None):
        self.n += 1
        return self.es.enter_context(self.nc.psum_tensor(f"{name or 'p'}{self.n}", list(shape), dt))

    def close(self):
        self.es.close()


class WStream:
    def __init__(self, cx, nbuf, nbytes, name, lookahead=None, q="pool"):
        self.cx = cx
        self.P = cx.P
        self.bufs = [cx.sb([128, nbytes // 2], BF16, name) for _ in range(nbuf)]
        self.res = [Res(f"{name}{i}") for i in range(nbuf)]
        self.reqs = []
        self.issued = 0
        self.taken = 0
        self.la = lookahead if lookahead is not None else nbuf - 1
        self.q = q

    def plan(self, dram_ap, shape):
        self.reqs.append((dram_ap, tuple(shape)))

    def _view(self, i, shape):
        n = int(np.prod(shape))
        b = self.bufs[i % len(self.bufs)]
        v = b[:, 0:n]
        if len(shape) == 2:
            v = v.rearrange("p (a b) -> p a b", a=shape[0])
        return v

    def _issue(self, i):
        ap, shape = self.reqs[i]
        r = self.res[i % len(self.bufs)]
        self.P.dma(self.q, self._view(i, shape), ap, writes=[r])

    def next(self):
        i = self.taken
        while self.issued < min(len(self.reqs), i + 1 + self.la):
            self._issue(self.issued)
            self.issued += 1
        self.taken += 1
        return self.res[i % len(self.bufs)], self._view(i, self.reqs[i][1])


def layer_norm_rows(P, nc, x_ap, xres, gB, bB, stats, mv, rstd, sres, cres, eps=LN_EPS):
    P.op("dve", lambda e: e.bn_stats(out=stats[:, 0, :], in_=x_ap[:, 0:512]), reads=[xres], writes=[sres])
    P.op("dve", lambda e: e.bn_stats(out=stats[:, 1, :], in_=x_ap[:, 512:1024]), reads=[xres], writes=[sres])
    P.op("dve", lambda e: e.bn_aggr(out=mv[:, :], in_=stats[:, :, :].rearrange("p a b -> p (a b)")),
         reads=[sres], writes=[sres])
    P.op("act", lambda e: e.activation(out=rstd[:, :], in_=mv[:, 1:2], func=AF.Sqrt, bias=float(eps), scale=1.0),
         reads=[sres], writes=[sres])
    P.op("dve", lambda e: e.reciprocal(out=rstd[:, :], in_=rstd[:, :]), reads=[sres], writes=[sres])
    P.op("dve", lambda e: e.tensor_scalar(out=x_ap, in0=x_ap, scalar1=mv[:, 0:1], scalar2=rstd[:, 0:1],
                                          op0=ALU.subtract, op1=ALU.mult), reads=[sres, xres], writes=[xres])
    P.op("dve", lambda e: e.tensor_tensor(out=x_ap, in0=x_ap, in1=gB, op=ALU.mult),
         reads=[xres, cres], writes=[xres])
    P.op("dve", lambda e: e.tensor_tensor(out=x_ap, in0=x_ap, in1=bB, op=ALU.add),
         reads=[xres, cres], writes=[xres])


def decl_merge(nc, pre, moe, ne_override=None, ff_override=None):
    dram = lambda n, s_, dt=F32: nc.dram_tensor(pre + n, list(s_), dt, kind="ExternalInput").ap()
    A = dict(
        w_gate=dram("w_gate", [D, NBR * D]), b_gate=dram("b_gate", [128, NBR * 8]), w_up=dram("w_up", [NBR, BW, D]),
        w_out=dram("w_out", [D, D]), lnB=dram("lnB", [6, 128, D]), ident=dram("ident", [128, 128]))
    if moe:
        FF = ff_override or D_FF_E
        NE = ne_override or N_EXP
        A.update(w1=dram("w1", [NE, D, FF]), w3=dram("w3", [NE, D, FF]), w2=dram("w2", [NE, FF, D]),
                 router_w=dram("router_w", [D, N_EXP]), router_b=dram("router_b", [128, N_EXP]))
    else:
        FF, NE = D_FF, 1
        A.update(w1=dram("w1", [1, D, FF]), w3=dram("w3", [1, D, FF]), w2=dram("w2", [1, FF, D]))
    A["FF"], A["NE"] = FF, NE
    return A


def build_merge(NT, do_ln0, moe, ne_override=None, ff_override=None):
    nc = bass.Bass("TRN2", target_bir_lowering=False)
    A = decl_merge(nc, "", moe, ne_override, ff_override)
    x_d = nc.dram_tensor("x", [NT, D], F32, kind="ExternalInput").ap()
    oT_d = nc.dram_tensor("oT", [NBR * BW, NT], BF16, kind="ExternalInput").ap()
    y_d = nc.dram_tensor("y", [NT, D], F32, kind="ExternalOutput").ap()
    cx0 = Ctx(nc)
    x_rows = lambda t0, n: x_d[t0:t0 + n, :]
    o_rows = lambda br, half, t0, n: oT_d[br * BW + half * 256: br * BW + (half + 1) * 256, t0:t0 + n]
    tags, cx = emit_merge(nc, cx0.P, A, NT, do_ln0, moe, x_rows, o_rows, y_d)
    cx0.P.finish(tags)
    cx.close()
    cx0.close()
    return nc


def emit_merge(nc, P, A, NT, do_ln0, moe, x_rows, o_rows, y_d, after_store=None):
    cx = Ctx(nc, P)
    wg_d, bg_d, wup_d, wout_d, lnB_d, ident_d = (A[k] for k in ("w_gate", "b_gate", "w_up", "w_out", "lnB", "ident"))
    FF, NE = A["FF"], A["NE"]
    w1_d, w3_d, w2_d = A["w1"], A["w3"], A["w2"]
    if moe:
        rw_d, rb_d = A["router_w"], A["router_b"]
    NFS = FF // 128

    TT2 = min(NT, 1024)
    TT1 = min(NT, 512)
    NS2 = TT2 // 128
    NS1 = TT1 // 128
    NH2 = TT2 // TT1
    n_super = NT // TT2

    ident = cx.sb([128, 128], F32, "ident")
    lnB = cx.sb([128, 6, D], F32, "lnB")
    bg = cx.sb([128, NBR * 8], F32, "bg")
    xs = cx.sb([128, NS2, D], F32, "xs")
    xT = cx.sb([128, 8, TT2], BF16, "xT")
    xTf = cx.sb([128, 8, 128], F32, "xTf") if moe else None
    U = cx.sb([128, 28 * TT2], BF16, "U")
    gate_sb = [cx.sb([128, TT1], F32, "gate") for _ in range(3)]
    prod_sb = [cx.sb([128, TT1], F32, "prod") for _ in range(3)]
    silu_sb = [cx.sb([128, 512], BF16, "silu") for _ in range(2)]
    stats = cx.sb([128, 2, 6], F32, "stats")
    mv = cx.sb([128, 2], F32, "mv")
    rstd = cx.sb([128, 1], F32, "rstd")
    if moe:
        rw = cx.sb([128, 8, N_EXP], F32, "rw")
        rb = cx.sb([128, N_EXP], F32, "rb")
        comb = cx.sb([128, NS2, N_EXP], F32, "comb")
        rt = [cx.sb([128, N_EXP], F32, "rt") for _ in range(6)]
        rs = [cx.sb([128, 1], F32, "rs") for _ in range(4)]
    TR = cx.ps([128, 1024], F32, "TR")
    TM = cx.ps([128, 1024], F32, "TM")
    FM = [cx.ps([128, 512], F32, "FM") for _ in range(4)]
    rTR, rTM = Res("TR", True), Res("TM", True)
    rFM = [Res(f"FM{i}", True) for i in range(4)]

    r_ident, r_lnB, r_bg = Res("ident"), Res("lnB"), Res("bg")
    r_xs = [Res(f"xs{i}") for i in range(NS2)]
    r_xT = [Res(f"xT{i}") for i in range(NS2)]
    r_xTf = Res("xTf")
    r_stat = Res("stat")
    r_gate = [Res(f"gate{i}") for i in range(3)]
    r_prod = [Res(f"prod{i}") for i in range(3)]
    r_silu = [Res(f"silu{i}") for i in range(2)]
    r_rt = Res("rt")
    r_comb = [Res(f"comb{i}") for i in range(NS2)]
    r_const = Res("const")

    oT_v = U[:, 0:12 * TT1].rearrange("p (a t) -> p a t", a=12)
    mT_v = U[:, 12 * TT1:20 * TT1].rearrange("p (a t) -> p a t", a=8)
    hT_v = U[:, 0:NFS * TT2].rearrange("p (a t) -> p a t", a=NFS)
    r_oT = [Res(f"oT{i}") for i in range(3)]
    r_mT = [Res(f"mT{i}") for i in range(8)]
    r_hT = [Res(f"hT{i}") for i in range(NFS)]

    P.dma("sp", ident[:, :], ident_d[:, :], writes=[r_ident])
    P.dma("sp", lnB[:, :, :], lnB_d.rearrange("a p d -> p a d"), writes=[r_lnB])
    P.dma("sp", bg[:, :], bg_d[:, :], writes=[r_bg])
    if moe:
        P.dma("sp", rw[:, :, :], rw_d.rearrange("(kc p) e -> p kc e", p=128), writes=[r_const])
        r_rb = Res("rb")
        P.dma("sp", rb[:, :], rb_d[:, :], writes=[r_rb])

    wsA = WStream(cx, 10, 2048, "wA", lookahead=6)
    wsB = WStream(cx, 16 if moe else 12, 2048, "wB", lookahead=8 if moe else 4)
    wg_v = wg_d.rearrange("(kc p) c -> p kc c", p=128)
    wup_v = wup_d.rearrange("b (kc p) c -> b p kc c", p=128)
    w1_v = w1_d.rearrange("e (kc p) c -> e p kc c", p=128)
    w3_v = w3_d.rearrange("e (kc p) c -> e p kc c", p=128)
    for st in range(n_super):
        for h in range(NH2):
            for fs in range(8):
                for br in range(NBR):
                    wsA.plan(wg_v[:, :, br * D + fs * 128: br * D + (fs + 1) * 128], (8, 128))
                    wsA.plan(wup_v[br][:, :, fs * 128:(fs + 1) * 128], (4, 128))
            for kc in range(8):
                wsB.plan(wout_d[kc * 128:(kc + 1) * 128, :], (1024,))
        for e in range(NE):
            for fs in range(NFS):
                wsA.plan(w1_v[e][:, :, fs * 128:(fs + 1) * 128], (8, 128))
                wsA.plan(w3_v[e][:, :, fs * 128:(fs + 1) * 128], (8, 128))
            for kc in range(NFS):
                wsB.plan(w2_d[e, kc * 128:(kc + 1) * 128, :], (1024,))

    out_tags = []

    def transpose_sub(s, want_f32):
        for kc in range(8):
            P.op("pe", lambda e, kc=kc: e.transpose(out=TR[:, kc * 128:(kc + 1) * 128],
                                                    in_=xs[:, s, kc * 128:(kc + 1) * 128], identity=ident[:, :]),
                 reads=[r_xs[s], r_ident], writes=[rTR], inc=(kc == 7))
        if want_f32:
            P.op("dve", lambda e: e.tensor_copy(out=xTf[:, :, :], in_=TR[:, :].rearrange("p (a t) -> p a t", a=8)),
                 reads=[rTR], writes=[r_xTf])
            P.op("act", lambda e: e.copy(out=xT[:, :, s * 128:(s + 1) * 128], in_=xTf[:, :, :]),
                 reads=[r_xTf], writes=[r_xT[s]])
        else:
            P.op("act", lambda e: e.copy(out=xT[:, :, s * 128:(s + 1) * 128],
                                         in_=TR[:, :].rearrange("p (a t) -> p a t", a=8)),
                 reads=[rTR], writes=[r_xT[s]])

    fm_i = [0]

    def next_fm():
        i = fm_i[0] % 4
        fm_i[0] += 1
        return FM[i], rFM[i]

    def ln_sub(s, which):
        layer_norm_rows(P, nc, xs[:, s, :], r_xs[s], lnB[:, 2 * which, :], lnB[:, 2 * which + 1, :],
                        stats, mv, rstd, r_stat, r_lnB)

    for st in range(n_super):
        t0 = st * TT2
        for s in range(NS2):
            P.dma("sp", xs[:, s, :], x_rows(t0 + s * 128, 128), writes=[r_xs[s]])
        for s in range(NS2):
            if do_ln0:
                ln_sub(s, 0)
            transpose_sub(s, False)
        for r in r_oT + r_mT:
            r.absorb(r_hT)
        for h in range(NH2):
            c0 = h * TT1
            for br in range(NBR):
                for half in range(2):
                    P.dma("sp", oT_v[:, br * 4 + half * 2:br * 4 + half * 2 + 2, :],
                          o_rows(br, half, t0 + c0, TT1).rearrange("(kc p) t -> p kc t", p=128),
                          writes=[r_oT[br]])
            for fs in range(8):
                for br in range(NBR):
                    gw_r, gw = wsA.next()
                    uw_r, uw = wsA.next()
                    gp, gp_r = next_fm()
                    for kc in range(8):
                        P.op("pe", lambda e, kc=kc: e.matmul(gp[:, 0:TT1], lhsT=gw[:, kc, :],
                                                             rhs=xT[:, kc, c0:c0 + TT1],
                                                             start=(kc == 0), stop=(kc == 7)),
                             reads=[gw_r] + r_xT[h * NS1:(h + 1) * NS1], writes=[gp_r], inc=(kc == 7))
                    up, up_r = next_fm()
                    for kc in range(4):
                        P.op("pe", lambda e, kc=kc: e.matmul(up[:, 0:TT1], lhsT=uw[:, kc, :],
                                                             rhs=oT_v[:, br * 4 + kc, :],
                                                             start=(kc == 0), stop=(kc == 3)),
                             reads=[uw_r, r_oT[br]], writes=[up_r], inc=(kc == 3))
                    P.op("act", lambda e: e.activation(out=gate_sb[br][:, :], in_=gp[:, 0:TT1], func=AF.Sigmoid,
                                                       bias=bg[:, br * 8 + fs: br * 8 + fs + 1], scale=1.0),
                         reads=[gp_r, r_bg], writes=[r_gate[br]])
                    P.op("dve", lambda e: e.tensor_tensor(out=prod_sb[br][:, :], in0=up[:, 0:TT1],
                                                          in1=gate_sb[br][:, :], op=ALU.mult),
                         reads=[up_r, r_gate[br]], writes=[r_prod[br]])
                P.op("dve", lambda e: e.tensor_tensor(out=prod_sb[0][:, :], in0=prod_sb[0][:, :],
                                                       in1=prod_sb[1][:, :], op=ALU.add),
                     reads=[r_prod[0], r_prod[1]], writes=[r_prod[0]])
                P.op("dve", lambda e: e.tensor_tensor(out=mT_v[:, fs, :], in0=prod_sb[0][:, :],
                                                       in1=prod_sb[2][:, :], op=ALU.add),
                     reads=[r_prod[0], r_prod[2]], writes=[r_mT[fs]])
            wo = [wsB.next() for _ in range(8)]
            for sl in range(NS1):
                s = h * NS1 + sl
                for half in range(2):
                    for kc in range(8):
                        P.op("pe", lambda e, kc=kc, half=half: e.matmul(
                            TM[:, half * 512:(half + 1) * 512], lhsT=mT_v[:, kc, sl * 128:(sl + 1) * 128],
                            rhs=wo[kc][1][:, half * 512:(half + 1) * 512], start=(kc == 0), stop=(kc == 7)),
                             reads=[r_mT[kc], wo[kc][0]], writes=[rTM], inc=(kc == 7 and half == 1))
                P.op("dve", lambda e: e.scalar_tensor_tensor(out=xs[:, s, :], in0=xs[:, s, :], scalar=float(ALPHA),
                                                             in1=TM[:, :], op0=ALU.mult, op1=ALU.add),
                     reads=[rTM, r_xs[s]], writes=[r_xs[s]])
                ln_sub(s, 1)
        for s in range(NS2):
            transpose_sub(s, moe)
            if moe:
                lg, lg_r = next_fm()
                for kc in range(8):
                    P.op("pe", lambda e, kc=kc: e.matmul(lg[:, 0:N_EXP], lhsT=xTf[:, kc, :], rhs=rw[:, kc, :],
                                                         start=(kc == 0), stop=(kc == 7)),
                         reads=[r_xTf, r_const], writes=[lg_r], inc=(kc == 7))
                L, M1, K1, L2, K2, T6 = rt
                m1, m2, g1, g2 = rs
                ops = [
                    lambda e: e.tensor_tensor(out=L[:, :], in0=lg[:, 0:N_EXP], in1=rb[:, :], op=ALU.add),
                    lambda e: e.tensor_reduce(out=m1[:, :], in_=L[:, :], axis=AX.X, op=ALU.max),
                    lambda e: e.tensor_scalar(out=K1[:, :], in0=L[:, :], scalar1=m1[:, 0:1], scalar2=None,
                                              op0=ALU.is_ge),
                    lambda e: e.scalar_tensor_tensor(out=L2[:, :], in0=K1[:, :], scalar=-1e30, in1=L[:, :],
                                                     op0=ALU.mult, op1=ALU.add),
                    lambda e: e.tensor_reduce(out=m2[:, :], in_=L2[:, :], axis=AX.X, op=ALU.max),
                    lambda e: e.tensor_scalar(out=K2[:, :], in0=L2[:, :], scalar1=m2[:, 0:1], scalar2=None,
                                              op0=ALU.is_ge),
                    lambda e: e.tensor_tensor(out=g2[:, :], in0=m2[:, :], in1=m1[:, :], op=ALU.subtract),
                ]
                for i, f in enumerate(ops):
                    P.op("dve", f, reads=[lg_r, r_rt, r_rb] if i == 0 else [r_rt], writes=[r_rt])
                P.op("act", lambda e: e.activation(out=g2[:, :], in_=g2[:, :], func=AF.Sigmoid),
                     reads=[r_rt], writes=[r_rt])
                ops2 = [
                    lambda e: e.tensor_scalar(out=g1[:, :], in0=g2[:, :], scalar1=-1.0, scalar2=1.0,
                                              op0=ALU.mult, op1=ALU.add),
                    lambda e: e.tensor_scalar(out=K2[:, :], in0=K2[:, :], scalar1=g2[:, 0:1], scalar2=None,
                                              op0=ALU.mult),
                ]
                for f in ops2:
                    P.op("dve", f, reads=[r_rt], writes=[r_rt])
                P.op("dve", lambda e: e.scalar_tensor_tensor(out=comb[:, s, :], in0=K1[:, :], scalar=g1[:, 0:1],
                                                             in1=K2[:, :], op0=ALU.mult, op1=ALU.add),
                     reads=[r_rt], writes=[r_comb[s]])
        for s in range(NS2):
            P.op("act", lambda e: e.mul(out=xs[:, s, :], in_=xs[:, s, :], mul=float(ALPHA)),
                 reads=[r_xs[s]], writes=[r_xs[s]])
        for r in r_hT:
            r.absorb(r_oT + r_mT)
        for e_i in range(NE):
            for fs in range(NFS):
                w1r, w1t = wsA.next()
                w3r, w3t = wsA.next()
                for hh in range(TT2 // 512 if TT2 >= 512 else 1):
                    n = min(512, TT2)
                    c0 = hh * n
                    subs = r_xT[c0 // 128:(c0 + n) // 128]
                    a, a_r = next_fm()
                    for kc in range(8):
                        P.op("pe", lambda e, kc=kc: e.matmul(a[:, 0:n], lhsT=w1t[:, kc, :], rhs=xT[:, kc, c0:c0 + n],
                                                             start=(kc == 0), stop=(kc == 7)),
                             reads=[w1r] + subs, writes=[a_r], inc=(kc == 7))
                    b, b_r = next_fm()
                    for kc in range(8):
                        P.op("pe", lambda e, kc=kc: e.matmul(b[:, 0:n], lhsT=w3t[:, kc, :], rhs=xT[:, kc, c0:c0 + n],
                                                             start=(kc == 0), stop=(kc == 7)),
                             reads=[w3r] + subs, writes=[b_r], inc=(kc == 7))
                    si = (fs * 2 + hh) % 2
                    P.op("act", lambda e: e.activation(out=silu_sb[si][:, 0:n], in_=a[:, 0:n], func=AF.Silu),
                         reads=[a_r], writes=[r_silu[si]])
                    P.op("dve", lambda e: e.tensor_tensor(out=hT_v[:, fs, c0:c0 + n], in0=b[:, 0:n],
                                                          in1=silu_sb[si][:, 0:n], op=ALU.mult),
                         reads=[b_r, r_silu[si]], writes=[r_hT[fs]])
            GK = 8
            for g0 in range(0, NFS, GK):
                gks = list(range(g0, min(NFS, g0 + GK)))
                wts = [wsB.next() for _ in gks]
                for s in range(NS2):
                    for half in range(2):
                        for j, kc in enumerate(gks):
                            P.op("pe", lambda e, j=j, kc=kc, half=half: e.matmul(
                                TM[:, half * 512:(half + 1) * 512], lhsT=hT_v[:, kc, s * 128:(s + 1) * 128],
                                rhs=wts[j][1][:, half * 512:(half + 1) * 512],
                                start=(j == 0), stop=(j == len(gks) - 1)),
                                 reads=[r_hT[kc], wts[j][0]], writes=[rTM],
                                 inc=(j == len(gks) - 1 and half == 1))
                    if moe:
                        P.op("dve", lambda e: e.scalar_tensor_tensor(
                            out=xs[:, s, :], in0=TM[:, :], scalar=comb[:, s, e_i:e_i + 1], in1=xs[:, s, :],
                            op0=ALU.mult, op1=ALU.add), reads=[rTM, r_xs[s], r_comb[s]], writes=[r_xs[s]])
                    else:
                        P.op("dve", lambda e: e.tensor_tensor(out=xs[:, s, :], in0=TM[:, :], in1=xs[:, s, :],
                                                              op=ALU.add),
                             reads=[rTM, r_xs[s]], writes=[r_xs[s]])
        for s in range(NS2):
            ln_sub(s, 2)
            out_tags.append(P.dma("sp", y_d[t0 + s * 128: t0 + (s + 1) * 128, :], xs[:, s, :], reads=[r_xs[s]]))
            if after_store is not None:
                after_store(t0 + s * 128, out_tags[-1])
    return out_tags, cx


RWKV_COLS = 3 * 512 + 64 + 64 + 128
CONV_COLS = 3 * 512
FOX_COLS = 3 * 512 + 8
GATE_OFF = RWKV_COLS + CONV_COLS + FOX_COLS
IDENT = np.eye(128, dtype=np.float32)


def _bc(v):
    return np.ascontiguousarray(np.broadcast_to(np.asarray(v, np.float32)[None, :], (128, v.shape[0])))


def merge_inputs(p, l, moe):
    w_in = p["w_in"][l]
    common = {
        "ident": IDENT,
        "w_gate": np.ascontiguousarray(w_in[:, GATE_OFF:GATE_OFF + NBR * D]),
        "b_gate": np.ascontiguousarray(p["b_gate"][l].reshape(NBR, 8, 128).transpose(2, 0, 1).reshape(128, NBR * 8)),
        "w_up": np.ascontiguousarray(np.stack([p["w_up_rwkv"][l], p["w_up_conv"][l], p["w_up_attn"][l]], 0)),
        "w_out": np.ascontiguousarray(p["w_out"][l]),
        "lnB": np.ascontiguousarray(np.stack([_bc(p["ln0_g"]), _bc(p["ln0_b"]), _bc(p["ln1_g"][l]), _bc(p["ln1_b"][l]),
                                              _bc(p["ln2_g"][l]), _bc(p["ln2_b"][l])], 0)),
    }
    i = l // 2
    if moe:
        common.update({
            "w1": p["moe_w1"][i], "w3": p["moe_w3"][i], "w2": p["moe_w2"][i],
            "router_w": np.ascontiguousarray(p["router_w"][i]), "router_b": _bc(p["router_b"][i]),
        })
    else:
        common.update({"w1": p["ffn_w1"][i][None], "w3": p["ffn_w3"][i][None], "w2": p["ffn_w2"][i][None]})
    return common


def run_merge(x_flat, oT, p, l, do_ln0, moe, n_cores=8):
    ntok = x_flat.shape[0]
    NT = ntok // n_cores
    nc = build_merge(NT, do_ln0, moe)
    common = merge_inputs(p, l, moe)
    in_maps = []
    for c in range(n_cores):
        m = dict(common)
        m["x"] = np.ascontiguousarray(x_flat[c * NT:(c + 1) * NT])
        m["oT"] = np.ascontiguousarray(oT[:, c * NT:(c + 1) * NT])
        in_maps.append(m)
    res = run_bass_kernel_spmd(nc, in_maps, core_ids=list(range(n_cores)))
    return np.concatenate([r["y"] for r in res.results], 0)


NEG = -30000.0


def decl_fox(nc, pre):
    dram = lambda n, s_, dt=F32: nc.dram_tensor(pre + n, list(s_), dt, kind="ExternalInput").ap()
    return dict(wc=dram("wc", [D, 768]), wq=dram("wq", [D, 256]), wk=dram("wk", [D, 256]), wv=dram("wv", [D, 256]),
                wf=dram("wf", [D, 4]), bf=dram("bf", [4, 1]), cw=dram("cw", [128, 6]), lnB=dram("lnB", [2, 128, D]),
                ident=dram("ident", [128, 128]), mask=dram("mask", [128, 128]))


def build_fox(T, do_ln0):
    nc = bass.Bass("TRN2", target_bir_lowering=False)
    A = decl_fox(nc, "")
    x_d = nc.dram_tensor("x", [T, D], F32, kind="ExternalInput").ap()
    obT_d = nc.dram_tensor("obT", [256, T], BF16, kind="ExternalOutput").ap()
    ocT_d = nc.dram_tensor("ocT", [256, T], BF16, kind="ExternalOutput").ap()
    cx0 = Ctx(nc)
    TTf = min(T, 512)
    tags, cx = emit_fox(nc, cx0.P, A, T, do_ln0, lambda t0, n: x_d[t0:t0 + n, :],
                        lambda ti: obT_d[:, ti * TTf:(ti + 1) * TTf], lambda ti: ocT_d[:, ti * TTf:(ti + 1) * TTf])
    cx0.P.finish(tags)
    cx.close()
    cx0.close()
    return nc


def emit_fox(nc, P, A, T, do_ln0, xsrc, ob_dst, oc_dst, after_store=None):
    cx = Ctx(nc, P)
    wc_d, wq_d, wk_d, wv_d, wf_d, bf_d, cw_d, lnB_d, ident_d, mask_d = (
        A[k] for k in ("wc", "wq", "wk", "wv", "wf", "bf", "cw", "lnB", "ident", "mask"))

    TT = min(T, 512)
    NS = TT // 128
    NSB = T // TT
    NB = T // 128
    H = 4

    ident = cx.sb([128, 128], F32, "ident")
    identb = cx.sb([128, 128], BF16, "identb")
    maskf = cx.sb([128, 128], F32, "maskf")
    maskb = cx.sb([128, 128], BF16, "maskb")
    lnB = cx.sb([128, 2, D], F32, "lnB")
    cw = cx.sb([128, 6], F32, "cw")
    bf = cx.sb([4, 1], F32, "bf")
    nbf = cx.sb([4, 1], F32, "nbf")
    wc = cx.sb([128, 8, 768], BF16, "wc")
    wq = cx.sb([128, 8, 256], BF16, "wq")
    wk = cx.sb([128, 8, 256], BF16, "wk")
    wv = cx.sb([128, 8, 256], BF16, "wv")
    wf = cx.sb([128, 8, 4], BF16, "wf")
    xs = cx.sb([128, 2, D], F32, "xs")
    xT = cx.sb([128, 8, TT], BF16, "xT")
    Kaug = [cx.sb([70, T], BF16, "Kaug") for _ in range(H)]
    Vaug = cx.sb([128, NB, H, 65], BF16, "Vaug")
    Qaug = [[cx.sb([70, TT], BF16, "Qaug") for _ in range(H)] for _ in range(2)]
    PT = [cx.sb([128, TT], BF16, "PT") for _ in range(3)]
    cvT = [cx.sb([128, 2, TT], F32, "cvT") for _ in range(2)]
    uT = cx.sb([128, 2, 2 + TT], F32, "uT")
    yT = cx.sb([128, 2, TT], F32, "yT")
    y2T = cx.sb([128, 2, TT], F32, "y2T")
    obT = cx.sb([128, 2, TT], BF16, "obT")
    ones4 = cx.sb([4, TT], F32, "ones4")
    lf = cx.sb([4, TT], F32, "lf")
    cc = cx.sb([4, TT], F32, "cc")
    cprev = cx.sb([4, 1], F32, "cprev")
    csp = cx.sb([4, 3, TT], BF16, "csp")
    csn = cx.sb([4, 3, TT], BF16, "csn")
    ctmp = cx.sb([4, TT], F32, "ctmp")
    ctmp2 = lf
    oc = [cx.sb([128, NS, 256], F32, "oc") for _ in range(2)]
    ocT = [cx.sb([128, 2, TT], BF16, "ocT") for _ in range(2)]
    r_ocT = [Res("ocT0"), Res("ocT1")]
    rinv = cx.sb([128, 4], F32, "rinv")
    stats = cx.sb([128, 2, 6], F32, "stats")
    mv = cx.sb([128, 2], F32, "mv")
    rstd = cx.sb([128, 1], F32, "rstd")

    TR = cx.ps([128, 1024], F32, "TR")
    FM = [cx.ps([128, 512], F32, "FM") for _ in range(2)]
    ST = [cx.ps([128, 512], F32, "ST") for _ in range(2)]
    OA = [cx.ps([128, 512], F32, "OA") for _ in range(2)]
    rTR = Res("TR", True)
    rFM = [Res("FM0", True), Res("FM1", True)]
    rST = [Res("ST0", True), Res("ST1", True)]
    rOA = [Res("OA0", True), Res("OA1", True)]

    r_c = Res("consts")
    r_w = Res("weights")
    r_xs = [Res(f"xs{i}") for i in range(2)]
    r_xT = Res("xT")
    r_K = [[Res(f"K{h}_{i}") for i in range(NSB)] for h in range(H)]
    r_V = [Res(f"V{i}") for i in range(NSB)]
    r_Q = [[Res(f"Q{b}{h}") for h in range(H)] for b in range(2)]
    r_PT = [Res(f"PT{i}") for i in range(3)]
    r_cv = [Res("cvb"), Res("cvc")]
    r_u, r_y, r_ob, r_y2 = Res("u"), Res("y"), Res("ob"), Res("y2")
    r_f = Res("f")
    r_cs = Res("cs")
    r_oc = [Res("oc0"), Res("oc1")]
    r_rinv = Res("rinv")
    r_stat = Res("stat")

    P.dma("sp", ident[:, :], ident_d[:, :], writes=[r_c])
    P.dma("sp", maskf[:, :], mask_d[:, :], writes=[r_c])
    P.dma("sp", lnB[:, :, :], lnB_d.rearrange("a p d -> p a d"), writes=[r_c])
    P.dma("sp", cw[:, :], cw_d[:, :], writes=[r_c])
    P.dma("sp", bf[:, :], bf_d[:, :], writes=[r_c])
    for wt, wd_, n in ((wc, wc_d, 768), (wq, wq_d, 256), (wk, wk_d, 256), (wv, wv_d, 256), (wf, wf_d, 4)):
        P.dma("pool", wt[:, :, :], wd_.rearrange("(kc p) c -> p kc c", p=128), writes=[r_w])
    r_c2 = Res("consts2")
    P.op("dve", lambda e: e.tensor_copy(out=identb[:, :], in_=ident[:, :]), reads=[r_c], writes=[r_c2])
    P.op("dve", lambda e: e.tensor_copy(out=maskb[:, :], in_=maskf[:, :]), reads=[r_c], writes=[r_c2])
    P.op("dve", lambda e: e.tensor_scalar(out=nbf[:, :], in0=bf[:, :], scalar1=-1.0, scalar2=None, op0=ALU.mult),
         reads=[r_c], writes=[r_c2])
    P.op("dve", lambda e: e.memset(ones4[:, :], 1.0), writes=[r_c2])
    P.op("dve", lambda e: e.memset(cprev[:, :], 0.0), writes=[r_cs])
    P.op("dve", lambda e: e.memset(uT[:, :, :], 0.0), writes=[r_u])
    P.op("pool", lambda e: e.memset(Vaug[:, :, :, :], 1.0), writes=r_V)
    for h in range(H):
        P.op("pool", lambda e, h=h: e.memset(Kaug[h][64:70, :], 1.0), writes=r_K[h])
        for b in range(2):
            P.op("pool", lambda e, h=h, b=b: e.memset(Qaug[b][h][64:70, :], 1.0), writes=[r_Q[b][h]])

    fm_i = [0]

    def next_fm():
        i = fm_i[0] % 2
        fm_i[0] += 1
        return FM[i], rFM[i]

    out_tags = []

    def proj(sb_i):
        t0 = sb_i * TT
        qb = sb_i % 2
        for s in range(NS):
            yield
            xb_ = s % 2
            P.dma("sp", xs[:, xb_, :], xsrc(t0 + s * 128, 128), writes=[r_xs[xb_]])
            if do_ln0:
                layer_norm_rows(P, nc, xs[:, xb_, :], r_xs[xb_], lnB[:, 0, :], lnB[:, 1, :], stats, mv, rstd, r_stat, r_c)
            for kc in range(8):
                P.op("pe", lambda e, kc=kc: e.transpose(out=TR[:, kc * 128:(kc + 1) * 128],
                                                        in_=xs[:, xb_, kc * 128:(kc + 1) * 128], identity=ident[:, :]),
                     reads=[r_xs[xb_], r_c], writes=[rTR], inc=(kc == 7))
            P.op("dve", lambda e: e.tensor_copy(out=xT[:, :, s * 128:(s + 1) * 128],
                                                in_=TR[:, :].rearrange("p (a t) -> p a t", a=8)),
                 reads=[rTR], writes=[r_xT])
        yield
        for grp in range(3):
            for hf in range(2):
                yield
                ps, ps_r = next_fm()
                c0 = grp * 256 + hf * 128
                for kc in range(8):
                    P.op("pe", lambda e, kc=kc: e.matmul(ps[:, 0:TT], lhsT=wc[:, kc, c0:c0 + 128], rhs=xT[:, kc, :],
                                                         start=(kc == 0), stop=(kc == 7)),
                         reads=[r_w, r_xT], writes=[ps_r], inc=(kc == 7))
                if grp < 2:
                    P.op("dve", lambda e: e.tensor_copy(out=cvT[grp][:, hf, :], in_=ps[:, 0:TT]),
                         reads=[ps_r], writes=[r_cv[grp]])
                else:
                    P.op("dve", lambda e: e.tensor_tensor(out=uT[:, hf, 2:2 + TT], in0=ps[:, 0:TT],
                                                          in1=cvT[1][:, hf, :], op=ALU.mult),
                         reads=[ps_r, r_cv[1]], writes=[r_u])
        yield
        for hf in range(2):
            P.op("pool", lambda e: e.tensor_scalar(out=yT[:, hf, :], in0=uT[:, hf, 0:TT],
                                                   scalar1=cw[:, hf * 3:hf * 3 + 1], scalar2=None, op0=ALU.mult),
                 reads=[r_u, r_c], writes=[r_y])
            for tap in (1, 2):
                P.op("pool", lambda e, tap=tap: e.tensor_scalar(out=y2T[:, hf, :], in0=uT[:, hf, tap:tap + TT],
                                                                scalar1=cw[:, hf * 3 + tap:hf * 3 + tap + 1],
                                                                scalar2=None, op0=ALU.mult),
                     reads=[r_u, r_c], writes=[r_y2])
                P.op("pool", lambda e: e.tensor_tensor(out=yT[:, hf, :], in0=yT[:, hf, :], in1=y2T[:, hf, :],
                                                       op=ALU.add),
                     reads=[r_y, r_y2], writes=[r_y])
            P.op("pool", lambda e: e.tensor_tensor(out=obT[:, hf, :], in0=yT[:, hf, :], in1=cvT[0][:, hf, :],
                                                   op=ALU.mult),
                 reads=[r_y, r_cv[0]], writes=[r_ob])
        P.op("pool", lambda e: e.tensor_copy(out=uT[:, :, 0:2], in_=uT[:, :, TT:TT + 2]), reads=[r_u], writes=[r_u])
        out_tags.append(P.dma("sp", ob_dst(sb_i).rearrange("(hf p) t -> p hf t", p=128), obT[:, :, :],
                              reads=[r_ob]))
        if after_store is not None:
            after_store(sb_i, out_tags[-1])
        yield
        ps, ps_r = next_fm()
        for kc in range(8):
            P.op("pe", lambda e, kc=kc: e.matmul(ps[0:4, 0:TT], lhsT=wf[:, kc, :], rhs=xT[:, kc, :],
                                                 start=(kc == 0), stop=(kc == 7)),
                 reads=[r_w, r_xT], writes=[ps_r], inc=(kc == 7))
        P.op("act", lambda e: e.activation(out=lf[:, :], in_=ps[0:4, 0:TT], func=AF.Exp, bias=nbf[:, 0:1], scale=-1.0),
             reads=[ps_r, r_c2], writes=[r_f])
        P.op("act", lambda e: e.activation(out=lf[:, :], in_=lf[:, :], func=AF.Ln, bias=1.0, scale=1.0),
             reads=[r_f], writes=[r_f])
        P.op("dve", lambda e: e.tensor_scalar(out=lf[:, :], in0=lf[:, :], scalar1=-1.0, scalar2=None, op0=ALU.mult),
             reads=[r_f], writes=[r_f])
        P.op("dve", lambda e: e.tensor_tensor_scan(out=cc[:, :], data0=ones4[:, :], data1=lf[:, :],
                                                   initial=cprev[:, 0:1], op0=ALU.mult, op1=ALU.add),
             reads=[r_f, r_cs, r_c2], writes=[r_cs])
        P.op("dve", lambda e: e.tensor_copy(out=cprev[:, :], in_=cc[:, TT - 1:TT]), reads=[r_cs], writes=[r_cs])
        P.op("dve", lambda e: e.tensor_copy(out=csp[:, 0, :], in_=cc[:, :]), reads=[r_cs], writes=[r_cs])
        P.op("dve", lambda e: e.tensor_tensor(out=ctmp[:, :], in0=cc[:, :], in1=csp[:, 0, :], op=ALU.subtract),
             reads=[r_cs], writes=[r_cs])
        P.op("dve", lambda e: e.tensor_copy(out=csp[:, 1, :], in_=ctmp[:, :]), reads=[r_cs], writes=[r_cs])
        P.op("dve", lambda e: e.tensor_tensor(out=ctmp2[:, :], in0=ctmp[:, :], in1=csp[:, 1, :], op=ALU.subtract),
             reads=[r_cs], writes=[r_cs])
        P.op("dve", lambda e: e.tensor_copy(out=csp[:, 2, :], in_=ctmp2[:, :]), reads=[r_cs], writes=[r_cs])
        P.op("dve", lambda e: e.tensor_scalar(out=csn[:, :, :], in0=csp[:, :, :], scalar1=-1.0, scalar2=None,
                                              op0=ALU.mult), reads=[r_cs], writes=[r_cs])
        yield
        for h in range(H):
            yield
            ps, ps_r = next_fm()
            for kc in range(8):
                P.op("pe", lambda e, kc=kc: e.matmul(ps[0:64, 0:TT], lhsT=wq[:, kc, h * 64:(h + 1) * 64], rhs=xT[:, kc, :],
                                                     start=(kc == 0), stop=(kc == 7)),
                     reads=[r_w, r_xT], writes=[ps_r], inc=(kc == 7))
            P.op("act", lambda e: e.mul(out=Qaug[qb][h][0:64, :], in_=ps[0:64, 0:TT], mul=0.125),
                 reads=[ps_r], writes=[r_Q[qb][h]])
            for jj in range(3):
                P.dma("sp", Qaug[qb][h][64 + jj:65 + jj, :], csp[h:h + 1, jj, :], reads=[r_cs], writes=[r_Q[qb][h]],
                      sem_res=r_Q[qb][h])
            ps, ps_r = next_fm()
            for kc in range(8):
                P.op("pe", lambda e, kc=kc: e.matmul(ps[0:64, 0:TT], lhsT=wk[:, kc, h * 64:(h + 1) * 64], rhs=xT[:, kc, :],
                                                     start=(kc == 0), stop=(kc == 7)),
                     reads=[r_w, r_xT], writes=[ps_r], inc=(kc == 7))
            P.op("dve", lambda e: e.tensor_copy(out=Kaug[h][0:64, t0:t0 + TT], in_=ps[0:64, 0:TT]),
                 reads=[ps_r], writes=[r_K[h][sb_i]])
            for jj in range(3):
                P.dma("sp", Kaug[h][67 + jj:68 + jj, t0:t0 + TT], csn[h:h + 1, jj, :], reads=[r_cs],
                      writes=[r_K[h][sb_i]], sem_res=r_K[h][sb_i])
        for s in range(NS):
            yield
            ps, ps_r = next_fm()
            for kc in range(8):
                P.op("pe", lambda e, kc=kc: e.matmul(ps[:, 0:256], lhsT=xT[:, kc, s * 128:(s + 1) * 128], rhs=wv[:, kc, :],
                                                     start=(kc == 0), stop=(kc == 7)),
                     reads=[r_w, r_xT], writes=[ps_r], inc=(kc == 7))
            P.op("dve", lambda e: e.tensor_copy(out=Vaug[:, sb_i * NS + s, :, 0:64],
                                                in_=ps[:, 0:256].rearrange("p (h d) -> p h d", h=H)),
                 reads=[ps_r], writes=[r_V[sb_i]])
        yield
    gen = proj(0)
    for _ in gen:
        pass
    for sb_i in range(NSB):
        t0 = sb_i * TT
        qb = sb_i % 2
        gen = proj(sb_i + 1) if sb_i + 1 < NSB else iter(())
        items = []
        for h in range(H):
            nkb = sb_i * NS + NS
            for j in range(nkb):
                items.append((h, j))
        ob_i = sb_i % 2

        def emit_S(idx):
            h, j = items[idx]
            st, st_r = ST[idx % 2], rST[idx % 2]
            dj = j - sb_i * NS
            qlo = max(0, dj) * 128
            N = TT - qlo
            if dj >= 0:
                P.op("pe", lambda e: e.matmul(st[:, 0:128], lhsT=Kaug[h][0:70, j * 128:(j + 1) * 128],
                                              rhs=Qaug[qb][h][0:70, qlo:qlo + 128], start=True, stop=False),
                     reads=[r_K[h][j // NS], r_Q[qb][h]], writes=[st_r], inc=False)
                P.op("pe", lambda e: e.matmul(st[:, 0:128], lhsT=identb[:, :], rhs=maskb[:, :], start=False, stop=True),
                     reads=[r_c2], writes=[st_r], inc=(N == 128))
                if N > 128:
                    P.op("pe", lambda e: e.matmul(st[:, 128:N], lhsT=Kaug[h][0:70, j * 128:(j + 1) * 128],
                                                  rhs=Qaug[qb][h][0:70, qlo + 128:TT], start=True, stop=True),
                         reads=[r_K[h][j // NS], r_Q[qb][h]], writes=[st_r])
            else:
                P.op("pe", lambda e: e.matmul(st[:, 0:N], lhsT=Kaug[h][0:70, j * 128:(j + 1) * 128],
                                              rhs=Qaug[qb][h][0:70, qlo:TT], start=True, stop=True),
                     reads=[r_K[h][j // NS], r_Q[qb][h]], writes=[st_r])

        def emit_rest(idx):
            h, j = items[idx]
            st, st_r = ST[idx % 2], rST[idx % 2]
            pt, pt_r = PT[idx % 3], r_PT[idx % 3]
            dj = j - sb_i * NS
            qlo = max(0, dj) * 128
            N = TT - qlo
            nkb = sb_i * NS + NS
            oa, oa_r = OA[h % 2], rOA[h % 2]
            P.op("act", lambda e: e.activation(out=pt[:, 0:N], in_=st[:, 0:N], func=AF.Exp), reads=[st_r], writes=[pt_r])
            for qq in range(qlo // 128, NS):
                last_j = sb_i * NS + qq
                P.op("pe", lambda e, qq=qq: e.matmul(oa[:, qq * 128:qq * 128 + 65],
                                                     lhsT=pt[:, qq * 128 - qlo:qq * 128 - qlo + 128],
                                                     rhs=Vaug[:, j, h, :], start=(j == 0 and qq == 0),
                                                     stop=(j == last_j), skip_group_check=True),
                     reads=[pt_r, r_V[j // NS]], writes=[oa_r], inc=(qq == NS - 1))
            if j == nkb - 1:
                oav = oa[:, :].rearrange("p (q c) -> p q c", q=4)
                P.op("dve", lambda e: e.reciprocal(out=rinv[:, 0:NS], in_=oav[:, 0:NS, 64]), reads=[oa_r], writes=[r_rinv])
                for qq in range(NS):
                    P.op("dve", lambda e, qq=qq: e.tensor_scalar(out=oc[ob_i][:, qq, h * 64:(h + 1) * 64],
                                                                 in0=oa[:, qq * 128:qq * 128 + 64],
                                                                 scalar1=rinv[:, qq:qq + 1], scalar2=None, op0=ALU.mult),
                         reads=[oa_r, r_rinv], writes=[r_oc[ob_i]])

        emit_S(0)
        nsteps = max(1, -(-48 // len(items)))
        for idx in range(len(items)):
            if idx + 1 < len(items):
                emit_S(idx + 1)
            emit_rest(idx)
            for _ in range(nsteps):
                next(gen, None)
        for _ in gen:
            pass
        for hf in range(2):
            for qq in range(NS):
                P.op("pe", lambda e, hf=hf, qq=qq: e.transpose(out=TR[:, hf * 512 + qq * 128: hf * 512 + (qq + 1) * 128],
                                                               in_=oc[ob_i][:, qq, hf * 128:(hf + 1) * 128],
                                                               identity=ident[:, :]),
                     reads=[r_oc[ob_i], r_c], writes=[rTR], inc=(hf == 1 and qq == NS - 1))
        P.op("act", lambda e: e.copy(out=ocT[ob_i][:, :, :],
                                     in_=TR[:, :].rearrange("p (a t) -> p a t", a=2)[:, :, 0:TT]),
             reads=[rTR], writes=[r_ocT[ob_i]])
        out_tags.append(P.dma("sp", oc_dst(sb_i).rearrange("(hf p) t -> p hf t", p=128), ocT[ob_i][:, :, :],
                              reads=[r_ocT[ob_i]]))
        if after_store is not None:
            after_store(sb_i, out_tags[-1])
    return out_tags, cx


MASK = np.where(np.arange(128)[None, :] >= np.arange(128)[:, None], 0.0, NEG).astype(np.float32)


def fox_inputs(p, l, g):
    w_in = p["w_in"][l]
    c_off = RWKV_COLS
    f_off = RWKV_COLS + CONV_COLS
    if True:
        cs = slice(g * 256, (g + 1) * 256)
        wcv = np.concatenate([w_in[:, c_off + k * 512 + g * 256: c_off + k * 512 + (g + 1) * 256] for k in range(3)], 1)
        m = {
            "wc": np.ascontiguousarray(wcv),
            "wq": np.ascontiguousarray(w_in[:, f_off + g * 256: f_off + (g + 1) * 256]),
            "wk": np.ascontiguousarray(w_in[:, f_off + 512 + g * 256: f_off + 512 + (g + 1) * 256]),
            "wv": np.ascontiguousarray(w_in[:, f_off + 1024 + g * 256: f_off + 1024 + (g + 1) * 256]),
            "wf": np.ascontiguousarray(w_in[:, f_off + 1536 + g * 4: f_off + 1536 + (g + 1) * 4]),
            "bf": np.ascontiguousarray(p["b_forget"][l][g * 4:(g + 1) * 4].reshape(4, 1)),
            "cw": np.ascontiguousarray(p["conv_w"][l][:, cs].reshape(3, 2, 128).transpose(2, 1, 0).reshape(128, 6)),
            "lnB": np.ascontiguousarray(np.stack([_bc(p["ln0_g"]), _bc(p["ln0_b"])], 0)),
            "ident": IDENT, "mask": MASK,
        }
    return m


def run_fox(xb, p, l, do_ln0, n_cores=8):
    B, T, _ = xb.shape
    nc = build_fox(T, do_ln0)
    in_maps = []
    for c in range(n_cores):
        b, g = c // 2, c % 2
        m = fox_inputs(p, l, g)
        m["x"] = np.ascontiguousarray(xb[b])
        in_maps.append(m)
    res = run_bass_kernel_spmd(nc, in_maps, core_ids=list(range(n_cores)))
    obT = np.stack([np.concatenate([res.results[2 * b + g]["obT"] for g in range(2)], 0) for b in range(B)], 0)
    ocT = np.stack([np.concatenate([res.results[2 * b + g]["ocT"] for g in range(2)], 0) for b in range(B)], 0)
    return obT, ocT


RWKV_LN_EPS = 64e-5
RWKV_WINDOW = 16
NPJ = 2
DECAY_SCALE = -0.6065306597126334


class Sched:
    def __init__(self):
        self.tasks = []
        self.done = set()

    def add(self, name, gen, deps=()):
        self.tasks.append([name, gen, set(deps)])

    def run(self, window=10):
        active = []
        pending = list(self.tasks)
        while pending or active:
            i = 0
            while i < len(pending) and len(active) < window:
                t = pending[i]
                if t[2] <= self.done:
                    active.append(t)
                    pending.pop(i)
                else:
                    i += 1
            assert active, ("deadlock", [t[0] for t in pending[:5]])
            for t in list(active):
                try:
                    next(t[1])
                except StopIteration:
                    self.done.add(t[0])
                    active.remove(t)


def decl_rwkv(nc, pre):
    dram = lambda n, s_, dt=F32: nc.dram_tensor(pre + n, list(s_), dt, kind="ExternalInput").ap()
    return dict(wall=dram("wall", [D, 1024]), muB=dram("muB", [128, 1024]), chv=dram("chv", [128, 2, 8]),
                w2ia=dram("w2ia", [128, 256]), w2g=dram("w2g", [128, 256]), lnxB=dram("lnxB", [2, 128, 256]),
                lnB=dram("lnB", [2, 128, D]), ident=dram("ident", [128, 128]), mask4=dram("mask4", [128, 512]),
                maskL=dram("maskL", [128, 128]), bones=dram("bones", [128, 128]), sel=dram("sel", [128, 2]),
                smask=dram("smask", [128, 512]))


def build_rwkv(T, do_ln0):
    nc = bass.Bass("TRN2", target_bir_lowering=False)
    A = decl_rwkv(nc, "")
    x_d = nc.dram_tensor("x", [T, D], F32, kind="ExternalInput").ap()
    oaT_d = nc.dram_tensor("oaT", [256, T], BF16, kind="ExternalOutput").ap()
    cx0 = Ctx(nc)
    TTr = min(T, 512)
    tags, cx = emit_rwkv(nc, cx0.P, A, T, do_ln0, lambda t0, n: x_d[t0:t0 + n, :],
                         lambda ti: oaT_d[:, ti * TTr:(ti + 1) * TTr])
    cx0.P.finish(tags)
    cx.close()
    cx0.close()
    return nc


def emit_rwkv(nc, P, A, T, do_ln0, xsrc, oa_dst, after_store=None):
    cx = Ctx(nc, P)
    (wall_d, muB_d, chv_d, w2ia_d, w2g_d, lnxB_d, lnB_d, ident_d, mask4_d, maskL_d, bones_d, sel_d, smask_d) = (
        A[k] for k in ("wall", "muB", "chv", "w2ia", "w2g", "lnxB", "lnB", "ident", "mask4", "maskL", "bones", "sel",
                       "smask"))

    TT = min(T, 512)
    NCH = TT // 128
    NTI = T // TT

    ident = cx.sb([128, 128], F32, "ident")
    identb = cx.sb([128, 128], BF16, "identb")
    mask4 = cx.sb([128, 512], F32, "mask4")
    maskL = cx.sb([128, 128], F32, "maskL")
    bones = cx.sb([128, 128], F32, "bones")
    self_ = cx.sb([128, 2], F32, "self")
    selb = cx.sb([128, 2], BF16, "selb")
    smask = cx.sb([128, 512], F32, "smask")
    lnB = cx.sb([128, 2, D], F32, "lnB") if do_ln0 else None
    lnxB = cx.sb([128, 2, 256], F32, "lnxB")
    chv = cx.sb([128, 2, 8], F32, "chv")
    omka = cx.sb([128, 2], F32, "omka")
    muB = cx.sb([128, 1024], F32, "muB")
    wtmp = cx.sb([128, 512], F32, "wtmp")
    wtmp2 = cx.sb([128, 512], F32, "wtmp2")
    W0 = cx.sb([128, 8, 1024], BF16, "W0")
    W1 = cx.sb([128, 8, 1024], BF16, "W1")
    w2iaf = cx.sb([128, 256], F32, "w2iaf")
    w2ia = cx.sb([128, 256], BF16, "w2ia")
    w2gf = cx.sb([128, 256], F32, "w2gf")
    w2g = cx.sb([128, 256], BF16, "w2g")
    r_c = Res("consts")
    r_c2 = Res("consts2")
    r_wt = Res("wtmp")
    r_W = Res("W")

    for t_, d_ in ((ident, ident_d), (mask4, mask4_d), (maskL, maskL_d), (bones, bones_d), (self_, sel_d),
                   (smask, smask_d), (muB, muB_d), (w2iaf, w2ia_d), (w2gf, w2g_d)):
        P.dma("sp", t_[:, :], d_[:, :], writes=[r_c])
    if do_ln0:
        P.dma("sp", lnB[:, :, :], lnB_d.rearrange("a p d -> p a d"), writes=[r_c])
    P.dma("sp", lnxB[:, :, :], lnxB_d.rearrange("a p d -> p a d"), writes=[r_c])
    P.dma("sp", chv[:, :, :], chv_d[:, :, :], writes=[r_c])
    P.op("dve", lambda e: e.tensor_copy(out=identb[:, :], in_=ident[:, :]), reads=[r_c], writes=[r_c2])
    P.op("dve", lambda e: e.tensor_copy(out=selb[:, :], in_=self_[:, :]), reads=[r_c], writes=[r_c2])
    P.op("dve", lambda e: e.tensor_copy(out=w2ia[:, :], in_=w2iaf[:, :]), reads=[r_c], writes=[r_c2])
    P.op("dve", lambda e: e.tensor_copy(out=w2g[:, :], in_=w2gf[:, :]), reads=[r_c], writes=[r_c2])
    P.op("dve", lambda e: e.tensor_scalar(out=omka[:, :], in0=chv[:, :, 3], scalar1=-1.0, scalar2=1.0,
                                          op0=ALU.mult, op1=ALU.add), reads=[r_c], writes=[r_c2])
    for kc in range(8):
        for hf in range(2):
            fs_ = slice(hf * 512, (hf + 1) * 512)
            P.dma("sp", wtmp[:, :], wall_d[kc * 128:(kc + 1) * 128, fs_], writes=[r_wt])
            P.op("dve", lambda e: e.tensor_tensor(out=wtmp2[:, :], in0=wtmp[:, :], in1=muB[:, fs_], op=ALU.mult),
                 reads=[r_wt, r_c], writes=[r_W])
            P.op("dve", lambda e, kc=kc: e.tensor_copy(out=W1[:, kc, fs_], in_=wtmp2[:, :]), reads=[r_W], writes=[r_W])
            P.op("dve", lambda e, kc=kc: e.tensor_tensor(out=W0[:, kc, fs_], in0=wtmp[:, :], in1=wtmp2[:, :],
                                                         op=ALU.subtract),
                 reads=[r_wt, r_W], writes=[r_W])

    xs = cx.sb([128, 2, D], F32, "xs")
    xT = cx.sb([128, 8, 1 + TT], BF16, "xT")
    stats = cx.sb([128, 2, 6], F32, "stats")
    mv = cx.sb([128, 2], F32, "mv")
    rstd = cx.sb([128, 1], F32, "rstd")
    r_xs = [Res(f"xs{i}") for i in range(2)]
    r_xT = Res("xT")
    r_stat = Res("stat")
    def pt(name, dt=F32, share=True):
        t_ = cx.sb([128, TT], dt, name)
        return [t_, t_] if share else [t_, cx.sb([128, TT], dt, name)]
    rT, kT, aT, lw, cumI, cumE, Dexcl, invD, E2, kkr, sq, rn, kk, tmpa, kp, bT = (
        pt(n) for n in ("rT", "kT", "aT", "lw", "cumI", "cumE", "Dexcl", "invD", "E2", "kkr", "sq", "rn", "kk",
                        "tmpa", "kp", "bT"))
    BhT, KhT = pt("BhT", share=False), pt("KhT", share=False)
    Dincl1 = cx.sb([128, TT], F32, "Dincl1")
    r_bk = [Res("bhkh0"), Res("bhkh1")]
    lo0T = cx.sb([128, TT], BF16, "lo0T")
    sgT = cx.sb([128, TT], BF16, "sgT")
    _tres = {}

    def qr(t_):
        return _tres.setdefault(id(t_), Res("tmp"))
    r_lo = Res("lo")
    ARt = [[cx.sb([128, NCH, 256], BF16, "ARt") for _ in range(2)] for _ in range(2)]
    BtT = [[cx.sb([128, TT], BF16, "BtT") for _ in range(2)] for _ in range(2)]
    KtT = [[cx.sb([128, TT], BF16, "KtT") for _ in range(2)] for _ in range(2)]
    rkT = [[cx.sb([128, TT], BF16, "rkT") for _ in range(2)] for _ in range(2)]
    DC = [[cx.sb([128, NCH], F32, "DC") for _ in range(2)] for _ in range(2)]
    BKh = [[cx.sb([128, 512], BF16, "BKh") for _ in range(NCH)] for _ in range(2)]
    Vb = [[cx.sb([128, 256], BF16, "Vb") for _ in range(NCH)] for _ in range(2)]
    Gt = [[cx.sb([128, 256], BF16, "Gt") for _ in range(NCH)] for _ in range(2)]
    r_AR = [[Res(f"AR{a}{b}") for b in range(2)] for a in range(2)]
    r_Bt = [[Res(f"Bt{a}{b}") for b in range(2)] for a in range(2)]
    r_Kt = [[Res(f"Kt{a}{b}") for b in range(2)] for a in range(2)]
    r_rk = [[Res(f"rk{a}{b}") for b in range(2)] for a in range(2)]
    r_Di = [[Res(f"Di{a}{b}") for b in range(2)] for a in range(2)]
    r_BKh = [[Res(f"BKh{a}{c}") for c in range(NCH)] for a in range(2)]
    r_Vb = [[Res(f"Vb{a}{c}") for c in range(NCH)] for a in range(2)]
    r_Gt = [[Res(f"Gt{a}{c}") for c in range(NCH)] for a in range(2)]
    NSLOT = NCH
    NM = [[cx.sb([128, 512], BF16, "NM") for _ in range(4)] for _ in range(NSLOT)]
    r_NM = [[Res(f"NM{a}{h}") for h in range(4)] for a in range(NSLOT)]
    Xb = [[[cx.sb([128, 128], BF16, "X") for _ in range(2)] for _ in range(4)] for _ in range(NSLOT)]
    r_X = [[[Res(f"X{a}{h}{k}") for k in range(2)] for h in range(4)] for a in range(NSLOT)]
    NL = [[[cx.sb([128, 256], BF16, "NL") for _ in range(2)] for _ in range(4)] for _ in range(NSLOT)]
    r_NL = [[[Res(f"NL{a}{h}{k}") for k in range(2)] for h in range(4)] for a in range(NSLOT)]
    Sf = [cx.sb([128, 128], F32, "Sf") for _ in range(2)]
    Sb = [cx.sb([128, 128], BF16, "Sb") for _ in range(2)]
    r_S = [Res("S0"), Res("S1")]
    brp = [cx.sb([128, 128], BF16, "brp") for _ in range(2)]
    Wbp = [cx.sb([128, 128], BF16, "Wbp") for _ in range(2)]
    r_br = [Res("br0"), Res("br1")]
    r_Wb = [Res("Wb0"), Res("Wb1")]
    Ysb = [cx.sb([128, 256], F32, "Ysb") for _ in range(2)]
    r_Y = [[Res(f"Y{a}{p}") for p in range(2)] for a in range(2)]
    yn = cx.sb([128, 256], F32, "yn")
    ost = cx.sb([128, 4, 6], F32, "ost")
    omv = cx.sb([128, 4, 2], F32, "omv")
    orstd = cx.sb([128, 4], F32, "orstd")
    bsb = cx.sb([128, 4], F32, "bsb")
    ob = [cx.sb([128, 256], F32, "ob") for _ in range(2)]
    oaT = [cx.sb([128, 2, TT], BF16, "oaT") for _ in range(2)]
    r_o = Res("ostuff")
    r_ob = [Res("ob0"), Res("ob1")]
    r_oaT = [Res("oaT0"), Res("oaT1")]

    PJ = [cx.ps([128, 512], F32, "PJ") for _ in range(NPJ)]
    INV = [cx.ps([128, 512], F32, "INV") for _ in range(6 - NPJ)]
    SEQ = [cx.ps([128, 512], F32, "SEQ") for _ in range(2)]
    rPJ = [Res(f"PJ{i}", True) for i in range(NPJ)]
    rINV = [Res(f"INV{i}", True) for i in range(6 - NPJ)]
    rSEQ = [Res("SEQ0", True), Res("SEQ1", True)]
    pj_i = [0]
    inv_i = [0]

    def next_pj():
        i = pj_i[0] % NPJ
        pj_i[0] += 1
        return PJ[i], rPJ[i]

    def next_inv():
        i = inv_i[0] % (6 - NPJ)
        inv_i[0] += 1
        return INV[i], rINV[i]

    P.op("dve", lambda e: e.memset(xT[:, :, :], 0.0), writes=[r_xT])
    for p in range(2):
        P.op("dve", lambda e, p=p: e.memset(Sf[p][:, :], 0.0), writes=[r_S[p]])
        P.op("dve", lambda e, p=p: e.memset(Sb[p][:, :], 0.0), writes=[r_S[p]])

    out_tags = []
    chs = lambda p, i: chv[:, p, i:i + 1]

    def c3(ap):
        return ap.rearrange("p (c t) -> p c t", c=NCH)

    def prep(ti):
        par = ti % 2
        t0 = ti * TT
        if ti > 0:
            P.op("pool", lambda e: e.tensor_copy(out=xT[:, :, 0:1], in_=xT[:, :, TT:TT + 1]), reads=[r_xT], writes=[r_xT])
        for s in range(NCH):
            xb_ = s % 2
            P.dma("sp", xs[:, xb_, :], xsrc(t0 + s * 128, 128), writes=[r_xs[xb_]])
            if do_ln0:
                layer_norm_rows(P, nc, xs[:, xb_, :], r_xs[xb_], lnB[:, 0, :], lnB[:, 1, :], stats, mv, rstd, r_stat, r_c)
            for half in range(2):
                TR, rTR = next_pj()
                for k4 in range(4):
                    kc = half * 4 + k4
                    P.op("pe", lambda e, kc=kc, k4=k4: e.transpose(out=TR[:, k4 * 128:(k4 + 1) * 128],
                                                                   in_=xs[:, xb_, kc * 128:(kc + 1) * 128],
                                                                   identity=ident[:, :]),
                         reads=[r_xs[xb_], r_c], writes=[rTR], inc=(k4 == 3))
                P.op("act", lambda e, half=half: e.copy(out=xT[:, half * 4:(half + 1) * 4, 1 + s * 128:1 + (s + 1) * 128],
                                                        in_=TR[:, :].rearrange("p (a t) -> p a t", a=4)),
                     reads=[rTR], writes=[r_xT])
            yield

        def proj_cm(c0, ncols=128):
            ps, ps_r = next_pj()
            for kc in range(8):
                P.op("pe", lambda e, kc=kc: e.matmul(ps[0:ncols, 0:TT], lhsT=W0[:, kc, c0:c0 + ncols],
                                                     rhs=xT[:, kc, 1:1 + TT], start=(kc == 0), stop=False),
                     reads=[r_W, r_xT], writes=[ps_r], inc=False)
            for kc in range(8):
                P.op("pe", lambda e, kc=kc: e.matmul(ps[0:ncols, 0:TT], lhsT=W1[:, kc, c0:c0 + ncols],
                                                     rhs=xT[:, kc, 0:TT], start=False, stop=(kc == 7)),
                     reads=[r_W, r_xT], writes=[ps_r], inc=(kc == 7))
            return ps, ps_r

        ps, ps_r = proj_cm(768)
        P.op("act", lambda e: e.activation(out=lo0T[0:64, :], in_=ps[0:64, 0:TT], func=AF.Tanh), reads=[ps_r], writes=[r_lo])
        P.op("act", lambda e: e.copy(out=lo0T[64:128, :], in_=ps[64:128, 0:TT]), reads=[ps_r], writes=[r_lo])
        ps, ps_r = proj_cm(896)
        P.op("act", lambda e: e.activation(out=sgT[:, :], in_=ps[:, 0:TT], func=AF.Sigmoid), reads=[ps_r], writes=[r_lo])
        yield
        for p in range(2):
            ps, ps_r = proj_cm(p * 128)
            P.op("act", lambda e: e.copy(out=rT[p][:, :], in_=ps[:, 0:TT]), reads=[ps_r], writes=[qr(rT[p])])
            ps, ps_r = proj_cm(256 + p * 128)
            P.op("act", lambda e: e.copy(out=kT[p][:, :], in_=ps[:, 0:TT]), reads=[ps_r], writes=[qr(kT[p])])
            ps, ps_r = next_pj()
            P.op("pe", lambda e: e.matmul(ps[:, 0:TT], lhsT=w2ia[0:64, p * 128:(p + 1) * 128], rhs=lo0T[0:64, :],
                                          start=True, stop=True), reads=[r_c2, r_lo], writes=[ps_r])
            P.op("act", lambda e: e.activation(out=lw[p][:, :], in_=ps[:, 0:TT], func=AF.Sigmoid, bias=chs(p, 0), scale=1.0),
                 reads=[ps_r, r_c], writes=[qr(lw[p])])
            ps, ps_r = next_pj()
            P.op("pe", lambda e: e.matmul(ps[:, 0:TT], lhsT=w2ia[64:128, p * 128:(p + 1) * 128], rhs=lo0T[64:128, :],
                                          start=True, stop=True), reads=[r_c2, r_lo], writes=[ps_r])
            P.op("act", lambda e: e.activation(out=aT[p][:, :], in_=ps[:, 0:TT], func=AF.Sigmoid, bias=chs(p, 1), scale=1.0),
                 reads=[ps_r, r_c], writes=[qr(aT[p])])
            yield
            P.op("dve", lambda e: e.tensor_scalar(out=lw[p][:, :], in0=lw[p][:, :], scalar1=DECAY_SCALE, scalar2=None,
                                                  op0=ALU.mult), reads=[qr(lw[p])], writes=[qr(lw[p])])
            P.op("dve", lambda e: e.tensor_tensor_scan(out=cumI[p][:, :], data0=smask[:, 0:TT], data1=lw[p][:, :],
                                                       initial=0.0, op0=ALU.mult, op1=ALU.add),
                 reads=[qr(lw[p]), r_c], writes=[qr(cumI[p])])
            P.op("pool", lambda e: e.tensor_tensor(out=cumE[p][:, :], in0=cumI[p][:, :], in1=lw[p][:, :], op=ALU.subtract),
                 reads=[qr(cumI[p]), qr(lw[p])], writes=[qr(cumE[p])])
            P.op("act", lambda e: e.activation(out=Dincl1[:, :], in_=cumI[p][:, :], func=AF.Exp),
                 reads=[qr(cumI[p])], writes=[qr(Dincl1)])
            P.op("dve", lambda e: e.tensor_copy(out=DC[par][p][:, :], in_=c3(Dincl1[:, :])[:, :, 127]),
                 reads=[qr(Dincl1)], writes=[r_Di[par][p]])
            P.op("act", lambda e: e.activation(out=invD[p][:, :], in_=cumI[p][:, :], func=AF.Exp, scale=-1.0),
                 reads=[qr(cumI[p])], writes=[qr(invD[p])])
            P.op("act", lambda e: e.activation(out=Dexcl[p][:, :], in_=cumE[p][:, :], func=AF.Exp),
                 reads=[qr(cumE[p])], writes=[qr(Dexcl[p])])
            for c in range(NCH):
                P.op("act", lambda e, c=c: e.activation(out=E2[p][:, c * 128:(c + 1) * 128],
                                                        in_=cumI[p][:, c * 128:(c + 1) * 128], func=AF.Exp,
                                                        bias=cumI[p][:, c * 128 + 127:c * 128 + 128], scale=-1.0),
                     reads=[qr(cumI[p])], writes=[qr(E2[p])])
            yield
            P.op("pool", lambda e: e.tensor_scalar(out=kkr[p][:, :], in0=kT[p][:, :], scalar1=chs(p, 2), scalar2=None,
                                                   op0=ALU.mult), reads=[qr(kT[p]), r_c], writes=[qr(kkr[p])])
            P.op("pool", lambda e: e.tensor_tensor(out=sq[p][:, :], in0=kkr[p][:, :], in1=kkr[p][:, :], op=ALU.mult),
                 reads=[qr(kkr[p])], writes=[qr(sq[p])])
            ps, ps_r = next_pj()
            P.op("pe", lambda e: e.matmul(ps[:, 0:TT], lhsT=bones[:, :], rhs=sq[p][:, :], start=True, stop=True),
                 reads=[r_c, qr(sq[p])], writes=[ps_r])
            P.op("act", lambda e: e.activation(out=rn[p][:, :], in_=ps[:, 0:TT], func=AF.Sqrt), reads=[ps_r],
                 writes=[qr(rn[p])])
            P.op("dve", lambda e: e.tensor_scalar(out=rn[p][:, :], in0=rn[p][:, :], scalar1=1e-12, scalar2=None,
                                                  op0=ALU.max), reads=[qr(rn[p])], writes=[qr(rn[p])])
            P.op("dve", lambda e: e.reciprocal(out=rn[p][:, :], in_=rn[p][:, :]), reads=[qr(rn[p])], writes=[qr(rn[p])])
            P.op("dve", lambda e: e.tensor_tensor(out=kk[p][:, :], in0=kkr[p][:, :], in1=rn[p][:, :], op=ALU.mult),
                 reads=[qr(kkr[p]), qr(rn[p])], writes=[qr(kk[p])])
            P.op("pool", lambda e: e.tensor_scalar(out=tmpa[p][:, :], in0=aT[p][:, :], scalar1=chs(p, 3),
                                                   scalar2=omka[:, p:p + 1], op0=ALU.mult, op1=ALU.add),
                 reads=[qr(aT[p]), r_c, r_c2], writes=[qr(tmpa[p])])
            P.op("pool", lambda e: e.tensor_tensor(out=kp[p][:, :], in0=kT[p][:, :], in1=tmpa[p][:, :], op=ALU.mult),
                 reads=[qr(kT[p]), qr(tmpa[p])], writes=[qr(kp[p])])
            yield
            P.op("dve", lambda e: e.scalar_tensor_tensor(out=ARt[par][p][:, :, 0:128], in0=c3(kk[p][:, :]), scalar=-1.0,
                                                         in1=c3(Dexcl[p][:, :]), op0=ALU.mult, op1=ALU.mult),
                 reads=[qr(kk[p]), qr(Dexcl[p])], writes=[r_AR[par][p]])
            P.op("pool", lambda e: e.tensor_tensor(out=ARt[par][p][:, :, 128:256], in0=c3(rT[p][:, :]),
                                                   in1=c3(Dincl1[:, :]), op=ALU.mult),
                 reads=[qr(rT[p]), qr(Dincl1)], writes=[r_AR[par][p]])
            P.op("pool", lambda e: e.tensor_tensor(out=bT[p][:, :], in0=kk[p][:, :], in1=aT[p][:, :], op=ALU.mult),
                 reads=[qr(kk[p]), qr(aT[p])], writes=[qr(bT[p])])
            P.op("dve", lambda e: e.tensor_tensor(out=BtT[par][p][:, :], in0=bT[p][:, :], in1=invD[p][:, :], op=ALU.mult),
                 reads=[qr(bT[p]), qr(invD[p])], writes=[r_Bt[par][p]])
            P.op("pool", lambda e: e.tensor_tensor(out=KtT[par][p][:, :], in0=kp[p][:, :], in1=invD[p][:, :], op=ALU.mult),
                 reads=[qr(kp[p]), qr(invD[p])], writes=[r_Kt[par][p]])
            P.op("dve", lambda e: e.tensor_tensor(out=BhT[p][:, :], in0=bT[p][:, :], in1=E2[p][:, :], op=ALU.mult),
                 reads=[qr(bT[p]), qr(E2[p])], writes=[r_bk[p]])
            P.op("pool", lambda e: e.tensor_tensor(out=KhT[p][:, :], in0=kp[p][:, :], in1=E2[p][:, :], op=ALU.mult),
                 reads=[qr(kp[p]), qr(E2[p]), r_bk[p]], writes=[r_bk[p]])
            P.op("dve", lambda e: e.scalar_tensor_tensor(out=rkT[par][p][:, :], in0=rT[p][:, :], scalar=chs(p, 4),
                                                         in1=kp[p][:, :], op0=ALU.mult, op1=ALU.mult),
                 reads=[qr(rT[p]), qr(kp[p]), r_c], writes=[r_rk[par][p]])
            yield
        for c in range(NCH):
            ps, ps_r = next_pj()
            for kc in range(8):
                P.op("pe", lambda e, kc=kc: e.matmul(ps[:, 0:256], lhsT=xT[:, kc, 1 + c * 128:1 + (c + 1) * 128],
                                                     rhs=W0[:, kc, 512:768], start=(kc == 0), stop=False),
                     reads=[r_W, r_xT], writes=[ps_r], inc=False)
            for kc in range(8):
                P.op("pe", lambda e, kc=kc: e.matmul(ps[:, 0:256], lhsT=xT[:, kc, c * 128:(c + 1) * 128],
                                                     rhs=W1[:, kc, 512:768], start=False, stop=(kc == 7)),
                     reads=[r_W, r_xT], writes=[ps_r], inc=(kc == 7))
            P.op("act", lambda e: e.copy(out=Vb[par][c][:, :], in_=ps[:, 0:256]), reads=[ps_r], writes=[r_Vb[par][c]])
            ps, ps_r = next_pj()
            P.op("pe", lambda e: e.matmul(ps[:, 0:256], lhsT=sgT[:, c * 128:(c + 1) * 128], rhs=w2g[:, :], start=True, stop=True),
                 reads=[r_lo, r_c2], writes=[ps_r])
            P.op("act", lambda e: e.copy(out=Gt[par][c][:, :], in_=ps[:, 0:256]), reads=[ps_r], writes=[r_Gt[par][c]])
            TR, rTR = next_pj()
            for q in range(4):
                src = (BhT, KhT)[q // 2][q % 2]
                P.op("pe", lambda e, q=q, src=src: e.transpose(out=TR[:, q * 128:(q + 1) * 128],
                                                               in_=src[:, c * 128:(c + 1) * 128], identity=ident[:, :]),
                     reads=[r_bk[q % 2], r_c], writes=[rTR], inc=(q == 3))
            P.op("dve", lambda e: e.tensor_copy(out=BKh[par][c][:, :], in_=TR[:, :]), reads=[rTR], writes=[r_BKh[par][c]])
            yield

    def inv(ti, c, h):
        par = ti % 2
        slot = c
        sp = slot
        p, hb = h // 2, (h % 2) * 64
        cs = slice(c * 128, (c + 1) * 128)
        a12, a12_r = next_inv()
        P.op("pe", lambda e: e.matmul(a12[:, 0:256], lhsT=BtT[par][p][hb:hb + 64, cs], rhs=ARt[par][p][hb:hb + 64, c, :],
                                      start=True, stop=True),
             reads=[r_Bt[par][p], r_AR[par][p]], writes=[a12_r], inc=False)
        P.op("pe", lambda e: e.matmul(a12[:, 256:512], lhsT=KtT[par][p][hb:hb + 64, cs], rhs=ARt[par][p][hb:hb + 64, c, :],
                                      start=False, stop=True, skip_group_check=True),
             reads=[r_Kt[par][p], r_AR[par][p]], writes=[a12_r])
        a3, a3_r = next_inv()
        P.op("pe", lambda e: e.matmul(a3[:, 0:128], lhsT=ARt[par][p][hb:hb + 64, c, 0:128], rhs=BtT[par][p][hb:hb + 64, cs],
                                      start=True, stop=True),
             reads=[r_Bt[par][p], r_AR[par][p]], writes=[a3_r])
        nm, nm_r = NM[slot][h], r_NM[slot][h]
        P.op("dve", lambda e: e.tensor_tensor(out=nm[:, :], in0=a12[:, :], in1=mask4[:, :], op=ALU.mult),
             reads=[a12_r, r_c], writes=[nm_r])
        nl = NL[sp][h]
        nl_r = r_NL[sp][h]
        P.op("dve", lambda e: e.tensor_tensor(out=nl[0][:, 128:256], in0=a3[:, 0:128], in1=maskL[:, :], op=ALU.mult),
             reads=[a3_r, r_c], writes=[nl_r[0]])
        X, X_r = Xb[slot][h], r_X[slot][h]
        P.op("pool", lambda e: e.tensor_tensor(out=X[0][:, :], in0=nm[:, 0:128], in1=identb[:, :], op=ALU.add),
             reads=[nm_r, r_c2], writes=[X_r[0]])
        yield
        Np, Np_r = nm[:, 0:128], nm_r
        Lp, Lp_r = nl[0][:, 128:256], nl_r[0]
        xi = 0
        for rnd in range(6):
            last = (rnd == 5)
            pp = (rnd + 1) % 2
            bk, bk_r = next_inv()
            if not last:
                P.op("pe", lambda e: e.matmul(bk[:, 0:128], lhsT=Lp, rhs=Np, start=True, stop=True),
                     reads=[Lp_r, Np_r], writes=[bk_r], inc=False)
            P.op("pe", lambda e: e.matmul(bk[:, 128:256], lhsT=Np, rhs=Lp, start=last, stop=True,
                                          skip_group_check=True),
                 reads=[Lp_r, Np_r], writes=[bk_r])
            if not last:
                P.op("act", lambda e: e.copy(out=nl[pp][:, :], in_=bk[:, 0:256]), reads=[bk_r], writes=[nl_r[pp]])
            else:
                P.op("act", lambda e: e.copy(out=nl[pp][:, 128:256], in_=bk[:, 128:256]), reads=[bk_r], writes=[nl_r[pp]])
            Np, Np_r = nl[pp][:, 0:128], nl_r[pp]
            Lp, Lp_r = nl[pp][:, 128:256], nl_r[pp]
            yield
            px, px_r = next_inv()
            P.op("pe", lambda e: e.matmul(px[:, 0:128], lhsT=Lp, rhs=X[xi][:, :], start=True, stop=True),
                 reads=[Lp_r, X_r[xi]], writes=[px_r])
            P.op("dve", lambda e: e.tensor_tensor(out=X[1 - xi][:, :], in0=px[:, 0:128], in1=X[xi][:, :], op=ALU.add),
                 reads=[px_r, X_r[xi]], writes=[X_r[1 - xi]])
            xi = 1 - xi
            yield
        assert xi == 0

    def seq(ti, c, p):
        par = ti % 2
        slot = c
        sq_, sq_r = SEQ[p], rSEQ[p]
        cs = slice(c * 128, (c + 1) * 128)
        for hh in range(2):
            h = 2 * p + hh
            hb = hh * 64
            P.op("pe", lambda e: e.matmul(sq_[:, hh * 64:(hh + 1) * 64], lhsT=ARt[par][p][hb:hb + 64, c, 0:128],
                                          rhs=Sb[p][hb:hb + 64, hh * 64:(hh + 1) * 64], start=(hh == 0), stop=False,
                                          skip_group_check=True),
                 reads=[r_AR[par][p], r_S[p]], writes=[sq_r], inc=False)
            P.op("pe", lambda e: e.matmul(sq_[:, hh * 64:(hh + 1) * 64], lhsT=NM[slot][h][:, 256:384],
                                          rhs=Vb[par][c][:, h * 64:(h + 1) * 64], start=False, stop=True,
                                          skip_group_check=True),
                 reads=[r_NM[slot][h], r_Vb[par][c]], writes=[sq_r], inc=(hh == 1))
        P.op("act", lambda e: e.copy(out=brp[p][:, :], in_=sq_[:, 0:128]), reads=[sq_r], writes=[r_br[p]])
        yield
        for hh in range(2):
            h = 2 * p + hh
            P.op("pe", lambda e: e.matmul(sq_[:, 128 + hh * 64:128 + (hh + 1) * 64], lhsT=Xb[slot][h][0][:, :],
                                          rhs=brp[p][:, hh * 64:(hh + 1) * 64], start=False, stop=True,
                                          skip_group_check=True),
                 reads=[r_X[slot][h][0], r_br[p]], writes=[sq_r], inc=(hh == 1))
        P.op("dve", lambda e: e.tensor_copy(out=Wbp[p][:, :], in_=sq_[:, 128:256]), reads=[sq_r], writes=[r_Wb[p]])
        yield
        P.op("pe", lambda e: e.matmul(sq_[:, 256:384], lhsT=BKh[par][c][:, p * 128:(p + 1) * 128], rhs=Wbp[p][:, :],
                                      start=False, stop=False, skip_group_check=True),
             reads=[r_BKh[par][c], r_Wb[p]], writes=[sq_r], inc=False)
        P.op("pe", lambda e: e.matmul(sq_[:, 256:384], lhsT=BKh[par][c][:, 256 + p * 128:256 + (p + 1) * 128],
                                      rhs=Vb[par][c][:, p * 128:(p + 1) * 128], start=False, stop=True,
                                      skip_group_check=True),
             reads=[r_BKh[par][c], r_Vb[par][c]], writes=[sq_r], inc=False)
        for hh in range(2):
            h = 2 * p + hh
            hb = hh * 64
            yc = slice(384 + hh * 64, 384 + (hh + 1) * 64)
            P.op("pe", lambda e: e.matmul(sq_[:, yc], lhsT=ARt[par][p][hb:hb + 64, c, 128:256],
                                          rhs=Sb[p][hb:hb + 64, hh * 64:(hh + 1) * 64], start=False, stop=False,
                                          skip_group_check=True),
                 reads=[r_AR[par][p], r_S[p]], writes=[sq_r], inc=False)
            P.op("pe", lambda e: e.matmul(sq_[:, yc], lhsT=NM[slot][h][:, 128:256], rhs=Wbp[p][:, hh * 64:(hh + 1) * 64],
                                          start=False, stop=False, skip_group_check=True),
                 reads=[r_NM[slot][h], r_Wb[p]], writes=[sq_r], inc=False)
            P.op("pe", lambda e: e.matmul(sq_[:, yc], lhsT=NM[slot][h][:, 384:512], rhs=Vb[par][c][:, h * 64:(h + 1) * 64],
                                          start=False, stop=True, skip_group_check=True),
                 reads=[r_NM[slot][h], r_Vb[par][c]], writes=[sq_r], inc=(hh == 1))
        P.op("dve", lambda e: e.scalar_tensor_tensor(out=Sf[p][:, :], in0=Sf[p][:, :],
                                                     scalar=DC[par][p][:, c:c + 1],
                                                     in1=sq_[:, 256:384], op0=ALU.mult, op1=ALU.add),
             reads=[sq_r, r_S[p], r_Di[par][p]], writes=[r_S[p]])
        P.op("act", lambda e: e.copy(out=Sb[p][:, :], in_=Sf[p][:, :]), reads=[r_S[p]], writes=[r_S[p]])
        P.op("dve", lambda e: e.tensor_copy(out=Ysb[c % 2][:, p * 128:(p + 1) * 128], in_=sq_[:, 384:512]),
             reads=[sq_r], writes=[r_Y[c % 2][p]])
        yield

    def outp(ti, c):
        par = ti % 2
        t0 = ti * TT + c * 128
        Y = Ysb[c % 2]
        ry = r_Y[c % 2]
        cs = slice(c * 128, (c + 1) * 128)
        for h in range(4):
            P.op("dve", lambda e, h=h: e.bn_stats(out=ost[:, h, :], in_=Y[:, h * 64:(h + 1) * 64]), reads=ry, writes=[r_o])
        for h in range(4):
            P.op("dve", lambda e, h=h: e.bn_aggr(out=omv[:, h, :], in_=ost[:, h, :]), reads=[r_o], writes=[r_o])
        P.op("act", lambda e: e.activation(out=orstd[:, :], in_=omv[:, :, 1], func=AF.Sqrt, bias=float(RWKV_LN_EPS), scale=1.0),
             reads=[r_o], writes=[r_o])
        P.op("dve", lambda e: e.reciprocal(out=orstd[:, :], in_=orstd[:, :]), reads=[r_o], writes=[r_o])
        yield
        for h in range(4):
            P.op("dve", lambda e, h=h: e.tensor_scalar(out=yn[:, h * 64:(h + 1) * 64], in0=Y[:, h * 64:(h + 1) * 64],
                                                       scalar1=omv[:, h, 0:1], scalar2=orstd[:, h:h + 1],
                                                       op0=ALU.subtract, op1=ALU.mult),
                 reads=ry + [r_o], writes=[r_o])
        P.op("pool", lambda e: e.tensor_tensor(out=yn[:, :], in0=yn[:, :], in1=lnxB[:, 0, :], op=ALU.mult),
             reads=[r_o, r_c], writes=[r_o])
        P.op("pool", lambda e: e.tensor_tensor(out=yn[:, :], in0=yn[:, :], in1=lnxB[:, 1, :], op=ALU.add),
             reads=[r_o, r_c], writes=[r_o])
        ps, ps_r = next_pj()
        for p in range(2):
            P.op("pe", lambda e, p=p: e.matmul(ps[:, p * 2:(p + 1) * 2], lhsT=rkT[par][p][:, cs], rhs=selb[:, :],
                                               start=(p == 0), stop=True, skip_group_check=True),
                 reads=[r_rk[par][p], r_c2], writes=[ps_r], inc=(p == 1))
        P.op("dve", lambda e: e.tensor_copy(out=bsb[:, :], in_=ps[:, 0:4]), reads=[ps_r], writes=[r_o])
        yield
        for h in range(4):
            P.op("dve", lambda e, h=h: e.scalar_tensor_tensor(out=yn[:, h * 64:(h + 1) * 64],
                                                              in0=Vb[par][c][:, h * 64:(h + 1) * 64],
                                                              scalar=bsb[:, h:h + 1], in1=yn[:, h * 64:(h + 1) * 64],
                                                              op0=ALU.mult, op1=ALU.add),
                 reads=[r_o, r_Vb[par][c]], writes=[r_o])
        oi = c % 2
        P.op("pool", lambda e: e.tensor_tensor(out=ob[oi][:, :], in0=yn[:, :], in1=Gt[par][c][:, :], op=ALU.mult),
             reads=[r_o, r_Gt[par][c]], writes=[r_ob[oi]])
        TR, rTR = next_pj()
        for pp in range(2):
            P.op("pe", lambda e, pp=pp: e.transpose(out=TR[:, pp * 128:(pp + 1) * 128], in_=ob[oi][:, pp * 128:(pp + 1) * 128],
                                                    identity=ident[:, :]),
                 reads=[r_ob[oi], r_c], writes=[rTR], inc=(pp == 1))
        P.op("act", lambda e: e.copy(out=oaT[par][:, :, c * 128:(c + 1) * 128],
                                     in_=TR[:, 0:256].rearrange("p (a t) -> p a t", a=2)),
             reads=[rTR], writes=[r_oaT[par]])
        if c == NCH - 1:
            out_tags.append(P.dma("sp", oa_dst(ti).rearrange("(pp p) t -> p pp t", p=128),
                                  oaT[par][:, :, :], reads=[r_oaT[par]]))
            if after_store is not None:
                after_store(ti, out_tags[-1])
        yield

    S = Sched()

    def add_prep(ti):
        deps = [f"prep{ti - 1}"] if ti > 0 else []
        if ti > 1:
            deps += [f"out{ti - 2}_{NCH - 1}"]
        S.add(f"prep{ti}", prep(ti), deps)

    add_prep(0)
    for ti in range(NTI):
        if ti + 1 < NTI:
            add_prep(ti + 1)
        for c in range(NCH):
            for h in range(4):
                d = [f"prep{ti}"]
                if ti >= 1:
                    d.append(f"seq{ti - 1}_{c}_{h // 2}")
                S.add(f"inv{ti}_{c}_{h}", inv(ti, c, h), d)
        for c in range(NCH):
            for p in range(2):
                d = [f"inv{ti}_{c}_{2 * p}", f"inv{ti}_{c}_{2 * p + 1}"]
                if c > 0:
                    d.append(f"seq{ti}_{c - 1}_{p}")
                elif ti > 0:
                    d.append(f"seq{ti - 1}_{NCH - 1}_{p}")
                S.add(f"seq{ti}_{c}_{p}", seq(ti, c, p), d)
            d = [f"seq{ti}_{c}_0", f"seq{ti}_{c}_1"]
            if c > 0:
                d.append(f"out{ti}_{c - 1}")
            elif ti > 0:
                d.append(f"out{ti - 1}_{NCH - 1}")
            S.add(f"out{ti}_{c}", outp(ti, c), d)
    S.run(window=RWKV_WINDOW)
    return out_tags, cx


_ar = np.arange(128)
MASK4 = np.concatenate([(_ar[:, None] < _ar[None, :]), (_ar[:, None] <= _ar[None, :])] * 2, 1).astype(np.float32)
MASKL = (_ar[None, :] < _ar[:, None]).astype(np.float32)
BONES = (_ar[:, None] // 64 == _ar[None, :] // 64).astype(np.float32)
SEL = (_ar[:, None] // 64 == np.arange(2)[None, :]).astype(np.float32)
SMASK = np.ascontiguousarray(np.broadcast_to((np.arange(512) % 128 != 0).astype(np.float32)[None, :], (128, 512)))


def rwkv_inputs(p, l, g):
    w_in = p["w_in"][l]
    mu = p["mu_shift"][l]
    if True:
        gc = slice(g * 256, (g + 1) * 256)
        cols = np.concatenate([np.arange(g * 256, (g + 1) * 256), 512 + np.arange(g * 256, (g + 1) * 256),
                               1024 + np.arange(g * 256, (g + 1) * 256), np.arange(1536, 1792)])
        vecs = [p["w0_decay"][l][gc], p["a0"][l][gc], p["k_k"][l][gc], p["k_a"][l][gc], p["r_k"][l].reshape(-1)[gc]]
        chv = np.zeros((128, 2, 8), np.float32)
        for i, v in enumerate(vecs):
            chv[:, :, i] = v.reshape(2, 128).T
        m = {
            "wall": np.ascontiguousarray(w_in[:, cols]),
            "muB": _bc(mu[cols]),
            "chv": chv,
            "w2ia": np.ascontiguousarray(np.concatenate([p["w2_decay"][l][:, gc], p["w2_iclr"][l][:, gc]], 0)),
            "w2g": np.ascontiguousarray(p["w2_gate"][l][:, gc]),
            "lnxB": np.ascontiguousarray(np.stack([_bc(p["lnx_g"][l][gc]), _bc(p["lnx_b"][l][gc])], 0)),
            "lnB": np.ascontiguousarray(np.stack([_bc(p["ln0_g"]), _bc(p["ln0_b"])], 0)),
            "ident": IDENT, "mask4": MASK4, "maskL": MASKL, "bones": BONES, "sel": SEL, "smask": SMASK,
        }
    return m


def run_rwkv(xb, p, l, do_ln0, n_cores=8):
    B, T, _ = xb.shape
    nc = build_rwkv(T, do_ln0)
    in_maps = []
    for c in range(n_cores):
        b, g = c // 2, c % 2
        m = rwkv_inputs(p, l, g)
        m["x"] = np.ascontiguousarray(xb[b])
        in_maps.append(m)
    res = run_bass_kernel_spmd(nc, in_maps, core_ids=list(range(n_cores)))
    oaT = np.stack([np.concatenate([res.results[2 * b + g]["oaT"] for g in range(2)], 0) for b in range(B)], 0)
    return oaT


PAIRS = [[0, 1], [2, 3], [4, 5], [6, 7]]


def build_fused(T, ff_override=None):
    nc = bass.Bass("TRN2", target_bir_lowering=False)
    NT = T // 2
    TT = min(T, 512)
    CW = min(1024, NT)
    NCK = T // CW
    HCK = NCK // 2
    RY = min(512, NT)
    NYC = NT // RY
    x_d = nc.dram_tensor("x", [T, D], F32, kind="ExternalInput").ap()
    y_d = nc.dram_tensor("y", [NT, D], F32, kind="ExternalOutput").ap()
    A_r = [decl_rwkv(nc, f"r{l}_") for l in range(DEPTH)]
    A_f = [decl_fox(nc, f"f{l}_") for l in range(DEPTH)]
    A_m = [decl_merge(nc, f"m{l}_", moe=(l % 2 == 1), ff_override=ff_override) for l in range(DEPTH)]
    ola = [nc.dram_tensor(f"ola{l}", [NCK, 256, CW], BF16).ap() for l in range(DEPTH)]
    olf = [nc.dram_tensor(f"olf{l}", [NCK, 512, CW], BF16).ap() for l in range(DEPTH)]
    oalla = [nc.dram_tensor(f"oalla{l}", [NCK, 512, CW], BF16).ap() for l in range(DEPTH)]
    oallf = [nc.dram_tensor(f"oallf{l}", [NCK, 1024, CW], BF16).ap() for l in range(DEPTH)]
    omia = [nc.dram_tensor(f"omia{l}", [HCK, 512, CW], BF16).ap() for l in range(DEPTH)]
    omif = [nc.dram_tensor(f"omif{l}", [HCK, 1024, CW], BF16).ap() for l in range(DEPTH)]
    yloc = nc.dram_tensor("yloc", [NT, D], F32).ap()
    yall = nc.dram_tensor("yall", [NYC, 2 * RY, D], F32).ap()
    xh = nc.dram_tensor("xh", [NT, D], F32).ap()
    cx0 = Ctx(nc)
    P = cx0.P
    pid = nc.sync.partition_id()
    g = pid % 2
    for i in range(2):
        P.dma("sp", xh[i * (NT // 2):(i + 1) * (NT // 2), :], x_d[bass.ds(g * NT + i * (NT // 2), NT // 2), :])

    def odst(buf, r0):
        def f(ti):
            j, c = (ti * TT) // CW, (ti * TT) % CW
            return buf[j, r0:r0 + 256, c:c + TT]
        return f

    def chunk_gather(src, dst, per_chunk):
        acc = {}

        def cb(ti, tag):
            j = (ti * TT) // CW
            acc.setdefault(j, []).append(tag)
            if len(acc[j]) == per_chunk:
                P.collective_allgather(src[j].opt(), dst[j].opt(), PAIRS, acc[j])
        return cb

    def xsrc1(t0, n):
        rank, j, r = t0 // NT, (t0 % NT) // RY, t0 % RY
        return yall[j, rank * RY + r: rank * RY + r + n, :]

    xsrc = lambda t0, n: x_d[t0:t0 + n, :]
    tags_m = []
    for l in range(DEPTH):
        do_ln0 = (l == 0)
        tags_r, cx = emit_rwkv(nc, P, A_r[l], T, do_ln0, xsrc, odst(ola[l], 0),
                               after_store=chunk_gather(ola[l], oalla[l], CW // TT))
        P.barrier()
        cx.close()
        tags_f, cx = emit_fox(nc, P, A_f[l], T, do_ln0, xsrc, odst(olf[l], 0), odst(olf[l], 256),
                              after_store=chunk_gather(olf[l], oallf[l], 2 * (CW // TT)))
        P.barrier()
        cx.close()
        P.dma("sp", omia[l].rearrange("j r c -> (j r) c"),
              oalla[l].rearrange("j r c -> (j r) c")[bass.ds(g * (HCK * 512), HCK * 512), :])
        P.dma("sp", omif[l].rearrange("j r c -> (j r) c"),
              oallf[l].rearrange("j r c -> (j r) c")[bass.ds(g * (HCK * 1024), HCK * 1024), :])
        P.barrier()
        if l == 0:
            x_rows = lambda t0, n: xh[t0:t0 + n, :]
        else:
            x_rows = lambda t0, n: yloc[t0:t0 + n, :]

        def o_rows(br, half, t0, n, l=l):
            jj, c = t0 // CW, t0 % CW
            if br == 0:
                return omia[l][jj, half * 256:(half + 1) * 256, c:c + n]
            return omif[l][jj, half * 512 + (br - 1) * 256: half * 512 + br * 256, c:c + n]

        last = (l == DEPTH - 1)
        dst = y_d if last else yloc
        cb = None
        if not last:
            accy = {}

            def cb(t0, tag):
                j = t0 // RY
                accy.setdefault(j, []).append(tag)
                if len(accy[j]) == RY // 128:
                    P.collective_allgather(yloc[j * RY:(j + 1) * RY, :].opt(), yall[j].opt(), PAIRS, accy[j])
        tags_m, cx = emit_merge(nc, P, A_m[l], NT, do_ln0, l % 2 == 1, x_rows, o_rows, dst, after_store=cb)
        P.barrier()
        cx.close()
        if not last:
            xsrc = xsrc1
    P.finish(tags_m)
    cx0.close()
    return nc


def fused_in_maps(x, p, n_cores=8):
    in_maps = []
    mcommon = [merge_inputs(p, l, l % 2 == 1) for l in range(DEPTH)]
    for c in range(n_cores):
        b, g = c // 2, c % 2
        m = {"x": np.ascontiguousarray(x[b])}
        for l in range(DEPTH):
            for k, v in rwkv_inputs(p, l, g).items():
                m[f"r{l}_{k}"] = v
            for k, v in fox_inputs(p, l, g).items():
                m[f"f{l}_{k}"] = v
            for k, v in mcommon[l].items():
                m[f"m{l}_{k}"] = v
        in_maps.append(m)
    return in_maps


def kernel(**inputs):
    p = {k: np.asarray(v) for k, v in inputs.items()}
    x = np.ascontiguousarray(p["x"], dtype=np.float32)
    B, T, _ = x.shape
    nc = build_fused(T)
    in_maps = fused_in_maps(x, p)
    res = run_bass_kernel_spmd(nc, in_maps, core_ids=list(range(8)))
    NT = T // 2
    out = np.empty((B, T, D), np.float32)
    for c in range(8):
        b, g = c // 2, c % 2
        out[b, g * NT:(g + 1) * NT] = res.results[c]["y"]
    return out
```

```python
import contextlib
import numpy as np
import ml_dtypes
import concourse.bass as bass
import concourse.mybir as mybir
from concourse.bass_utils import run_bass_kernel_spmd

F32 = mybir.dt.float32
BF16 = mybir.dt.bfloat16
AF = mybir.ActivationFunctionType
ALU = mybir.AluOpType
AX = mybir.AxisListType

D = 1024
DEPTH = 2
ALPHA = (2 * DEPTH) ** 0.25
LN_EPS = 1e-5
D_FF = 2816
N_EXP = 8
D_FF_E = 3584
NBR = 3
BW = 512


class Res:
    __slots__ = ("name", "w", "r", "dsem", "dval", "excl")

    def __init__(self, name, excl=False):
        self.name = name
        self.excl = excl
        self.w = None
        self.r = {}
        self.dsem = None
        self.dval = 0

    def absorb(self, others):
        for o in others:
            if o.w is not None:
                self.r[("w", id(o))] = o.w
            for k, t in o.r.items():
                self.r[(k, id(o))] = t


class Prog:
    ROLL = 20000

    NPOOL = 20

    def __init__(self, nc, es):
        self.nc = nc
        self.es = es
        self.dpool = {q: dict(sems=[], vals=[], idx=0) for q in ("sp", "act", "pool")}
        self.cc_tags = []
        self.E = {}
        for name, obj in (("pe", nc.tensor), ("dve", nc.vector), ("act", nc.scalar),
                          ("pool", nc.gpsimd), ("sp", nc.sync)):
            self.E[name] = dict(obj=obj, sem=None, val=0, seen={}, nsem=0, pending=False)
        self.nsems = 0
        self.n_inst = 0

    def new_sem(self, name):
        self.nsems += 1
        return self.es.enter_context(self.nc.semaphore(f"{name}_{self.nsems}"))

    def _eng_sem(self, en):
        E = self.E[en]
        if E["sem"] is None or E["val"] >= self.ROLL:
            assert not E["pending"]
            E["sem"] = self.new_sem("e" + en)
            E["val"] = 0
        return E

    def _wait(self, en, tag, same_ok=True):
        E = self.E[en]
        ten, sem, val = tag
        if ten == en and sem is E["sem"]:
            if en == "pe":
                return
            if val < E["val"] - 1:
                return
        elif ten == en:
            return
        key = id(sem)
        if E["seen"].get(key, 0) >= val:
            return
        E["obj"].wait_ge(sem, val)
        E["seen"][key] = val
        self.n_inst += 1

    def _deps(self, en, reads, writes):
        for r in reads:
            if r.w is not None:
                self._wait(en, r.w)
            if r.excl:
                for t in r.r.values():
                    if t[0] != en:
                        self._wait(en, t)
        for w in writes:
            if w.w is not None:
                self._wait(en, w.w)
            for t in w.r.values():
                if t[0] == en and t[1] is self.E[en]["sem"] and en != "pe":
                    continue
                self._wait(en, t)

    def op(self, en, fn, reads=(), writes=(), inc=True):
        E = self._eng_sem(en)
        self._deps(en, reads, writes)
        ins = fn(E["obj"])
        self.n_inst += 1
        if inc:
            E["val"] += 1
            ins.then_inc(E["sem"], 1)
            tag = (en, E["sem"], E["val"])
            E["pending"] = False
        else:
            tag = (en, E["sem"], E["val"] + 1)
            E["pending"] = True
        for r in reads:
            r.r[en] = tag
        for w in writes:
            w.w = tag
            w.r = {}
        return ins

    def dma(self, q, out, in_, reads=(), writes=(), sem_res=None):
        E = self.E[q]
        self._deps(q, reads, writes)
        pool = self.dpool[q]
        i = pool["idx"] % self.NPOOL
        pool["idx"] += 1
        if len(pool["sems"]) <= i:
            pool["sems"].append(self.new_sem("d" + q))
            pool["vals"].append(0)
        sem, prev = pool["sems"][i], pool["vals"][i]
        if prev > 0:
            self._wait(q, ("dma", sem, prev))
        ins = E["obj"].dma_start(out=out, in_=in_)
        ins.then_inc(sem, 16)
        pool["vals"][i] = prev + 16
        self.n_inst += 1
        tag = ("dma", sem, prev + 16)
        for r in reads:
            r.r[("dma", id(sem))] = tag
        for w in writes:
            w.w = tag
            w.r = {}
        return tag

    def collective_allgather(self, in_ap, out_ap, groups, dep_tags):
        for t in dep_tags:
            self._wait("pool", t)
        sem = self.new_sem("cc")
        ins = self.nc.gpsimd.collective_compute("AllGather", ALU.bypass, replica_groups=groups,
                                                ins=[in_ap], outs=[out_ap])
        ins.then_inc(sem)
        self.n_inst += 1
        tag = ("cc", sem, 1)
        self.cc_tags.append(tag)
        return tag

    def barrier(self, extra_tags=()):
        tags = [(en, E["sem"], E["val"]) for en, E in self.E.items() if E["sem"] is not None and E["val"] > 0]
        for pool in self.dpool.values():
            for sem, v in zip(pool["sems"], pool["vals"]):
                if v > 0:
                    tags.append(("dma", sem, v))
        tags += list(self.cc_tags) + list(extra_tags)
        for en, E in self.E.items():
            assert not E["pending"], en
            for t in tags:
                if t[0] == en:
                    continue
                self._wait(en, t)

    def finish(self, out_tags):
        for t in out_tags:
            self._wait("sp", t)
        for en, E in self.E.items():
            assert not E["pending"], en


class Ctx:
    _uid = [0]

    def __init__(self, nc, P=None):
        self.nc = nc
        self.es = contextlib.ExitStack()
        self.P = P if P is not None else Prog(nc, self.es)
        Ctx._uid[0] += 1
        self.n = Ctx._uid[0] * 1000

    def sb(self, shape, dt, name=None):
        self.n += 1
        return self.es.enter_context(self.nc.sbuf_tensor(f"{name or 't'}{self.n}", list(shape), dt))

    def ps(self, shape, dt=F32, name=None):
        self.n += 1
        return self.es.enter_context(self.nc.psum_tensor(f"{name or 'p'}{self.n}", list(shape), dt))

    def close(self):
        self.es.close()


class WStream:
    def __init__(self, cx, nbuf, nbytes, name, lookahead=None, q="pool"):
        self.cx = cx
        self.P = cx.P
        self.bufs = [cx.sb([128, nbytes // 2], BF16, name) for _ in range(nbuf)]
        self.res = [Res(f"{name}{i}") for i in range(nbuf)]
        self.reqs = []
        self.issued = 0
        self.taken = 0
        self.la = lookahead if lookahead is not None else nbuf - 1
        self.q = q

    def plan(self, dram_ap, shape):
        self.reqs.append((dram_ap, tuple(shape)))

    def _view(self, i, shape):
        n = int(np.prod(shape))
        b = self.bufs[i % len(self.bufs)]
        v = b[:, 0:n]
        if len(shape) == 2:
            v = v.rearrange("p (a b) -> p a b", a=shape[0])
        return v

    def _issue(self, i):
        ap, shape = self.reqs[i]
        r = self.res[i % len(self.bufs)]
        self.P.dma(self.q, self._view(i, shape), ap, writes=[r])

    def next(self):
        i = self.taken
        while self.issued < min(len(self.reqs), i + 1 + self.la):
            self._issue(self.issued)
            self.issued += 1
        self.taken += 1
        return self.res[i % len(self.bufs)], self._view(i, self.reqs[i][1])


def layer_norm_rows(P, nc, x_ap, xres, gB, bB, stats, mv, rstd, sres, cres, eps=LN_EPS):
    P.op("dve", lambda e: e.bn_stats(out=stats[:, 0, :], in_=x_ap[:, 0:512]), reads=[xres], writes=[sres])
    P.op("dve", lambda e: e.bn_stats(out=stats[:, 1, :], in_=x_ap[:, 512:1024]), reads=[xres], writes=[sres])
    P.op("dve", lambda e: e.bn_aggr(out=mv[:, :], in_=stats[:, :, :].rearrange("p a b -> p (a b)")),
         reads=[sres], writes=[sres])
    P.op("act", lambda e: e.activation(out=rstd[:, :], in_=mv[:, 1:2], func=AF.Sqrt, bias=float(eps), scale=1.0),
         reads=[sres], writes=[sres])
    P.op("dve", lambda e: e.reciprocal(out=rstd[:, :], in_=rstd[:, :]), reads=[sres], writes=[sres])
    P.op("dve", lambda e: e.tensor_scalar(out=x_ap, in0=x_ap, scalar1=mv[:, 0:1], scalar2=rstd[:, 0:1],
                                          op0=ALU.subtract, op1=ALU.mult), reads=[sres, xres], writes=[xres])
    P.op("dve", lambda e: e.tensor_tensor(out=x_ap, in0=x_ap, in1=gB, op=ALU.mult),
         reads=[xres, cres], writes=[xres])
    P.op("dve", lambda e: e.tensor_tensor(out=x_ap, in0=x_ap, in1=bB, op=ALU.add),
         reads=[xres, cres], writes=[xres])


def decl_merge(nc, pre, moe, ne_override=None, ff_override=None):
    dram = lambda n, s_, dt=F32: nc.dram_tensor(pre + n, list(s_), dt, kind="ExternalInput").ap()
    A = dict(
        w_gate=dram("w_gate", [D, NBR * D]), b_gate=dram("b_gate", [128, NBR * 8]), w_up=dram("w_up", [NBR, BW, D]),
        w_out=dram("w_out", [D, D]), lnB=dram("lnB", [6, 128, D]), ident=dram("ident", [128, 128]))
    if moe:
        FF = ff_override or D_FF_E
        NE = ne_override or N_EXP
        A.update(w1=dram("w1", [NE, D, FF]), w3=dram("w3", [NE, D, FF]), w2=dram("w2", [NE, FF, D]),
                 router_w=dram("router_w", [D, N_EXP]), router_b=dram("router_b", [128, N_EXP]))
    else:
        FF, NE = D_FF, 1
        A.update(w1=dram("w1", [1, D, FF]), w3=dram("w3", [1, D, FF]), w2=dram("w2", [1, FF, D]))
    A["FF"], A["NE"] = FF, NE
    return A


def build_merge(NT, do_ln0, moe, ne_override=None, ff_override=None):
    nc = bass.Bass("TRN2", target_bir_lowering=False)
    A = decl_merge(nc, "", moe, ne_override, ff_override)
    x_d = nc.dram_tensor("x", [NT, D], F32, kind="ExternalInput").ap()
    oT_d = nc.dram_tensor("oT", [NBR * BW, NT], BF16, kind="ExternalInput").ap()
    y_d = nc.dram_tensor("y", [NT, D], F32, kind="ExternalOutput").ap()
    cx0 = Ctx(nc)
    x_rows = lambda t0, n: x_d[t0:t0 + n, :]
    o_rows = lambda br, half, t0, n: oT_d[br * BW + half * 256: br * BW + (half + 1) * 256, t0:t0 + n]
    tags, cx = emit_merge(nc, cx0.P, A, NT, do_ln0, moe, x_rows, o_rows, y_d)
    cx0.P.finish(tags)
    cx.close()
    cx0.close()
    return nc


def emit_merge(nc, P, A, NT, do_ln0, moe, x_rows, o_rows, y_d, after_store=None):
    cx = Ctx(nc, P)
    wg_d, bg_d, wup_d, wout_d, lnB_d, ident_d = (A[k] for k in ("w_gate", "b_gate", "w_up", "w_out", "lnB", "ident"))
    FF, NE = A["FF"], A["NE"]
    w1_d, w3_d, w2_d = A["w1"], A["w3"], A["w2"]
    if moe:
        rw_d, rb_d = A["router_w"], A["router_b"]
    NFS = FF // 128

    TT2 = min(NT, 1024)
    TT1 = min(NT, 512)
    NS2 = TT2 // 128
    NS1 = TT1 // 128
    NH2 = TT2 // TT1
    n_super = NT // TT2

    ident = cx.sb([128, 128], F32, "ident")
    lnB = cx.sb([128, 6, D], F32, "lnB")
    bg = cx.sb([128, NBR * 8], F32, "bg")
    xs = cx.sb([128, NS2, D], F32, "xs")
    xT = cx.sb([128, 8, TT2], BF16, "xT")
    xTf = cx.sb([128, 8, 128], F32, "xTf") if moe else None
    U = cx.sb([128, 28 * TT2], BF16, "U")
    gate_sb = [cx.sb([128, TT1], F32, "gate") for _ in range(3)]
    prod_sb = [cx.sb([128, TT1], F32, "prod") for _ in range(3)]
    silu_sb = [cx.sb([128, 512], BF16, "silu") for _ in range(2)]
    stats = cx.sb([128, 2, 6], F32, "stats")
    mv = cx.sb([128, 2], F32, "mv")
    rstd = cx.sb([128, 1], F32, "rstd")
    if moe:
        rw = cx.sb([128, 8, N_EXP], F32, "rw")
        rb = cx.sb([128, N_EXP], F32, "rb")
        comb = cx.sb([128, NS2, N_EXP], F32, "comb")
        rt = [cx.sb([128, N_EXP], F32, "rt") for _ in range(6)]
        rs = [cx.sb([128, 1], F32, "rs") for _ in range(4)]
    TR = cx.ps([128, 1024], F32, "TR")
    TM = cx.ps([128, 1024], F32, "TM")
    FM = [cx.ps([128, 512], F32, "FM") for _ in range(4)]
    rTR, rTM = Res("TR", True), Res("TM", True)
    rFM = [Res(f"FM{i}", True) for i in range(4)]

    r_ident, r_lnB, r_bg = Res("ident"), Res("lnB"), Res("bg")
    r_xs = [Res(f"xs{i}") for i in range(NS2)]
    r_xT = [Res(f"xT{i}") for i in range(NS2)]
    r_xTf = Res("xTf")
    r_stat = Res("stat")
    r_gate = [Res(f"gate{i}") for i in range(3)]
    r_prod = [Res(f"prod{i}") for i in range(3)]
    r_silu = [Res(f"silu{i}") for i in range(2)]
    r_rt = Res("rt")
    r_comb = [Res(f"comb{i}") for i in range(NS2)]
    r_const = Res("const")

    oT_v = U[:, 0:12 * TT1].rearrange("p (a t) -> p a t", a=12)
    mT_v = U[:, 12 * TT1:20 * TT1].rearrange("p (a t) -> p a t", a=8)
    hT_v = U[:, 0:NFS * TT2].rearrange("p (a t) -> p a t", a=NFS)
    r_oT = [Res(f"oT{i}") for i in range(3)]
    r_mT = [Res(f"mT{i}") for i in range(8)]
    r_hT = [Res(f"hT{i}") for i in range(NFS)]

    P.dma("sp", ident[:, :], ident_d[:, :], writes=[r_ident])
    P.dma("sp", lnB[:, :, :], lnB_d.rearrange("a p d -> p a d"), writes=[r_lnB])
    P.dma("sp", bg[:, :], bg_d[:, :], writes=[r_bg])
    if moe:
        P.dma("sp", rw[:, :, :], rw_d.rearrange("(kc p) e -> p kc e", p=128), writes=[r_const])
        r_rb = Res("rb")
        P.dma("sp", rb[:, :], rb_d[:, :], writes=[r_rb])

    wsA = WStream(cx, 10, 2048, "wA", lookahead=6)
    wsB = WStream(cx, 16 if moe else 12, 2048, "wB", lookahead=8 if moe else 4)
    wg_v = wg_d.rearrange("(kc p) c -> p kc c", p=128)
    wup_v = wup_d.rearrange("b (kc p) c -> b p kc c", p=128)
    w1_v = w1_d.rearrange("e (kc p) c -> e p kc c", p=128)
    w3_v = w3_d.rearrange("e (kc p) c -> e p kc c", p=128)
    for st in range(n_super):
        for h in range(NH2):
            for fs in range(8):
                for br in range(NBR):
                    wsA.plan(wg_v[:, :, br * D + fs * 128: br * D + (fs + 1) * 128], (8, 128))
                    wsA.plan(wup_v[br][:, :, fs * 128:(fs + 1) * 128], (4, 128))
            for kc in range(8):
                wsB.plan(wout_d[kc * 128:(kc + 1) * 128, :], (1024,))
        for e in range(NE):
            for fs in range(NFS):
                wsA.plan(w1_v[e][:, :, fs * 128:(fs + 1) * 128], (8, 128))
                wsA.plan(w3_v[e][:, :, fs * 128:(fs + 1) * 128], (8, 128))
            for kc in range(NFS):
                wsB.plan(w2_d[e, kc * 128:(kc + 1) * 128, :], (1024,))

    out_tags = []

    def transpose_sub(s, want_f32):
        for kc in range(8):
            P.op("pe", lambda e, kc=kc: e.transpose(out=TR[:, kc * 128:(kc + 1) * 128],
                                                    in_=xs[:, s, kc * 128:(kc + 1) * 128], identity=ident[:, :]),
                 reads=[r_xs[s], r_ident], writes=[rTR], inc=(kc == 7))
        if want_f32:
            P.op("dve", lambda e: e.tensor_copy(out=xTf[:, :, :], in_=TR[:, :].rearrange("p (a t) -> p a t", a=8)),
                 reads=[rTR], writes=[r_xTf])
            P.op("act", lambda e: e.copy(out=xT[:, :, s * 128:(s + 1) * 128], in_=xTf[:, :, :]),
                 reads=[r_xTf], writes=[r_xT[s]])
        else:
            P.op("act", lambda e: e.copy(out=xT[:, :, s * 128:(s + 1) * 128],
                                         in_=TR[:, :].rearrange("p (a t) -> p a t", a=8)),
                 reads=[rTR], writes=[r_xT[s]])

    fm_i = [0]

    def next_fm():
        i = fm_i[0] % 4
        fm_i[0] += 1
        return FM[i], rFM[i]

    def ln_sub(s, which):
        layer_norm_rows(P, nc, xs[:, s, :], r_xs[s], lnB[:, 2 * which, :], lnB[:, 2 * which + 1, :],
                        stats, mv, rstd, r_stat, r_lnB)

    for st in range(n_super):
        t0 = st * TT2
        for s in range(NS2):
            P.dma("sp", xs[:, s, :], x_rows(t0 + s * 128, 128), writes=[r_xs[s]])
        for s in range(NS2):
            if do_ln0:
                ln_sub(s, 0)
            transpose_sub(s, False)
        for r in r_oT + r_mT:
            r.absorb(r_hT)
        for h in range(NH2):
            c0 = h * TT1
            for br in range(NBR):
                for half in range(2):
                    P.dma("sp", oT_v[:, br * 4 + half * 2:br * 4 + half * 2 + 2, :],
                          o_rows(br, half, t0 + c0, TT1).rearrange("(kc p) t -> p kc t", p=128),
                          writes=[r_oT[br]])
            for fs in range(8):
                for br in range(NBR):
                    gw_r, gw = wsA.next()
                    uw_r, uw = wsA.next()
                    gp, gp_r = next_fm()
                    for kc in range(8):
                        P.op("pe", lambda e, kc=kc: e.matmul(gp[:, 0:TT1], lhsT=gw[:, kc, :],
                                                             rhs=xT[:, kc, c0:c0 + TT1],
                                                             start=(kc == 0), stop=(kc == 7)),
                             reads=[gw_r] + r_xT[h * NS1:(h + 1) * NS1], writes=[gp_r], inc=(kc == 7))
                    up, up_r = next_fm()
                    for kc in range(4):
                        P.op("pe", lambda e, kc=kc: e.matmul(up[:, 0:TT1], lhsT=uw[:, kc, :],
                                                             rhs=oT_v[:, br * 4 + kc, :],
                                                             start=(kc == 0), stop=(kc == 3)),
                             reads=[uw_r, r_oT[br]], writes=[up_r], inc=(kc == 3))
                    P.op("act", lambda e: e.activation(out=gate_sb[br][:, :], in_=gp[:, 0:TT1], func=AF.Sigmoid,
                                                       bias=bg[:, br * 8 + fs: br * 8 + fs + 1], scale=1.0),
                         reads=[gp_r, r_bg], writes=[r_gate[br]])
                    P.op("dve", lambda e: e.tensor_tensor(out=prod_sb[br][:, :], in0=up[:, 0:TT1],
                                                          in1=gate_sb[br][:, :], op=ALU.mult),
                         reads=[up_r, r_gate[br]], writes=[r_prod[br]])
                P.op("dve", lambda e: e.tensor_tensor(out=prod_sb[0][:, :], in0=prod_sb[0][:, :],
                                                       in1=prod_sb[1][:, :], op=ALU.add),
                     reads=[r_prod[0], r_prod[1]], writes=[r_prod[0]])
                P.op("dve", lambda e: e.tensor_tensor(out=mT_v[:, fs, :], in0=prod_sb[0][:, :],
                                                       in1=prod_sb[2][:, :], op=ALU.add),
                     reads=[r_prod[0], r_prod[2]], writes=[r_mT[fs]])
            wo = [wsB.next() for _ in range(8)]
            for sl in range(NS1):
                s = h * NS1 + sl
                TMa, rTMa = (TM, rTM) if s % 2 == 0 else (TR, rTR)
                for half in range(2):
                    for kc in range(8):
                        P.op("pe", lambda e, kc=kc, half=half, TMa=TMa: e.matmul(
                            TMa[:, half * 512:(half + 1) * 512], lhsT=mT_v[:, kc, sl * 128:(sl + 1) * 128],
                            rhs=wo[kc][1][:, half * 512:(half + 1) * 512], start=(kc == 0), stop=(kc == 7)),
                             reads=[r_mT[kc], wo[kc][0]], writes=[rTMa], inc=(kc == 7 and half == 1))
                P.op("dve", lambda e, TMa=TMa: e.scalar_tensor_tensor(out=xs[:, s, :], in0=xs[:, s, :], scalar=float(ALPHA),
                                                             in1=TMa[:, :], op0=ALU.mult, op1=ALU.add),
                     reads=[rTMa, r_xs[s]], writes=[r_xs[s]])
                ln_sub(s, 1)
        for s in range(NS2):
            transpose_sub(s, moe)
            if moe:
                lg, lg_r = next_fm()
                for kc in range(8):
                    P.op("pe", lambda e, kc=kc: e.matmul(lg[:, 0:N_EXP], lhsT=xTf[:, kc, :], rhs=rw[:, kc, :],
                                                         start=(kc == 0), stop=(kc == 7)),
                         reads=[r_xTf, r_const], writes=[lg_r], inc=(kc == 7))
                L, M1, K1, L2, K2, T6 = rt
                m1, m2, g1, g2 = rs
                ops = [
                    lambda e: e.tensor_tensor(out=L[:, :], in0=lg[:, 0:N_EXP], in1=rb[:, :], op=ALU.add),
                    lambda e: e.tensor_reduce(out=m1[:, :], in_=L[:, :], axis=AX.X, op=ALU.max),
                    lambda e: e.tensor_scalar(out=K1[:, :], in0=L[:, :], scalar1=m1[:, 0:1], scalar2=None,
                                              op0=ALU.is_ge),
                    lambda e: e.scalar_tensor_tensor(out=L2[:, :], in0=K1[:, :], scalar=-1e30, in1=L[:, :],
                                                     op0=ALU.mult, op1=ALU.add),
                    lambda e: e.tensor_reduce(out=m2[:, :], in_=L2[:, :], axis=AX.X, op=ALU.max),
                    lambda e: e.tensor_scalar(out=K2[:, :], in0=L2[:, :], scalar1=m2[:, 0:1], scalar2=None,
                                              op0=ALU.is_ge),
                    lambda e: e.tensor_tensor(out=g2[:, :], in0=m2[:, :], in1=m1[:, :], op=ALU.subtract),
                ]
                for i, f in enumerate(ops):
                    P.op("dve", f, reads=[lg_r, r_rt, r_rb] if i == 0 else [r_rt], writes=[r_rt])
                P.op("act", lambda e: e.activation(out=g2[:, :], in_=g2[:, :], func=AF.Sigmoid),
                     reads=[r_rt], writes=[r_rt])
                ops2 = [
                    lambda e: e.tensor_scalar(out=g1[:, :], in0=g2[:, :], scalar1=-1.0, scalar2=1.0,
                                              op0=ALU.mult, op1=ALU.add),
                    lambda e: e.tensor_scalar(out=K2[:, :], in0=K2[:, :], scalar1=g2[:, 0:1], scalar2=None,
                                              op0=ALU.mult),
                ]
                for f in ops2:
                    P.op("dve", f, reads=[r_rt], writes=[r_rt])
                P.op("dve", lambda e: e.scalar_tensor_tensor(out=comb[:, s, :], in0=K1[:, :], scalar=g1[:, 0:1],
                                                             in1=K2[:, :], op0=ALU.mult, op1=ALU.add),
                     reads=[r_rt], writes=[r_comb[s]])
        for s in range(NS2):
            P.op("act", lambda e: e.mul(out=xs[:, s, :], in_=xs[:, s, :], mul=float(ALPHA)),
                 reads=[r_xs[s]], writes=[r_xs[s]])
        for r in r_hT:
            r.absorb(r_oT + r_mT)
        for e_i in range(NE):
            for fs in range(NFS):
                w1r, w1t = wsA.next()
                w3r, w3t = wsA.next()
                for hh in range(TT2 // 512 if TT2 >= 512 else 1):
                    n = min(512, TT2)
                    c0 = hh * n
                    subs = r_xT[c0 // 128:(c0 + n) // 128]
                    a, a_r = next_fm()
                    for kc in range(8):
                        P.op("pe", lambda e, kc=kc: e.matmul(a[:, 0:n], lhsT=w1t[:, kc, :], rhs=xT[:, kc, c0:c0 + n],
                                                             start=(kc == 0), stop=(kc == 7)),
                             reads=[w1r] + subs, writes=[a_r], inc=(kc == 7))
                    b, b_r = next_fm()
                    for kc in range(8):
                        P.op("pe", lambda e, kc=kc: e.matmul(b[:, 0:n], lhsT=w3t[:, kc, :], rhs=xT[:, kc, c0:c0 + n],
                                                             start=(kc == 0), stop=(kc == 7)),
                             reads=[w3r] + subs, writes=[b_r], inc=(kc == 7))
                    si = (fs * 2 + hh) % 2
                    P.op("act", lambda e: e.activation(out=silu_sb[si][:, 0:n], in_=a[:, 0:n], func=AF.Silu),
                         reads=[a_r], writes=[r_silu[si]])
                    P.op("dve", lambda e: e.tensor_tensor(out=hT_v[:, fs, c0:c0 + n], in0=b[:, 0:n],
                                                          in1=silu_sb[si][:, 0:n], op=ALU.mult),
                         reads=[b_r, r_silu[si]], writes=[r_hT[fs]])
            GK = 8
            for g0 in range(0, NFS, GK):
                gks = list(range(g0, min(NFS, g0 + GK)))
                wts = [wsB.next() for _ in gks]
                for s in range(NS2):
                    TMa, rTMa = (TM, rTM) if s % 2 == 0 else (TR, rTR)
                    for half in range(2):
                        for j, kc in enumerate(gks):
                            P.op("pe", lambda e, j=j, kc=kc, half=half, TMa=TMa: e.matmul(
                                TMa[:, half * 512:(half + 1) * 512], lhsT=hT_v[:, kc, s * 128:(s + 1) * 128],
                                rhs=wts[j][1][:, half * 512:(half + 1) * 512],
                                start=(j == 0), stop=(j == len(gks) - 1)),
                                 reads=[r_hT[kc], wts[j][0]], writes=[rTMa],
                                 inc=(j == len(gks) - 1 and half == 1))
                    if moe:
                        P.op("dve", lambda e, TMa=TMa: e.scalar_tensor_tensor(
                            out=xs[:, s, :], in0=TMa[:, :], scalar=comb[:, s, e_i:e_i + 1], in1=xs[:, s, :],
                            op0=ALU.mult, op1=ALU.add), reads=[rTMa, r_xs[s], r_comb[s]], writes=[r_xs[s]])
                    else:
                        P.op("dve", lambda e, TMa=TMa: e.tensor_tensor(out=xs[:, s, :], in0=TMa[:, :], in1=xs[:, s, :],
                                                              op=ALU.add),
                             reads=[rTMa, r_xs[s]], writes=[r_xs[s]])
        for s in range(NS2):
            ln_sub(s, 2)
            out_tags.append(P.dma("sp", y_d[t0 + s * 128: t0 + (s + 1) * 128, :], xs[:, s, :], reads=[r_xs[s]]))
            if after_store is not None:
                after_store(t0 + s * 128, out_tags[-1])
    return out_tags, cx


RWKV_COLS = 3 * 512 + 64 + 64 + 128
CONV_COLS = 3 * 512
FOX_COLS = 3 * 512 + 8
GATE_OFF = RWKV_COLS + CONV_COLS + FOX_COLS
IDENT = np.eye(128, dtype=np.float32)


def _bc(v):
    return np.ascontiguousarray(np.broadcast_to(np.asarray(v, np.float32)[None, :], (128, v.shape[0])))


def merge_inputs(p, l, moe):
    w_in = p["w_in"][l]
    common = {
        "ident": IDENT,
        "w_gate": np.ascontiguousarray(w_in[:, GATE_OFF:GATE_OFF + NBR * D]),
        "b_gate": np.ascontiguousarray(p["b_gate"][l].reshape(NBR, 8, 128).transpose(2, 0, 1).reshape(128, NBR * 8)),
        "w_up": np.ascontiguousarray(np.stack([p["w_up_rwkv"][l], p["w_up_conv"][l], p["w_up_attn"][l]], 0)),
        "w_out": np.ascontiguousarray(p["w_out"][l]),
        "lnB": np.ascontiguousarray(np.stack([_bc(p["ln0_g"]), _bc(p["ln0_b"]), _bc(p["ln1_g"][l]), _bc(p["ln1_b"][l]),
                                              _bc(p["ln2_g"][l]), _bc(p["ln2_b"][l])], 0)),
    }
    i = l // 2
    if moe:
        common.update({
            "w1": p["moe_w1"][i], "w3": p["moe_w3"][i], "w2": p["moe_w2"][i],
            "router_w": np.ascontiguousarray(p["router_w"][i]), "router_b": _bc(p["router_b"][i]),
        })
    else:
        common.update({"w1": p["ffn_w1"][i][None], "w3": p["ffn_w3"][i][None], "w2": p["ffn_w2"][i][None]})
    return common


def run_merge(x_flat, oT, p, l, do_ln0, moe, n_cores=8):
    ntok = x_flat.shape[0]
    NT = ntok // n_cores
    nc = build_merge(NT, do_ln0, moe)
    common = merge_inputs(p, l, moe)
    in_maps = []
    for c in range(n_cores):
        m = dict(common)
        m["x"] = np.ascontiguousarray(x_flat[c * NT:(c + 1) * NT])
        m["oT"] = np.ascontiguousarray(oT[:, c * NT:(c + 1) * NT])
        in_maps.append(m)
    res = run_bass_kernel_spmd(nc, in_maps, core_ids=list(range(n_cores)))
    return np.concatenate([r["y"] for r in res.results], 0)


NEG = -30000.0


def decl_fox(nc, pre):
    dram = lambda n, s_, dt=F32: nc.dram_tensor(pre + n, list(s_), dt, kind="ExternalInput").ap()
    return dict(wc=dram("wc", [D, 768]), wq=dram("wq", [D, 256]), wk=dram("wk", [D, 256]), wv=dram("wv", [D, 256]),
                wf=dram("wf", [D, 4]), bf=dram("bf", [4, 1]), cw=dram("cw", [128, 6]), lnB=dram("lnB", [2, 128, D]),
                ident=dram("ident", [128, 128]), mask=dram("mask", [128, 128]))


def build_fox(T, do_ln0):
    nc = bass.Bass("TRN2", target_bir_lowering=False)
    A = decl_fox(nc, "")
    x_d = nc.dram_tensor("x", [T, D], F32, kind="ExternalInput").ap()
    obT_d = nc.dram_tensor("obT", [256, T], BF16, kind="ExternalOutput").ap()
    ocT_d = nc.dram_tensor("ocT", [256, T], BF16, kind="ExternalOutput").ap()
    cx0 = Ctx(nc)
    TTf = min(T, 512)
    tags, cx = emit_fox(nc, cx0.P, A, T, do_ln0, lambda t0, n: x_d[t0:t0 + n, :],
                        lambda ti: obT_d[:, ti * TTf:(ti + 1) * TTf], lambda ti: ocT_d[:, ti * TTf:(ti + 1) * TTf])
    cx0.P.finish(tags)
    cx.close()
    cx0.close()
    return nc


def emit_fox(nc, P, A, T, do_ln0, xsrc, ob_dst, oc_dst, after_store=None):
    cx = Ctx(nc, P)
    wc_d, wq_d, wk_d, wv_d, wf_d, bf_d, cw_d, lnB_d, ident_d, mask_d = (
        A[k] for k in ("wc", "wq", "wk", "wv", "wf", "bf", "cw", "lnB", "ident", "mask"))

    TT = min(T, 512)
    NS = TT // 128
    NSB = T // TT
    NB = T // 128
    H = 4

    ident = cx.sb([128, 128], F32, "ident")
    identb = cx.sb([128, 128], BF16, "identb")
    maskf = cx.sb([128, 128], F32, "maskf")
    maskb = cx.sb([128, 128], BF16, "maskb")
    lnB = cx.sb([128, 2, D], F32, "lnB")
    cw = cx.sb([128, 6], F32, "cw")
    bf = cx.sb([4, 1], F32, "bf")
    nbf = cx.sb([4, 1], F32, "nbf")
    wc = cx.sb([128, 8, 768], BF16, "wc")
    wq = cx.sb([128, 8, 256], BF16, "wq")
    wk = cx.sb([128, 8, 256], BF16, "wk")
    wv = cx.sb([128, 8, 256], BF16, "wv")
    wf = cx.sb([128, 8, 4], BF16, "wf")
    xs = cx.sb([128, 2, D], F32, "xs")
    xT = cx.sb([128, 8, TT], BF16, "xT")
    Kaug = [cx.sb([70, T], BF16, "Kaug") for _ in range(H)]
    Vaug = cx.sb([128, NB, H, 65], BF16, "Vaug")
    Qaug = [[cx.sb([70, TT], BF16, "Qaug") for _ in range(H)] for _ in range(2)]
    PT = [cx.sb([128, TT], BF16, "PT") for _ in range(3)]
    cvT = [cx.sb([128, 2, TT], F32, "cvT") for _ in range(2)]
    uT = cx.sb([128, 2, 2 + TT], F32, "uT")
    yT = cx.sb([128, 2, TT], F32, "yT")
    y2T = cx.sb([128, 2, TT], F32, "y2T")
    obT = cx.sb([128, 2, TT], BF16, "obT")
    ones4 = cx.sb([4, TT], F32, "ones4")
    lf = cx.sb([4, TT], F32, "lf")
    cc = cx.sb([4, TT], F32, "cc")
    cprev = cx.sb([4, 1], F32, "cprev")
    csp = cx.sb([4, 3, TT], BF16, "csp")
    csn = cx.sb([4, 3, TT], BF16, "csn")
    ctmp = cx.sb([4, TT], F32, "ctmp")
    ctmp2 = lf
    oc = [cx.sb([128, NS, 256], F32, "oc") for _ in range(2)]
    ocT = [cx.sb([128, 2, TT], BF16, "ocT") for _ in range(2)]
    r_ocT = [Res("ocT0"), Res("ocT1")]
    rinv = cx.sb([128, 4], F32, "rinv")
    stats = cx.sb([128, 2, 6], F32, "stats")
    mv = cx.sb([128, 2], F32, "mv")
    rstd = cx.sb([128, 1], F32, "rstd")

    TR = cx.ps([128, 1024], F32, "TR")
    FM = [cx.ps([128, 512], F32, "FM") for _ in range(2)]
    ST = [cx.ps([128, 512], F32, "ST") for _ in range(2)]
    OA = [cx.ps([128, 512], F32, "OA") for _ in range(2)]
    rTR = Res("TR", True)
    rFM = [Res("FM0", True), Res("FM1", True)]
    rST = [Res("ST0", True), Res("ST1", True)]
    rOA = [Res("OA0", True), Res("OA1", True)]

    r_c = Res("consts")
    r_w = Res("weights")
    r_xs = [Res(f"xs{i}") for i in range(2)]
    r_xT = Res("xT")
    r_K = [[Res(f"K{h}_{i}") for i in range(NSB)] for h in range(H)]
    r_V = [Res(f"V{i}") for i in range(NSB)]
    r_Q = [[Res(f"Q{b}{h}") for h in range(H)] for b in range(2)]
    r_PT = [Res(f"PT{i}") for i in range(3)]
    r_cv = [Res("cvb"), Res("cvc")]
    r_u, r_y, r_ob, r_y2 = Res("u"), Res("y"), Res("ob"), Res("y2")
    r_f = Res("f")
    r_cs = Res("cs")
    r_oc = [Res("oc0"), Res("oc1")]
    r_rinv = Res("rinv")
    r_stat = Res("stat")

    P.dma("sp", ident[:, :], ident_d[:, :], writes=[r_c])
    P.dma("sp", maskf[:, :], mask_d[:, :], writes=[r_c])
    P.dma("sp", lnB[:, :, :], lnB_d.rearrange("a p d -> p a d"), writes=[r_c])
    P.dma("sp", cw[:, :], cw_d[:, :], writes=[r_c])
    P.dma("sp", bf[:, :], bf_d[:, :], writes=[r_c])
    for wt, wd_, n in ((wc, wc_d, 768), (wq, wq_d, 256), (wk, wk_d, 256), (wv, wv_d, 256), (wf, wf_d, 4)):
        P.dma("pool", wt[:, :, :], wd_.rearrange("(kc p) c -> p kc c", p=128), writes=[r_w])
    r_c2 = Res("consts2")
    P.op("dve", lambda e: e.tensor_copy(out=identb[:, :], in_=ident[:, :]), reads=[r_c], writes=[r_c2])
    P.op("dve", lambda e: e.tensor_copy(out=maskb[:, :], in_=maskf[:, :]), reads=[r_c], writes=[r_c2])
    P.op("dve", lambda e: e.tensor_scalar(out=nbf[:, :], in0=bf[:, :], scalar1=-1.0, scalar2=None, op0=ALU.mult),
         reads=[r_c], writes=[r_c2])
    P.op("dve", lambda e: e.memset(ones4[:, :], 1.0), writes=[r_c2])
    P.op("dve", lambda e: e.memset(cprev[:, :], 0.0), writes=[r_cs])
    P.op("dve", lambda e: e.memset(uT[:, :, :], 0.0), writes=[r_u])
    P.op("pool", lambda e: e.memset(Vaug[:, :, :, :], 1.0), writes=r_V)
    for h in range(H):
        P.op("pool", lambda e, h=h: e.memset(Kaug[h][64:70, :], 1.0), writes=r_K[h])
        for b in range(2):
            P.op("pool", lambda e, h=h, b=b: e.memset(Qaug[b][h][64:70, :], 1.0), writes=[r_Q[b][h]])

    fm_i = [0]

    def next_fm():
        i = fm_i[0] % 2
        fm_i[0] += 1
        return FM[i], rFM[i]

    out_tags = []

    def proj(sb_i):
        t0 = sb_i * TT
        qb = sb_i % 2
        for s in range(NS):
            yield
            xb_ = s % 2
            P.dma("sp", xs[:, xb_, :], xsrc(t0 + s * 128, 128), writes=[r_xs[xb_]])
            if do_ln0:
                layer_norm_rows(P, nc, xs[:, xb_, :], r_xs[xb_], lnB[:, 0, :], lnB[:, 1, :], stats, mv, rstd, r_stat, r_c)
            for kc in range(8):
                P.op("pe", lambda e, kc=kc: e.transpose(out=TR[:, kc * 128:(kc + 1) * 128],
                                                        in_=xs[:, xb_, kc * 128:(kc + 1) * 128], identity=ident[:, :]),
                     reads=[r_xs[xb_], r_c], writes=[rTR], inc=(kc == 7))
            P.op("dve", lambda e: e.tensor_copy(out=xT[:, :, s * 128:(s + 1) * 128],
                                                in_=TR[:, :].rearrange("p (a t) -> p a t", a=8)),
                 reads=[rTR], writes=[r_xT])
        yield
        for grp in range(3):
            for hf in range(2):
                yield
                ps, ps_r = next_fm()
                c0 = grp * 256 + hf * 128
                for kc in range(8):
                    P.op("pe", lambda e, kc=kc: e.matmul(ps[:, 0:TT], lhsT=wc[:, kc, c0:c0 + 128], rhs=xT[:, kc, :],
                                                         start=(kc == 0), stop=(kc == 7)),
                         reads=[r_w, r_xT], writes=[ps_r], inc=(kc == 7))
                if grp < 2:
                    P.op("dve", lambda e: e.tensor_copy(out=cvT[grp][:, hf, :], in_=ps[:, 0:TT]),
                         reads=[ps_r], writes=[r_cv[grp]])
                else:
                    P.op("dve", lambda e: e.tensor_tensor(out=uT[:, hf, 2:2 + TT], in0=ps[:, 0:TT],
                                                          in1=cvT[1][:, hf, :], op=ALU.mult),
                         reads=[ps_r, r_cv[1]], writes=[r_u])
        yield
        for hf in range(2):
            P.op("dve", lambda e: e.tensor_scalar(out=yT[:, hf, :], in0=uT[:, hf, 0:TT],
                                                  scalar1=cw[:, hf * 3:hf * 3 + 1], scalar2=None, op0=ALU.mult),
                 reads=[r_u, r_c], writes=[r_y])
            for tap in (1, 2):
                P.op("dve", lambda e, tap=tap: e.scalar_tensor_tensor(out=yT[:, hf, :], in0=uT[:, hf, tap:tap + TT],
                                                                      scalar=cw[:, hf * 3 + tap:hf * 3 + tap + 1],
                                                                      in1=yT[:, hf, :], op0=ALU.mult, op1=ALU.add),
                     reads=[r_u, r_y, r_c], writes=[r_y])
            P.op("pool", lambda e: e.tensor_tensor(out=obT[:, hf, :], in0=yT[:, hf, :], in1=cvT[0][:, hf, :],
                                                   op=ALU.mult),
                 reads=[r_y, r_cv[0]], writes=[r_ob])
        P.op("pool", lambda e: e.tensor_copy(out=uT[:, :, 0:2], in_=uT[:, :, TT:TT + 2]), reads=[r_u], writes=[r_u])
        out_tags.append(P.dma("sp", ob_dst(sb_i).rearrange("(hf p) t -> p hf t", p=128), obT[:, :, :],
                              reads=[r_ob]))
        if after_store is not None:
            after_store(sb_i, out_tags[-1])
        yield
        ps, ps_r = next_fm()
        for kc in range(8):
            P.op("pe", lambda e, kc=kc: e.matmul(ps[0:4, 0:TT], lhsT=wf[:, kc, :], rhs=xT[:, kc, :],
                                                 start=(kc == 0), stop=(kc == 7)),
                 reads=[r_w, r_xT], writes=[ps_r], inc=(kc == 7))
        P.op("act", lambda e: e.activation(out=lf[:, :], in_=ps[0:4, 0:TT], func=AF.Exp, bias=nbf[:, 0:1], scale=-1.0),
             reads=[ps_r, r_c2], writes=[r_f])
        P.op("act", lambda e: e.activation(out=lf[:, :], in_=lf[:, :], func=AF.Ln, bias=1.0, scale=1.0),
             reads=[r_f], writes=[r_f])
        P.op("dve", lambda e: e.tensor_scalar(out=lf[:, :], in0=lf[:, :], scalar1=-1.0, scalar2=None, op0=ALU.mult),
             reads=[r_f], writes=[r_f])
        P.op("dve", lambda e: e.tensor_tensor_scan(out=cc[:, :], data0=ones4[:, :], data1=lf[:, :],
                                                   initial=cprev[:, 0:1], op0=ALU.mult, op1=ALU.add),
             reads=[r_f, r_cs, r_c2], writes=[r_cs])
        P.op("dve", lambda e: e.tensor_copy(out=cprev[:, :], in_=cc[:, TT - 1:TT]), reads=[r_cs], writes=[r_cs])
        P.op("dve", lambda e: e.tensor_copy(out=csp[:, 0, :], in_=cc[:, :]), reads=[r_cs], writes=[r_cs])
        P.op("dve", lambda e: e.tensor_tensor(out=ctmp[:, :], in0=cc[:, :], in1=csp[:, 0, :], op=ALU.subtract),
             reads=[r_cs], writes=[r_cs])
        P.op("dve", lambda e: e.tensor_copy(out=csp[:, 1, :], in_=ctmp[:, :]), reads=[r_cs], writes=[r_cs])
        P.op("dve", lambda e: e.tensor_tensor(out=ctmp2[:, :], in0=ctmp[:, :], in1=csp[:, 1, :], op=ALU.subtract),
             reads=[r_cs], writes=[r_cs])
        P.op("dve", lambda e: e.tensor_copy(out=csp[:, 2, :], in_=ctmp2[:, :]), reads=[r_cs], writes=[r_cs])
        P.op("dve", lambda e: e.tensor_scalar(out=csn[:, :, :], in0=csp[:, :, :], scalar1=-1.0, scalar2=None,
                                              op0=ALU.mult), reads=[r_cs], writes=[r_cs])
        yield
        for h in range(H):
            yield
            ps, ps_r = next_fm()
            for kc in range(8):
                P.op("pe", lambda e, kc=kc: e.matmul(ps[0:64, 0:TT], lhsT=wq[:, kc, h * 64:(h + 1) * 64], rhs=xT[:, kc, :],
                                                     start=(kc == 0), stop=(kc == 7)),
                     reads=[r_w, r_xT], writes=[ps_r], inc=(kc == 7))
            P.op("act", lambda e: e.mul(out=Qaug[qb][h][0:64, :], in_=ps[0:64, 0:TT], mul=0.125),
                 reads=[ps_r], writes=[r_Q[qb][h]])
            for jj in range(3):
                P.dma("sp", Qaug[qb][h][64 + jj:65 + jj, :], csp[h:h + 1, jj, :], reads=[r_cs], writes=[r_Q[qb][h]],
                      sem_res=r_Q[qb][h])
            ps, ps_r = next_fm()
            for kc in range(8):
                P.op("pe", lambda e, kc=kc: e.matmul(ps[0:64, 0:TT], lhsT=wk[:, kc, h * 64:(h + 1) * 64], rhs=xT[:, kc, :],
                                                     start=(kc == 0), stop=(kc == 7)),
                     reads=[r_w, r_xT], writes=[ps_r], inc=(kc == 7))
            P.op("dve", lambda e: e.tensor_copy(out=Kaug[h][0:64, t0:t0 + TT], in_=ps[0:64, 0:TT]),
                 reads=[ps_r], writes=[r_K[h][sb_i]])
            for jj in range(3):
                P.dma("sp", Kaug[h][67 + jj:68 + jj, t0:t0 + TT], csn[h:h + 1, jj, :], reads=[r_cs],
                      writes=[r_K[h][sb_i]], sem_res=r_K[h][sb_i])
        for s in range(NS):
            yield
            ps, ps_r = next_fm()
            for kc in range(8):
                P.op("pe", lambda e, kc=kc: e.matmul(ps[:, 0:256], lhsT=xT[:, kc, s * 128:(s + 1) * 128], rhs=wv[:, kc, :],
                                                     start=(kc == 0), stop=(kc == 7)),
                     reads=[r_w, r_xT], writes=[ps_r], inc=(kc == 7))
            P.op("dve", lambda e: e.tensor_copy(out=Vaug[:, sb_i * NS + s, :, 0:64],
                                                in_=ps[:, 0:256].rearrange("p (h d) -> p h d", h=H)),
                 reads=[ps_r], writes=[r_V[sb_i]])
        yield
    gen = proj(0)
    for _ in gen:
        pass
    for sb_i in range(NSB):
        t0 = sb_i * TT
        qb = sb_i % 2
        gen = proj(sb_i + 1) if sb_i + 1 < NSB else iter(())
        items = []
        for h in range(H):
            nkb = sb_i * NS + NS
            for j in range(nkb):
                items.append((h, j))
        ob_i = sb_i % 2

        def emit_S(idx):
            h, j = items[idx]
            st, st_r = ST[idx % 2], rST[idx % 2]
            dj = j - sb_i * NS
            qlo = max(0, dj) * 128
            N = TT - qlo
            if dj >= 0:
                P.op("pe", lambda e: e.matmul(st[:, 0:128], lhsT=Kaug[h][0:70, j * 128:(j + 1) * 128],
                                              rhs=Qaug[qb][h][0:70, qlo:qlo + 128], start=True, stop=False),
                     reads=[r_K[h][j // NS], r_Q[qb][h]], writes=[st_r], inc=False)
                P.op("pe", lambda e: e.matmul(st[:, 0:128], lhsT=identb[:, :], rhs=maskb[:, :], start=False, stop=True),
                     reads=[r_c2], writes=[st_r], inc=(N == 128))
                if N > 128:
                    P.op("pe", lambda e: e.matmul(st[:, 128:N], lhsT=Kaug[h][0:70, j * 128:(j + 1) * 128],
                                                  rhs=Qaug[qb][h][0:70, qlo + 128:TT], start=True, stop=True),
                         reads=[r_K[h][j // NS], r_Q[qb][h]], writes=[st_r])
            else:
                P.op("pe", lambda e: e.matmul(st[:, 0:N], lhsT=Kaug[h][0:70, j * 128:(j + 1) * 128],
                                              rhs=Qaug[qb][h][0:70, qlo:TT], start=True, stop=True),
                     reads=[r_K[h][j // NS], r_Q[qb][h]], writes=[st_r])

        def emit_rest(idx):
            h, j = items[idx]
            st, st_r = ST[idx % 2], rST[idx % 2]
            pt, pt_r = PT[idx % 3], r_PT[idx % 3]
            dj = j - sb_i * NS
            qlo = max(0, dj) * 128
            N = TT - qlo
            nkb = sb_i * NS + NS
            oa, oa_r = OA[h % 2], rOA[h % 2]
            P.op("act", lambda e: e.activation(out=pt[:, 0:N], in_=st[:, 0:N], func=AF.Exp), reads=[st_r], writes=[pt_r])
            for qq in range(qlo // 128, NS):
                last_j = sb_i * NS + qq
                P.op("pe", lambda e, qq=qq: e.matmul(oa[:, qq * 128:qq * 128 + 65],
                                                     lhsT=pt[:, qq * 128 - qlo:qq * 128 - qlo + 128],
                                                     rhs=Vaug[:, j, h, :], start=(j == 0 and qq == 0),
                                                     stop=(j == last_j), skip_group_check=True),
                     reads=[pt_r, r_V[j // NS]], writes=[oa_r], inc=(qq == NS - 1))
            if j == nkb - 1:
                oav = oa[:, :].rearrange("p (q c) -> p q c", q=4)
                P.op("dve", lambda e: e.reciprocal(out=rinv[:, 0:NS], in_=oav[:, 0:NS, 64]), reads=[oa_r], writes=[r_rinv])
                for qq in range(NS):
                    P.op("dve", lambda e, qq=qq: e.tensor_scalar(out=oc[ob_i][:, qq, h * 64:(h + 1) * 64],
                                                                 in0=oa[:, qq * 128:qq * 128 + 64],
                                                                 scalar1=rinv[:, qq:qq + 1], scalar2=None, op0=ALU.mult),
                         reads=[oa_r, r_rinv], writes=[r_oc[ob_i]])

        emit_S(0)
        nsteps = max(1, -(-48 // len(items)))
        for idx in range(len(items)):
            if idx + 1 < len(items):
                emit_S(idx + 1)
            emit_rest(idx)
            for _ in range(nsteps):
                next(gen, None)
        for _ in gen:
            pass
        for hf in range(2):
            for qq in range(NS):
                P.op("pe", lambda e, hf=hf, qq=qq: e.transpose(out=TR[:, hf * 512 + qq * 128: hf * 512 + (qq + 1) * 128],
                                                               in_=oc[ob_i][:, qq, hf * 128:(hf + 1) * 128],
                                                               identity=ident[:, :]),
                     reads=[r_oc[ob_i], r_c], writes=[rTR], inc=(hf == 1 and qq == NS - 1))
        P.op("act", lambda e: e.copy(out=ocT[ob_i][:, :, :],
                                     in_=TR[:, :].rearrange("p (a t) -> p a t", a=2)[:, :, 0:TT]),
             reads=[rTR], writes=[r_ocT[ob_i]])
        out_tags.append(P.dma("sp", oc_dst(sb_i).rearrange("(hf p) t -> p hf t", p=128), ocT[ob_i][:, :, :],
                              reads=[r_ocT[ob_i]]))
        if after_store is not None:
            after_store(sb_i, out_tags[-1])
    return out_tags, cx


MASK = np.where(np.arange(128)[None, :] >= np.arange(128)[:, None], 0.0, NEG).astype(np.float32)


def fox_inputs(p, l, g):
    w_in = p["w_in"][l]
    c_off = RWKV_COLS
    f_off = RWKV_COLS + CONV_COLS
    if True:
        cs = slice(g * 256, (g + 1) * 256)
        wcv = np.concatenate([w_in[:, c_off + k * 512 + g * 256: c_off + k * 512 + (g + 1) * 256] for k in range(3)], 1)
        m = {
            "wc": np.ascontiguousarray(wcv),
            "wq": np.ascontiguousarray(w_in[:, f_off + g * 256: f_off + (g + 1) * 256]),
            "wk": np.ascontiguousarray(w_in[:, f_off + 512 + g * 256: f_off + 512 + (g + 1) * 256]),
            "wv": np.ascontiguousarray(w_in[:, f_off + 1024 + g * 256: f_off + 1024 + (g + 1) * 256]),
            "wf": np.ascontiguousarray(w_in[:, f_off + 1536 + g * 4: f_off + 1536 + (g + 1) * 4]),
            "bf": np.ascontiguousarray(p["b_forget"][l][g * 4:(g + 1) * 4].reshape(4, 1)),
            "cw": np.ascontiguousarray(p["conv_w"][l][:, cs].reshape(3, 2, 128).transpose(2, 1, 0).reshape(128, 6)),
            "lnB": np.ascontiguousarray(np.stack([_bc(p["ln0_g"]), _bc(p["ln0_b"])], 0)),
            "ident": IDENT, "mask": MASK,
        }
    return m


def run_fox(xb, p, l, do_ln0, n_cores=8):
    B, T, _ = xb.shape
    nc = build_fox(T, do_ln0)
    in_maps = []
    for c in range(n_cores):
        b, g = c // 2, c % 2
        m = fox_inputs(p, l, g)
        m["x"] = np.ascontiguousarray(xb[b])
        in_maps.append(m)
    res = run_bass_kernel_spmd(nc, in_maps, core_ids=list(range(n_cores)))
    obT = np.stack([np.concatenate([res.results[2 * b + g]["obT"] for g in range(2)], 0) for b in range(B)], 0)
    ocT = np.stack([np.concatenate([res.results[2 * b + g]["ocT"] for g in range(2)], 0) for b in range(B)], 0)
    return obT, ocT


RWKV_LN_EPS = 64e-5
RWKV_WINDOW = 16
NPJ = 2
DECAY_SCALE = -0.6065306597126334


class Sched:
    def __init__(self):
        self.tasks = []
        self.done = set()

    def add(self, name, gen, deps=()):
        self.tasks.append([name, gen, set(deps)])

    def run(self, window=10):
        active = []
        pending = list(self.tasks)
        while pending or active:
            i = 0
            while i < len(pending) and len(active) < window:
                t = pending[i]
                if t[2] <= self.done:
                    active.append(t)
                    pending.pop(i)
                else:
                    i += 1
            assert active, ("deadlock", [t[0] for t in pending[:5]])
            for t in list(active):
                try:
                    next(t[1])
                except StopIteration:
                    self.done.add(t[0])
                    active.remove(t)


def decl_rwkv(nc, pre):
    dram = lambda n, s_, dt=F32: nc.dram_tensor(pre + n, list(s_), dt, kind="ExternalInput").ap()
    return dict(wall=dram("wall", [D, 1024]), muB=dram("muB", [128, 1024]), chv=dram("chv", [128, 2, 8]),
                w2ia=dram("w2ia", [128, 256]), w2g=dram("w2g", [128, 256]), lnxB=dram("lnxB", [2, 128, 256]),
                lnB=dram("lnB", [2, 128, D]), ident=dram("ident", [128, 128]), mask4=dram("mask4", [128, 512]),
                maskL=dram("maskL", [128, 128]), bones=dram("bones", [128, 128]), sel=dram("sel", [128, 2]),
                smask=dram("smask", [128, 512]))


def build_rwkv(T, do_ln0):
    nc = bass.Bass("TRN2", target_bir_lowering=False)
    A = decl_rwkv(nc, "")
    x_d = nc.dram_tensor("x", [T, D], F32, kind="ExternalInput").ap()
    oaT_d = nc.dram_tensor("oaT", [256, T], BF16, kind="ExternalOutput").ap()
    cx0 = Ctx(nc)
    TTr = min(T, 512)
    tags, cx = emit_rwkv(nc, cx0.P, A, T, do_ln0, lambda t0, n: x_d[t0:t0 + n, :],
                         lambda ti: oaT_d[:, ti * TTr:(ti + 1) * TTr])
    cx0.P.finish(tags)
    cx.close()
    cx0.close()
    return nc


def emit_rwkv(nc, P, A, T, do_ln0, xsrc, oa_dst, after_store=None):
    cx = Ctx(nc, P)
    (wall_d, muB_d, chv_d, w2ia_d, w2g_d, lnxB_d, lnB_d, ident_d, mask4_d, maskL_d, bones_d, sel_d, smask_d) = (
        A[k] for k in ("wall", "muB", "chv", "w2ia", "w2g", "lnxB", "lnB", "ident", "mask4", "maskL", "bones", "sel",
                       "smask"))

    TT = min(T, 512)
    NCH = TT // 128
    NTI = T // TT

    ident = cx.sb([128, 128], F32, "ident")
    identb = cx.sb([128, 128], BF16, "identb")
    mask4 = cx.sb([128, 512], F32, "mask4")
    maskL = cx.sb([128, 128], F32, "maskL")
    bones = cx.sb([128, 128], F32, "bones")
    self_ = cx.sb([128, 2], F32, "self")
    selb = cx.sb([128, 2], BF16, "selb")
    smask = cx.sb([128, 512], F32, "smask")
    lnB = cx.sb([128, 2, D], F32, "lnB") if do_ln0 else None
    lnxB = cx.sb([128, 2, 256], F32, "lnxB")
    chv = cx.sb([128, 2, 8], F32, "chv")
    omka = cx.sb([128, 2], F32, "omka")
    muB = cx.sb([128, 1024], F32, "muB")
    wtmp = cx.sb([128, 512], F32, "wtmp")
    wtmp2 = cx.sb([128, 512], F32, "wtmp2")
    W0 = cx.sb([128, 8, 1024], BF16, "W0")
    W1 = cx.sb([128, 8, 1024], BF16, "W1")
    w2iaf = cx.sb([128, 256], F32, "w2iaf")
    w2ia = cx.sb([128, 256], BF16, "w2ia")
    w2gf = cx.sb([128, 256], F32, "w2gf")
    w2g = cx.sb([128, 256], BF16, "w2g")
    r_c = Res("consts")
    r_c2 = Res("consts2")
    r_wt = Res("wtmp")
    r_W = Res("W")

    for t_, d_ in ((ident, ident_d), (mask4, mask4_d), (maskL, maskL_d), (bones, bones_d), (self_, sel_d),
                   (smask, smask_d), (muB, muB_d), (w2iaf, w2ia_d), (w2gf, w2g_d)):
        P.dma("sp", t_[:, :], d_[:, :], writes=[r_c])
    if do_ln0:
        P.dma("sp", lnB[:, :, :], lnB_d.rearrange("a p d -> p a d"), writes=[r_c])
    P.dma("sp", lnxB[:, :, :], lnxB_d.rearrange("a p d -> p a d"), writes=[r_c])
    P.dma("sp", chv[:, :, :], chv_d[:, :, :], writes=[r_c])
    P.op("dve", lambda e: e.tensor_copy(out=identb[:, :], in_=ident[:, :]), reads=[r_c], writes=[r_c2])
    P.op("dve", lambda e: e.tensor_copy(out=selb[:, :], in_=self_[:, :]), reads=[r_c], writes=[r_c2])
    P.op("dve", lambda e: e.tensor_copy(out=w2ia[:, :], in_=w2iaf[:, :]), reads=[r_c], writes=[r_c2])
    P.op("dve", lambda e: e.tensor_copy(out=w2g[:, :], in_=w2gf[:, :]), reads=[r_c], writes=[r_c2])
    P.op("dve", lambda e: e.tensor_scalar(out=omka[:, :], in0=chv[:, :, 3], scalar1=-1.0, scalar2=1.0,
                                          op0=ALU.mult, op1=ALU.add), reads=[r_c], writes=[r_c2])
    for kc in range(8):
        for hf in range(2):
            fs_ = slice(hf * 512, (hf + 1) * 512)
            P.dma("sp", wtmp[:, :], wall_d[kc * 128:(kc + 1) * 128, fs_], writes=[r_wt])
            P.op("dve", lambda e: e.tensor_tensor(out=wtmp2[:, :], in0=wtmp[:, :], in1=muB[:, fs_], op=ALU.mult),
                 reads=[r_wt, r_c], writes=[r_W])
            P.op("dve", lambda e, kc=kc: e.tensor_copy(out=W1[:, kc, fs_], in_=wtmp2[:, :]), reads=[r_W], writes=[r_W])
            P.op("dve", lambda e, kc=kc: e.tensor_tensor(out=W0[:, kc, fs_], in0=wtmp[:, :], in1=wtmp2[:, :],
                                                         op=ALU.subtract),
                 reads=[r_wt, r_W], writes=[r_W])

    xs = cx.sb([128, 2, D], F32, "xs")
    xT = cx.sb([128, 8, 1 + TT], BF16, "xT")
    stats = cx.sb([128, 2, 6], F32, "stats")
    mv = cx.sb([128, 2], F32, "mv")
    rstd = cx.sb([128, 1], F32, "rstd")
    r_xs = [Res(f"xs{i}") for i in range(2)]
    r_xT = Res("xT")
    r_stat = Res("stat")
    def pt(name, dt=F32, share=True):
        t_ = cx.sb([128, TT], dt, name)
        return [t_, t_] if share else [t_, cx.sb([128, TT], dt, name)]
    rT, kT, aT, lw, cumI, cumE, Dexcl, invD, E2, kkr, sq, rn, kk, tmpa, kp, bT = (
        pt(n) for n in ("rT", "kT", "aT", "lw", "cumI", "cumE", "Dexcl", "invD", "E2", "kkr", "sq", "rn", "kk",
                        "tmpa", "kp", "bT"))
    BhT, KhT = pt("BhT", share=False), pt("KhT", share=False)
    Dincl1 = cx.sb([128, TT], F32, "Dincl1")
    r_bk = [Res("bhkh0"), Res("bhkh1")]
    lo0T = cx.sb([128, TT], BF16, "lo0T")
    sgT = cx.sb([128, TT], BF16, "sgT")
    _tres = {}

    def qr(t_):
        return _tres.setdefault(id(t_), Res("tmp"))
    r_lo = Res("lo")
    ARt = [[cx.sb([128, NCH, 256], BF16, "ARt") for _ in range(2)] for _ in range(2)]
    BtT = [[cx.sb([128, TT], BF16, "BtT") for _ in range(2)] for _ in range(2)]
    KtT = [[cx.sb([128, TT], BF16, "KtT") for _ in range(2)] for _ in range(2)]
    rkT = [[cx.sb([128, TT], BF16, "rkT") for _ in range(2)] for _ in range(2)]
    DC = [[cx.sb([128, NCH], F32, "DC") for _ in range(2)] for _ in range(2)]
    BKh = [[cx.sb([128, 512], BF16, "BKh") for _ in range(NCH)] for _ in range(2)]
    Vb = [[cx.sb([128, 256], BF16, "Vb") for _ in range(NCH)] for _ in range(2)]
    Gt = [[cx.sb([128, 256], BF16, "Gt") for _ in range(NCH)] for _ in range(2)]
    r_AR = [[Res(f"AR{a}{b}") for b in range(2)] for a in range(2)]
    r_Bt = [[Res(f"Bt{a}{b}") for b in range(2)] for a in range(2)]
    r_Kt = [[Res(f"Kt{a}{b}") for b in range(2)] for a in range(2)]
    r_rk = [[Res(f"rk{a}{b}") for b in range(2)] for a in range(2)]
    r_Di = [[Res(f"Di{a}{b}") for b in range(2)] for a in range(2)]
    r_BKh = [[Res(f"BKh{a}{c}") for c in range(NCH)] for a in range(2)]
    r_Vb = [[Res(f"Vb{a}{c}") for c in range(NCH)] for a in range(2)]
    r_Gt = [[Res(f"Gt{a}{c}") for c in range(NCH)] for a in range(2)]
    NSLOT = NCH
    NM = [[cx.sb([128, 512], BF16, "NM") for _ in range(4)] for _ in range(NSLOT)]
    r_NM = [[Res(f"NM{a}{h}") for h in range(4)] for a in range(NSLOT)]
    Xb = [[[cx.sb([128, 128], BF16, "X") for _ in range(2)] for _ in range(4)] for _ in range(NSLOT)]
    r_X = [[[Res(f"X{a}{h}{k}") for k in range(2)] for h in range(4)] for a in range(NSLOT)]
    NL = [[[cx.sb([128, 256], BF16, "NL") for _ in range(2)] for _ in range(4)] for _ in range(NSLOT)]
    r_NL = [[[Res(f"NL{a}{h}{k}") for k in range(2)] for h in range(4)] for a in range(NSLOT)]
    Sf = [cx.sb([128, 128], F32, "Sf") for _ in range(2)]
    Sb = [cx.sb([128, 128], BF16, "Sb") for _ in range(2)]
    r_S = [Res("S0"), Res("S1")]
    brp = [cx.sb([128, 128], BF16, "brp") for _ in range(2)]
    Wbp = [cx.sb([128, 128], BF16, "Wbp") for _ in range(2)]
    r_br = [Res("br0"), Res("br1")]
    r_Wb = [Res("Wb0"), Res("Wb1")]
    Ysb = [cx.sb([128, 256], F32, "Ysb") for _ in range(2)]
    r_Y = [[Res(f"Y{a}{p}") for p in range(2)] for a in range(2)]
    yn = cx.sb([128, 256], F32, "yn")
    ost = cx.sb([128, 4, 6], F32, "ost")
    omv = cx.sb([128, 4, 2], F32, "omv")
    orstd = cx.sb([128, 4], F32, "orstd")
    bsb = cx.sb([128, 4], F32, "bsb")
    ob = [cx.sb([128, 256], F32, "ob") for _ in range(2)]
    oaT = [cx.sb([128, 2, TT], BF16, "oaT") for _ in range(2)]
    r_o = Res("ostuff")
    r_ob = [Res("ob0"), Res("ob1")]
    r_oaT = [Res("oaT0"), Res("oaT1")]

    PJ = [cx.ps([128, 512], F32, "PJ") for _ in range(NPJ)]
    INV = [cx.ps([128, 512], F32, "INV") for _ in range(6 - NPJ)]
    SEQ = [cx.ps([128, 512], F32, "SEQ") for _ in range(2)]
    rPJ = [Res(f"PJ{i}", True) for i in range(NPJ)]
    rINV = [Res(f"INV{i}", True) for i in range(6 - NPJ)]
    rSEQ = [Res("SEQ0", True), Res("SEQ1", True)]
    pj_i = [0]
    inv_i = [0]

    def next_pj():
        i = pj_i[0] % NPJ
        pj_i[0] += 1
        return PJ[i], rPJ[i]

    def next_inv():
        i = inv_i[0] % (6 - NPJ)
        inv_i[0] += 1
        return INV[i], rINV[i]

    P.op("dve", lambda e: e.memset(xT[:, :, :], 0.0), writes=[r_xT])
    for p in range(2):
        P.op("dve", lambda e, p=p: e.memset(Sf[p][:, :], 0.0), writes=[r_S[p]])
        P.op("dve", lambda e, p=p: e.memset(Sb[p][:, :], 0.0), writes=[r_S[p]])

    out_tags = []
    chs = lambda p, i: chv[:, p, i:i + 1]

    def c3(ap):
        return ap.rearrange("p (c t) -> p c t", c=NCH)

    def prep(ti):
        par = ti % 2
        t0 = ti * TT
        if ti > 0:
            P.op("pool", lambda e: e.tensor_copy(out=xT[:, :, 0:1], in_=xT[:, :, TT:TT + 1]), reads=[r_xT], writes=[r_xT])
        for s in range(NCH):
            xb_ = s % 2
            P.dma("sp", xs[:, xb_, :], xsrc(t0 + s * 128, 128), writes=[r_xs[xb_]])
            if do_ln0:
                layer_norm_rows(P, nc, xs[:, xb_, :], r_xs[xb_], lnB[:, 0, :], lnB[:, 1, :], stats, mv, rstd, r_stat, r_c)
            for half in range(2):
                TR, rTR = next_pj()
                for k4 in range(4):
                    kc = half * 4 + k4
                    P.op("pe", lambda e, kc=kc, k4=k4: e.transpose(out=TR[:, k4 * 128:(k4 + 1) * 128],
                                                                   in_=xs[:, xb_, kc * 128:(kc + 1) * 128],
                                                                   identity=ident[:, :]),
                         reads=[r_xs[xb_], r_c], writes=[rTR], inc=(k4 == 3))
                P.op("act", lambda e, half=half: e.copy(out=xT[:, half * 4:(half + 1) * 4, 1 + s * 128:1 + (s + 1) * 128],
                                                        in_=TR[:, :].rearrange("p (a t) -> p a t", a=4)),
                     reads=[rTR], writes=[r_xT])
            yield

        def proj_cm(c0, ncols=128):
            ps, ps_r = next_pj()
            for kc in range(8):
                P.op("pe", lambda e, kc=kc: e.matmul(ps[0:ncols, 0:TT], lhsT=W0[:, kc, c0:c0 + ncols],
                                                     rhs=xT[:, kc, 1:1 + TT], start=(kc == 0), stop=False),
                     reads=[r_W, r_xT], writes=[ps_r], inc=False)
            for kc in range(8):
                P.op("pe", lambda e, kc=kc: e.matmul(ps[0:ncols, 0:TT], lhsT=W1[:, kc, c0:c0 + ncols],
                                                     rhs=xT[:, kc, 0:TT], start=False, stop=(kc == 7)),
                     reads=[r_W, r_xT], writes=[ps_r], inc=(kc == 7))
            return ps, ps_r

        ps, ps_r = proj_cm(768)
        P.op("act", lambda e: e.activation(out=lo0T[0:64, :], in_=ps[0:64, 0:TT], func=AF.Tanh), reads=[ps_r], writes=[r_lo])
        P.op("act", lambda e: e.copy(out=lo0T[64:128, :], in_=ps[64:128, 0:TT]), reads=[ps_r], writes=[r_lo])
        ps, ps_r = proj_cm(896)
        P.op("act", lambda e: e.activation(out=sgT[:, :], in_=ps[:, 0:TT], func=AF.Sigmoid), reads=[ps_r], writes=[r_lo])
        yield
        for p in range(2):
            ps, ps_r = proj_cm(p * 128)
            P.op("act", lambda e: e.copy(out=rT[p][:, :], in_=ps[:, 0:TT]), reads=[ps_r], writes=[qr(rT[p])])
            ps, ps_r = proj_cm(256 + p * 128)
            P.op("act", lambda e: e.copy(out=kT[p][:, :], in_=ps[:, 0:TT]), reads=[ps_r], writes=[qr(kT[p])])
            ps, ps_r = next_pj()
            P.op("pe", lambda e: e.matmul(ps[:, 0:TT], lhsT=w2ia[0:64, p * 128:(p + 1) * 128], rhs=lo0T[0:64, :],
                                          start=True, stop=True), reads=[r_c2, r_lo], writes=[ps_r])
            P.op("act", lambda e: e.activation(out=lw[p][:, :], in_=ps[:, 0:TT], func=AF.Sigmoid, bias=chs(p, 0), scale=1.0),
                 reads=[ps_r, r_c], writes=[qr(lw[p])])
            ps, ps_r = next_pj()
            P.op("pe", lambda e: e.matmul(ps[:, 0:TT], lhsT=w2ia[64:128, p * 128:(p + 1) * 128], rhs=lo0T[64:128, :],
                                          start=True, stop=True), reads=[r_c2, r_lo], writes=[ps_r])
            P.op("act", lambda e: e.activation(out=aT[p][:, :], in_=ps[:, 0:TT], func=AF.Sigmoid, bias=chs(p, 1), scale=1.0),
                 reads=[ps_r, r_c], writes=[qr(aT[p])])
            yield
            P.op("dve", lambda e: e.tensor_scalar(out=lw[p][:, :], in0=lw[p][:, :], scalar1=DECAY_SCALE, scalar2=None,
                                                  op0=ALU.mult), reads=[qr(lw[p])], writes=[qr(lw[p])])
            P.op("dve", lambda e: e.tensor_tensor_scan(out=cumI[p][:, :], data0=smask[:, 0:TT], data1=lw[p][:, :],
                                                       initial=0.0, op0=ALU.mult, op1=ALU.add),
                 reads=[qr(lw[p]), r_c], writes=[qr(cumI[p])])
            P.op("pool", lambda e: e.tensor_tensor(out=cumE[p][:, :], in0=cumI[p][:, :], in1=lw[p][:, :], op=ALU.subtract),
                 reads=[qr(cumI[p]), qr(lw[p])], writes=[qr(cumE[p])])
            P.op("act", lambda e: e.activation(out=Dincl1[:, :], in_=cumI[p][:, :], func=AF.Exp),
                 reads=[qr(cumI[p])], writes=[qr(Dincl1)])
            P.op("dve", lambda e: e.tensor_copy(out=DC[par][p][:, :], in_=c3(Dincl1[:, :])[:, :, 127]),
                 reads=[qr(Dincl1)], writes=[r_Di[par][p]])
            P.op("act", lambda e: e.activation(out=invD[p][:, :], in_=cumI[p][:, :], func=AF.Exp, scale=-1.0),
                 reads=[qr(cumI[p])], writes=[qr(invD[p])])
            P.op("act", lambda e: e.activation(out=Dexcl[p][:, :], in_=cumE[p][:, :], func=AF.Exp),
                 reads=[qr(cumE[p])], writes=[qr(Dexcl[p])])
            for c in range(NCH):
                P.op("act", lambda e, c=c: e.activation(out=E2[p][:, c * 128:(c + 1) * 128],
                                                        in_=cumI[p][:, c * 128:(c + 1) * 128], func=AF.Exp,
                                                        bias=cumI[p][:, c * 128 + 127:c * 128 + 128], scale=-1.0),
                     reads=[qr(cumI[p])], writes=[qr(E2[p])])
            yield
            P.op("pool", lambda e: e.tensor_scalar(out=kkr[p][:, :], in0=kT[p][:, :], scalar1=chs(p, 2), scalar2=None,
                                                   op0=ALU.mult), reads=[qr(kT[p]), r_c], writes=[qr(kkr[p])])
            P.op("pool", lambda e: e.tensor_tensor(out=sq[p][:, :], in0=kkr[p][:, :], in1=kkr[p][:, :], op=ALU.mult),
                 reads=[qr(kkr[p])], writes=[qr(sq[p])])
            ps, ps_r = next_pj()
            P.op("pe", lambda e: e.matmul(ps[:, 0:TT], lhsT=bones[:, :], rhs=sq[p][:, :], start=True, stop=True),
                 reads=[r_c, qr(sq[p])], writes=[ps_r])
            P.op("act", lambda e: e.activation(out=rn[p][:, :], in_=ps[:, 0:TT], func=AF.Sqrt), reads=[ps_r],
                 writes=[qr(rn[p])])
            P.op("dve", lambda e: e.tensor_scalar(out=rn[p][:, :], in0=rn[p][:, :], scalar1=1e-12, scalar2=None,
                                                  op0=ALU.max), reads=[qr(rn[p])], writes=[qr(rn[p])])
            P.op("dve", lambda e: e.reciprocal(out=rn[p][:, :], in_=rn[p][:, :]), reads=[qr(rn[p])], writes=[qr(rn[p])])
            P.op("dve", lambda e: e.tensor_tensor(out=kk[p][:, :], in0=kkr[p][:, :], in1=rn[p][:, :], op=ALU.mult),
                 reads=[qr(kkr[p]), qr(rn[p])], writes=[qr(kk[p])])
            P.op("pool", lambda e: e.tensor_scalar(out=tmpa[p][:, :], in0=aT[p][:, :], scalar1=chs(p, 3),
                                                   scalar2=omka[:, p:p + 1], op0=ALU.mult, op1=ALU.add),
                 reads=[qr(aT[p]), r_c, r_c2], writes=[qr(tmpa[p])])
            P.op("pool", lambda e: e.tensor_tensor(out=kp[p][:, :], in0=kT[p][:, :], in1=tmpa[p][:, :], op=ALU.mult),
                 reads=[qr(kT[p]), qr(tmpa[p])], writes=[qr(kp[p])])
            yield
            P.op("dve", lambda e: e.scalar_tensor_tensor(out=ARt[par][p][:, :, 0:128], in0=c3(kk[p][:, :]), scalar=-1.0,
                                                         in1=c3(Dexcl[p][:, :]), op0=ALU.mult, op1=ALU.mult),
                 reads=[qr(kk[p]), qr(Dexcl[p])], writes=[r_AR[par][p]])
            P.op("pool", lambda e: e.tensor_tensor(out=ARt[par][p][:, :, 128:256], in0=c3(rT[p][:, :]),
                                                   in1=c3(Dincl1[:, :]), op=ALU.mult),
                 reads=[qr(rT[p]), qr(Dincl1)], writes=[r_AR[par][p]])
            P.op("pool", lambda e: e.tensor_tensor(out=bT[p][:, :], in0=kk[p][:, :], in1=aT[p][:, :], op=ALU.mult),
                 reads=[qr(kk[p]), qr(aT[p])], writes=[qr(bT[p])])
            P.op("dve", lambda e: e.tensor_tensor(out=BtT[par][p][:, :], in0=bT[p][:, :], in1=invD[p][:, :], op=ALU.mult),
                 reads=[qr(bT[p]), qr(invD[p])], writes=[r_Bt[par][p]])
            P.op("pool", lambda e: e.tensor_tensor(out=KtT[par][p][:, :], in0=kp[p][:, :], in1=invD[p][:, :], op=ALU.mult),
                 reads=[qr(kp[p]), qr(invD[p])], writes=[r_Kt[par][p]])
            P.op("dve", lambda e: e.tensor_tensor(out=BhT[p][:, :], in0=bT[p][:, :], in1=E2[p][:, :], op=ALU.mult),
                 reads=[qr(bT[p]), qr(E2[p])], writes=[r_bk[p]])
            P.op("pool", lambda e: e.tensor_tensor(out=KhT[p][:, :], in0=kp[p][:, :], in1=E2[p][:, :], op=ALU.mult),
                 reads=[qr(kp[p]), qr(E2[p]), r_bk[p]], writes=[r_bk[p]])
            P.op("dve", lambda e: e.scalar_tensor_tensor(out=rkT[par][p][:, :], in0=rT[p][:, :], scalar=chs(p, 4),
                                                         in1=kp[p][:, :], op0=ALU.mult, op1=ALU.mult),
                 reads=[qr(rT[p]), qr(kp[p]), r_c], writes=[r_rk[par][p]])
            yield
        for c in range(NCH):
            ps, ps_r = next_pj()
            for kc in range(8):
                P.op("pe", lambda e, kc=kc: e.matmul(ps[:, 0:256], lhsT=xT[:, kc, 1 + c * 128:1 + (c + 1) * 128],
                                                     rhs=W0[:, kc, 512:768], start=(kc == 0), stop=False),
                     reads=[r_W, r_xT], writes=[ps_r], inc=False)
            for kc in range(8):
                P.op("pe", lambda e, kc=kc: e.matmul(ps[:, 0:256], lhsT=xT[:, kc, c * 128:(c + 1) * 128],
                                                     rhs=W1[:, kc, 512:768], start=False, stop=(kc == 7)),
                     reads=[r_W, r_xT], writes=[ps_r], inc=(kc == 7))
            P.op("act", lambda e: e.copy(out=Vb[par][c][:, :], in_=ps[:, 0:256]), reads=[ps_r], writes=[r_Vb[par][c]])
            ps, ps_r = next_pj()
            P.op("pe", lambda e: e.matmul(ps[:, 0:256], lhsT=sgT[:, c * 128:(c + 1) * 128], rhs=w2g[:, :], start=True, stop=True),
                 reads=[r_lo, r_c2], writes=[ps_r])
            P.op("act", lambda e: e.copy(out=Gt[par][c][:, :], in_=ps[:, 0:256]), reads=[ps_r], writes=[r_Gt[par][c]])
            TR, rTR = next_pj()
            for q in range(4):
                src = (BhT, KhT)[q // 2][q % 2]
                P.op("pe", lambda e, q=q, src=src: e.transpose(out=TR[:, q * 128:(q + 1) * 128],
                                                               in_=src[:, c * 128:(c + 1) * 128], identity=ident[:, :]),
                     reads=[r_bk[q % 2], r_c], writes=[rTR], inc=(q == 3))
            P.op("dve", lambda e: e.tensor_copy(out=BKh[par][c][:, :], in_=TR[:, :]), reads=[rTR], writes=[r_BKh[par][c]])
            yield

    def inv(ti, c, h):
        par = ti % 2
        slot = c
        sp = slot
        p, hb = h // 2, (h % 2) * 64
        cs = slice(c * 128, (c + 1) * 128)
        a12, a12_r = next_inv()
        P.op("pe", lambda e: e.matmul(a12[:, 0:256], lhsT=BtT[par][p][hb:hb + 64, cs], rhs=ARt[par][p][hb:hb + 64, c, :],
                                      start=True, stop=True),
             reads=[r_Bt[par][p], r_AR[par][p]], writes=[a12_r], inc=False)
        P.op("pe", lambda e: e.matmul(a12[:, 256:512], lhsT=KtT[par][p][hb:hb + 64, cs], rhs=ARt[par][p][hb:hb + 64, c, :],
                                      start=False, stop=True, skip_group_check=True),
             reads=[r_Kt[par][p], r_AR[par][p]], writes=[a12_r])
        a3, a3_r = next_inv()
        P.op("pe", lambda e: e.matmul(a3[:, 0:128], lhsT=ARt[par][p][hb:hb + 64, c, 0:128], rhs=BtT[par][p][hb:hb + 64, cs],
                                      start=True, stop=True),
             reads=[r_Bt[par][p], r_AR[par][p]], writes=[a3_r])
        nm, nm_r = NM[slot][h], r_NM[slot][h]
        P.op("dve", lambda e: e.tensor_tensor(out=nm[:, :], in0=a12[:, :], in1=mask4[:, :], op=ALU.mult),
             reads=[a12_r, r_c], writes=[nm_r])
        nl = NL[sp][h]
        nl_r = r_NL[sp][h]
        P.op("dve", lambda e: e.tensor_tensor(out=nl[0][:, 128:256], in0=a3[:, 0:128], in1=maskL[:, :], op=ALU.mult),
             reads=[a3_r, r_c], writes=[nl_r[0]])
        X, X_r = Xb[slot][h], r_X[slot][h]
        P.op("pool", lambda e: e.tensor_tensor(out=X[0][:, :], in0=nm[:, 0:128], in1=identb[:, :], op=ALU.add),
             reads=[nm_r, r_c2], writes=[X_r[0]])
        yield
        Np, Np_r = nm[:, 0:128], nm_r
        Lp, Lp_r = nl[0][:, 128:256], nl_r[0]
        xi = 0
        for rnd in range(6):
            last = (rnd == 5)
            pp = (rnd + 1) % 2
            bk, bk_r = next_inv()
            if not last:
                P.op("pe", lambda e: e.matmul(bk[:, 0:128], lhsT=Lp, rhs=Np, start=True, stop=True),
                     reads=[Lp_r, Np_r], writes=[bk_r], inc=False)
            P.op("pe", lambda e: e.matmul(bk[:, 128:256], lhsT=Np, rhs=Lp, start=last, stop=True,
                                          skip_group_check=True),
                 reads=[Lp_r, Np_r], writes=[bk_r])
            if not last:
                P.op("act", lambda e: e.copy(out=nl[pp][:, :], in_=bk[:, 0:256]), reads=[bk_r], writes=[nl_r[pp]])
            else:
                P.op("act", lambda e: e.copy(out=nl[pp][:, 128:256], in_=bk[:, 128:256]), reads=[bk_r], writes=[nl_r[pp]])
            Np, Np_r = nl[pp][:, 0:128], nl_r[pp]
            Lp, Lp_r = nl[pp][:, 128:256], nl_r[pp]
            yield
            px, px_r = next_inv()
            P.op("pe", lambda e: e.matmul(px[:, 0:128], lhsT=Lp, rhs=X[xi][:, :], start=True, stop=True),
                 reads=[Lp_r, X_r[xi]], writes=[px_r])
            P.op("dve", lambda e: e.tensor_tensor(out=X[1 - xi][:, :], in0=px[:, 0:128], in1=X[xi][:, :], op=ALU.add),
                 reads=[px_r, X_r[xi]], writes=[X_r[1 - xi]])
            xi = 1 - xi
            yield
        assert xi == 0

    def seq(ti, c, p):
        par = ti % 2
        slot = c
        sq_, sq_r = SEQ[p], rSEQ[p]
        cs = slice(c * 128, (c + 1) * 128)
        for hh in range(2):
            h = 2 * p + hh
            hb = hh * 64
            P.op("pe", lambda e: e.matmul(sq_[:, hh * 64:(hh + 1) * 64], lhsT=ARt[par][p][hb:hb + 64, c, 0:128],
                                          rhs=Sb[p][hb:hb + 64, hh * 64:(hh + 1) * 64], start=(hh == 0), stop=False,
                                          skip_group_check=True),
                 reads=[r_AR[par][p], r_S[p]], writes=[sq_r], inc=False)
            P.op("pe", lambda e: e.matmul(sq_[:, hh * 64:(hh + 1) * 64], lhsT=NM[slot][h][:, 256:384],
                                          rhs=Vb[par][c][:, h * 64:(h + 1) * 64], start=False, stop=True,
                                          skip_group_check=True),
                 reads=[r_NM[slot][h], r_Vb[par][c]], writes=[sq_r], inc=(hh == 1))
        P.op("act", lambda e: e.copy(out=brp[p][:, :], in_=sq_[:, 0:128]), reads=[sq_r], writes=[r_br[p]])
        yield
        for hh in range(2):
            h = 2 * p + hh
            P.op("pe", lambda e: e.matmul(sq_[:, 128 + hh * 64:128 + (hh + 1) * 64], lhsT=Xb[slot][h][0][:, :],
                                          rhs=brp[p][:, hh * 64:(hh + 1) * 64], start=False, stop=True,
                                          skip_group_check=True),
                 reads=[r_X[slot][h][0], r_br[p]], writes=[sq_r], inc=(hh == 1))
        P.op("dve", lambda e: e.tensor_copy(out=Wbp[p][:, :], in_=sq_[:, 128:256]), reads=[sq_r], writes=[r_Wb[p]])
        yield
        P.op("pe", lambda e: e.matmul(sq_[:, 256:384], lhsT=BKh[par][c][:, p * 128:(p + 1) * 128], rhs=Wbp[p][:, :],
                                      start=False, stop=False, skip_group_check=True),
             reads=[r_BKh[par][c], r_Wb[p]], writes=[sq_r], inc=False)
        P.op("pe", lambda e: e.matmul(sq_[:, 256:384], lhsT=BKh[par][c][:, 256 + p * 128:256 + (p + 1) * 128],
                                      rhs=Vb[par][c][:, p * 128:(p + 1) * 128], start=False, stop=True,
                                      skip_group_check=True),
             reads=[r_BKh[par][c], r_Vb[par][c]], writes=[sq_r], inc=False)
        for hh in range(2):
            h = 2 * p + hh
            hb = hh * 64
            yc = slice(384 + hh * 64, 384 + (hh + 1) * 64)
            P.op("pe", lambda e: e.matmul(sq_[:, yc], lhsT=ARt[par][p][hb:hb + 64, c, 128:256],
                                          rhs=Sb[p][hb:hb + 64, hh * 64:(hh + 1) * 64], start=False, stop=False,
                                          skip_group_check=True),
                 reads=[r_AR[par][p], r_S[p]], writes=[sq_r], inc=False)
            P.op("pe", lambda e: e.matmul(sq_[:, yc], lhsT=NM[slot][h][:, 128:256], rhs=Wbp[p][:, hh * 64:(hh + 1) * 64],
                                          start=False, stop=False, skip_group_check=True),
                 reads=[r_NM[slot][h], r_Wb[p]], writes=[sq_r], inc=False)
            P.op("pe", lambda e: e.matmul(sq_[:, yc], lhsT=NM[slot][h][:, 384:512], rhs=Vb[par][c][:, h * 64:(h + 1) * 64],
                                          start=False, stop=True, skip_group_check=True),
                 reads=[r_NM[slot][h], r_Vb[par][c]], writes=[sq_r], inc=(hh == 1))
        P.op("dve", lambda e: e.scalar_tensor_tensor(out=Sf[p][:, :], in0=Sf[p][:, :],
                                                     scalar=DC[par][p][:, c:c + 1],
                                                     in1=sq_[:, 256:384], op0=ALU.mult, op1=ALU.add),
             reads=[sq_r, r_S[p], r_Di[par][p]], writes=[r_S[p]])
        P.op("act", lambda e: e.copy(out=Sb[p][:, :], in_=Sf[p][:, :]), reads=[r_S[p]], writes=[r_S[p]])
        P.op("dve", lambda e: e.tensor_copy(out=Ysb[c % 2][:, p * 128:(p + 1) * 128], in_=sq_[:, 384:512]),
             reads=[sq_r], writes=[r_Y[c % 2][p]])
        yield

    def outp(ti, c):
        par = ti % 2
        t0 = ti * TT + c * 128
        Y = Ysb[c % 2]
        ry = r_Y[c % 2]
        cs = slice(c * 128, (c + 1) * 128)
        for h in range(4):
            P.op("dve", lambda e, h=h: e.bn_stats(out=ost[:, h, :], in_=Y[:, h * 64:(h + 1) * 64]), reads=ry, writes=[r_o])
        for h in range(4):
            P.op("dve", lambda e, h=h: e.bn_aggr(out=omv[:, h, :], in_=ost[:, h, :]), reads=[r_o], writes=[r_o])
        P.op("act", lambda e: e.activation(out=orstd[:, :], in_=omv[:, :, 1], func=AF.Sqrt, bias=float(RWKV_LN_EPS), scale=1.0),
             reads=[r_o], writes=[r_o])
        P.op("dve", lambda e: e.reciprocal(out=orstd[:, :], in_=orstd[:, :]), reads=[r_o], writes=[r_o])
        yield
        for h in range(4):
            P.op("dve", lambda e, h=h: e.tensor_scalar(out=yn[:, h * 64:(h + 1) * 64], in0=Y[:, h * 64:(h + 1) * 64],
                                                       scalar1=omv[:, h, 0:1], scalar2=orstd[:, h:h + 1],
                                                       op0=ALU.subtract, op1=ALU.mult),
                 reads=ry + [r_o], writes=[r_o])
        P.op("pool", lambda e: e.tensor_tensor(out=yn[:, :], in0=yn[:, :], in1=lnxB[:, 0, :], op=ALU.mult),
             reads=[r_o, r_c], writes=[r_o])
        P.op("pool", lambda e: e.tensor_tensor(out=yn[:, :], in0=yn[:, :], in1=lnxB[:, 1, :], op=ALU.add),
             reads=[r_o, r_c], writes=[r_o])
        ps, ps_r = next_pj()
        for p in range(2):
            P.op("pe", lambda e, p=p: e.matmul(ps[:, p * 2:(p + 1) * 2], lhsT=rkT[par][p][:, cs], rhs=selb[:, :],
                                               start=(p == 0), stop=True, skip_group_check=True),
                 reads=[r_rk[par][p], r_c2], writes=[ps_r], inc=(p == 1))
        P.op("dve", lambda e: e.tensor_copy(out=bsb[:, :], in_=ps[:, 0:4]), reads=[ps_r], writes=[r_o])
        yield
        for h in range(4):
            P.op("dve", lambda e, h=h: e.scalar_tensor_tensor(out=yn[:, h * 64:(h + 1) * 64],
                                                              in0=Vb[par][c][:, h * 64:(h + 1) * 64],
                                                              scalar=bsb[:, h:h + 1], in1=yn[:, h * 64:(h + 1) * 64],
                                                              op0=ALU.mult, op1=ALU.add),
                 reads=[r_o, r_Vb[par][c]], writes=[r_o])
        oi = c % 2
        P.op("pool", lambda e: e.tensor_tensor(out=ob[oi][:, :], in0=yn[:, :], in1=Gt[par][c][:, :], op=ALU.mult),
             reads=[r_o, r_Gt[par][c]], writes=[r_ob[oi]])
        TR, rTR = next_pj()
        for pp in range(2):
            P.op("pe", lambda e, pp=pp: e.transpose(out=TR[:, pp * 128:(pp + 1) * 128], in_=ob[oi][:, pp * 128:(pp + 1) * 128],
                                                    identity=ident[:, :]),
                 reads=[r_ob[oi], r_c], writes=[rTR], inc=(pp == 1))
        P.op("act", lambda e: e.copy(out=oaT[par][:, :, c * 128:(c + 1) * 128],
                                     in_=TR[:, 0:256].rearrange("p (a t) -> p a t", a=2)),
             reads=[rTR], writes=[r_oaT[par]])
        if c == NCH - 1:
            out_tags.append(P.dma("sp", oa_dst(ti).rearrange("(pp p) t -> p pp t", p=128),
                                  oaT[par][:, :, :], reads=[r_oaT[par]]))
            if after_store is not None:
                after_store(ti, out_tags[-1])
        yield

    S = Sched()

    def add_prep(ti):
        deps = [f"prep{ti - 1}"] if ti > 0 else []
        if ti > 1:
            deps += [f"out{ti - 2}_{NCH - 1}"]
        S.add(f"prep{ti}", prep(ti), deps)

    add_prep(0)
    for ti in range(NTI):
        if ti + 1 < NTI:
            add_prep(ti + 1)
        for c in range(NCH):
            for h in range(4):
                d = [f"prep{ti}"]
                if ti >= 1:
                    d.append(f"seq{ti - 1}_{c}_{h // 2}")
                S.add(f"inv{ti}_{c}_{h}", inv(ti, c, h), d)
        for c in range(NCH):
            for p in range(2):
                d = [f"inv{ti}_{c}_{2 * p}", f"inv{ti}_{c}_{2 * p + 1}"]
                if c > 0:
                    d.append(f"seq{ti}_{c - 1}_{p}")
                elif ti > 0:
                    d.append(f"seq{ti - 1}_{NCH - 1}_{p}")
                S.add(f"seq{ti}_{c}_{p}", seq(ti, c, p), d)
            d = [f"seq{ti}_{c}_0", f"seq{ti}_{c}_1"]
            if c > 0:
                d.append(f"out{ti}_{c - 1}")
            elif ti > 0:
                d.append(f"out{ti - 1}_{NCH - 1}")
            S.add(f"out{ti}_{c}", outp(ti, c), d)
    S.run(window=RWKV_WINDOW)
    return out_tags, cx


_ar = np.arange(128)
MASK4 = np.concatenate([(_ar[:, None] < _ar[None, :]), (_ar[:, None] <= _ar[None, :])] * 2, 1).astype(np.float32)
MASKL = (_ar[None, :] < _ar[:, None]).astype(np.float32)
BONES = (_ar[:, None] // 64 == _ar[None, :] // 64).astype(np.float32)
SEL = (_ar[:, None] // 64 == np.arange(2)[None, :]).astype(np.float32)
SMASK = np.ascontiguousarray(np.broadcast_to((np.arange(512) % 128 != 0).astype(np.float32)[None, :], (128, 512)))


def rwkv_inputs(p, l, g):
    w_in = p["w_in"][l]
    mu = p["mu_shift"][l]
    if True:
        gc = slice(g * 256, (g + 1) * 256)
        cols = np.concatenate([np.arange(g * 256, (g + 1) * 256), 512 + np.arange(g * 256, (g + 1) * 256),
                               1024 + np.arange(g * 256, (g + 1) * 256), np.arange(1536, 1792)])
        vecs = [p["w0_decay"][l][gc], p["a0"][l][gc], p["k_k"][l][gc], p["k_a"][l][gc], p["r_k"][l].reshape(-1)[gc]]
        chv = np.zeros((128, 2, 8), np.float32)
        for i, v in enumerate(vecs):
            chv[:, :, i] = v.reshape(2, 128).T
        m = {
            "wall": np.ascontiguousarray(w_in[:, cols]),
            "muB": _bc(mu[cols]),
            "chv": chv,
            "w2ia": np.ascontiguousarray(np.concatenate([p["w2_decay"][l][:, gc], p["w2_iclr"][l][:, gc]], 0)),
            "w2g": np.ascontiguousarray(p["w2_gate"][l][:, gc]),
            "lnxB": np.ascontiguousarray(np.stack([_bc(p["lnx_g"][l][gc]), _bc(p["lnx_b"][l][gc])], 0)),
            "lnB": np.ascontiguousarray(np.stack([_bc(p["ln0_g"]), _bc(p["ln0_b"])], 0)),
            "ident": IDENT, "mask4": MASK4, "maskL": MASKL, "bones": BONES, "sel": SEL, "smask": SMASK,
        }
    return m


def run_rwkv(xb, p, l, do_ln0, n_cores=8):
    B, T, _ = xb.shape
    nc = build_rwkv(T, do_ln0)
    in_maps = []
    for c in range(n_cores):
        b, g = c // 2, c % 2
        m = rwkv_inputs(p, l, g)
        m["x"] = np.ascontiguousarray(xb[b])
        in_maps.append(m)
    res = run_bass_kernel_spmd(nc, in_maps, core_ids=list(range(n_cores)))
    oaT = np.stack([np.concatenate([res.results[2 * b + g]["oaT"] for g in range(2)], 0) for b in range(B)], 0)
    return oaT


PAIRS = [[0, 1], [2, 3], [4, 5], [6, 7]]


def build_fused(T, ff_override=None):
    nc = bass.Bass("TRN2", target_bir_lowering=False)
    NT = T // 2
    TT = min(T, 512)
    CW = min(1024, NT)
    NCK = T // CW
    HCK = NCK // 2
    RY = min(512, NT)
    NYC = NT // RY
    x_d = nc.dram_tensor("x", [T, D], F32, kind="ExternalInput").ap()
    y_d = nc.dram_tensor("y", [NT, D], F32, kind="ExternalOutput").ap()
    A_r = [decl_rwkv(nc, f"r{l}_") for l in range(DEPTH)]
    A_f = [decl_fox(nc, f"f{l}_") for l in range(DEPTH)]
    A_m = [decl_merge(nc, f"m{l}_", moe=(l % 2 == 1), ff_override=ff_override) for l in range(DEPTH)]
    ola = [nc.dram_tensor(f"ola{l}", [NCK, 256, CW], BF16).ap() for l in range(DEPTH)]
    olf = [nc.dram_tensor(f"olf{l}", [NCK, 512, CW], BF16).ap() for l in range(DEPTH)]
    oalla = [nc.dram_tensor(f"oalla{l}", [NCK, 512, CW], BF16).ap() for l in range(DEPTH)]
    oallf = [nc.dram_tensor(f"oallf{l}", [NCK, 1024, CW], BF16).ap() for l in range(DEPTH)]
    omia = [nc.dram_tensor(f"omia{l}", [HCK, 512, CW], BF16).ap() for l in range(DEPTH)]
    omif = [nc.dram_tensor(f"omif{l}", [HCK, 1024, CW], BF16).ap() for l in range(DEPTH)]
    yloc = nc.dram_tensor("yloc", [NT, D], F32).ap()
    yall = nc.dram_tensor("yall", [NYC, 2 * RY, D], F32).ap()
    xh = nc.dram_tensor("xh", [NT, D], F32).ap()
    cx0 = Ctx(nc)
    P = cx0.P
    pid = nc.sync.partition_id()
    g = pid % 2
    for i in range(2):
        P.dma("sp", xh[i * (NT // 2):(i + 1) * (NT // 2), :], x_d[bass.ds(g * NT + i * (NT // 2), NT // 2), :])

    def odst(buf, r0):
        def f(ti):
            j, c = (ti * TT) // CW, (ti * TT) % CW
            return buf[j, r0:r0 + 256, c:c + TT]
        return f

    def chunk_gather(src, dst, per_chunk):
        acc = {}

        def cb(ti, tag):
            j = (ti * TT) // CW
            acc.setdefault(j, []).append(tag)
            if len(acc[j]) == per_chunk:
                P.collective_allgather(src[j].opt(), dst[j].opt(), PAIRS, acc[j])
        return cb

    def xsrc1(t0, n):
        rank, j, r = t0 // NT, (t0 % NT) // RY, t0 % RY
        return yall[j, rank * RY + r: rank * RY + r + n, :]

    xsrc = lambda t0, n: x_d[t0:t0 + n, :]
    tags_m = []
    for l in range(DEPTH):
        do_ln0 = (l == 0)
        tags_r, cx = emit_rwkv(nc, P, A_r[l], T, do_ln0, xsrc, odst(ola[l], 0),
                               after_store=chunk_gather(ola[l], oalla[l], CW // TT))
        P.barrier()
        cx.close()
        tags_f, cx = emit_fox(nc, P, A_f[l], T, do_ln0, xsrc, odst(olf[l], 0), odst(olf[l], 256),
                              after_store=chunk_gather(olf[l], oallf[l], 2 * (CW // TT)))
        P.barrier()
        cx.close()
        P.dma("sp", omia[l].rearrange("j r c -> (j r) c"),
              oalla[l].rearrange("j r c -> (j r) c")[bass.ds(g * (HCK * 512), HCK * 512), :])
        P.dma("sp", omif[l].rearrange("j r c -> (j r) c"),
              oallf[l].rearrange("j r c -> (j r) c")[bass.ds(g * (HCK * 1024), HCK * 1024), :])
        P.barrier()
        if l == 0:
            x_rows = lambda t0, n: xh[t0:t0 + n, :]
        else:
            x_rows = lambda t0, n: yloc[t0:t0 + n, :]

        def o_rows(br, half, t0, n, l=l):
            jj, c = t0 // CW, t0 % CW
            if br == 0:
                return omia[l][jj, half * 256:(half + 1) * 256, c:c + n]
            return omif[l][jj, half * 512 + (br - 1) * 256: half * 512 + br * 256, c:c + n]

        last = (l == DEPTH - 1)
        dst = y_d if last else yloc
        cb = None
        if not last:
            accy = {}

            def cb(t0, tag):
                j = t0 // RY
                accy.setdefault(j, []).append(tag)
                if len(accy[j]) == RY // 128:
                    P.collective_allgather(yloc[j * RY:(j + 1) * RY, :].opt(), yall[j].opt(), PAIRS, accy[j])
        tags_m, cx = emit_merge(nc, P, A_m[l], NT, do_ln0, l % 2 == 1, x_rows, o_rows, dst, after_store=cb)
        P.barrier()
        cx.close()
        if not last:
            xsrc = xsrc1
    P.finish(tags_m)
    cx0.close()
    return nc


def fused_in_maps(x, p, n_cores=8):
    in_maps = []
    mcommon = [merge_inputs(p, l, l % 2 == 1) for l in range(DEPTH)]
    for c in range(n_cores):
        b, g = c // 2, c % 2
        m = {"x": np.ascontiguousarray(x[b])}
        for l in range(DEPTH):
            for k, v in rwkv_inputs(p, l, g).items():
                m[f"r{l}_{k}"] = v
            for k, v in fox_inputs(p, l, g).items():
                m[f"f{l}_{k}"] = v
            for k, v in mcommon[l].items():
                m[f"m{l}_{k}"] = v
        in_maps.append(m)
    return in_maps


def kernel(**inputs):
    p = {k: np.asarray(v) for k, v in inputs.items()}
    x = np.ascontiguousarray(p["x"], dtype=np.float32)
    B, T, _ = x.shape
    nc = build_fused(T)
    in_maps = fused_in_maps(x, p)
    res = run_bass_kernel_spmd(nc, in_maps, core_ids=list(range(8)))
    NT = T // 2
    out = np.empty((B, T, D), np.float32)
    for c in range(8):
        b, g = c // 2, c % 2
        out[b, g * NT:(g + 1) * NT] = res.results[c]["y"]
    return out
```

```python
import contextlib
import numpy as np
import ml_dtypes
import concourse.bass as bass
import concourse.mybir as mybir
from concourse.bass_utils import run_bass_kernel_spmd

F32 = mybir.dt.float32
BF16 = mybir.dt.bfloat16
AF = mybir.ActivationFunctionType
ALU = mybir.AluOpType
AX = mybir.AxisListType

D = 1024
DEPTH = 2
ALPHA = (2 * DEPTH) ** 0.25
LN_EPS = 1e-5
D_FF = 2816
N_EXP = 8
D_FF_E = 3584
NBR = 3
BW = 512


class Res:
    __slots__ = ("name", "w", "r", "dsem", "dval", "excl")

    def __init__(self, name, excl=False):
        self.name = name
        self.excl = excl
        self.w = None
        self.r = {}
        self.dsem = None
        self.dval = 0

    def absorb(self, others):
        for o in others:
            if o.w is not None:
                self.r[("w", id(o))] = o.w
            for k, t in o.r.items():
                self.r[(k, id(o))] = t


class Prog:
    ROLL = 20000

    NPOOL = 20

    def __init__(self, nc, es):
        self.nc = nc
        self.es = es
        self.dpool = {q: dict(sems=[], vals=[], idx=0) for q in ("sp", "act", "pool")}
        self.cc_tags = []
        self.E = {}
        for name, obj in (("pe", nc.tensor), ("dve", nc.vector), ("act", nc.scalar),
                          ("pool", nc.gpsimd), ("sp", nc.sync)):
            self.E[name] = dict(obj=obj, sem=None, val=0, seen={}, nsem=0, pending=False)
        self.nsems = 0
        self.n_inst = 0

    def new_sem(self, name):
        self.nsems += 1
        return self.es.enter_context(self.nc.semaphore(f"{name}_{self.nsems}"))

    def _eng_sem(self, en):
        E = self.E[en]
        if E["sem"] is None or E["val"] >= self.ROLL:
            assert not E["pending"]
            E["sem"] = self.new_sem("e" + en)
            E["val"] = 0
        return E

    def _wait(self, en, tag, same_ok=True):
        E = self.E[en]
        ten, sem, val = tag
        if ten == en and sem is E["sem"]:
            if en == "pe":
                return
            if val < E["val"] - 1:
                return
        elif ten == en:
            return
        key = id(sem)
        if E["seen"].get(key, 0) >= val:
            return
        E["obj"].wait_ge(sem, val)
        E["seen"][key] = val
        self.n_inst += 1

    def _deps(self, en, reads, writes):
        for r in reads:
            if r.w is not None:
                self._wait(en, r.w)
            if r.excl:
                for t in r.r.values():
                    if t[0] != en:
                        self._wait(en, t)
        for w in writes:
            if w.w is not None:
                self._wait(en, w.w)
            for t in w.r.values():
                if t[0] == en and t[1] is self.E[en]["sem"] and en != "pe":
                    continue
                self._wait(en, t)

    def op(self, en, fn, reads=(), writes=(), inc=True):
        E = self._eng_sem(en)
        self._deps(en, reads, writes)
        ins = fn(E["obj"])
        self.n_inst += 1
        if inc:
            E["val"] += 1
            ins.then_inc(E["sem"], 1)
            tag = (en, E["sem"], E["val"])
            E["pending"] = False
        else:
            tag = (en, E["sem"], E["val"] + 1)
            E["pending"] = True
        for r in reads:
            r.r[en] = tag
        for w in writes:
            w.w = tag
            w.r = {}
        return ins

    def dma(self, q, out, in_, reads=(), writes=(), sem_res=None):
        E = self.E[q]
        self._deps(q, reads, writes)
        pool = self.dpool[q]
        i = pool["idx"] % self.NPOOL
        pool["idx"] += 1
        if len(pool["sems"]) <= i:
            pool["sems"].append(self.new_sem("d" + q))
            pool["vals"].append(0)
        sem, prev = pool["sems"][i], pool["vals"][i]
        if prev > 0:
            self._wait(q, ("dma", sem, prev))
        ins = E["obj"].dma_start(out=out, in_=in_)
        ins.then_inc(sem, 16)
        pool["vals"][i] = prev + 16
        self.n_inst += 1
        tag = ("dma", sem, prev + 16)
        for r in reads:
            r.r[("dma", id(sem))] = tag
        for w in writes:
            w.w = tag
            w.r = {}
        return tag

    def collective_allgather(self, in_ap, out_ap, groups, dep_tags):
        for t in dep_tags:
            self._wait("pool", t)
        sem = self.new_sem("cc")
        ins = self.nc.gpsimd.collective_compute("AllGather", ALU.bypass, replica_groups=groups,
                                                ins=[in_ap], outs=[out_ap])
        ins.then_inc(sem)
        self.n_inst += 1
        tag = ("cc", sem, 1)
        self.cc_tags.append(tag)
        return tag

    def barrier(self, extra_tags=()):
        tags = [(en, E["sem"], E["val"]) for en, E in self.E.items() if E["sem"] is not None and E["val"] > 0]
        for pool in self.dpool.values():
            for sem, v in zip(pool["sems"], pool["vals"]):
                if v > 0:
                    tags.append(("dma", sem, v))
        tags += list(self.cc_tags) + list(extra_tags)
        for en, E in self.E.items():
            assert not E["pending"], en
            for t in tags:
                if t[0] == en:
                    continue
                self._wait(en, t)

    def finish(self, out_tags):
        for t in out_tags:
            self._wait("sp", t)
        for en, E in self.E.items():
            assert not E["pending"], en


class Ctx:
    _uid = [0]

    def __init__(self, nc, P=None):
        self.nc = nc
        self.es = contextlib.ExitStack()
        self.P = P if P is not None else Prog(nc, self.es)
        Ctx._uid[0] += 1
        self.n = Ctx._uid[0] * 1000

    def sb(self, shape, dt, name=None):
        self.n += 1
        return self.es.enter_context(self.nc.sbuf_tensor(f"{name or 't'}{self.n}", list(shape), dt))

    def ps(self, shape, dt=F32, name=None):
        self.n += 1
        return self.es.enter_context(self.nc.psum_tensor(f"{name or 'p'}{self.n}", list(shape), dt))

    def close(self):
        self.es.close()


class WStream:
    def __init__(self, cx, nbuf, nbytes, name, lookahead=None, q="pool"):
        self.cx = cx
        self.P = cx.P
        self.bufs = [cx.sb([128, nbytes // 2], BF16, name) for _ in range(nbuf)]
        self.res = [Res(f"{name}{i}") for i in range(nbuf)]
        self.reqs = []
        self.issued = 0
        self.taken = 0
        self.la = lookahead if lookahead is not None else nbuf - 1
        self.q = q

    def plan(self, dram_ap, shape):
        self.reqs.append((dram_ap, tuple(shape)))

    def _view(self, i, shape):
        n = int(np.prod(shape))
        b = self.bufs[i % len(self.bufs)]
        v = b[:, 0:n]
        if len(shape) == 2:
            v = v.rearrange("p (a b) -> p a b", a=shape[0])
        return v

    def _issue(self, i):
        ap, shape = self.reqs[i]
        r = self.res[i % len(self.bufs)]
        self.P.dma(self.q, self._view(i, shape), ap, writes=[r])

    def next(self):
        i = self.taken
        while self.issued < min(len(self.reqs), i + 1 + self.la):
            self._issue(self.issued)
            self.issued += 1
        self.taken += 1
        return self.res[i % len(self.bufs)], self._view(i, self.reqs[i][1])


def layer_norm_rows(P, nc, x_ap, xres, gB, bB, stats, mv, rstd, sres, cres, eps=LN_EPS):
    P.op("dve", lambda e: e.bn_stats(out=stats[:, 0, :], in_=x_ap[:, 0:512]), reads=[xres], writes=[sres])
    P.op("dve", lambda e: e.bn_stats(out=stats[:, 1, :], in_=x_ap[:, 512:1024]), reads=[xres], writes=[sres])
    P.op("dve", lambda e: e.bn_aggr(out=mv[:, :], in_=stats[:, :, :].rearrange("p a b -> p (a b)")),
         reads=[sres], writes=[sres])
    P.op("act", lambda e: e.activation(out=rstd[:, :], in_=mv[:, 1:2], func=AF.Sqrt, bias=float(eps), scale=1.0),
         reads=[sres], writes=[sres])
    P.op("dve", lambda e: e.reciprocal(out=rstd[:, :], in_=rstd[:, :]), reads=[sres], writes=[sres])
    P.op("dve", lambda e: e.tensor_scalar(out=x_ap, in0=x_ap, scalar1=mv[:, 0:1], scalar2=rstd[:, 0:1],
                                          op0=ALU.subtract, op1=ALU.mult), reads=[sres, xres], writes=[xres])
    P.op("dve", lambda e: e.tensor_tensor(out=x_ap, in0=x_ap, in1=gB, op=ALU.mult),
         reads=[xres, cres], writes=[xres])
    P.op("dve", lambda e: e.tensor_tensor(out=x_ap, in0=x_ap, in1=bB, op=ALU.add),
         reads=[xres, cres], writes=[xres])


def decl_merge(nc, pre, moe, ne_override=None, ff_override=None):
    dram = lambda n, s_, dt=F32: nc.dram_tensor(pre + n, list(s_), dt, kind="ExternalInput").ap()
    A = dict(
        w_gate=dram("w_gate", [D, NBR * D]), b_gate=dram("b_gate", [128, NBR * 8]), w_up=dram("w_up", [NBR, BW, D]),
        w_out=dram("w_out", [D, D]), lnB=dram("lnB", [6, 128, D]), ident=dram("ident", [128, 128]))
    if moe:
        FF = ff_override or D_FF_E
        NE = ne_override or N_EXP
        A.update(w1=dram("w1", [NE, D, FF]), w3=dram("w3", [NE, D, FF]), w2=dram("w2", [NE, FF, D]),
                 router_w=dram("router_w", [D, N_EXP]), router_b=dram("router_b", [128, N_EXP]))
    else:
        FF, NE = D_FF, 1
        A.update(w1=dram("w1", [1, D, FF]), w3=dram("w3", [1, D, FF]), w2=dram("w2", [1, FF, D]))
    A["FF"], A["NE"] = FF, NE
    return A


def build_merge(NT, do_ln0, moe, ne_override=None, ff_override=None):
    nc = bass.Bass("TRN2", target_bir_lowering=False)
    A = decl_merge(nc, "", moe, ne_override, ff_override)
    x_d = nc.dram_tensor("x", [NT, D], F32, kind="ExternalInput").ap()
    oT_d = nc.dram_tensor("oT", [NBR * BW, NT], BF16, kind="ExternalInput").ap()
    y_d = nc.dram_tensor("y", [NT, D], F32, kind="ExternalOutput").ap()
    cx0 = Ctx(nc)
    x_rows = lambda t0, n: x_d[t0:t0 + n, :]
    o_rows = lambda br, half, t0, n: oT_d[br * BW + half * 256: br * BW + (half + 1) * 256, t0:t0 + n]
    tags, cx = emit_merge(nc, cx0.P, A, NT, do_ln0, moe, x_rows, o_rows, y_d)
    cx0.P.finish(tags)
    cx.close()
    cx0.close()
    return nc


def emit_merge(nc, P, A, NT, do_ln0, moe, x_rows, o_rows, y_d, after_store=None):
    cx = Ctx(nc, P)
    wg_d, bg_d, wup_d, wout_d, lnB_d, ident_d = (A[k] for k in ("w_gate", "b_gate", "w_up", "w_out", "lnB", "ident"))
    FF, NE = A["FF"], A["NE"]
    w1_d, w3_d, w2_d = A["w1"], A["w3"], A["w2"]
    if moe:
        rw_d, rb_d = A["router_w"], A["router_b"]
    NFS = FF // 128

    TT2 = min(NT, 1024)
    TT1 = min(NT, 512)
    NS2 = TT2 // 128
    NS1 = TT1 // 128
    NH2 = TT2 // TT1
    n_super = NT // TT2

    ident = cx.sb([128, 128], F32, "ident")
    lnB = cx.sb([128, 6, D], F32, "lnB")
    bg = cx.sb([128, NBR * 8], F32, "bg")
    xs = cx.sb([128, NS2, D], F32, "xs")
    xT = cx.sb([128, 8, TT2], BF16, "xT")
    xTf = cx.sb([128, 8, 128], F32, "xTf") if moe else None
    U = cx.sb([128, 28 * TT2], BF16, "U")
    gate_sb = [cx.sb([128, TT1], F32, "gate") for _ in range(3)]
    prod_sb = [cx.sb([128, TT1], F32, "prod") for _ in range(3)]
    silu_sb = [cx.sb([128, 512], BF16, "silu") for _ in range(2)]
    stats = cx.sb([128, 2, 6], F32, "stats")
    mv = cx.sb([128, 2], F32, "mv")
    rstd = cx.sb([128, 1], F32, "rstd")
    if moe:
        rw = cx.sb([128, 8, N_EXP], F32, "rw")
        rb = cx.sb([128, N_EXP], F32, "rb")
        comb = cx.sb([128, NS2, N_EXP], F32, "comb")
        rt = [cx.sb([128, N_EXP], F32, "rt") for _ in range(6)]
        rs = [cx.sb([128, 1], F32, "rs") for _ in range(4)]
    TR = cx.ps([128, 1024], F32, "TR")
    TM = cx.ps([128, 1024], F32, "TM")
    FM = [cx.ps([128, 512], F32, "FM") for _ in range(4)]
    rTR, rTM = Res("TR", True), Res("TM", True)
    rFM = [Res(f"FM{i}", True) for i in range(4)]

    r_ident, r_lnB, r_bg = Res("ident"), Res("lnB"), Res("bg")
    r_xs = [Res(f"xs{i}") for i in range(NS2)]
    r_xT = [Res(f"xT{i}") for i in range(NS2)]
    r_xTf = Res("xTf")
    r_stat = Res("stat")
    r_gate = [Res(f"gate{i}") for i in range(3)]
    r_prod = [Res(f"prod{i}") for i in range(3)]
    r_silu = [Res(f"silu{i}") for i in range(2)]
    r_rt = Res("rt")
    r_comb = [Res(f"comb{i}") for i in range(NS2)]
    r_const = Res("const")

    oT_v = U[:, 0:12 * TT1].rearrange("p (a t) -> p a t", a=12)
    mT_v = U[:, 12 * TT1:20 * TT1].rearrange("p (a t) -> p a t", a=8)
    hT_v = U[:, 0:NFS * TT2].rearrange("p (a t) -> p a t", a=NFS)
    r_oT = [Res(f"oT{i}") for i in range(3)]
    r_mT = [Res(f"mT{i}") for i in range(8)]
    r_hT = [Res(f"hT{i}") for i in range(NFS)]

    P.dma("sp", ident[:, :], ident_d[:, :], writes=[r_ident])
    P.dma("sp", lnB[:, :, :], lnB_d.rearrange("a p d -> p a d"), writes=[r_lnB])
    P.dma("sp", bg[:, :], bg_d[:, :], writes=[r_bg])
    if moe:
        P.dma("sp", rw[:, :, :], rw_d.rearrange("(kc p) e -> p kc e", p=128), writes=[r_const])
        r_rb = Res("rb")
        P.dma("sp", rb[:, :], rb_d[:, :], writes=[r_rb])

    wsA = WStream(cx, 10, 2048, "wA", lookahead=6)
    wsB = WStream(cx, 16 if moe else 12, 2048, "wB", lookahead=8 if moe else 4)
    wg_v = wg_d.rearrange("(kc p) c -> p kc c", p=128)
    wup_v = wup_d.rearrange("b (kc p) c -> b p kc c", p=128)
    w1_v = w1_d.rearrange("e (kc p) c -> e p kc c", p=128)
    w3_v = w3_d.rearrange("e (kc p) c -> e p kc c", p=128)
    for st in range(n_super):
        for h in range(NH2):
            for fs in range(8):
                for br in range(NBR):
                    wsA.plan(wg_v[:, :, br * D + fs * 128: br * D + (fs + 1) * 128], (8, 128))
                    wsA.plan(wup_v[br][:, :, fs * 128:(fs + 1) * 128], (4, 128))
            for kc in range(8):
                wsB.plan(wout_d[kc * 128:(kc + 1) * 128, :], (1024,))
        for e in range(NE):
            for fs in range(NFS):
                wsA.plan(w1_v[e][:, :, fs * 128:(fs + 1) * 128], (8, 128))
                wsA.plan(w3_v[e][:, :, fs * 128:(fs + 1) * 128], (8, 128))
            for kc in range(NFS):
                wsB.plan(w2_d[e, kc * 128:(kc + 1) * 128, :], (1024,))

    out_tags = []

    def transpose_sub(s, want_f32):
        for kc in range(8):
            P.op("pe", lambda e, kc=kc: e.transpose(out=TR[:, kc * 128:(kc + 1) * 128],
                                                    in_=xs[:, s, kc * 128:(kc + 1) * 128], identity=ident[:, :]),
                 reads=[r_xs[s], r_ident], writes=[rTR], inc=(kc == 7))
        if want_f32:
            P.op("dve", lambda e: e.tensor_copy(out=xTf[:, :, :], in_=TR[:, :].rearrange("p (a t) -> p a t", a=8)),
                 reads=[rTR], writes=[r_xTf])
            P.op("act", lambda e: e.copy(out=xT[:, :, s * 128:(s + 1) * 128], in_=xTf[:, :, :]),
                 reads=[r_xTf], writes=[r_xT[s]])
        else:
            P.op("act", lambda e: e.copy(out=xT[:, :, s * 128:(s + 1) * 128],
                                         in_=TR[:, :].rearrange("p (a t) -> p a t", a=8)),
                 reads=[rTR], writes=[r_xT[s]])

    fm_i = [0]

    def next_fm():
        i = fm_i[0] % 4
        fm_i[0] += 1
        return FM[i], rFM[i]

    def ln_sub(s, which):
        layer_norm_rows(P, nc, xs[:, s, :], r_xs[s], lnB[:, 2 * which, :], lnB[:, 2 * which + 1, :],
                        stats, mv, rstd, r_stat, r_lnB)

    for st in range(n_super):
        t0 = st * TT2
        for s in range(NS2):
            P.dma("sp", xs[:, s, :], x_rows(t0 + s * 128, 128), writes=[r_xs[s]])
        for s in range(NS2):
            if do_ln0:
                ln_sub(s, 0)
            transpose_sub(s, False)
        for r in r_oT + r_mT:
            r.absorb(r_hT)
        for h in range(NH2):
            c0 = h * TT1
            for br in range(NBR):
                for half in range(2):
                    P.dma("sp", oT_v[:, br * 4 + half * 2:br * 4 + half * 2 + 2, :],
                          o_rows(br, half, t0 + c0, TT1).rearrange("(kc p) t -> p kc t", p=128),
                          writes=[r_oT[br]])
            for fs in range(8):
                for br in range(NBR):
                    gw_r, gw = wsA.next()
                    uw_r, uw = wsA.next()
                    gp, gp_r = next_fm()
                    for kc in range(8):
                        P.op("pe", lambda e, kc=kc: e.matmul(gp[:, 0:TT1], lhsT=gw[:, kc, :],
                                                             rhs=xT[:, kc, c0:c0 + TT1],
                                                             start=(kc == 0), stop=(kc == 7)),
                             reads=[gw_r] + r_xT[h * NS1:(h + 1) * NS1], writes=[gp_r], inc=(kc == 7))
                    up, up_r = next_fm()
                    for kc in range(4):
                        P.op("pe", lambda e, kc=kc: e.matmul(up[:, 0:TT1], lhsT=uw[:, kc, :],
                                                             rhs=oT_v[:, br * 4 + kc, :],
                                                             start=(kc == 0), stop=(kc == 3)),
                             reads=[uw_r, r_oT[br]], writes=[up_r], inc=(kc == 3))
                    P.op("act", lambda e: e.activation(out=gate_sb[br][:, :], in_=gp[:, 0:TT1], func=AF.Sigmoid,
                                                       bias=bg[:, br * 8 + fs: br * 8 + fs + 1], scale=1.0),
                         reads=[gp_r, r_bg], writes=[r_gate[br]])
                    P.op("dve", lambda e: e.tensor_tensor(out=prod_sb[br][:, :], in0=up[:, 0:TT1],
                                                          in1=gate_sb[br][:, :], op=ALU.mult),
                         reads=[up_r, r_gate[br]], writes=[r_prod[br]])
                P.op("dve", lambda e: e.tensor_tensor(out=prod_sb[0][:, :], in0=prod_sb[0][:, :],
                                                       in1=prod_sb[1][:, :], op=ALU.add),
                     reads=[r_prod[0], r_prod[1]], writes=[r_prod[0]])
                P.op("dve", lambda e: e.tensor_tensor(out=mT_v[:, fs, :], in0=prod_sb[0][:, :],
                                                       in1=prod_sb[2][:, :], op=ALU.add),
                     reads=[r_prod[0], r_prod[2]], writes=[r_mT[fs]])
            wo = [wsB.next() for _ in range(8)]
            for sl in range(NS1):
                s = h * NS1 + sl
                TMa, rTMa = (TM, rTM) if s % 2 == 0 else (TR, rTR)
                for half in range(2):
                    for kc in range(8):
                        P.op("pe", lambda e, kc=kc, half=half, TMa=TMa: e.matmul(
                            TMa[:, half * 512:(half + 1) * 512], lhsT=mT_v[:, kc, sl * 128:(sl + 1) * 128],
                            rhs=wo[kc][1][:, half * 512:(half + 1) * 512], start=(kc == 0), stop=(kc == 7)),
                             reads=[r_mT[kc], wo[kc][0]], writes=[rTMa], inc=(kc == 7 and half == 1))
                P.op("dve", lambda e, TMa=TMa: e.scalar_tensor_tensor(out=xs[:, s, :], in0=xs[:, s, :], scalar=float(ALPHA),
                                                             in1=TMa[:, :], op0=ALU.mult, op1=ALU.add),
                     reads=[rTMa, r_xs[s]], writes=[r_xs[s]])
                ln_sub(s, 1)
        for s in range(NS2):
            transpose_sub(s, moe)
            if moe:
                lg, lg_r = next_fm()
                for kc in range(8):
                    P.op("pe", lambda e, kc=kc: e.matmul(lg[:, 0:N_EXP], lhsT=xTf[:, kc, :], rhs=rw[:, kc, :],
                                                         start=(kc == 0), stop=(kc == 7)),
                         reads=[r_xTf, r_const], writes=[lg_r], inc=(kc == 7))
                L, M1, K1, L2, K2, T6 = rt
                m1, m2, g1, g2 = rs
                ops = [
                    lambda e: e.tensor_tensor(out=L[:, :], in0=lg[:, 0:N_EXP], in1=rb[:, :], op=ALU.add),
                    lambda e: e.tensor_reduce(out=m1[:, :], in_=L[:, :], axis=AX.X, op=ALU.max),
                    lambda e: e.tensor_scalar(out=K1[:, :], in0=L[:, :], scalar1=m1[:, 0:1], scalar2=None,
                                              op0=ALU.is_ge),
                    lambda e: e.scalar_tensor_tensor(out=L2[:, :], in0=K1[:, :], scalar=-1e30, in1=L[:, :],
                                                     op0=ALU.mult, op1=ALU.add),
                    lambda e: e.tensor_reduce(out=m2[:, :], in_=L2[:, :], axis=AX.X, op=ALU.max),
                    lambda e: e.tensor_scalar(out=K2[:, :], in0=L2[:, :], scalar1=m2[:, 0:1], scalar2=None,
                                              op0=ALU.is_ge),
                    lambda e: e.tensor_tensor(out=g2[:, :], in0=m2[:, :], in1=m1[:, :], op=ALU.subtract),
                ]
                for i, f in enumerate(ops):
                    P.op("dve", f, reads=[lg_r, r_rt, r_rb] if i == 0 else [r_rt], writes=[r_rt])
                P.op("act", lambda e: e.activation(out=g2[:, :], in_=g2[:, :], func=AF.Sigmoid),
                     reads=[r_rt], writes=[r_rt])
                ops2 = [
                    lambda e: e.tensor_scalar(out=g1[:, :], in0=g2[:, :], scalar1=-1.0, scalar2=1.0,
                                              op0=ALU.mult, op1=ALU.add),
                    lambda e: e.tensor_scalar(out=K2[:, :], in0=K2[:, :], scalar1=g2[:, 0:1], scalar2=None,
                                              op0=ALU.mult),
                ]
                for f in ops2:
                    P.op("dve", f, reads=[r_rt], writes=[r_rt])
                P.op("dve", lambda e: e.scalar_tensor_tensor(out=comb[:, s, :], in0=K1[:, :], scalar=g1[:, 0:1],
                                                             in1=K2[:, :], op0=ALU.mult, op1=ALU.add),
                     reads=[r_rt], writes=[r_comb[s]])
        for s in range(NS2):
            P.op("act", lambda e: e.mul(out=xs[:, s, :], in_=xs[:, s, :], mul=float(ALPHA)),
                 reads=[r_xs[s]], writes=[r_xs[s]])
        for r in r_hT:
            r.absorb(r_oT + r_mT)
        for e_i in range(NE):
            for fs in range(NFS):
                w1r, w1t = wsA.next()
                w3r, w3t = wsA.next()
                for hh in range(TT2 // 512 if TT2 >= 512 else 1):
                    n = min(512, TT2)
                    c0 = hh * n
                    subs = r_xT[c0 // 128:(c0 + n) // 128]
                    a, a_r = next_fm()
                    for kc in range(8):
                        P.op("pe", lambda e, kc=kc: e.matmul(a[:, 0:n], lhsT=w1t[:, kc, :], rhs=xT[:, kc, c0:c0 + n],
                                                             start=(kc == 0), stop=(kc == 7)),
                             reads=[w1r] + subs, writes=[a_r], inc=(kc == 7))
                    b, b_r = next_fm()
                    for kc in range(8):
                        P.op("pe", lambda e, kc=kc: e.matmul(b[:, 0:n], lhsT=w3t[:, kc, :], rhs=xT[:, kc, c0:c0 + n],
                                                             start=(kc == 0), stop=(kc == 7)),
                             reads=[w3r] + subs, writes=[b_r], inc=(kc == 7))
                    si = (fs * 2 + hh) % 2
                    P.op("act", lambda e: e.activation(out=silu_sb[si][:, 0:n], in_=a[:, 0:n], func=AF.Silu),
                         reads=[a_r], writes=[r_silu[si]])
                    P.op("dve", lambda e: e.tensor_tensor(out=hT_v[:, fs, c0:c0 + n], in0=b[:, 0:n],
                                                          in1=silu_sb[si][:, 0:n], op=ALU.mult),
                         reads=[b_r, r_silu[si]], writes=[r_hT[fs]])
            GK = 8
            for g0 in range(0, NFS, GK):
                gks = list(range(g0, min(NFS, g0 + GK)))
                wts = [wsB.next() for _ in gks]
                for s in range(NS2):
                    TMa, rTMa = (TM, rTM) if s % 2 == 0 else (TR, rTR)
                    for half in range(2):
                        for j, kc in enumerate(gks):
                            P.op("pe", lambda e, j=j, kc=kc, half=half, TMa=TMa: e.matmul(
                                TMa[:, half * 512:(half + 1) * 512], lhsT=hT_v[:, kc, s * 128:(s + 1) * 128],
                                rhs=wts[j][1][:, half * 512:(half + 1) * 512],
                                start=(j == 0), stop=(j == len(gks) - 1)),
                                 reads=[r_hT[kc], wts[j][0]], writes=[rTMa],
                                 inc=(j == len(gks) - 1 and half == 1))
                    if moe:
                        P.op("dve", lambda e, TMa=TMa: e.scalar_tensor_tensor(
                            out=xs[:, s, :], in0=TMa[:, :], scalar=comb[:, s, e_i:e_i + 1], in1=xs[:, s, :],
                            op0=ALU.mult, op1=ALU.add), reads=[rTMa, r_xs[s], r_comb[s]], writes=[r_xs[s]])
                    else:
                        P.op("dve", lambda e, TMa=TMa: e.tensor_tensor(out=xs[:, s, :], in0=TMa[:, :], in1=xs[:, s, :],
                                                              op=ALU.add),
                             reads=[rTMa, r_xs[s]], writes=[r_xs[s]])
        for s in range(NS2):
            ln_sub(s, 2)
            out_tags.append(P.dma("sp", y_d[t0 + s * 128: t0 + (s + 1) * 128, :], xs[:, s, :], reads=[r_xs[s]]))
            if after_store is not None:
                after_store(t0 + s * 128, out_tags[-1])
    return out_tags, cx


RWKV_COLS = 3 * 512 + 64 + 64 + 128
CONV_COLS = 3 * 512
FOX_COLS = 3 * 512 + 8
GATE_OFF = RWKV_COLS + CONV_COLS + FOX_COLS
IDENT = np.eye(128, dtype=np.float32)


def _bc(v):
    return np.ascontiguousarray(np.broadcast_to(np.asarray(v, np.float32)[None, :], (128, v.shape[0])))


def merge_inputs(p, l, moe):
    w_in = p["w_in"][l]
    common = {
        "ident": IDENT,
        "w_gate": np.ascontiguousarray(w_in[:, GATE_OFF:GATE_OFF + NBR * D]),
        "b_gate": np.ascontiguousarray(p["b_gate"][l].reshape(NBR, 8, 128).transpose(2, 0, 1).reshape(128, NBR * 8)),
        "w_up": np.ascontiguousarray(np.stack([p["w_up_rwkv"][l], p["w_up_conv"][l], p["w_up_attn"][l]], 0)),
        "w_out": np.ascontiguousarray(p["w_out"][l]),
        "lnB": np.ascontiguousarray(np.stack([_bc(p["ln0_g"]), _bc(p["ln0_b"]), _bc(p["ln1_g"][l]), _bc(p["ln1_b"][l]),
                                              _bc(p["ln2_g"][l]), _bc(p["ln2_b"][l])], 0)),
    }
    i = l // 2
    if moe:
        common.update({
            "w1": p["moe_w1"][i], "w3": p["moe_w3"][i], "w2": p["moe_w2"][i],
            "router_w": np.ascontiguousarray(p["router_w"][i]), "router_b": _bc(p["router_b"][i]),
        })
    else:
        common.update({"w1": p["ffn_w1"][i][None], "w3": p["ffn_w3"][i][None], "w2": p["ffn_w2"][i][None]})
    return common


def run_merge(x_flat, oT, p, l, do_ln0, moe, n_cores=8):
    ntok = x_flat.shape[0]
    NT = ntok // n_cores
    nc = build_merge(NT, do_ln0, moe)
    common = merge_inputs(p, l, moe)
    in_maps = []
    for c in range(n_cores):
        m = dict(common)
        m["x"] = np.ascontiguousarray(x_flat[c * NT:(c + 1) * NT])
        m["oT"] = np.ascontiguousarray(oT[:, c * NT:(c + 1) * NT])
        in_maps.append(m)
    res = run_bass_kernel_spmd(nc, in_maps, core_ids=list(range(n_cores)))
    return np.concatenate([r["y"] for r in res.results], 0)


NEG = -30000.0


def decl_fox(nc, pre):
    dram = lambda n, s_, dt=F32: nc.dram_tensor(pre + n, list(s_), dt, kind="ExternalInput").ap()
    return dict(wc=dram("wc", [D, 768]), wq=dram("wq", [D, 256]), wk=dram("wk", [D, 256]), wv=dram("wv", [D, 256]),
                wf=dram("wf", [D, 4]), bf=dram("bf", [4, 1]), cw=dram("cw", [128, 6]), lnB=dram("lnB", [2, 128, D]),
                ident=dram("ident", [128, 128]), mask=dram("mask", [128, 128]))


def build_fox(T, do_ln0):
    nc = bass.Bass("TRN2", target_bir_lowering=False)
    A = decl_fox(nc, "")
    x_d = nc.dram_tensor("x", [T, D], F32, kind="ExternalInput").ap()
    obT_d = nc.dram_tensor("obT", [256, T], BF16, kind="ExternalOutput").ap()
    ocT_d = nc.dram_tensor("ocT", [256, T], BF16, kind="ExternalOutput").ap()
    cx0 = Ctx(nc)
    TTf = min(T, 512)
    tags, cx = emit_fox(nc, cx0.P, A, T, do_ln0, lambda t0, n: x_d[t0:t0 + n, :],
                        lambda ti: obT_d[:, ti * TTf:(ti + 1) * TTf], lambda ti: ocT_d[:, ti * TTf:(ti + 1) * TTf])
    cx0.P.finish(tags)
    cx.close()
    cx0.close()
    return nc


def emit_fox(nc, P, A, T, do_ln0, xsrc, ob_dst, oc_dst, after_store=None):
    cx = Ctx(nc, P)
    wc_d, wq_d, wk_d, wv_d, wf_d, bf_d, cw_d, lnB_d, ident_d, mask_d = (
        A[k] for k in ("wc", "wq", "wk", "wv", "wf", "bf", "cw", "lnB", "ident", "mask"))

    TT = min(T, 512)
    NS = TT // 128
    NSB = T // TT
    NB = T // 128
    H = 4

    ident = cx.sb([128, 128], F32, "ident")
    identb = cx.sb([128, 128], BF16, "identb")
    maskf = cx.sb([128, 128], F32, "maskf")
    maskb = cx.sb([128, 128], BF16, "maskb")
    lnB = cx.sb([128, 2, D], F32, "lnB")
    cw = cx.sb([128, 6], F32, "cw")
    bf = cx.sb([4, 1], F32, "bf")
    nbf = cx.sb([4, 1], F32, "nbf")
    wc = cx.sb([128, 8, 768], BF16, "wc")
    wq = cx.sb([128, 8, 256], BF16, "wq")
    wk = cx.sb([128, 8, 256], BF16, "wk")
    wv = cx.sb([128, 8, 256], BF16, "wv")
    wf = cx.sb([128, 8, 4], BF16, "wf")
    xs = cx.sb([128, 2, D], F32, "xs")
    xT = cx.sb([128, 8, TT], BF16, "xT")
    Kaug = [cx.sb([70, T], BF16, "Kaug") for _ in range(H)]
    Vaug = cx.sb([128, NB, H, 65], BF16, "Vaug")
    Qaug = [[cx.sb([70, TT], BF16, "Qaug") for _ in range(H)] for _ in range(2)]
    PT = [cx.sb([128, TT], BF16, "PT") for _ in range(3)]
    cvT = [cx.sb([128, 2, TT], F32, "cvT") for _ in range(2)]
    uT = cx.sb([128, 2, 2 + TT], F32, "uT")
    yT = cx.sb([128, 2, TT], F32, "yT")
    y2T = cx.sb([128, 2, TT], F32, "y2T")
    obT = cx.sb([128, 2, TT], BF16, "obT")
    ones4 = cx.sb([4, TT], F32, "ones4")
    lf = cx.sb([4, TT], F32, "lf")
    cc = cx.sb([4, TT], F32, "cc")
    cprev = cx.sb([4, 1], F32, "cprev")
    csp = cx.sb([4, 3, TT], BF16, "csp")
    csn = cx.sb([4, 3, TT], BF16, "csn")
    ctmp = cx.sb([4, TT], F32, "ctmp")
    ctmp2 = lf
    oc = [cx.sb([128, NS, 256], F32, "oc") for _ in range(2)]
    ocT = [cx.sb([128, 2, TT], BF16, "ocT") for _ in range(2)]
    r_ocT = [Res("ocT0"), Res("ocT1")]
    rinv = cx.sb([128, 4], F32, "rinv")
    stats = cx.sb([128, 2, 6], F32, "stats")
    mv = cx.sb([128, 2], F32, "mv")
    rstd = cx.sb([128, 1], F32, "rstd")

    TR = cx.ps([128, 1024], F32, "TR")
    FM = [cx.ps([128, 512], F32, "FM") for _ in range(2)]
    ST = [cx.ps([128, 512], F32, "ST") for _ in range(2)]
    OA = [cx.ps([128, 512], F32, "OA") for _ in range(2)]
    rTR = Res("TR", True)
    rFM = [Res("FM0", True), Res("FM1", True)]
    rST = [Res("ST0", True), Res("ST1", True)]
    rOA = [Res("OA0", True), Res("OA1", True)]

    r_c = Res("consts")
    r_w = Res("weights")
    r_xs = [Res(f"xs{i}") for i in range(2)]
    r_xT = Res("xT")
    r_K = [[Res(f"K{h}_{i}") for i in range(NSB)] for h in range(H)]
    r_V = [Res(f"V{i}") for i in range(NSB)]
    r_Q = [[Res(f"Q{b}{h}") for h in range(H)] for b in range(2)]
    r_PT = [Res(f"PT{i}") for i in range(3)]
    r_cv = [Res("cvb"), Res("cvc")]
    r_u, r_y, r_ob, r_y2 = Res("u"), Res("y"), Res("ob"), Res("y2")
    r_f = Res("f")
    r_cs = Res("cs")
    r_oc = [Res("oc0"), Res("oc1")]
    r_rinv = Res("rinv")
    r_stat = Res("stat")

    P.dma("sp", ident[:, :], ident_d[:, :], writes=[r_c])
    P.dma("sp", maskf[:, :], mask_d[:, :], writes=[r_c])
    P.dma("sp", lnB[:, :, :], lnB_d.rearrange("a p d -> p a d"), writes=[r_c])
    P.dma("sp", cw[:, :], cw_d[:, :], writes=[r_c])
    P.dma("sp", bf[:, :], bf_d[:, :], writes=[r_c])
    for wt, wd_, n in ((wc, wc_d, 768), (wq, wq_d, 256), (wk, wk_d, 256), (wv, wv_d, 256), (wf, wf_d, 4)):
        P.dma("pool", wt[:, :, :], wd_.rearrange("(kc p) c -> p kc c", p=128), writes=[r_w])
    r_c2 = Res("consts2")
    P.op("dve", lambda e: e.tensor_copy(out=identb[:, :], in_=ident[:, :]), reads=[r_c], writes=[r_c2])
    P.op("dve", lambda e: e.tensor_copy(out=maskb[:, :], in_=maskf[:, :]), reads=[r_c], writes=[r_c2])
    P.op("dve", lambda e: e.tensor_scalar(out=nbf[:, :], in0=bf[:, :], scalar1=-1.0, scalar2=None, op0=ALU.mult),
         reads=[r_c], writes=[r_c2])
    P.op("dve", lambda e: e.memset(ones4[:, :], 1.0), writes=[r_c2])
    P.op("dve", lambda e: e.memset(cprev[:, :], 0.0), writes=[r_cs])
    P.op("dve", lambda e: e.memset(uT[:, :, :], 0.0), writes=[r_u])
    P.op("pool", lambda e: e.memset(Vaug[:, :, :, :], 1.0), writes=r_V)
    for h in range(H):
        P.op("pool", lambda e, h=h: e.memset(Kaug[h][64:70, :], 1.0), writes=r_K[h])
        for b in range(2):
            P.op("pool", lambda e, h=h, b=b: e.memset(Qaug[b][h][64:70, :], 1.0), writes=[r_Q[b][h]])

    fm_i = [0]

    def next_fm():
        i = fm_i[0] % 2
        fm_i[0] += 1
        return FM[i], rFM[i]

    out_tags = []

    def proj(sb_i):
        t0 = sb_i * TT
        qb = sb_i % 2
        for s in range(NS):
            yield
            xb_ = s % 2
            P.dma("sp", xs[:, xb_, :], xsrc(t0 + s * 128, 128), writes=[r_xs[xb_]])
            if do_ln0:
                layer_norm_rows(P, nc, xs[:, xb_, :], r_xs[xb_], lnB[:, 0, :], lnB[:, 1, :], stats, mv, rstd, r_stat, r_c)
            for kc in range(8):
                P.op("pe", lambda e, kc=kc: e.transpose(out=TR[:, kc * 128:(kc + 1) * 128],
                                                        in_=xs[:, xb_, kc * 128:(kc + 1) * 128], identity=ident[:, :]),
                     reads=[r_xs[xb_], r_c], writes=[rTR], inc=(kc == 7))
            P.op("dve", lambda e: e.tensor_copy(out=xT[:, :, s * 128:(s + 1) * 128],
                                                in_=TR[:, :].rearrange("p (a t) -> p a t", a=8)),
                 reads=[rTR], writes=[r_xT])
        yield
        for grp in range(3):
            for hf in range(2):
                yield
                ps, ps_r = next_fm()
                c0 = grp * 256 + hf * 128
                for kc in range(8):
                    P.op("pe", lambda e, kc=kc: e.matmul(ps[:, 0:TT], lhsT=wc[:, kc, c0:c0 + 128], rhs=xT[:, kc, :],
                                                         start=(kc == 0), stop=(kc == 7)),
                         reads=[r_w, r_xT], writes=[ps_r], inc=(kc == 7))
                if grp < 2:
                    P.op("dve", lambda e: e.tensor_copy(out=cvT[grp][:, hf, :], in_=ps[:, 0:TT]),
                         reads=[ps_r], writes=[r_cv[grp]])
                else:
                    P.op("dve", lambda e: e.tensor_tensor(out=uT[:, hf, 2:2 + TT], in0=ps[:, 0:TT],
                                                          in1=cvT[1][:, hf, :], op=ALU.mult),
                         reads=[ps_r, r_cv[1]], writes=[r_u])
        yield
        for hf in range(2):
            P.op("dve", lambda e: e.tensor_scalar(out=yT[:, hf, :], in0=uT[:, hf, 0:TT],
                                                  scalar1=cw[:, hf * 3:hf * 3 + 1], scalar2=None, op0=ALU.mult),
                 reads=[r_u, r_c], writes=[r_y])
            for tap in (1, 2):
                P.op("dve", lambda e, tap=tap: e.scalar_tensor_tensor(out=yT[:, hf, :], in0=uT[:, hf, tap:tap + TT],
                                                                      scalar=cw[:, hf * 3 + tap:hf * 3 + tap + 1],
                                                                      in1=yT[:, hf, :], op0=ALU.mult, op1=ALU.add),
                     reads=[r_u, r_y, r_c], writes=[r_y])
            P.op("pool", lambda e: e.tensor_tensor(out=obT[:, hf, :], in0=yT[:, hf, :], in1=cvT[0][:, hf, :],
                                                   op=ALU.mult),
                 reads=[r_y, r_cv[0]], writes=[r_ob])
        P.op("pool", lambda e: e.tensor_copy(out=uT[:, :, 0:2], in_=uT[:, :, TT:TT + 2]), reads=[r_u], writes=[r_u])
        out_tags.append(P.dma("sp", ob_dst(sb_i).rearrange("(hf p) t -> p hf t", p=128), obT[:, :, :],
                              reads=[r_ob]))
        if after_store is not None:
            after_store(sb_i, out_tags[-1])
        yield
        ps, ps_r = next_fm()
        for kc in range(8):
            P.op("pe", lambda e, kc=kc: e.matmul(ps[0:4, 0:TT], lhsT=wf[:, kc, :], rhs=xT[:, kc, :],
                                                 start=(kc == 0), stop=(kc == 7)),
                 reads=[r_w, r_xT], writes=[ps_r], inc=(kc == 7))
        P.op("act", lambda e: e.activation(out=lf[:, :], in_=ps[0:4, 0:TT], func=AF.Exp, bias=nbf[:, 0:1], scale=-1.0),
             reads=[ps_r, r_c2], writes=[r_f])
        P.op("act", lambda e: e.activation(out=lf[:, :], in_=lf[:, :], func=AF.Ln, bias=1.0, scale=1.0),
             reads=[r_f], writes=[r_f])
        P.op("dve", lambda e: e.tensor_scalar(out=lf[:, :], in0=lf[:, :], scalar1=-1.0, scalar2=None, op0=ALU.mult),
             reads=[r_f], writes=[r_f])
        P.op("dve", lambda e: e.tensor_tensor_scan(out=cc[:, :], data0=ones4[:, :], data1=lf[:, :],
                                                   initial=cprev[:, 0:1], op0=ALU.mult, op1=ALU.add),
             reads=[r_f, r_cs, r_c2], writes=[r_cs])
        P.op("dve", lambda e: e.tensor_copy(out=cprev[:, :], in_=cc[:, TT - 1:TT]), reads=[r_cs], writes=[r_cs])
        P.op("dve", lambda e: e.tensor_copy(out=csp[:, 0, :], in_=cc[:, :]), reads=[r_cs], writes=[r_cs])
        P.op("dve", lambda e: e.tensor_tensor(out=ctmp[:, :], in0=cc[:, :], in1=csp[:, 0, :], op=ALU.subtract),
             reads=[r_cs], writes=[r_cs])
        P.op("dve", lambda e: e.tensor_copy(out=csp[:, 1, :], in_=ctmp[:, :]), reads=[r_cs], writes=[r_cs])
        P.op("dve", lambda e: e.tensor_tensor(out=ctmp2[:, :], in0=ctmp[:, :], in1=csp[:, 1, :], op=ALU.subtract),
             reads=[r_cs], writes=[r_cs])
        P.op("dve", lambda e: e.tensor_copy(out=csp[:, 2, :], in_=ctmp2[:, :]), reads=[r_cs], writes=[r_cs])
        P.op("dve", lambda e: e.tensor_scalar(out=csn[:, :, :], in0=csp[:, :, :], scalar1=-1.0, scalar2=None,
                                              op0=ALU.mult), reads=[r_cs], writes=[r_cs])
        yield
        for h in range(H):
            yield
            ps, ps_r = next_fm()
            for kc in range(8):
                P.op("pe", lambda e, kc=kc: e.matmul(ps[0:64, 0:TT], lhsT=wq[:, kc, h * 64:(h + 1) * 64], rhs=xT[:, kc, :],
                                                     start=(kc == 0), stop=(kc == 7)),
                     reads=[r_w, r_xT], writes=[ps_r], inc=(kc == 7))
            P.op("act", lambda e: e.mul(out=Qaug[qb][h][0:64, :], in_=ps[0:64, 0:TT], mul=0.125),
                 reads=[ps_r], writes=[r_Q[qb][h]])
            for jj in range(3):
                P.dma("sp", Qaug[qb][h][64 + jj:65 + jj, :], csp[h:h + 1, jj, :], reads=[r_cs], writes=[r_Q[qb][h]],
                      sem_res=r_Q[qb][h])
            ps, ps_r = next_fm()
            for kc in range(8):
                P.op("pe", lambda e, kc=kc: e.matmul(ps[0:64, 0:TT], lhsT=wk[:, kc, h * 64:(h + 1) * 64], rhs=xT[:, kc, :],
                                                     start=(kc == 0), stop=(kc == 7)),
                     reads=[r_w, r_xT], writes=[ps_r], inc=(kc == 7))
            P.op("dve", lambda e: e.tensor_copy(out=Kaug[h][0:64, t0:t0 + TT], in_=ps[0:64, 0:TT]),
                 reads=[ps_r], writes=[r_K[h][sb_i]])
            for jj in range(3):
                P.dma("sp", Kaug[h][67 + jj:68 + jj, t0:t0 + TT], csn[h:h + 1, jj, :], reads=[r_cs],
                      writes=[r_K[h][sb_i]], sem_res=r_K[h][sb_i])
        for s in range(NS):
            yield
            ps, ps_r = next_fm()
            for kc in range(8):
                P.op("pe", lambda e, kc=kc: e.matmul(ps[:, 0:256], lhsT=xT[:, kc, s * 128:(s + 1) * 128], rhs=wv[:, kc, :],
                                                     start=(kc == 0), stop=(kc == 7)),
                     reads=[r_w, r_xT], writes=[ps_r], inc=(kc == 7))
            P.op("dve", lambda e: e.tensor_copy(out=Vaug[:, sb_i * NS + s, :, 0:64],
                                                in_=ps[:, 0:256].rearrange("p (h d) -> p h d", h=H)),
                 reads=[ps_r], writes=[r_V[sb_i]])
        yield
    gen = proj(0)
    for _ in gen:
        pass
    for sb_i in range(NSB):
        t0 = sb_i * TT
        qb = sb_i % 2
        gen = proj(sb_i + 1) if sb_i + 1 < NSB else iter(())
        items = []
        for h in range(H):
            nkb = sb_i * NS + NS
            for j in range(nkb):
                items.append((h, j))
        ob_i = sb_i % 2

        def emit_S(idx):
            h, j = items[idx]
            st, st_r = ST[idx % 2], rST[idx % 2]
            dj = j - sb_i * NS
            qlo = max(0, dj) * 128
            N = TT - qlo
            if dj >= 0:
                P.op("pe", lambda e: e.matmul(st[:, 0:128], lhsT=Kaug[h][0:70, j * 128:(j + 1) * 128],
                                              rhs=Qaug[qb][h][0:70, qlo:qlo + 128], start=True, stop=False),
                     reads=[r_K[h][j // NS], r_Q[qb][h]], writes=[st_r], inc=False)
                P.op("pe", lambda e: e.matmul(st[:, 0:128], lhsT=identb[:, :], rhs=maskb[:, :], start=False, stop=True),
                     reads=[r_c2], writes=[st_r], inc=(N == 128))
                if N > 128:
                    P.op("pe", lambda e: e.matmul(st[:, 128:N], lhsT=Kaug[h][0:70, j * 128:(j + 1) * 128],
                                                  rhs=Qaug[qb][h][0:70, qlo + 128:TT], start=True, stop=True),
                         reads=[r_K[h][j // NS], r_Q[qb][h]], writes=[st_r])
            else:
                P.op("pe", lambda e: e.matmul(st[:, 0:N], lhsT=Kaug[h][0:70, j * 128:(j + 1) * 128],
                                              rhs=Qaug[qb][h][0:70, qlo:TT], start=True, stop=True),
                     reads=[r_K[h][j // NS], r_Q[qb][h]], writes=[st_r])

        def emit_rest(idx):
            h, j = items[idx]
            st, st_r = ST[idx % 2], rST[idx % 2]
            pt, pt_r = PT[idx % 3], r_PT[idx % 3]
            dj = j - sb_i * NS
            qlo = max(0, dj) * 128
            N = TT - qlo
            nkb = sb_i * NS + NS
            oa, oa_r = OA[h % 2], rOA[h % 2]
            P.op("act", lambda e: e.activation(out=pt[:, 0:N], in_=st[:, 0:N], func=AF.Exp), reads=[st_r], writes=[pt_r])
            for qq in range(qlo // 128, NS):
                last_j = sb_i * NS + qq
                P.op("pe", lambda e, qq=qq: e.matmul(oa[:, qq * 128:qq * 128 + 65],
                                                     lhsT=pt[:, qq * 128 - qlo:qq * 128 - qlo + 128],
                                                     rhs=Vaug[:, j, h, :], start=(j == 0 and qq == 0),
                                                     stop=(j == last_j), skip_group_check=True),
                     reads=[pt_r, r_V[j // NS]], writes=[oa_r], inc=(qq == NS - 1))
            if j == nkb - 1:
                oav = oa[:, :].rearrange("p (q c) -> p q c", q=4)
                P.op("dve", lambda e: e.reciprocal(out=rinv[:, 0:NS], in_=oav[:, 0:NS, 64]), reads=[oa_r], writes=[r_rinv])
                for qq in range(NS):
                    P.op("dve", lambda e, qq=qq: e.tensor_scalar(out=oc[ob_i][:, qq, h * 64:(h + 1) * 64],
                                                                 in0=oa[:, qq * 128:qq * 128 + 64],
                                                                 scalar1=rinv[:, qq:qq + 1], scalar2=None, op0=ALU.mult),
                         reads=[oa_r, r_rinv], writes=[r_oc[ob_i]])

        emit_S(0)
        nsteps = max(1, -(-48 // len(items)))
        for idx in range(len(items)):
            if idx + 1 < len(items):
                emit_S(idx + 1)
            emit_rest(idx)
            for _ in range(nsteps):
                next(gen, None)
        for _ in gen:
            pass
        for hf in range(2):
            for qq in range(NS):
                P.op("pe", lambda e, hf=hf, qq=qq: e.transpose(out=TR[:, hf * 512 + qq * 128: hf * 512 + (qq + 1) * 128],
                                                               in_=oc[ob_i][:, qq, hf * 128:(hf + 1) * 128],
                                                               identity=ident[:, :]),
                     reads=[r_oc[ob_i], r_c], writes=[rTR], inc=(hf == 1 and qq == NS - 1))
        P.op("act", lambda e: e.copy(out=ocT[ob_i][:, :, :],
                                     in_=TR[:, :].rearrange("p (a t) -> p a t", a=2)[:, :, 0:TT]),
             reads=[rTR], writes=[r_ocT[ob_i]])
        out_tags.append(P.dma("sp", oc_dst(sb_i).rearrange("(hf p) t -> p hf t", p=128), ocT[ob_i][:, :, :],
                              reads=[r_ocT[ob_i]]))
        if after_store is not None:
            after_store(sb_i, out_tags[-1])
    return out_tags, cx


MASK = np.where(np.arange(128)[None, :] >= np.arange(128)[:, None], 0.0, NEG).astype(np.float32)


def fox_inputs(p, l, g):
    w_in = p["w_in"][l]
    c_off = RWKV_COLS
    f_off = RWKV_COLS + CONV_COLS
    if True:
        cs = slice(g * 256, (g + 1) * 256)
        wcv = np.concatenate([w_in[:, c_off + k * 512 + g * 256: c_off + k * 512 + (g + 1) * 256] for k in range(3)], 1)
        m = {
            "wc": np.ascontiguousarray(wcv),
            "wq": np.ascontiguousarray(w_in[:, f_off + g * 256: f_off + (g + 1) * 256]),
            "wk": np.ascontiguousarray(w_in[:, f_off + 512 + g * 256: f_off + 512 + (g + 1) * 256]),
            "wv": np.ascontiguousarray(w_in[:, f_off + 1024 + g * 256: f_off + 1024 + (g + 1) * 256]),
            "wf": np.ascontiguousarray(w_in[:, f_off + 1536 + g * 4: f_off + 1536 + (g + 1) * 4]),
            "bf": np.ascontiguousarray(p["b_forget"][l][g * 4:(g + 1) * 4].reshape(4, 1)),
            "cw": np.ascontiguousarray(p["conv_w"][l][:, cs].reshape(3, 2, 128).transpose(2, 1, 0).reshape(128, 6)),
            "lnB": np.ascontiguousarray(np.stack([_bc(p["ln0_g"]), _bc(p["ln0_b"])], 0)),
            "ident": IDENT, "mask": MASK,
        }
    return m


def run_fox(xb, p, l, do_ln0, n_cores=8):
    B, T, _ = xb.shape
    nc = build_fox(T, do_ln0)
    in_maps = []
    for c in range(n_cores):
        b, g = c // 2, c % 2
        m = fox_inputs(p, l, g)
        m["x"] = np.ascontiguousarray(xb[b])
        in_maps.append(m)
    res = run_bass_kernel_spmd(nc, in_maps, core_ids=list(range(n_cores)))
    obT = np.stack([np.concatenate([res.results[2 * b + g]["obT"] for g in range(2)], 0) for b in range(B)], 0)
    ocT = np.stack([np.concatenate([res.results[2 * b + g]["ocT"] for g in range(2)], 0) for b in range(B)], 0)
    return obT, ocT


RWKV_LN_EPS = 64e-5
RWKV_WINDOW = 16
NPJ = 2
DECAY_SCALE = -0.6065306597126334


class Sched:
    def __init__(self):
        self.tasks = []
        self.done = set()

    def add(self, name, gen, deps=()):
        self.tasks.append([name, gen, set(deps)])

    def run(self, window=10):
        active = []
        pending = list(self.tasks)
        while pending or active:
            i = 0
            while i < len(pending) and len(active) < window:
                t = pending[i]
                if t[2] <= self.done:
                    active.append(t)
                    pending.pop(i)
                else:
                    i += 1
            assert active, ("deadlock", [t[0] for t in pending[:5]])
            for t in list(active):
                try:
                    next(t[1])
                except StopIteration:
                    self.done.add(t[0])
                    active.remove(t)


def decl_rwkv(nc, pre):
    dram = lambda n, s_, dt=F32: nc.dram_tensor(pre + n, list(s_), dt, kind="ExternalInput").ap()
    return dict(wall=dram("wall", [D, 1024]), muB=dram("muB", [128, 1024]), chv=dram("chv", [128, 2, 8]),
                w2ia=dram("w2ia", [128, 256]), w2g=dram("w2g", [128, 256]), lnxB=dram("lnxB", [2, 128, 256]),
                lnB=dram("lnB", [2, 128, D]), ident=dram("ident", [128, 128]), mask4=dram("mask4", [128, 512]),
                maskL=dram("maskL", [128, 128]), bones=dram("bones", [128, 128]), sel=dram("sel", [128, 2]),
                smask=dram("smask", [128, 512]))


def build_rwkv(T, do_ln0):
    nc = bass.Bass("TRN2", target_bir_lowering=False)
    A = decl_rwkv(nc, "")
    x_d = nc.dram_tensor("x", [T, D], F32, kind="ExternalInput").ap()
    oaT_d = nc.dram_tensor("oaT", [256, T], BF16, kind="ExternalOutput").ap()
    cx0 = Ctx(nc)
    TTr = min(T, 512)
    tags, cx = emit_rwkv(nc, cx0.P, A, T, do_ln0, lambda t0, n: x_d[t0:t0 + n, :],
                         lambda ti: oaT_d[:, ti * TTr:(ti + 1) * TTr])
    cx0.P.finish(tags)
    cx.close()
    cx0.close()
    return nc


def emit_rwkv(nc, P, A, T, do_ln0, xsrc, oa_dst, after_store=None):
    cx = Ctx(nc, P)
    (wall_d, muB_d, chv_d, w2ia_d, w2g_d, lnxB_d, lnB_d, ident_d, mask4_d, maskL_d, bones_d, sel_d, smask_d) = (
        A[k] for k in ("wall", "muB", "chv", "w2ia", "w2g", "lnxB", "lnB", "ident", "mask4", "maskL", "bones", "sel",
                       "smask"))

    TT = min(T, 512)
    NCH = TT // 128
    NTI = T // TT

    ident = cx.sb([128, 128], F32, "ident")
    identb = cx.sb([128, 128], BF16, "identb")
    mask4 = cx.sb([128, 512], F32, "mask4")
    maskL = cx.sb([128, 128], F32, "maskL")
    bones = cx.sb([128, 128], F32, "bones")
    self_ = cx.sb([128, 2], F32, "self")
    selb = cx.sb([128, 2], BF16, "selb")
    smask = cx.sb([128, 512], F32, "smask")
    lnB = cx.sb([128, 2, D], F32, "lnB") if do_ln0 else None
    lnxB = cx.sb([128, 2, 256], F32, "lnxB")
    chv = cx.sb([128, 2, 8], F32, "chv")
    omka = cx.sb([128, 2], F32, "omka")
    muB = cx.sb([128, 1024], F32, "muB")
    wtmp = cx.sb([128, 512], F32, "wtmp")
    wtmp2 = cx.sb([128, 512], F32, "wtmp2")
    W0 = cx.sb([128, 8, 1024], BF16, "W0")
    W1 = cx.sb([128, 8, 1024], BF16, "W1")
    w2iaf = cx.sb([128, 256], F32, "w2iaf")
    w2ia = cx.sb([128, 256], BF16, "w2ia")
    w2gf = cx.sb([128, 256], F32, "w2gf")
    w2g = cx.sb([128, 256], BF16, "w2g")
    r_c = Res("consts")
    r_c2 = Res("consts2")
    r_wt = Res("wtmp")
    r_W = Res("W")

    for t_, d_ in ((ident, ident_d), (mask4, mask4_d), (maskL, maskL_d), (bones, bones_d), (self_, sel_d),
                   (smask, smask_d), (muB, muB_d), (w2iaf, w2ia_d), (w2gf, w2g_d)):
        P.dma("sp", t_[:, :], d_[:, :], writes=[r_c])
    if do_ln0:
        P.dma("sp", lnB[:, :, :], lnB_d.rearrange("a p d -> p a d"), writes=[r_c])
    P.dma("sp", lnxB[:, :, :], lnxB_d.rearrange("a p d -> p a d"), writes=[r_c])
    P.dma("sp", chv[:, :, :], chv_d[:, :, :], writes=[r_c])
    P.op("dve", lambda e: e.tensor_copy(out=identb[:, :], in_=ident[:, :]), reads=[r_c], writes=[r_c2])
    P.op("dve", lambda e: e.tensor_copy(out=selb[:, :], in_=self_[:, :]), reads=[r_c], writes=[r_c2])
    P.op("dve", lambda e: e.tensor_copy(out=w2ia[:, :], in_=w2iaf[:, :]), reads=[r_c], writes=[r_c2])
    P.op("dve", lambda e: e.tensor_copy(out=w2g[:, :], in_=w2gf[:, :]), reads=[r_c], writes=[r_c2])
    P.op("dve", lambda e: e.tensor_scalar(out=omka[:, :], in0=chv[:, :, 3], scalar1=-1.0, scalar2=1.0,
                                          op0=ALU.mult, op1=ALU.add), reads=[r_c], writes=[r_c2])
    for kc in range(8):
        for hf in range(2):
            fs_ = slice(hf * 512, (hf + 1) * 512)
            P.dma("sp", wtmp[:, :], wall_d[kc * 128:(kc + 1) * 128, fs_], writes=[r_wt])
            P.op("dve", lambda e: e.tensor_tensor(out=wtmp2[:, :], in0=wtmp[:, :], in1=muB[:, fs_], op=ALU.mult),
                 reads=[r_wt, r_c], writes=[r_W])
            P.op("dve", lambda e, kc=kc: e.tensor_copy(out=W1[:, kc, fs_], in_=wtmp2[:, :]), reads=[r_W], writes=[r_W])
            P.op("dve", lambda e, kc=kc: e.tensor_tensor(out=W0[:, kc, fs_], in0=wtmp[:, :], in1=wtmp2[:, :],
                                                         op=ALU.subtract),
                 reads=[r_wt, r_W], writes=[r_W])

    xs = cx.sb([128, 2, D], F32, "xs")
    xT = cx.sb([128, 8, 1 + TT], BF16, "xT")
    stats = cx.sb([128, 2, 6], F32, "stats")
    mv = cx.sb([128, 2], F32, "mv")
    rstd = cx.sb([128, 1], F32, "rstd")
    r_xs = [Res(f"xs{i}") for i in range(2)]
    r_xT = Res("xT")
    r_stat = Res("stat")
    def pt(name, dt=F32, share=True):
        t_ = cx.sb([128, TT], dt, name)
        return [t_, t_] if share else [t_, cx.sb([128, TT], dt, name)]
    rT, kT, aT, lw, cumI, cumE, Dexcl, invD, E2, kkr, sq, rn, kk, tmpa, kp, bT = (
        pt(n) for n in ("rT", "kT", "aT", "lw", "cumI", "cumE", "Dexcl", "invD", "E2", "kkr", "sq", "rn", "kk",
                        "tmpa", "kp", "bT"))
    BhT, KhT = pt("BhT", share=False), pt("KhT", share=False)
    Dincl1 = cx.sb([128, TT], F32, "Dincl1")
    r_bk = [Res("bhkh0"), Res("bhkh1")]
    lo0T = cx.sb([128, TT], BF16, "lo0T")
    sgT = cx.sb([128, TT], BF16, "sgT")
    _tres = {}

    def qr(t_):
        return _tres.setdefault(id(t_), Res("tmp"))
    r_lo = Res("lo")
    ARt = [[cx.sb([128, NCH, 256], BF16, "ARt") for _ in range(2)] for _ in range(2)]
    BtT = [[cx.sb([128, TT], BF16, "BtT") for _ in range(2)] for _ in range(2)]
    KtT = [[cx.sb([128, TT], BF16, "KtT") for _ in range(2)] for _ in range(2)]
    rkT = [[cx.sb([128, TT], BF16, "rkT") for _ in range(2)] for _ in range(2)]
    DC = [[cx.sb([128, NCH], F32, "DC") for _ in range(2)] for _ in range(2)]
    BKh = [[cx.sb([128, 512], BF16, "BKh") for _ in range(NCH)] for _ in range(2)]
    Vb = [[cx.sb([128, 256], BF16, "Vb") for _ in range(NCH)] for _ in range(2)]
    Gt = [[cx.sb([128, 256], BF16, "Gt") for _ in range(NCH)] for _ in range(2)]
    r_AR = [[Res(f"AR{a}{b}") for b in range(2)] for a in range(2)]
    r_Bt = [[Res(f"Bt{a}{b}") for b in range(2)] for a in range(2)]
    r_Kt = [[Res(f"Kt{a}{b}") for b in range(2)] for a in range(2)]
    r_rk = [[Res(f"rk{a}{b}") for b in range(2)] for a in range(2)]
    r_Di = [[Res(f"Di{a}{b}") for b in range(2)] for a in range(2)]
    r_BKh = [[Res(f"BKh{a}{c}") for c in range(NCH)] for a in range(2)]
    r_Vb = [[Res(f"Vb{a}{c}") for c in range(NCH)] for a in range(2)]
    r_Gt = [[Res(f"Gt{a}{c}") for c in range(NCH)] for a in range(2)]
    NSLOT = NCH
    NM = [[cx.sb([128, 512], BF16, "NM") for _ in range(4)] for _ in range(NSLOT)]
    r_NM = [[Res(f"NM{a}{h}") for h in range(4)] for a in range(NSLOT)]
    Xb = [[[cx.sb([128, 128], BF16, "X") for _ in range(2)] for _ in range(4)] for _ in range(NSLOT)]
    r_X = [[[Res(f"X{a}{h}{k}") for k in range(2)] for h in range(4)] for a in range(NSLOT)]
    NL = [[[cx.sb([128, 256], BF16, "NL") for _ in range(2)] for _ in range(4)] for _ in range(NSLOT)]
    r_NL = [[[Res(f"NL{a}{h}{k}") for k in range(2)] for h in range(4)] for a in range(NSLOT)]
    Sf = [cx.sb([128, 128], F32, "Sf") for _ in range(2)]
    Sb = [cx.sb([128, 128], BF16, "Sb") for _ in range(2)]
    r_S = [Res("S0"), Res("S1")]
    brp = [cx.sb([128, 128], BF16, "brp") for _ in range(2)]
    Wbp = [cx.sb([128, 128], BF16, "Wbp") for _ in range(2)]
    r_br = [Res("br0"), Res("br1")]
    r_Wb = [Res("Wb0"), Res("Wb1")]
    Ysb = [cx.sb([128, 256], F32, "Ysb") for _ in range(2)]
    r_Y = [[Res(f"Y{a}{p}") for p in range(2)] for a in range(2)]
    yn = cx.sb([128, 256], F32, "yn")
    ost = cx.sb([128, 4, 6], F32, "ost")
    omv = cx.sb([128, 4, 2], F32, "omv")
    orstd = cx.sb([128, 4], F32, "orstd")
    bsb = cx.sb([128, 4], F32, "bsb")
    ob = [cx.sb([128, 256], F32, "ob") for _ in range(2)]
    oaT = [cx.sb([128, 2, TT], BF16, "oaT") for _ in range(2)]
    r_o = Res("ostuff")
    r_ob = [Res("ob0"), Res("ob1")]
    r_oaT = [Res("oaT0"), Res("oaT1")]

    PJ = [cx.ps([128, 512], F32, "PJ") for _ in range(NPJ)]
    INV = [cx.ps([128, 512], F32, "INV") for _ in range(6 - NPJ)]
    SEQ = [cx.ps([128, 512], F32, "SEQ") for _ in range(2)]
    rPJ = [Res(f"PJ{i}", True) for i in range(NPJ)]
    rINV = [Res(f"INV{i}", True) for i in range(6 - NPJ)]
    rSEQ = [Res("SEQ0", True), Res("SEQ1", True)]
    pj_i = [0]
    inv_i = [0]

    def next_pj():
        i = pj_i[0] % NPJ
        pj_i[0] += 1
        return PJ[i], rPJ[i]

    def next_inv():
        i = inv_i[0] % (6 - NPJ)
        inv_i[0] += 1
        return INV[i], rINV[i]

    P.op("dve", lambda e: e.memset(xT[:, :, :], 0.0), writes=[r_xT])
    for p in range(2):
        P.op("dve", lambda e, p=p: e.memset(Sf[p][:, :], 0.0), writes=[r_S[p]])
        P.op("dve", lambda e, p=p: e.memset(Sb[p][:, :], 0.0), writes=[r_S[p]])

    out_tags = []
    chs = lambda p, i: chv[:, p, i:i + 1]

    def c3(ap):
        return ap.rearrange("p (c t) -> p c t", c=NCH)

    def prep(ti):
        par = ti % 2
        t0 = ti * TT
        if ti > 0:
            P.op("pool", lambda e: e.tensor_copy(out=xT[:, :, 0:1], in_=xT[:, :, TT:TT + 1]), reads=[r_xT], writes=[r_xT])
        for s in range(NCH):
            xb_ = s % 2
            P.dma("sp", xs[:, xb_, :], xsrc(t0 + s * 128, 128), writes=[r_xs[xb_]])
            if do_ln0:
                layer_norm_rows(P, nc, xs[:, xb_, :], r_xs[xb_], lnB[:, 0, :], lnB[:, 1, :], stats, mv, rstd, r_stat, r_c)
            for half in range(2):
                TR, rTR = next_pj()
                for k4 in range(4):
                    kc = half * 4 + k4
                    P.op("pe", lambda e, kc=kc, k4=k4: e.transpose(out=TR[:, k4 * 128:(k4 + 1) * 128],
                                                                   in_=xs[:, xb_, kc * 128:(kc + 1) * 128],
                                                                   identity=ident[:, :]),
                         reads=[r_xs[xb_], r_c], writes=[rTR], inc=(k4 == 3))
                P.op("act", lambda e, half=half: e.copy(out=xT[:, half * 4:(half + 1) * 4, 1 + s * 128:1 + (s + 1) * 128],
                                                        in_=TR[:, :].rearrange("p (a t) -> p a t", a=4)),
                     reads=[rTR], writes=[r_xT])
            yield

        def proj_cm(c0, ncols=128):
            ps, ps_r = next_pj()
            for kc in range(8):
                P.op("pe", lambda e, kc=kc: e.matmul(ps[0:ncols, 0:TT], lhsT=W0[:, kc, c0:c0 + ncols],
                                                     rhs=xT[:, kc, 1:1 + TT], start=(kc == 0), stop=False),
                     reads=[r_W, r_xT], writes=[ps_r], inc=False)
            for kc in range(8):
                P.op("pe", lambda e, kc=kc: e.matmul(ps[0:ncols, 0:TT], lhsT=W1[:, kc, c0:c0 + ncols],
                                                     rhs=xT[:, kc, 0:TT], start=False, stop=(kc == 7)),
                     reads=[r_W, r_xT], writes=[ps_r], inc=(kc == 7))
            return ps, ps_r

        ps, ps_r = proj_cm(768)
        P.op("act", lambda e: e.activation(out=lo0T[0:64, :], in_=ps[0:64, 0:TT], func=AF.Tanh), reads=[ps_r], writes=[r_lo])
        P.op("act", lambda e: e.copy(out=lo0T[64:128, :], in_=ps[64:128, 0:TT]), reads=[ps_r], writes=[r_lo])
        ps, ps_r = proj_cm(896)
        P.op("act", lambda e: e.activation(out=sgT[:, :], in_=ps[:, 0:TT], func=AF.Sigmoid), reads=[ps_r], writes=[r_lo])
        yield
        for p in range(2):
            ps, ps_r = proj_cm(p * 128)
            P.op("act", lambda e: e.copy(out=rT[p][:, :], in_=ps[:, 0:TT]), reads=[ps_r], writes=[qr(rT[p])])
            ps, ps_r = proj_cm(256 + p * 128)
            P.op("act", lambda e: e.copy(out=kT[p][:, :], in_=ps[:, 0:TT]), reads=[ps_r], writes=[qr(kT[p])])
            ps, ps_r = next_pj()
            P.op("pe", lambda e: e.matmul(ps[:, 0:TT], lhsT=w2ia[0:64, p * 128:(p + 1) * 128], rhs=lo0T[0:64, :],
                                          start=True, stop=True), reads=[r_c2, r_lo], writes=[ps_r])
            P.op("act", lambda e: e.activation(out=lw[p][:, :], in_=ps[:, 0:TT], func=AF.Sigmoid, bias=chs(p, 0), scale=1.0),
                 reads=[ps_r, r_c], writes=[qr(lw[p])])
            ps, ps_r = next_pj()
            P.op("pe", lambda e: e.matmul(ps[:, 0:TT], lhsT=w2ia[64:128, p * 128:(p + 1) * 128], rhs=lo0T[64:128, :],
                                          start=True, stop=True), reads=[r_c2, r_lo], writes=[ps_r])
            P.op("act", lambda e: e.activation(out=aT[p][:, :], in_=ps[:, 0:TT], func=AF.Sigmoid, bias=chs(p, 1), scale=1.0),
                 reads=[ps_r, r_c], writes=[qr(aT[p])])
            yield
            P.op("dve", lambda e: e.tensor_scalar(out=lw[p][:, :], in0=lw[p][:, :], scalar1=DECAY_SCALE, scalar2=None,
                                                  op0=ALU.mult), reads=[qr(lw[p])], writes=[qr(lw[p])])
            P.op("dve", lambda e: e.tensor_tensor_scan(out=cumI[p][:, :], data0=smask[:, 0:TT], data1=lw[p][:, :],
                                                       initial=0.0, op0=ALU.mult, op1=ALU.add),
                 reads=[qr(lw[p]), r_c], writes=[qr(cumI[p])])
            P.op("pool", lambda e: e.tensor_tensor(out=cumE[p][:, :], in0=cumI[p][:, :], in1=lw[p][:, :], op=ALU.subtract),
                 reads=[qr(cumI[p]), qr(lw[p])], writes=[qr(cumE[p])])
            P.op("act", lambda e: e.activation(out=Dincl1[:, :], in_=cumI[p][:, :], func=AF.Exp),
                 reads=[qr(cumI[p])], writes=[qr(Dincl1)])
            P.op("dve", lambda e: e.tensor_copy(out=DC[par][p][:, :], in_=c3(Dincl1[:, :])[:, :, 127]),
                 reads=[qr(Dincl1)], writes=[r_Di[par][p]])
            P.op("act", lambda e: e.activation(out=invD[p][:, :], in_=cumI[p][:, :], func=AF.Exp, scale=-1.0),
                 reads=[qr(cumI[p])], writes=[qr(invD[p])])
            P.op("act", lambda e: e.activation(out=Dexcl[p][:, :], in_=cumE[p][:, :], func=AF.Exp),
                 reads=[qr(cumE[p])], writes=[qr(Dexcl[p])])
            for c in range(NCH):
                P.op("act", lambda e, c=c: e.activation(out=E2[p][:, c * 128:(c + 1) * 128],
                                                        in_=cumI[p][:, c * 128:(c + 1) * 128], func=AF.Exp,
                                                        bias=cumI[p][:, c * 128 + 127:c * 128 + 128], scale=-1.0),
                     reads=[qr(cumI[p])], writes=[qr(E2[p])])
            yield
            P.op("dve", lambda e: e.tensor_scalar(out=kkr[p][:, :], in0=kT[p][:, :], scalar1=chs(p, 2), scalar2=None,
                                                   op0=ALU.mult), reads=[qr(kT[p]), r_c], writes=[qr(kkr[p])])
            P.op("dve", lambda e: e.tensor_tensor(out=sq[p][:, :], in0=kkr[p][:, :], in1=kkr[p][:, :], op=ALU.mult),
                 reads=[qr(kkr[p])], writes=[qr(sq[p])])
            ps, ps_r = next_pj()
            P.op("pe", lambda e: e.matmul(ps[:, 0:TT], lhsT=bones[:, :], rhs=sq[p][:, :], start=True, stop=True),
                 reads=[r_c, qr(sq[p])], writes=[ps_r])
            P.op("act", lambda e: e.activation(out=rn[p][:, :], in_=ps[:, 0:TT], func=AF.Sqrt), reads=[ps_r],
                 writes=[qr(rn[p])])
            P.op("dve", lambda e: e.tensor_scalar(out=rn[p][:, :], in0=rn[p][:, :], scalar1=1e-12, scalar2=None,
                                                  op0=ALU.max), reads=[qr(rn[p])], writes=[qr(rn[p])])
            P.op("dve", lambda e: e.reciprocal(out=rn[p][:, :], in_=rn[p][:, :]), reads=[qr(rn[p])], writes=[qr(rn[p])])
            P.op("dve", lambda e: e.tensor_tensor(out=kk[p][:, :], in0=kkr[p][:, :], in1=rn[p][:, :], op=ALU.mult),
                 reads=[qr(kkr[p]), qr(rn[p])], writes=[qr(kk[p])])
            P.op("pool", lambda e: e.tensor_scalar(out=tmpa[p][:, :], in0=aT[p][:, :], scalar1=chs(p, 3),
                                                   scalar2=omka[:, p:p + 1], op0=ALU.mult, op1=ALU.add),
                 reads=[qr(aT[p]), r_c, r_c2], writes=[qr(tmpa[p])])
            P.op("pool", lambda e: e.tensor_tensor(out=kp[p][:, :], in0=kT[p][:, :], in1=tmpa[p][:, :], op=ALU.mult),
                 reads=[qr(kT[p]), qr(tmpa[p])], writes=[qr(kp[p])])
            yield
            P.op("dve", lambda e: e.scalar_tensor_tensor(out=ARt[par][p][:, :, 0:128], in0=c3(kk[p][:, :]), scalar=-1.0,
                                                         in1=c3(Dexcl[p][:, :]), op0=ALU.mult, op1=ALU.mult),
                 reads=[qr(kk[p]), qr(Dexcl[p])], writes=[r_AR[par][p]])
            P.op("dve", lambda e: e.tensor_tensor(out=ARt[par][p][:, :, 128:256], in0=c3(rT[p][:, :]),
                                                   in1=c3(Dincl1[:, :]), op=ALU.mult),
                 reads=[qr(rT[p]), qr(Dincl1)], writes=[r_AR[par][p]])
            P.op("pool", lambda e: e.tensor_tensor(out=bT[p][:, :], in0=kk[p][:, :], in1=aT[p][:, :], op=ALU.mult),
                 reads=[qr(kk[p]), qr(aT[p])], writes=[qr(bT[p])])
            P.op("dve", lambda e: e.tensor_tensor(out=BtT[par][p][:, :], in0=bT[p][:, :], in1=invD[p][:, :], op=ALU.mult),
                 reads=[qr(bT[p]), qr(invD[p])], writes=[r_Bt[par][p]])
            P.op("dve", lambda e: e.tensor_tensor(out=KtT[par][p][:, :], in0=kp[p][:, :], in1=invD[p][:, :], op=ALU.mult),
                 reads=[qr(kp[p]), qr(invD[p])], writes=[r_Kt[par][p]])
            P.op("dve", lambda e: e.tensor_tensor(out=BhT[p][:, :], in0=bT[p][:, :], in1=E2[p][:, :], op=ALU.mult),
                 reads=[qr(bT[p]), qr(E2[p])], writes=[r_bk[p]])
            P.op("pool", lambda e: e.tensor_tensor(out=KhT[p][:, :], in0=kp[p][:, :], in1=E2[p][:, :], op=ALU.mult),
                 reads=[qr(kp[p]), qr(E2[p]), r_bk[p]], writes=[r_bk[p]])
            P.op("dve", lambda e: e.scalar_tensor_tensor(out=rkT[par][p][:, :], in0=rT[p][:, :], scalar=chs(p, 4),
                                                         in1=kp[p][:, :], op0=ALU.mult, op1=ALU.mult),
                 reads=[qr(rT[p]), qr(kp[p]), r_c], writes=[r_rk[par][p]])
            yield
        for c in range(NCH):
            ps, ps_r = next_pj()
            for kc in range(8):
                P.op("pe", lambda e, kc=kc: e.matmul(ps[:, 0:256], lhsT=xT[:, kc, 1 + c * 128:1 + (c + 1) * 128],
                                                     rhs=W0[:, kc, 512:768], start=(kc == 0), stop=False),
                     reads=[r_W, r_xT], writes=[ps_r], inc=False)
            for kc in range(8):
                P.op("pe", lambda e, kc=kc: e.matmul(ps[:, 0:256], lhsT=xT[:, kc, c * 128:(c + 1) * 128],
                                                     rhs=W1[:, kc, 512:768], start=False, stop=(kc == 7)),
                     reads=[r_W, r_xT], writes=[ps_r], inc=(kc == 7))
            P.op("act", lambda e: e.copy(out=Vb[par][c][:, :], in_=ps[:, 0:256]), reads=[ps_r], writes=[r_Vb[par][c]])
            ps, ps_r = next_pj()
            P.op("pe", lambda e: e.matmul(ps[:, 0:256], lhsT=sgT[:, c * 128:(c + 1) * 128], rhs=w2g[:, :], start=True, stop=True),
                 reads=[r_lo, r_c2], writes=[ps_r])
            P.op("act", lambda e: e.copy(out=Gt[par][c][:, :], in_=ps[:, 0:256]), reads=[ps_r], writes=[r_Gt[par][c]])
            TR, rTR = next_pj()
            for q in range(4):
                src = (BhT, KhT)[q // 2][q % 2]
                P.op("pe", lambda e, q=q, src=src: e.transpose(out=TR[:, q * 128:(q + 1) * 128],
                                                               in_=src[:, c * 128:(c + 1) * 128], identity=ident[:, :]),
                     reads=[r_bk[q % 2], r_c], writes=[rTR], inc=(q == 3))
            P.op("dve", lambda e: e.tensor_copy(out=BKh[par][c][:, :], in_=TR[:, :]), reads=[rTR], writes=[r_BKh[par][c]])
            yield

    def inv(ti, c, h):
        par = ti % 2
        slot = c
        sp = slot
        p, hb = h // 2, (h % 2) * 64
        cs = slice(c * 128, (c + 1) * 128)
        a12, a12_r = next_inv()
        P.op("pe", lambda e: e.matmul(a12[:, 0:256], lhsT=BtT[par][p][hb:hb + 64, cs], rhs=ARt[par][p][hb:hb + 64, c, :],
                                      start=True, stop=True),
             reads=[r_Bt[par][p], r_AR[par][p]], writes=[a12_r], inc=False)
        P.op("pe", lambda e: e.matmul(a12[:, 256:512], lhsT=KtT[par][p][hb:hb + 64, cs], rhs=ARt[par][p][hb:hb + 64, c, :],
                                      start=False, stop=True, skip_group_check=True),
             reads=[r_Kt[par][p], r_AR[par][p]], writes=[a12_r])
        a3, a3_r = next_inv()
        P.op("pe", lambda e: e.matmul(a3[:, 0:128], lhsT=ARt[par][p][hb:hb + 64, c, 0:128], rhs=BtT[par][p][hb:hb + 64, cs],
                                      start=True, stop=True),
             reads=[r_Bt[par][p], r_AR[par][p]], writes=[a3_r])
        nm, nm_r = NM[slot][h], r_NM[slot][h]
        P.op("dve", lambda e: e.tensor_tensor(out=nm[:, :], in0=a12[:, :], in1=mask4[:, :], op=ALU.mult),
             reads=[a12_r, r_c], writes=[nm_r])
        nl = NL[sp][h]
        nl_r = r_NL[sp][h]
        P.op("dve", lambda e: e.tensor_tensor(out=nl[0][:, 128:256], in0=a3[:, 0:128], in1=maskL[:, :], op=ALU.mult),
             reads=[a3_r, r_c], writes=[nl_r[0]])
        X, X_r = Xb[slot][h], r_X[slot][h]
        P.op("pool", lambda e: e.tensor_tensor(out=X[0][:, :], in0=nm[:, 0:128], in1=identb[:, :], op=ALU.add),
             reads=[nm_r, r_c2], writes=[X_r[0]])
        yield
        Np, Np_r = nm[:, 0:128], nm_r
        Lp, Lp_r = nl[0][:, 128:256], nl_r[0]
        xi = 0
        for rnd in range(6):
            last = (rnd == 5)
            pp = (rnd + 1) % 2
            bk, bk_r = next_inv()
            if not last:
                P.op("pe", lambda e: e.matmul(bk[:, 0:128], lhsT=Lp, rhs=Np, start=True, stop=True),
                     reads=[Lp_r, Np_r], writes=[bk_r], inc=False)
            P.op("pe", lambda e: e.matmul(bk[:, 128:256], lhsT=Np, rhs=Lp, start=last, stop=True,
                                          skip_group_check=True),
                 reads=[Lp_r, Np_r], writes=[bk_r])
            if not last:
                P.op("act", lambda e: e.copy(out=nl[pp][:, :], in_=bk[:, 0:256]), reads=[bk_r], writes=[nl_r[pp]])
            else:
                P.op("act", lambda e: e.copy(out=nl[pp][:, 128:256], in_=bk[:, 128:256]), reads=[bk_r], writes=[nl_r[pp]])
            Np, Np_r = nl[pp][:, 0:128], nl_r[pp]
            Lp, Lp_r = nl[pp][:, 128:256], nl_r[pp]
            yield
            px, px_r = next_inv()
            P.op("pe", lambda e: e.matmul(px[:, 0:128], lhsT=Lp, rhs=X[xi][:, :], start=True, stop=True),
                 reads=[Lp_r, X_r[xi]], writes=[px_r])
            P.op("dve", lambda e: e.tensor_tensor(out=X[1 - xi][:, :], in0=px[:, 0:128], in1=X[xi][:, :], op=ALU.add),
                 reads=[px_r, X_r[xi]], writes=[X_r[1 - xi]])
            xi = 1 - xi
            yield
        assert xi == 0

    def seq(ti, c, p):
        par = ti % 2
        slot = c
        sq_, sq_r = SEQ[p], rSEQ[p]
        cs = slice(c * 128, (c + 1) * 128)
        for hh in range(2):
            h = 2 * p + hh
            hb = hh * 64
            P.op("pe", lambda e: e.matmul(sq_[:, hh * 64:(hh + 1) * 64], lhsT=ARt[par][p][hb:hb + 64, c, 0:128],
                                          rhs=Sb[p][hb:hb + 64, hh * 64:(hh + 1) * 64], start=(hh == 0), stop=False,
                                          skip_group_check=True),
                 reads=[r_AR[par][p], r_S[p]], writes=[sq_r], inc=False)
            P.op("pe", lambda e: e.matmul(sq_[:, hh * 64:(hh + 1) * 64], lhsT=NM[slot][h][:, 256:384],
                                          rhs=Vb[par][c][:, h * 64:(h + 1) * 64], start=False, stop=True,
                                          skip_group_check=True),
                 reads=[r_NM[slot][h], r_Vb[par][c]], writes=[sq_r], inc=(hh == 1))
        P.op("act", lambda e: e.copy(out=brp[p][:, :], in_=sq_[:, 0:128]), reads=[sq_r], writes=[r_br[p]])
        yield
        for hh in range(2):
            h = 2 * p + hh
            P.op("pe", lambda e: e.matmul(sq_[:, 128 + hh * 64:128 + (hh + 1) * 64], lhsT=Xb[slot][h][0][:, :],
                                          rhs=brp[p][:, hh * 64:(hh + 1) * 64], start=False, stop=True,
                                          skip_group_check=True),
                 reads=[r_X[slot][h][0], r_br[p]], writes=[sq_r], inc=(hh == 1))
        P.op("dve", lambda e: e.tensor_copy(out=Wbp[p][:, :], in_=sq_[:, 128:256]), reads=[sq_r], writes=[r_Wb[p]])
        yield
        P.op("pe", lambda e: e.matmul(sq_[:, 256:384], lhsT=BKh[par][c][:, p * 128:(p + 1) * 128], rhs=Wbp[p][:, :],
                                      start=False, stop=False, skip_group_check=True),
             reads=[r_BKh[par][c], r_Wb[p]], writes=[sq_r], inc=False)
        P.op("pe", lambda e: e.matmul(sq_[:, 256:384], lhsT=BKh[par][c][:, 256 + p * 128:256 + (p + 1) * 128],
                                      rhs=Vb[par][c][:, p * 128:(p + 1) * 128], start=False, stop=True,
                                      skip_group_check=True),
             reads=[r_BKh[par][c], r_Vb[par][c]], writes=[sq_r], inc=False)
        for hh in range(2):
            h = 2 * p + hh
            hb = hh * 64
            yc = slice(384 + hh * 64, 384 + (hh + 1) * 64)
            P.op("pe", lambda e: e.matmul(sq_[:, yc], lhsT=ARt[par][p][hb:hb + 64, c, 128:256],
                                          rhs=Sb[p][hb:hb + 64, hh * 64:(hh + 1) * 64], start=False, stop=False,
                                          skip_group_check=True),
                 reads=[r_AR[par][p], r_S[p]], writes=[sq_r], inc=False)
            P.op("pe", lambda e: e.matmul(sq_[:, yc], lhsT=NM[slot][h][:, 128:256], rhs=Wbp[p][:, hh * 64:(hh + 1) * 64],
                                          start=False, stop=False, skip_group_check=True),
                 reads=[r_NM[slot][h], r_Wb[p]], writes=[sq_r], inc=False)
            P.op("pe", lambda e: e.matmul(sq_[:, yc], lhsT=NM[slot][h][:, 384:512], rhs=Vb[par][c][:, h * 64:(h + 1) * 64],
                                          start=False, stop=True, skip_group_check=True),
                 reads=[r_NM[slot][h], r_Vb[par][c]], writes=[sq_r], inc=(hh == 1))
        P.op("dve", lambda e: e.scalar_tensor_tensor(out=Sf[p][:, :], in0=Sf[p][:, :],
                                                     scalar=DC[par][p][:, c:c + 1],
                                                     in1=sq_[:, 256:384], op0=ALU.mult, op1=ALU.add),
             reads=[sq_r, r_S[p], r_Di[par][p]], writes=[r_S[p]])
        P.op("act", lambda e: e.copy(out=Sb[p][:, :], in_=Sf[p][:, :]), reads=[r_S[p]], writes=[r_S[p]])
        P.op("dve", lambda e: e.tensor_copy(out=Ysb[c % 2][:, p * 128:(p + 1) * 128], in_=sq_[:, 384:512]),
             reads=[sq_r], writes=[r_Y[c % 2][p]])
        yield

    def outp(ti, c):
        par = ti % 2
        t0 = ti * TT + c * 128
        Y = Ysb[c % 2]
        ry = r_Y[c % 2]
        cs = slice(c * 128, (c + 1) * 128)
        for h in range(4):
            P.op("dve", lambda e, h=h: e.bn_stats(out=ost[:, h, :], in_=Y[:, h * 64:(h + 1) * 64]), reads=ry, writes=[r_o])
        for h in range(4):
            P.op("dve", lambda e, h=h: e.bn_aggr(out=omv[:, h, :], in_=ost[:, h, :]), reads=[r_o], writes=[r_o])
        P.op("act", lambda e: e.activation(out=orstd[:, :], in_=omv[:, :, 1], func=AF.Sqrt, bias=float(RWKV_LN_EPS), scale=1.0),
             reads=[r_o], writes=[r_o])
        P.op("dve", lambda e: e.reciprocal(out=orstd[:, :], in_=orstd[:, :]), reads=[r_o], writes=[r_o])
        yield
        for h in range(4):
            P.op("dve", lambda e, h=h: e.tensor_scalar(out=yn[:, h * 64:(h + 1) * 64], in0=Y[:, h * 64:(h + 1) * 64],
                                                       scalar1=omv[:, h, 0:1], scalar2=orstd[:, h:h + 1],
                                                       op0=ALU.subtract, op1=ALU.mult),
                 reads=ry + [r_o], writes=[r_o])
        P.op("pool", lambda e: e.tensor_tensor(out=yn[:, :], in0=yn[:, :], in1=lnxB[:, 0, :], op=ALU.mult),
             reads=[r_o, r_c], writes=[r_o])
        P.op("pool", lambda e: e.tensor_tensor(out=yn[:, :], in0=yn[:, :], in1=lnxB[:, 1, :], op=ALU.add),
             reads=[r_o, r_c], writes=[r_o])
        ps, ps_r = next_pj()
        for p in range(2):
            P.op("pe", lambda e, p=p: e.matmul(ps[:, p * 2:(p + 1) * 2], lhsT=rkT[par][p][:, cs], rhs=selb[:, :],
                                               start=(p == 0), stop=True, skip_group_check=True),
                 reads=[r_rk[par][p], r_c2], writes=[ps_r], inc=(p == 1))
        P.op("dve", lambda e: e.tensor_copy(out=bsb[:, :], in_=ps[:, 0:4]), reads=[ps_r], writes=[r_o])
        yield
        for h in range(4):
            P.op("dve", lambda e, h=h: e.scalar_tensor_tensor(out=yn[:, h * 64:(h + 1) * 64],
                                                              in0=Vb[par][c][:, h * 64:(h + 1) * 64],
                                                              scalar=bsb[:, h:h + 1], in1=yn[:, h * 64:(h + 1) * 64],
                                                              op0=ALU.mult, op1=ALU.add),
                 reads=[r_o, r_Vb[par][c]], writes=[r_o])
        oi = c % 2
        P.op("pool", lambda e: e.tensor_tensor(out=ob[oi][:, :], in0=yn[:, :], in1=Gt[par][c][:, :], op=ALU.mult),
             reads=[r_o, r_Gt[par][c]], writes=[r_ob[oi]])
        TR, rTR = next_pj()
        for pp in range(2):
            P.op("pe", lambda e, pp=pp: e.transpose(out=TR[:, pp * 128:(pp + 1) * 128], in_=ob[oi][:, pp * 128:(pp + 1) * 128],
                                                    identity=ident[:, :]),
                 reads=[r_ob[oi], r_c], writes=[rTR], inc=(pp == 1))
        P.op("act", lambda e: e.copy(out=oaT[par][:, :, c * 128:(c + 1) * 128],
                                     in_=TR[:, 0:256].rearrange("p (a t) -> p a t", a=2)),
             reads=[rTR], writes=[r_oaT[par]])
        if c == NCH - 1:
            out_tags.append(P.dma("sp", oa_dst(ti).rearrange("(pp p) t -> p pp t", p=128),
                                  oaT[par][:, :, :], reads=[r_oaT[par]]))
            if after_store is not None:
                after_store(ti, out_tags[-1])
        yield

    S = Sched()

    def add_prep(ti):
        deps = [f"prep{ti - 1}"] if ti > 0 else []
        if ti > 1:
            deps += [f"out{ti - 2}_{NCH - 1}"]
        S.add(f"prep{ti}", prep(ti), deps)

    add_prep(0)
    for ti in range(NTI):
        if ti + 1 < NTI:
            add_prep(ti + 1)
        for c in range(NCH):
            for h in range(4):
                d = [f"prep{ti}"]
                if ti >= 1:
                    d.append(f"seq{ti - 1}_{c}_{h // 2}")
                S.add(f"inv{ti}_{c}_{h}", inv(ti, c, h), d)
        for c in range(NCH):
            for p in range(2):
                d = [f"inv{ti}_{c}_{2 * p}", f"inv{ti}_{c}_{2 * p + 1}"]
                if c > 0:
                    d.append(f"seq{ti}_{c - 1}_{p}")
                elif ti > 0:
                    d.append(f"seq{ti - 1}_{NCH - 1}_{p}")
                S.add(f"seq{ti}_{c}_{p}", seq(ti, c, p), d)
            d = [f"seq{ti}_{c}_0", f"seq{ti}_{c}_1"]
            if c > 0:
                d.append(f"out{ti}_{c - 1}")
            elif ti > 0:
                d.append(f"out{ti - 1}_{NCH - 1}")
            S.add(f"out{ti}_{c}", outp(ti, c), d)
    S.run(window=RWKV_WINDOW)
    return out_tags, cx


_ar = np.arange(128)
MASK4 = np.concatenate([(_ar[:, None] < _ar[None, :]), (_ar[:, None] <= _ar[None, :])] * 2, 1).astype(np.float32)
MASKL = (_ar[None, :] < _ar[:, None]).astype(np.float32)
BONES = (_ar[:, None] // 64 == _ar[None, :] // 64).astype(np.float32)
SEL = (_ar[:, None] // 64 == np.arange(2)[None, :]).astype(np.float32)
SMASK = np.ascontiguousarray(np.broadcast_to((np.arange(512) % 128 != 0).astype(np.float32)[None, :], (128, 512)))


def rwkv_inputs(p, l, g):
    w_in = p["w_in"][l]
    mu = p["mu_shift"][l]
    if True:
        gc = slice(g * 256, (g + 1) * 256)
        cols = np.concatenate([np.arange(g * 256, (g + 1) * 256), 512 + np.arange(g * 256, (g + 1) * 256),
                               1024 + np.arange(g * 256, (g + 1) * 256), np.arange(1536, 1792)])
        vecs = [p["w0_decay"][l][gc], p["a0"][l][gc], p["k_k"][l][gc], p["k_a"][l][gc], p["r_k"][l].reshape(-1)[gc]]
        chv = np.zeros((128, 2, 8), np.float32)
        for i, v in enumerate(vecs):
            chv[:, :, i] = v.reshape(2, 128).T
        m = {
            "wall": np.ascontiguousarray(w_in[:, cols]),
            "muB": _bc(mu[cols]),
            "chv": chv,
            "w2ia": np.ascontiguousarray(np.concatenate([p["w2_decay"][l][:, gc], p["w2_iclr"][l][:, gc]], 0)),
            "w2g": np.ascontiguousarray(p["w2_gate"][l][:, gc]),
            "lnxB": np.ascontiguousarray(np.stack([_bc(p["lnx_g"][l][gc]), _bc(p["lnx_b"][l][gc])], 0)),
            "lnB": np.ascontiguousarray(np.stack([_bc(p["ln0_g"]), _bc(p["ln0_b"])], 0)),
            "ident": IDENT, "mask4": MASK4, "maskL": MASKL, "bones": BONES, "sel": SEL, "smask": SMASK,
        }
    return m


def run_rwkv(xb, p, l, do_ln0, n_cores=8):
    B, T, _ = xb.shape
    nc = build_rwkv(T, do_ln0)
    in_maps = []
    for c in range(n_cores):
        b, g = c // 2, c % 2
        m = rwkv_inputs(p, l, g)
        m["x"] = np.ascontiguousarray(xb[b])
        in_maps.append(m)
    res = run_bass_kernel_spmd(nc, in_maps, core_ids=list(range(n_cores)))
    oaT = np.stack([np.concatenate([res.results[2 * b + g]["oaT"] for g in range(2)], 0) for b in range(B)], 0)
    return oaT


PAIRS = [[0, 1], [2, 3], [4, 5], [6, 7]]


def build_fused(T, ff_override=None):
    nc = bass.Bass("TRN2", target_bir_lowering=False)
    NT = T // 2
    TT = min(T, 512)
    CW = min(1024, NT)
    NCK = T // CW
    HCK = NCK // 2
    RY = min(512, NT)
    NYC = NT // RY
    x_d = nc.dram_tensor("x", [T, D], F32, kind="ExternalInput").ap()
    y_d = nc.dram_tensor("y", [NT, D], F32, kind="ExternalOutput").ap()
    A_r = [decl_rwkv(nc, f"r{l}_") for l in range(DEPTH)]
    A_f = [decl_fox(nc, f"f{l}_") for l in range(DEPTH)]
    A_m = [decl_merge(nc, f"m{l}_", moe=(l % 2 == 1), ff_override=ff_override) for l in range(DEPTH)]
    ola = [nc.dram_tensor(f"ola{l}", [NCK, 256, CW], BF16).ap() for l in range(DEPTH)]
    olf = [nc.dram_tensor(f"olf{l}", [NCK, 512, CW], BF16).ap() for l in range(DEPTH)]
    oalla = [nc.dram_tensor(f"oalla{l}", [NCK, 512, CW], BF16).ap() for l in range(DEPTH)]
    oallf = [nc.dram_tensor(f"oallf{l}", [NCK, 1024, CW], BF16).ap() for l in range(DEPTH)]
    omia = [nc.dram_tensor(f"omia{l}", [HCK, 512, CW], BF16).ap() for l in range(DEPTH)]
    omif = [nc.dram_tensor(f"omif{l}", [HCK, 1024, CW], BF16).ap() for l in range(DEPTH)]
    yloc = nc.dram_tensor("yloc", [NT, D], F32).ap()
    yall = nc.dram_tensor("yall", [NYC, 2 * RY, D], F32).ap()
    xh = nc.dram_tensor("xh", [NT, D], F32).ap()
    cx0 = Ctx(nc)
    P = cx0.P
    pid = nc.sync.partition_id()
    g = pid % 2
    for i in range(2):
        P.dma("sp", xh[i * (NT // 2):(i + 1) * (NT // 2), :], x_d[bass.ds(g * NT + i * (NT // 2), NT // 2), :])

    def odst(buf, r0):
        def f(ti):
            j, c = (ti * TT) // CW, (ti * TT) % CW
            return buf[j, r0:r0 + 256, c:c + TT]
        return f

    def chunk_gather(src, dst, per_chunk):
        acc = {}

        def cb(ti, tag):
            j = (ti * TT) // CW
            acc.setdefault(j, []).append(tag)
            if len(acc[j]) == per_chunk:
                P.collective_allgather(src[j].opt(), dst[j].opt(), PAIRS, acc[j])
        return cb

    def xsrc1(t0, n):
        rank, j, r = t0 // NT, (t0 % NT) // RY, t0 % RY
        return yall[j, rank * RY + r: rank * RY + r + n, :]

    xsrc = lambda t0, n: x_d[t0:t0 + n, :]
    tags_m = []
    for l in range(DEPTH):
        do_ln0 = (l == 0)
        tags_r, cx = emit_rwkv(nc, P, A_r[l], T, do_ln0, xsrc, odst(ola[l], 0),
                               after_store=chunk_gather(ola[l], oalla[l], CW // TT))
        P.barrier()
        cx.close()
        tags_f, cx = emit_fox(nc, P, A_f[l], T, do_ln0, xsrc, odst(olf[l], 0), odst(olf[l], 256),
                              after_store=chunk_gather(olf[l], oallf[l], 2 * (CW // TT)))
        P.barrier()
        cx.close()
        P.dma("sp", omia[l].rearrange("j r c -> (j r) c"),
              oalla[l].rearrange("j r c -> (j r) c")[bass.ds(g * (HCK * 512), HCK * 512), :])
        P.dma("sp", omif[l].rearrange("j r c -> (j r) c"),
              oallf[l].rearrange("j r c -> (j r) c")[bass.ds(g * (HCK * 1024), HCK * 1024), :])
        P.barrier()
        if l == 0:
            x_rows = lambda t0, n: xh[t0:t0 + n, :]
        else:
            x_rows = lambda t0, n: yloc[t0:t0 + n, :]

        def o_rows(br, half, t0, n, l=l):
            jj, c = t0 // CW, t0 % CW
            if br == 0:
                return omia[l][jj, half * 256:(half + 1) * 256, c:c + n]
            return omif[l][jj, half * 512 + (br - 1) * 256: half * 512 + br * 256, c:c + n]

        last = (l == DEPTH - 1)
        dst = y_d if last else yloc
        cb = None
        if not last:
            accy = {}

            def cb(t0, tag):
                j = t0 // RY
                accy.setdefault(j, []).append(tag)
                if len(accy[j]) == RY // 128:
                    P.collective_allgather(yloc[j * RY:(j + 1) * RY, :].opt(), yall[j].opt(), PAIRS, accy[j])
        tags_m, cx = emit_merge(nc, P, A_m[l], NT, do_ln0, l % 2 == 1, x_rows, o_rows, dst, after_store=cb)
        P.barrier()
        cx.close()
        if not last:
            xsrc = xsrc1
    P.finish(tags_m)
    cx0.close()
    return nc


def fused_in_maps(x, p, n_cores=8):
    in_maps = []
    mcommon = [merge_inputs(p, l, l % 2 == 1) for l in range(DEPTH)]
    for c in range(n_cores):
        b, g = c // 2, c % 2
        m = {"x": np.ascontiguousarray(x[b])}
        for l in range(DEPTH):
            for k, v in rwkv_inputs(p, l, g).items():
                m[f"r{l}_{k}"] = v
            for k, v in fox_inputs(p, l, g).items():
                m[f"f{l}_{k}"] = v
            for k, v in mcommon[l].items():
                m[f"m{l}_{k}"] = v
        in_maps.append(m)
    return in_maps


def kernel(**inputs):
    p = {k: np.asarray(v) for k, v in inputs.items()}
    x = np.ascontiguousarray(p["x"], dtype=np.float32)
    B, T, _ = x.shape
    nc = build_fused(T)
    in_maps = fused_in_maps(x, p)
    res = run_bass_kernel_spmd(nc, in_maps, core_ids=list(range(8)))
    NT = T // 2
    out = np.empty((B, T, D), np.float32)
    for c in range(8):
        b, g = c // 2, c % 2
        out[b, g * NT:(g + 1) * NT] = res.results[c]["y"]
    return out
```
